# Optimizing a Trainium2 kernel written in Bass

```python
import math
import jax, jax.numpy as jnp
from jax import lax
import numpy as np

D_MODEL = 1024
BATCH = 2
SEQ = 8192
DEPTH = 1
DEC_BATCH = 128
DEC_SEQ = 1
PAST_LEN = 2048
PAGE_SIZE = 128

N_META = 16
N_HEADS = D_MODEL // 128
HEAD_DIM = 64
ATTN_WIDTH = N_HEADS * HEAD_DIM
INDEX_HEADS = 8
INDEX_DIM = 64
TOPK_MAX = 256
NUM_BUCKETS = 32
MAX_DISTANCE = 128
POOL_WINDOWS = (2, 4, 8, 16)
POOL_GROUPS = 4
POOL_WIDTH = D_MODEL // 2
POOL_GROUP_DIM = POOL_WIDTH // POOL_GROUPS
POOL_STATE = 16 - 1
PEER_HEADS = 8
PEER_NKEYS = 128
PEER_EXPERTS = PEER_NKEYS * PEER_NKEYS
PEER_DKEY = 128
PEER_TOPK = 16
QBLK = 128
TOKBLK = 128
EPS = 1e-6
NEG = -1e30
IN_SPLITS = (ATTN_WIDTH, ATTN_WIDTH, ATTN_WIDTH, INDEX_HEADS * INDEX_DIM, INDEX_DIM,
             INDEX_HEADS, POOL_WIDTH, D_MODEL, D_MODEL)
IN_WIDTH = sum(IN_SPLITS)

kernel_name = 'dsa_pool_peer_hybrid_step'


def rms_norm(x, g):
    xf = x.astype(jnp.float32)
    y = xf * lax.rsqrt(jnp.mean(xf * xf, axis=-1, keepdims=True) + EPS)
    return (y * g.astype(jnp.float32)).astype(x.dtype)


def split_cols(z):
    outs, start = [], 0
    for w in IN_SPLITS:
        outs.append(z[..., start:start + w])
        start += w
    return outs


def rel_bucket(dist):
    n = jnp.maximum(dist, 0)
    max_exact = NUM_BUCKETS // 2
    nf = jnp.maximum(n, max_exact).astype(jnp.float32)
    large = max_exact + (jnp.log(nf / max_exact) / math.log(MAX_DISTANCE / max_exact)
                         * (NUM_BUCKETS - max_exact)).astype(jnp.int32)
    large = jnp.minimum(large, NUM_BUCKETS - 1)
    return jnp.where(n < max_exact, n, large)


def take_rows(a, idx):
    return jax.vmap(lambda ab, ib: ab[ib])(a, idx)


def index_scores(qi, wi, ki, q_pos, k_pos):
    dots = jnp.einsum('bqhd,bsd->bqhs', qi.astype(jnp.float32), ki.astype(jnp.float32)) * (INDEX_DIM ** -0.5)
    s = jnp.einsum('bqh,bqhs->bqs', wi.astype(jnp.float32) * (INDEX_HEADS ** -0.5), jax.nn.relu(dots))
    causal = (k_pos[None, :] <= q_pos[:, None])[None]
    return jnp.where(causal, s, NEG)


def sparse_attn(q, k_sel, v_sel, sel_pos, q_pos, rel_bias):
    logits = jnp.einsum('bqhd,bqkhd->bhqk', q.astype(jnp.float32), k_sel.astype(jnp.float32)) * (HEAD_DIM ** -0.5)
    dist = q_pos[None, :, None] - sel_pos
    bias = rel_bias.astype(jnp.float32)[rel_bucket(dist)]
    logits = logits + jnp.transpose(bias, (0, 3, 1, 2))
    logits = jnp.where((dist >= 0)[:, None], logits, NEG)
    p = jax.nn.softmax(logits, axis=-1)
    out = jnp.einsum('bhqk,bqkhd->bqhd', p, v_sel.astype(jnp.float32))
    return out.astype(q.dtype)


def prompt_attention(q, k, v, qi, wi, ki, rel_bias, k_sel):
    B, T = q.shape[:2]
    nblk = T // QBLK
    pos = jnp.arange(T, dtype=jnp.int32)

    def blk(args):
        qb, qib, wib, pb = args
        s = index_scores(qib, wib, ki, pb, pos)
        _, idx = lax.top_k(s, k_sel)
        return sparse_attn(qb, take_rows(k, idx), take_rows(v, idx), idx, pb, rel_bias)

    def to_blocks(a):
        return jnp.moveaxis(a.reshape((B, nblk, QBLK) + a.shape[2:]), 1, 0)

    out = lax.map(blk, (to_blocks(q), to_blocks(qi), to_blocks(wi), pos.reshape(nblk, QBLK)))
    return jnp.moveaxis(out, 0, 1).reshape(B, T, ATTN_WIDTH)


def sample_attention(q, k_new, v_new, qi, wi, ki_new, cache_k, cache_v, cache_ik, page_table, rel_bias, k_sel):
    Bd, Tn = q.shape[:2]
    past = page_table.shape[1] * PAGE_SIZE
    ki_past = cache_ik[page_table].reshape(Bd, past, INDEX_DIM)
    ki_all = jnp.concatenate([ki_past, ki_new.astype(ki_past.dtype)], axis=1)
    q_pos = past + jnp.arange(Tn, dtype=jnp.int32)
    k_pos = jnp.arange(past + Tn, dtype=jnp.int32)
    s = index_scores(qi, wi, ki_all, q_pos, k_pos)
    _, idx = lax.top_k(s, k_sel)
    in_past = idx < past
    pidx = jnp.minimum(idx, past - 1)
    phys = jax.vmap(lambda pt, i: pt[i])(page_table, pidx // PAGE_SIZE)
    off = pidx % PAGE_SIZE
    nidx = jnp.clip(idx - past, 0, Tn - 1)
    sel = in_past[..., None, None]
    k_sel_rows = jnp.where(sel, cache_k[phys, off].astype(k_new.dtype), take_rows(k_new, nidx))
    v_sel_rows = jnp.where(sel, cache_v[phys, off].astype(v_new.dtype), take_rows(v_new, nidx))
    out = sparse_attn(q, k_sel_rows, v_sel_rows, idx, q_pos, rel_bias)
    return out.reshape(Bd, Tn, ATTN_WIDTH)


def pool_means(u, n_prefix):
    uf = u.astype(jnp.float32)
    B, total, _ = u.shape
    cs = jnp.concatenate([jnp.zeros((B, 1, POOL_WIDTH), jnp.float32), lax.cumsum(uf, axis=1)], axis=1)
    j = jnp.arange(n_prefix, total, dtype=jnp.int32)
    hi = cs[:, n_prefix + 1:]
    cur = uf[:, n_prefix:]
    outs = []
    for g, w in enumerate(POOL_WINDOWS):
        sl = slice(g * POOL_GROUP_DIM, (g + 1) * POOL_GROUP_DIM)
        lo = jnp.maximum(j + 1 - w, 0)
        cnt = (j + 1 - lo).astype(jnp.float32)[None, :, None]
        outs.append((hi[..., sl] - cs[:, lo, sl]) / cnt - cur[..., sl])
    return jnp.stack(outs, axis=2)


def mixer_inputs(h, norm1_g, w_in, q_norm_g, k_norm_g):
    B, T, _ = h.shape
    hn = rms_norm(h, norm1_g)
    qa, ka, va, qi, ki, wi, u, ga, gb = split_cols(hn @ w_in)
    q = rms_norm(qa.reshape(B, T, N_HEADS, HEAD_DIM), q_norm_g)
    k = rms_norm(ka.reshape(B, T, N_HEADS, HEAD_DIM), k_norm_g)
    v = va.reshape(B, T, N_HEADS, HEAD_DIM)
    qi = qi.reshape(B, T, INDEX_HEADS, INDEX_DIM)
    return q, k, v, qi, ki, wi, u, ga, gb


def peer_block(xt, peer_wq, peer_subkeys, peer_u, peer_v):
    n = xt.shape[0]
    q = (xt @ peer_wq).astype(jnp.float32).reshape(n, PEER_HEADS, 2, PEER_DKEY // 2)
    s = jnp.einsum('nhpd,pkd->nhpk', q, peer_subkeys.astype(jnp.float32))
    s1, i1 = lax.top_k(s[:, :, 0], PEER_TOPK)
    s2, i2 = lax.top_k(s[:, :, 1], PEER_TOPK)
    ncand = PEER_TOPK * PEER_TOPK
    cand = (s1[..., :, None] + s2[..., None, :]).reshape(n, PEER_HEADS, ncand)
    cid = (i1[..., :, None] * PEER_NKEYS + i2[..., None, :]).reshape(n, PEER_HEADS, ncand)
    top_s, top_pos = lax.top_k(cand, PEER_TOPK)
    eid = jnp.take_along_axis(cid, top_pos, axis=-1)
    g = jax.nn.softmax(top_s, axis=-1)
    u = peer_u[eid]
    v = peer_v[eid]
    act = jax.nn.gelu(jnp.einsum('nd,nhkd->nhk', xt, u).astype(jnp.float32), approximate=False)
    return jnp.einsum('nhk,nhkd->nd', (g * act).astype(xt.dtype), v)


def peer_ffn(xn, peer_wq, peer_subkeys, peer_u, peer_v):
    N = xn.shape[0]
    n_pad = -(-N // TOKBLK) * TOKBLK
    xb = jnp.pad(xn, ((0, n_pad - N), (0, 0))).reshape(n_pad // TOKBLK, TOKBLK, D_MODEL)
    y = lax.map(lambda xt: peer_block(xt, peer_wq, peer_subkeys, peer_u, peer_v), xb)
    return y.reshape(n_pad, D_MODEL)[:N]


def merge_and_channel(h, attn_o, pool_m, ga, gb, w_pool, pool_scale, w_branch_attn, w_branch_pool,
                      w_out, norm2_g, peer_wq, peer_subkeys, peer_u, peer_v):
    B, T, _ = h.shape
    pool_o = (jnp.einsum('btgc,gcd->btgd', pool_m, w_pool.astype(jnp.float32)).reshape(B, T, POOL_WIDTH)
              * pool_scale.astype(jnp.float32)).astype(h.dtype)
    merged = jax.nn.sigmoid(ga) * (attn_o @ w_branch_attn) + jax.nn.sigmoid(gb) * (pool_o @ w_branch_pool)
    h = h + merged @ w_out
    xn = rms_norm(h, norm2_g).reshape(B * T, D_MODEL)
    return h + peer_ffn(xn, peer_wq, peer_subkeys, peer_u, peer_v).reshape(B, T, D_MODEL)


def setup_inputs(seed: int = 0) -> dict:
    key = jax.random.key(seed)
    ks = jax.random.split(key, 24)
    f32 = jnp.float32
    n_pages = PAST_LEN // PAGE_SIZE
    n_used = DEC_BATCH * n_pages
    n_phys = n_used + (n_used + 3) // 4

    def nrm(k, shape, scale):
        return jax.random.normal(k, shape, f32) * scale

    page_table = jax.random.permutation(ks[6], n_phys)[:n_used].reshape(DEC_BATCH, n_pages).astype(jnp.int32)
    return {
        'x_prompt': nrm(ks[0], (BATCH, SEQ, D_MODEL), 1.0),
        'x_sample': nrm(ks[1], (DEC_BATCH, DEC_SEQ, D_MODEL), 1.0),
        'cache_k': nrm(ks[2], (DEPTH, n_phys, PAGE_SIZE, N_HEADS, HEAD_DIM), 1.0),
        'cache_v': nrm(ks[3], (DEPTH, n_phys, PAGE_SIZE, N_HEADS, HEAD_DIM), 1.0),
        'cache_idx_k': nrm(ks[4], (DEPTH, n_phys, PAGE_SIZE, INDEX_DIM), 1.0),
        'state_pool': nrm(ks[5], (DEPTH, DEC_BATCH, POOL_STATE, POOL_WIDTH), 1.0),
        'page_table': page_table,
        'meta_tokens': nrm(ks[7], (N_META, D_MODEL), 1.0),
        'rel_bias': nrm(ks[8], (NUM_BUCKETS, N_HEADS), 0.5),
        'norm1_g': 1.0 + nrm(ks[9], (DEPTH, D_MODEL), 0.02),
        'w_in': nrm(ks[10], (DEPTH, D_MODEL, IN_WIDTH), D_MODEL ** -0.5),
        'q_norm_g': 1.0 + nrm(ks[11], (DEPTH, HEAD_DIM), 0.02),
        'k_norm_g': 1.0 + nrm(ks[12], (DEPTH, HEAD_DIM), 0.02),
        'w_pool': nrm(ks[13], (DEPTH, POOL_GROUPS, POOL_GROUP_DIM, POOL_GROUP_DIM), POOL_GROUP_DIM ** -0.5),
        'pool_scale': 1.0 + nrm(ks[14], (DEPTH, POOL_WIDTH), 0.02),
        'w_branch_attn': nrm(ks[15], (DEPTH, ATTN_WIDTH, D_MODEL), ATTN_WIDTH ** -0.5),
        'w_branch_pool': nrm(ks[16], (DEPTH, POOL_WIDTH, D_MODEL), POOL_WIDTH ** -0.5),
        'w_out': nrm(ks[17], (DEPTH, D_MODEL, D_MODEL), D_MODEL ** -0.5),
        'norm2_g': 1.0 + nrm(ks[18], (DEPTH, D_MODEL), 0.02),
        'peer_wq': nrm(ks[19], (DEPTH, D_MODEL, PEER_HEADS * PEER_DKEY), D_MODEL ** -0.5),
        'peer_subkeys': nrm(ks[20], (DEPTH, 2, PEER_NKEYS, PEER_DKEY // 2), (PEER_DKEY // 2) ** -0.5),
        'peer_u': nrm(ks[21], (DEPTH, PEER_EXPERTS, D_MODEL), D_MODEL ** -0.5),
        'peer_v': nrm(ks[22], (DEPTH, PEER_EXPERTS, D_MODEL), PEER_HEADS ** -0.5),
    }


def reference(x_prompt, x_sample, cache_k, cache_v, cache_idx_k, state_pool, page_table,
              meta_tokens, rel_bias, norm1_g, w_in, q_norm_g, k_norm_g, w_pool, pool_scale,
              w_branch_attn, w_branch_pool, w_out, norm2_g, peer_wq, peer_subkeys, peer_u, peer_v):
    B, S, D = x_prompt.shape
    T = S + N_META
    T_pad = -(-T // QBLK) * QBLK
    meta = jnp.broadcast_to(meta_tokens[None].astype(x_prompt.dtype), (B, N_META, D))
    hp = jnp.pad(jnp.concatenate([meta, x_prompt], axis=1), ((0, 0), (0, T_pad - T), (0, 0)))
    hs = x_sample
    Tn = hs.shape[1]
    past = page_table.shape[1] * PAGE_SIZE
    k_sel_p = min(TOPK_MAX, S // 4)
    k_sel_s = min(TOPK_MAX, (past + Tn) // 4)

    kp_l, vp_l, ip_l, pp_l, ks_l, vs_l, is_l, ps_l = [], [], [], [], [], [], [], []
    for l in range(DEPTH):
        q, k, v, qi, ki, wi, u, ga, gb = mixer_inputs(hp, norm1_g[l], w_in[l], q_norm_g[l], k_norm_g[l])
        attn_o = prompt_attention(q, k, v, qi, wi, ki, rel_bias, k_sel_p)
        pool_m = pool_means(u, 0)
        hp = merge_and_channel(hp, attn_o, pool_m, ga, gb, w_pool[l], pool_scale[l], w_branch_attn[l],
                               w_branch_pool[l], w_out[l], norm2_g[l], peer_wq[l], peer_subkeys[l],
                               peer_u[l], peer_v[l])
        kp_l.append(k[:, :T])
        vp_l.append(v[:, :T])
        ip_l.append(ki[:, :T])
        pp_l.append(u[:, T - POOL_STATE:T])

        q, k, v, qi, ki, wi, u, ga, gb = mixer_inputs(hs, norm1_g[l], w_in[l], q_norm_g[l], k_norm_g[l])
        attn_o = sample_attention(q, k, v, qi, wi, ki, cache_k[l], cache_v[l], cache_idx_k[l],
                                  page_table, rel_bias, k_sel_s)
        u_all = jnp.concatenate([state_pool[l].astype(u.dtype), u], axis=1)
        pool_m = pool_means(u_all, POOL_STATE)
        hs = merge_and_channel(hs, attn_o, pool_m, ga, gb, w_pool[l], pool_scale[l], w_branch_attn[l],
                               w_branch_pool[l], w_out[l], norm2_g[l], peer_wq[l], peer_subkeys[l],
                               peer_u[l], peer_v[l])
        ks_l.append(k)
        vs_l.append(v)
        is_l.append(ki)
        ps_l.append(u_all[:, -POOL_STATE:])

    y_prompt = hp[:, N_META:T]
    y_sample = hs
    return (y_prompt, y_sample, jnp.stack(kp_l), jnp.stack(vp_l), jnp.stack(ip_l), jnp.stack(pp_l),
            jnp.stack(ks_l), jnp.stack(vs_l), jnp.stack(is_l), jnp.stack(ps_l))
```

```python
import numpy as np
from contextlib import ExitStack
import concourse.bass as bass
import concourse.mybir as mybir
from concourse.bass_utils import run_bass_kernel_spmd

F32 = mybir.dt.float32
BF16 = mybir.dt.bfloat16
I32 = mybir.dt.int32
U32 = mybir.dt.uint32
ALU = mybir.AluOpType
AF = mybir.ActivationFunctionType
AX = mybir.AxisListType

D = 1024
NBLK_A = 68
NQ = 17
EPS = 1e-6
IN_W = 4680
CH_Q, CH_K, CH_V, CH_QI, CH_KW, CH_U, CH_GA0, CH_GA1, CH_GB0, CH_GB1 = range(10)
CHUNKS = [(0, 512), (512, 512), (1024, 512), (1536, 512), (2048, 72), (2120, 512),
          (2632, 512), (3144, 512), (3656, 512), (4168, 512)]
N_DMA_SEMS = 12
NIT = 16


class Buf:
    def __init__(self, t, name):
        self.t = t
        self.name = name
        self.w = None
        self.r = {}

    def __getitem__(self, idx):
        return self.t[idx]


class KB:
    def __init__(self, nc, es, plan=None):
        self.nc = nc
        self.es = es
        self.plan = plan
        self.targets = {e: set() for e in ("pe", "dve", "act", "pool", "sp")}
        self.rank = None
        if plan is not None:
            self.rank = {e: {idx: r + 1 for r, idx in enumerate(sorted(plan[e]))} for e in plan}
        self.eng = {"pe": nc.tensor, "dve": nc.vector, "act": nc.scalar, "pool": nc.gpsimd, "sp": nc.sync}
        self.sem = {e: es.enter_context(nc.semaphore("s_" + e)) for e in self.eng}
        self.cnt = {e: 0 for e in self.eng}
        self.seen = {e: {} for e in self.eng}
        self.dsem = [es.enter_context(nc.semaphore("d_%d" % i)) for i in range(N_DMA_SEMS)]
        self.dcnt = [0] * N_DMA_SEMS
        self.drr = 0
        self.nbuf = 0

    def sb(self, shape, dt, name=None):
        self.nbuf += 1
        name = "sb_" + (name or ("%d" % self.nbuf))
        return Buf(self.es.enter_context(self.nc.sbuf_tensor(name, list(shape), dt)), name)

    def ps(self, shape, dt, name=None):
        self.nbuf += 1
        name = name or ("ps%d" % self.nbuf)
        return Buf(self.es.enter_context(self.nc.psum_tensor(name, list(shape), dt)), name)

    def dram(self, name, shape, dt, kind="Internal"):
        return Buf(self.nc.dram_tensor(name, list(shape), dt, kind=kind).ap(), name)

    def _deps(self, e, reads, writes):
        need = {}

        def add(tok):
            if tok is None:
                return
            key, sem, val = tok
            if key == "pe" and e == "pe":
                return
            if need.get(key, (None, 0))[1] < val:
                need[key] = (sem, val)

        for b in reads:
            add(b.w)
        for b in writes:
            add(b.w)
            for tok in b.r.values():
                add(tok)
        eo = self.eng[e]
        for key, (sem, val) in need.items():
            if self.seen[e].get(key, 0) < val:
                self.seen[e][key] = val
                if key in self.targets:
                    if self.plan is None:
                        self.targets[key].add(val)
                    else:
                        eo.wait_ge(sem, self.rank[key][val])
                elif self.plan is not None:
                    eo.wait_ge(sem, val)

    def _record(self, tok, reads, writes):
        for b in reads:
            if b.r.get(tok[0], (None, None, 0))[2] < tok[2]:
                b.r[tok[0]] = tok
        for b in writes:
            b.w = tok
            b.r = {}

    def op(self, e, fn, reads=(), writes=()):
        self._deps(e, reads, writes)
        self.cnt[e] += 1
        if self.plan is not None:
            ins = fn(self.eng[e])
            if self.cnt[e] in self.plan[e]:
                ins.then_inc(self.sem[e], 1)
        self._record((e, self.sem[e], self.cnt[e]), reads, writes)

    def dma(self, q, out, in_, reads=(), writes=(), indirect=None):
        i = self.drr
        self.drr = (i + 1) % N_DMA_SEMS
        eo = self.eng[q]
        key = "d%d" % i
        if self.dcnt[i] > 0 and self.seen[q].get(key, 0) < 16 * self.dcnt[i]:
            if self.plan is not None:
                eo.wait_ge(self.dsem[i], 16 * self.dcnt[i])
            self.seen[q][key] = 16 * self.dcnt[i]
        self._deps(q, reads, writes)
        self.dcnt[i] += 1
        if self.plan is not None:
            if indirect is None:
                ins = eo.dma_start(out=out, in_=in_)
            else:
                ins = eo.indirect_dma_start(out=out, out_offset=None, in_=in_, in_offset=indirect)
            ins.then_inc(self.dsem[i], 16)
        self._record((key, self.dsem[i], 16 * self.dcnt[i]), reads, writes)

    def finish(self):
        eo = self.eng["sp"]
        if self.plan is None:
            for e in ("pe", "dve", "act", "pool"):
                if self.cnt[e] > 0:
                    self.targets[e].add(self.cnt[e])
            return
        for i in range(N_DMA_SEMS):
            if self.dcnt[i] > 0:
                eo.wait_ge(self.dsem[i], 16 * self.dcnt[i])
        for e in ("pe", "dve", "act", "pool"):
            if self.cnt[e] > 0:
                eo.wait_ge(self.sem[e], self.rank[e][self.cnt[e]])


def build(cfg):
    _, kb_dry = _build(cfg, None)
    nc, _ = _build(cfg, kb_dry.targets)
    return nc


def _build(cfg, plan):
    nblk_a = cfg.get("nblk_a", NBLK_A)
    nq = cfg.get("nq", NQ)
    do_attn = cfg.get("attn", True)
    do_tail = cfg.get("tail", True)
    nkeys = nblk_a * 128
    nc = bass.Bass("TRN2", target_bir_lowering=False)
    es = ExitStack()
    kb = KB(nc, es, plan)
    nc._es_keep = es

    def din(name, shape, dt=F32):
        return Buf(nc.dram_tensor(name, list(shape), dt, kind="ExternalInput").ap(), name)

    def dout(name, shape, dt=F32):
        return Buf(nc.dram_tensor(name, list(shape), dt, kind="ExternalOutput").ap(), name)

    xcat = din("xcat", [nkeys, D])
    xown = din("xown", [nq, 128, D])
    xprev = din("xprev", [nq, 16, D])
    w_in = din("w_in", [D, IN_W])
    norm1_g = din("norm1_g", [D])
    q_norm_g = din("q_norm_g", [64])
    k_norm_g = din("k_norm_g", [64])
    ident_in = din("ident", [128, 128])
    rel_bias = din("rel_bias", [32, 8])
    qs_in = din("qs", [128, 128])
    thrtab_in = din("thrtab", [128, 155])
    cmask_in = din("cmask", [128, 512])
    pw_in = din("pw", [128, NIT])
    w_ba = din("w_ba", [512, D])
    w_bp = din("w_bp", [512, D])
    w_out = din("w_out", [D, D])
    peer_wq = din("peer_wq", [D, D])
    w_pool = din("w_pool", [4, 128, 128])
    pool_scale = din("pool_scale", [512])
    norm2_g = din("norm2_g", [D])
    subkeys = din("subkeys", [2, 128, 64])
    peer_u = din("peer_u", [16384, D])
    peer_v = din("peer_v", [16384, D])
    rcnt_in = din("rcnt", [128, 4, 128])
    iota16_in = din("iota16", [128, 16])
    k_own = dout("k_own", [nq, 128, 512])
    v_own = dout("v_own", [nq, 128, 512])
    ki_own = dout("ki_own", [nq, 128, 64])
    y_own = dout("y_own", [nq, 128, D])
    u_last = dout("u_last", [128, 4, 144])
    y_s_out = dout("y_s", [128, D])
    do_sample = cfg.get("sample", True)
    if do_sample:
        xs_in = din("xs_own", [128, D])
        cache_k = din("cache_k", [2560 * 128, 512])
        cache_v = din("cache_v", [2560 * 128, 512])
        cache_ik = din("cache_ik", [2560, 8192])
        state_in = din("state_own", [16, 15, 512])
        pt_in = din("pt_own", [16, 16], I32)
        zsel_in = din("zsel", [128, 31])
        k_s_out = dout("ks_o", [128, 512])
        v_s_out = dout("vs_o", [128, 512])
        ki_s_out = dout("kis_o", [128, 64])
        pool_s_out = dout("pool_s", [16, 15, 512])
        qi_d = kb.dram("qi_d", [16, 512], F32)
        wi_d = kb.dram("wi_d", [16, 8], F32)
        q_d = kb.dram("q_d", [16, 512], F32)
        sc_d = kb.dram("sc_d", [16, 2048], F32)
    wba_bf = kb.dram("wba_bf", [128, 4, D], BF16)
    wbp_bf = kb.dram("wbp_bf", [128, 4, D], BF16)
    wout_bf = kb.dram("wout_bf", [128, 8, D], BF16)
    pwq_bf = kb.dram("pwq_bf", [128, 8, D], BF16)
    win_bf = kb.dram("win_bf", [128, 8, IN_W], BF16)
    kt_s = kb.dram("kt_s", [128, 4, nkeys], BF16)
    v_s = kb.dram("v_s", [nkeys, 8, 65], BF16)
    kit_s = kb.dram("kit_s", [128, nkeys], BF16)

    ident_f = kb.sb([128, 128], F32, "ident_f")
    ident_b = kb.sb([128, 128], BF16, "ident_b")
    g1col = kb.sb([128, 8], F32, "g1col")
    qg = kb.sb([128, 64], F32, "qg")
    kg = kb.sb([128, 64], F32, "kg")
    kb.dma("sp", ident_f[:], ident_in[:, :], writes=[ident_f])
    kb.op("dve", lambda e: e.tensor_copy(out=ident_b[:], in_=ident_f[:]), reads=[ident_f], writes=[ident_b])
    ident4 = kb.sb([128, 512], BF16, "ident4")
    for j4 in range(4):
        kb.op("dve", lambda e, j4=j4: e.tensor_copy(out=ident4[:, j4 * 128:(j4 + 1) * 128], in_=ident_f[:]),
              reads=[ident_f, ident4], writes=[ident4])
    with nc.allow_non_contiguous_dma(reason="tiny param loads"):
        kb.dma("sp", g1col[:], norm1_g.t.rearrange("(k p) -> p k", p=128), writes=[g1col])
        kb.dma("sp", qg[:], q_norm_g.t.partition_broadcast(128), writes=[qg])
        kb.dma("sp", kg[:], k_norm_g.t.partition_broadcast(128), writes=[kg])
    kb.op("dve", lambda e: e.tensor_scalar(out=qg[:], in0=qg[:], scalar1=0.125, scalar2=None, op0=ALU.mult),
          reads=[qg], writes=[qg])

    pb = [kb.ps([128, 512], F32, "bank%d" % i) for i in range(8)]

    ktc = [kb.sb([128, 4, 512], BF16, "ktc%d" % j) for j in range(2)]
    vch = [kb.sb([128, 4, 520], BF16, "vch%d" % j) for j in range(2)]

    class View:
        def __init__(self, owner, ap):
            self.owner = owner
            self.ap = ap

        def __getitem__(self, idx):
            return self.ap[idx]

        @property
        def w(self):
            return self.owner.w

        @w.setter
        def w(self, v):
            self.owner.w = v

        @property
        def r(self):
            return self.owner.r

        @r.setter
        def r(self, v):
            self.owner.r = v

    wst_v = [View(ktc[j], ktc[j].t[:].rearrange("p a b -> p (a b)").bitcast(F32)[:, 0:1024]) for j in range(2)]
    wsb_v = [View(vch[j], vch[j].t[:].rearrange("p a b -> p (a b)")[:, 0:1024]) for j in range(2)]
    pcount = [0]

    def conv_w(src_ap, dst_ap, ncol, scal):
        s = pcount[0] % 2
        pcount[0] += 1
        kb.dma("sp", wst_v[s][:, 0:ncol], src_ap, writes=[wst_v[s].owner])
        kb.op("dve", lambda e: e.tensor_scalar(out=wsb_v[s][:, 0:ncol], in0=wst_v[s][:, 0:ncol], scalar1=scal,
                                               scalar2=None, op0=ALU.mult),
              reads=[wst_v[s].owner, g1col], writes=[wsb_v[s].owner])
        kb.dma("sp", dst_ap, wsb_v[s][:, 0:ncol], reads=[wsb_v[s].owner], writes=[win_bf])

    for kc in range(8):
        for pc in range(5):
            conv_w(w_in[kc * 128:(kc + 1) * 128, pc * 936:(pc + 1) * 936], win_bf[:, kc, pc * 936:(pc + 1) * 936],
                   936, g1col[:, kc:kc + 1])
    if do_tail:
        for (wsrc, wdst, nkc) in ((w_ba, wba_bf, 4), (w_bp, wbp_bf, 4), (w_out, wout_bf, 8), (peer_wq, pwq_bf, 8)):
            for kc in range(nkc):
                conv_w(wsrc[kc * 128:(kc + 1) * 128, :], wdst[:, kc, :], 1024, 1.0)

    wslot = [kb.sb([128, 8, 512], BF16, "wslot%d" % i) for i in range(2)]
    wstate = {"i": 0}

    def load_w(src, c0, cw, nk=8):
        s = wslot[wstate["i"] % 2]
        wstate["i"] += 1
        kb.dma("sp", s[:, 0:nk, 0:cw], src[:, 0:nk, c0:c0 + cw], reads=[src], writes=[s])
        return s

    xt = [kb.sb([128, D], F32, "xt%d" % i) for i in range(2)]
    xn = kb.sb([128, D], F32, "xn")
    junk = kb.sb([128, D], BF16, "junk")
    ssq = kb.sb([128, 1], F32, "ssq")
    rstd = kb.sb([128, 1], F32, "rstd")
    xs = kb.sb([128, D], BF16, "xs")
    hnT = kb.sb([128, 8, 128], BF16, "hnT")
    tp_bank = pb[2]

    def norm_transpose(xb, dstT, ntok=128, gtile=None, keep=None, xowner=None):
        xo = xowner or xb
        kb.op("act", lambda e: e.activation(out=junk[0:ntok, :], in_=xb[0:ntok, :], func=AF.Square,
                                            accum_out=ssq[0:ntok, :]),
              reads=[xo], writes=[junk, ssq])
        kb.op("act", lambda e: e.activation(out=rstd[0:ntok, :], in_=ssq[0:ntok, :], func=AF.Sqrt,
                                            scale=1.0 / D, bias=EPS),
              reads=[ssq], writes=[rstd])
        kb.op("dve", lambda e: e.reciprocal(out=rstd[0:ntok, :], in_=rstd[0:ntok, :]), reads=[rstd], writes=[rstd])
        if gtile is None:
            kb.op("dve", lambda e: e.tensor_scalar(out=xs[0:ntok, :], in0=xb[0:ntok, :], scalar1=rstd[0:ntok, :],
                                                   scalar2=None, op0=ALU.mult),
                  reads=[xo, rstd], writes=[xs])
        else:
            kb.op("dve", lambda e: e.scalar_tensor_tensor(out=keep[0:ntok, :], in0=xb[0:ntok, :],
                                                          scalar=rstd[0:ntok, :], in1=gtile[0:ntok, :],
                                                          op0=ALU.mult, op1=ALU.mult),
                  reads=[xo, rstd, gtile], writes=[keep])
            kb.op("dve", lambda e: e.tensor_copy(out=xs[0:ntok, :], in_=keep[0:ntok, :]), reads=[keep], writes=[xs])
        tpv = tp_bank.t[:].bitcast(BF16)
        for kc in range(8):
            kb.op("pe", lambda e, kc=kc: e.transpose(out=tpv[:, kc * 128:kc * 128 + ntok],
                                                     in_=xs[0:ntok, kc * 128:(kc + 1) * 128],
                                                     identity=ident_b[0:ntok, 0:ntok]),
                  reads=[xs, ident_b], writes=[tp_bank])
        kb.op("dve", lambda e: e.tensor_copy(
            out=dstT[:, :, 0:ntok], in_=tpv.rearrange("p (k t) -> p k t", k=8)[:, :, 0:ntok]),
            reads=[tp_bank], writes=[dstT])

    def proj_tok(dst_bank, wsl, cw, srcT=None, ntok=128):
        srcT = srcT or hnT
        for kc in range(8):
            kb.op("pe", lambda e, kc=kc: e.matmul(dst_bank[0:ntok, 0:cw], lhsT=srcT[:, kc, 0:ntok],
                                                  rhs=wsl[:, kc, 0:cw], start=(kc == 0), stop=(kc == 7)),
                  reads=[srcT, wsl], writes=[dst_bank])

    hsq = kb.sb([128, 512], F32, "hsq")
    hss = kb.sb([128, 8], F32, "hss")
    hrs = kb.sb([128, 8], F32, "hrs")

    def head_norm(src_bank, gain, dst):
        kb.op("act", lambda e: e.activation(out=hsq[:], in_=src_bank[:, 0:512], func=AF.Square),
              reads=[src_bank], writes=[hsq])
        kb.op("dve", lambda e: e.tensor_reduce(out=hss[:], in_=hsq[:].rearrange("p (h d) -> p h d", h=8),
                                               axis=AX.X, op=ALU.add),
              reads=[hsq], writes=[hss])
        kb.op("act", lambda e: e.activation(out=hrs[:], in_=hss[:], func=AF.Sqrt, scale=1.0 / 64, bias=EPS),
              reads=[hss], writes=[hrs])
        kb.op("dve", lambda e: e.reciprocal(out=hrs[:], in_=hrs[:]), reads=[hrs], writes=[hrs])
        kb.op("dve", lambda e: e.tensor_tensor(out=hsq[:].rearrange("p (h d) -> p h d", h=8),
                                               in0=src_bank[:, 0:512].rearrange("p (h d) -> p h d", h=8),
                                               in1=hrs[:].unsqueeze(2).to_broadcast([128, 8, 64]), op=ALU.mult),
              reads=[src_bank, hrs], writes=[hsq])
        kb.op("dve", lambda e: e.tensor_tensor(out=dst[:].rearrange("p (h d) -> p h d", h=8),
                                               in0=hsq[:].rearrange("p (h d) -> p h d", h=8),
                                               in1=gain[:].unsqueeze(1).to_broadcast([128, 8, 64]), op=ALU.mult),
              reads=[hsq, gain], writes=[dst])

    wk = wslot[0]
    wv = wslot[1]
    wkw = kb.sb([128, 8, 72], BF16, "wkw")
    kb.dma("sp", wk[:], win_bf[:, :, 512:1024], reads=[win_bf], writes=[wk])
    kb.dma("sp", wv[:], win_bf[:, :, 1024:1536], reads=[win_bf], writes=[wv])
    kb.dma("sp", wkw[:], win_bf[:, :, 2048:2120], reads=[win_bf], writes=[wkw])
    kf = kb.sb([128, 512], F32, "kf")
    kbf = kb.sb([128, 512], BF16, "kbf")
    ktb = kb.sb([128, 4, 128], BF16, "ktb")
    vb = kb.sb([128, 8, 65], BF16, "vb")
    kib = kb.sb([128, 128], BF16, "kib")
    kitb = kb.sb([128, 128], BF16, "kitb")
    kb.op("dve", lambda e: e.memset(vb[:], 1.0), writes=[vb])
    for blk in range(nblk_a):
        xb = xt[blk % 2]
        kb.dma("sp", xb[:], xcat[blk * 128:(blk + 1) * 128, :], writes=[xb])
        norm_transpose(xb, hnT)
        proj_tok(pb[0], wk, 512)
        head_norm(pb[0], kg, kf)
        kb.op("dve", lambda e: e.tensor_copy(out=kbf[:], in_=kf[:]), reads=[kf], writes=[kbf])
        tpv = tp_bank.t[:].bitcast(BF16)
        for pr in range(4):
            kb.op("pe", lambda e, pr=pr: e.transpose(out=tpv[:, pr * 128:(pr + 1) * 128],
                                                     in_=kbf[:, pr * 128:(pr + 1) * 128], identity=ident_b[:]),
                  reads=[kbf, ident_b], writes=[tp_bank])
        kb.op("act", lambda e: e.copy(out=ktb[:], in_=tpv[:, 0:512].rearrange("p (a t) -> p a t", a=4)),
              reads=[tp_bank], writes=[ktb])
        kb.dma("sp", kt_s[:, :, blk * 128:(blk + 1) * 128], ktb[:], reads=[ktb], writes=[kt_s])
        proj_tok(pb[1], wv, 512)
        kb.op("act", lambda e: e.copy(out=vb[:, :, 0:64], in_=pb[1][:, 0:512].rearrange("p (h d) -> p h d", h=8)),
              reads=[pb[1]], writes=[vb])
        kb.dma("sp", v_s[blk * 128:(blk + 1) * 128, :, :], vb[:], reads=[vb], writes=[v_s])
        proj_tok(pb[0], wkw, 72)
        kb.op("dve", lambda e: e.tensor_copy(out=kib[:, 0:64], in_=pb[0][:, 0:64]), reads=[pb[0]], writes=[kib])
        kb.op("dve", lambda e: e.tensor_copy(out=kib[:, 64:128], in_=pb[0][:, 0:64]), reads=[pb[0]], writes=[kib])
        kb.op("pe", lambda e: e.transpose(out=tpv[:, 512:640], in_=kib[:], identity=ident_b[:]),
              reads=[kib, ident_b], writes=[tp_bank])
        kb.op("act", lambda e: e.copy(out=kitb[:], in_=tpv[:, 512:640]), reads=[tp_bank], writes=[kitb])
        kb.dma("sp", kit_s[:, blk * 128:(blk + 1) * 128], kitb[:], reads=[kitb], writes=[kit_s])

    ko = kb.sb([128, 512], F32, "ko")
    vo = kb.sb([128, 512], F32, "vo")
    kio = kb.sb([128, 64], F32, "kio")
    wis = kb.sb([128, 8], F32, "wis")
    dbg_ao = dout("dbg_ao", [nq, 128, 512]) if cfg.get("dbg") else None
    if do_attn:
        qf = kb.sb([128, 512], F32, "qf")
        qbf = kb.sb([128, 512], BF16, "qbf")
        qT2 = kb.sb([128, 4, 128], BF16, "qT2")
        qiT2 = kb.sb([128, 4, 128], BF16, "qiT2")
        kitc = [kb.sb([128, 512], BF16, "kitc%d" % j) for j in range(2)]
        rl = [kb.sb([128, 512], BF16, "rl%d" % j) for j in range(2)]
        Isc = kb.sb([128, max(nkeys, 8704)], F32, "Isc")
        junkI = kb.sb([128, 2176], BF16, "junkI")
        cnt4 = kb.sb([128, 4], F32, "cnt4")
        hi0 = kb.sb([128, 1], F32, "hi0")
        lo = kb.sb([128, 1], F32, "lo")
        mid = kb.sb([128, 1], F32, "mid")
        cntt = kb.sb([128, 1], F32, "cntt")
        dl = kb.sb([128, 1], F32, "dl")
        wh = kb.sb([128, NIT], F32, "wh")
        pw = kb.sb([128, NIT], F32, "pw")
        cmask = kb.sb([128, 512], F32, "cmask")
        negm = [kb.sb([128, 128], BF16, "negm%d" % j) for j in range(2)]
        addh = kb.sb([128, 8, 128], BF16, "addh")
        PT = [kb.sb([128, 8, 128], BF16, "PT%d" % j) for j in range(2)]
        rden = kb.sb([128, 8], F32, "rden")
        ao = kb.sb([128, 512], BF16, "ao")
        aof = kb.sb([128, 512], F32, "aof") if cfg.get("dbg") else None
        kb.dma("sp", pw[:], pw_in[:, :], writes=[pw])
        kb.dma("sp", cmask[:], cmask_in[:, :], writes=[cmask])
        qs = kb.sb([128, 128], F32, "qs")
        thrtab = kb.sb([128, 155], F32, "thrtab")
        rbb = kb.sb([128, 256], F32, "rbb")
        ndel = kb.sb([128, 256], F32, "ndel")
        ind = kb.sb([128, 128], F32, "ind")
        NB = [kb.sb([128, 8, 128], BF16, "NB%d" % j) for j in range(5)]
        NBt = kb.sb([128, 8, 128], F32, "h2")
        h2 = View(NBt, NBt.t[:].rearrange("p a b -> p (a b)"))
        kb.dma("sp", qs[:], qs_in[:, :], writes=[qs])
        kb.dma("sp", thrtab[:], thrtab_in[:, :], writes=[thrtab])
        with nc.allow_non_contiguous_dma(reason="tiny param loads"):
            kb.dma("sp", rbb[:], rel_bias.t.rearrange("b h -> (b h)").partition_broadcast(128), writes=[rbb])
        kb.op("dve", lambda e: e.tensor_tensor(out=ndel[:, 8:256], in0=rbb[:, 0:248], in1=rbb[:, 8:256],
                                               op=ALU.subtract), reads=[rbb], writes=[ndel])
        for r5 in range(5):
            kb.op("dve", lambda e: e.memset(NBt[:], 0.0), writes=[NBt])
            for b in range(1, 32):
                col = r5 * 31 + (b - 1)
                kb.op("dve", lambda e, col=col: e.tensor_scalar(out=ind[:], in0=qs[:], scalar1=thrtab[:, col:col + 1],
                                                                scalar2=None, op0=ALU.is_lt),
                      reads=[qs, thrtab], writes=[ind])
                for h in range(8):
                    kb.op("dve", lambda e, b=b, h=h: e.scalar_tensor_tensor(
                        out=NBt[:, h, :], in0=ind[:], scalar=ndel[:, b * 8 + h:b * 8 + h + 1], in1=NBt[:, h, :],
                        op0=ALU.mult, op1=ALU.add), reads=[ind, ndel, NBt], writes=[NBt])
            kb.op("dve", lambda e, r5=r5: e.tensor_copy(out=NB[r5][:], in_=NBt[:]), reads=[NBt], writes=[NB[r5]])
    if do_tail:
        aoT = kb.sb([128, 4, 128], BF16, "aoT")
        hnTp = kb.sb([128, 8, 16], BF16, "hnTp")
        xpv = kb.sb([16, D], F32, "xpv")
        uT = kb.sb([128, 4, 144], F32, "uT")
        s1 = kb.sb([128, 4, 144], F32, "s1")
        s2 = kb.sb([128, 4, 144], F32, "s2")
        s3 = kb.sb([128, 4, 144], F32, "s3")
        pmf = kb.sb([128, 4, 128], F32, "pmf")
        pmT = kb.sb([128, 4, 128], BF16, "pmT")
        poT = kb.sb([128, 4, 128], BF16, "poT")
        sga = kb.sb([128, 8, 128], BF16, "sga")
        sgb = kb.sb([128, 8, 128], BF16, "sgb")
        tmpA = kb.sb([128, 512], F32, "tmpA")
        tmpB = kb.sb([128, 512], F32, "tmpB")
        mT = kb.sb([128, 8, 128], BF16, "mT")
        xnT = sgb
        qpT = sga
        g2b = kb.sb([128, D], F32, "g2b")
        wpl = kb.sb([128, 4, 128], BF16, "wpl")
        wplf = kb.sb([128, 4, 128], F32, "wplf")
        pscol = kb.sb([128, 4], F32, "pscol")
        SKf = kb.sb([128, 256], F32, "SKf")
        sktmp = kb.sb([128, 128], F32, "sktmp")
        SK = kb.sb([128, 256], BF16, "SK")
        rcnt = kb.sb([128, 4, 128], F32, "rcnt")
        iota16 = kb.sb([128, 16], F32, "iota16")
        tv = kb.sb([128, 8, 2, 16], F32, "tv")
        ti = kb.sb([128, 8, 2, 16], U32, "ti")
        tif = kb.sb([128, 8, 2, 16], F32, "tif")
        tsv = kb.sb([128, 8, 16], F32, "tsv")
        tpos = kb.sb([128, 8, 16], U32, "tpos")
        pa = kb.sb([128, 8, 16], U32, "pa")
        pbb = kb.sb([128, 8, 16], U32, "pbb")
        paf = kb.sb([128, 8, 16], F32, "paf")
        pbf = kb.sb([128, 8, 16], F32, "pbf")
        i1s = kb.sb([128, 8, 16], F32, "i1s")
        i2s = kb.sb([128, 8, 16], F32, "i2s")
        eidf = kb.sb([128, 128], F32, "eidf")
        eidi = kb.sb([128, 128], I32, "eidi")
        gmx = kb.sb([128, 8], F32, "gmx")
        gk = kb.sb([128, 8, 16], F32, "gk")
        actv = kb.sb([128, 128], F32, "actv")
        wgt = kb.sb([128, 128], F32, "wgt")
        m8 = kb.sb([128, 8], F32, "m8")
        UbS = [View(o, o.t[:].rearrange("p a b -> p (a b)").bitcast(F32)[:, 0:1024]) for o in (ktc[0], ktc[1], vch[0], vch[1])]
        Ubo = [kb.sb([128, D], F32, "Ub%d" % j) for j in range(3)]
        Ub = [View(o, o.t[:]) for o in Ubo]
        kb.dma("sp", rcnt[:], rcnt_in[:, :, :], writes=[rcnt])
        kb.dma("sp", iota16[:], iota16_in[:, :], writes=[iota16])
        kb.op("dve", lambda e: e.memset(SKf[:], 0.0), writes=[SKf])
        with nc.allow_non_contiguous_dma(reason="small param loads"):
            kb.dma("sp", g2b[:], norm2_g.t.partition_broadcast(128), writes=[g2b])
            kb.dma("sp", pscol[:], pool_scale.t.rearrange("(g d) -> d g", d=128), writes=[pscol])
            kb.dma("sp", wplf[:], w_pool.t.rearrange("g c d -> c g d"), writes=[wplf])
            kb.dma("sp", sktmp[:, 0:64], subkeys.t[0], writes=[sktmp])
            kb.dma("sp", sktmp[:, 64:128], subkeys.t[1], writes=[sktmp])
        kb.op("pe", lambda e: e.transpose(out=pb[2][:, 0:128], in_=sktmp[:], identity=ident_f[:]),
              reads=[sktmp, ident_f], writes=[pb[2]])
        kb.op("dve", lambda e: e.tensor_copy(out=SKf[0:64, 0:128], in_=pb[2][0:64, 0:128]), reads=[pb[2], SKf], writes=[SKf])
        kb.op("dve", lambda e: e.tensor_copy(out=SKf[64:128, 128:256], in_=pb[2][64:128, 0:128]), reads=[pb[2], SKf],
              writes=[SKf])
        kb.op("dve", lambda e: e.tensor_copy(out=SK[:], in_=SKf[:]), reads=[SKf], writes=[SK])
        kb.op("dve", lambda e: e.tensor_copy(out=wpl[:], in_=wplf[:]), reads=[wplf], writes=[wpl])
        IscV = Isc.t[:]
        ssc = IscV[:, 0:2048]
        sscw = IscV[:, 2048:4096]
        cand = IscV[:, 4096:6144]
        candw = IscV[:, 6144:8192]
        ohv = IscV[:, 0:2048]
        ohv2 = IscV[:, 2048:4096]

    def tail_block(i, xb, sample=False):
        tpv = tp_bank.t[:].bitcast(BF16)
        for pr in range(4):
            kb.op("pe", lambda e, pr=pr: e.transpose(out=tpv[:, pr * 128:(pr + 1) * 128],
                                                     in_=ao[:, pr * 128:(pr + 1) * 128], identity=ident_b[:]),
                  reads=[ao, ident_b], writes=[tp_bank])
        kb.op("act", lambda e: e.copy(out=aoT[:], in_=tpv[:, 0:512].rearrange("p (a t) -> p a t", a=4)),
              reads=[tp_bank], writes=[aoT])
        if not sample:
            kb.dma("sp", xpv[:], xprev[i, :, :], writes=[xpv])
            norm_transpose(xpv, hnTp, ntok=16)
            wsl = load_w(win_bf, *CHUNKS[CH_U])
            for g in range(4):
                bk = pb[g // 2]
                c0 = (g % 2) * 144
                for kc in range(8):
                    kb.op("pe", lambda e, bk=bk, c0=c0, g=g, kc=kc, wsl=wsl: e.matmul(
                        bk[:, c0:c0 + 16], lhsT=wsl[:, kc, g * 128:(g + 1) * 128], rhs=hnTp[:, kc, :],
                        start=(kc == 0), stop=(kc == 7)), reads=[wsl, hnTp], writes=[bk])
                for kc in range(8):
                    kb.op("pe", lambda e, bk=bk, c0=c0, g=g, kc=kc, wsl=wsl: e.matmul(
                        bk[:, c0 + 16:c0 + 144], lhsT=wsl[:, kc, g * 128:(g + 1) * 128], rhs=hnT[:, kc, :],
                        start=(kc == 0), stop=(kc == 7)), reads=[wsl, hnT], writes=[bk])
            for hf in range(2):
                kb.op("act", lambda e, hf=hf: e.copy(out=uT[:, 2 * hf:2 * hf + 2, :],
                                                     in_=pb[hf][:, 0:288].rearrange("p (g t) -> p g t", g=2)),
                      reads=[pb[hf]], writes=[uT])
            if i == nq - 1:
                kb.dma("sp", u_last[:, :, :], uT[:], reads=[uT], writes=[u_last])
            kb.op("dve", lambda e: e.tensor_tensor(out=s1[:, :, 1:144], in0=uT[:, :, 1:144], in1=uT[:, :, 0:143],
                                                   op=ALU.add), reads=[uT], writes=[s1])
            kb.op("dve", lambda e: e.tensor_tensor(out=s2[:, 1:4, 3:144], in0=s1[:, 1:4, 3:144], in1=s1[:, 1:4, 1:142],
                                                   op=ALU.add), reads=[s1], writes=[s2])
            kb.op("dve", lambda e: e.tensor_tensor(out=s3[:, 2:4, 7:144], in0=s2[:, 2:4, 7:144], in1=s2[:, 2:4, 3:140],
                                                   op=ALU.add), reads=[s2], writes=[s3])
            kb.op("dve", lambda e: e.tensor_tensor(out=s1[:, 3, 15:144], in0=s3[:, 3, 15:144], in1=s3[:, 3, 7:136],
                                                   op=ALU.add), reads=[s3, s1], writes=[s1])
            wsum = [s1[:, 0, 16:144], s2[:, 1, 16:144], s3[:, 2, 16:144], s1[:, 3, 16:144]]
            wsrc = [s1, s2, s3, s1]
            for g in range(4):
                if i == 0:
                    kb.op("dve", lambda e, g=g: e.tensor_tensor(out=pmf[:, g, :], in0=wsum[g], in1=rcnt[:, g, :],
                                                                op=ALU.mult), reads=[wsrc[g], rcnt], writes=[pmf])
                    kb.op("dve", lambda e, g=g: e.tensor_tensor(out=pmT[:, g, :], in0=pmf[:, g, :], in1=uT[:, g, 16:144],
                                                                op=ALU.subtract), reads=[pmf, uT], writes=[pmT])
                else:
                    kb.op("dve", lambda e, g=g: e.scalar_tensor_tensor(
                        out=pmT[:, g, :], in0=wsum[g], scalar=1.0 / (2 ** (g + 1)), in1=uT[:, g, 16:144],
                        op0=ALU.mult, op1=ALU.subtract), reads=[wsrc[g], uT], writes=[pmT])
        for g in range(4):
            kb.op("pe", lambda e, g=g: e.matmul(pb[0][:, g * 128:(g + 1) * 128], lhsT=wpl[:, g, :], rhs=pmT[:, g, :],
                                                start=True, stop=True), reads=[wpl, pmT], writes=[pb[0]])
        kb.op("dve", lambda e: e.tensor_tensor(out=poT[:], in0=pb[0][:, 0:512].rearrange("p (g t) -> p g t", g=4),
                                               in1=pscol[:].unsqueeze(2).to_broadcast([128, 4, 128]), op=ALU.mult),
              reads=[pb[0], pscol], writes=[poT])
        for (chs, dst) in (((CH_GA0, CH_GA1), sga), ((CH_GB0, CH_GB1), sgb)):
            for hf, chn in enumerate(chs):
                wsl = load_w(win_bf, *CHUNKS[chn])
                bk = pb[hf]
                for ft in range(4):
                    for kc in range(8):
                        kb.op("pe", lambda e, bk=bk, ft=ft, kc=kc, wsl=wsl: e.matmul(
                            bk[:, ft * 128:(ft + 1) * 128], lhsT=wsl[:, kc, ft * 128:(ft + 1) * 128], rhs=hnT[:, kc, :],
                            start=(kc == 0), stop=(kc == 7)), reads=[wsl, hnT], writes=[bk])
                kb.op("act", lambda e, bk=bk, dst=dst, hf=hf: e.activation(
                    out=dst[:, 4 * hf:4 * hf + 4, :], in_=bk[:, 0:512].rearrange("p (f t) -> p f t", f=4),
                    func=AF.Sigmoid), reads=[bk], writes=[dst])
        for hf in range(2):
            wa = load_w(wba_bf, hf * 512, 512, nk=4)
            wb = load_w(wbp_bf, hf * 512, 512, nk=4)
            for ft in range(4):
                for kc in range(4):
                    kb.op("pe", lambda e, ft=ft, kc=kc, wa=wa: e.matmul(
                        pb[0][:, ft * 128:(ft + 1) * 128], lhsT=wa[:, kc, ft * 128:(ft + 1) * 128], rhs=aoT[:, kc, :],
                        start=(kc == 0), stop=(kc == 3)), reads=[wa, aoT], writes=[pb[0]])
                for kc in range(4):
                    kb.op("pe", lambda e, ft=ft, kc=kc, wb=wb: e.matmul(
                        pb[1][:, ft * 128:(ft + 1) * 128], lhsT=wb[:, kc, ft * 128:(ft + 1) * 128], rhs=poT[:, kc, :],
                        start=(kc == 0), stop=(kc == 3)), reads=[wb, poT], writes=[pb[1]])
            kb.op("dve", lambda e, hf=hf: e.tensor_tensor(
                out=tmpA[:], in0=pb[0][:, 0:512], in1=sga[:, 4 * hf:4 * hf + 4, :].rearrange("p f t -> p (f t)"),
                op=ALU.mult), reads=[pb[0], sga], writes=[tmpA])
            kb.op("dve", lambda e, hf=hf: e.tensor_tensor(
                out=tmpB[:], in0=pb[1][:, 0:512], in1=sgb[:, 4 * hf:4 * hf + 4, :].rearrange("p f t -> p (f t)"),
                op=ALU.mult), reads=[pb[1], sgb], writes=[tmpB])
            kb.op("dve", lambda e, hf=hf: e.tensor_tensor(
                out=mT[:, 4 * hf:4 * hf + 4, :].rearrange("p f t -> p (f t)"), in0=tmpA[:], in1=tmpB[:], op=ALU.add),
                reads=[tmpA, tmpB], writes=[mT])
        for hf in range(2):
            wo = load_w(wout_bf, hf * 512, 512, nk=8)
            for kc in range(8):
                kb.op("pe", lambda e, kc=kc, wo=wo, hf=hf: e.matmul(
                    pb[hf][:, 0:512], lhsT=mT[:, kc, :], rhs=wo[:, kc, :], start=(kc == 0), stop=(kc == 7)),
                    reads=[mT, wo], writes=[pb[hf]])
            kb.op("dve", lambda e, hf=hf: e.tensor_tensor(out=h2[:, hf * 512:(hf + 1) * 512], in0=pb[hf][:, 0:512],
                                                          in1=xb[:, hf * 512:(hf + 1) * 512], op=ALU.add),
                  reads=[pb[hf], xb], writes=[NBt])
        norm_transpose(h2, xnT, gtile=g2b, keep=xn, xowner=NBt)
        for hf in range(2):
            wq_ = load_w(pwq_bf, hf * 512, 512, nk=8)
            for ft in range(4):
                for kc in range(8):
                    kb.op("pe", lambda e, ft=ft, kc=kc, wq_=wq_, hf=hf: e.matmul(
                        pb[hf][:, ft * 128:(ft + 1) * 128], lhsT=wq_[:, kc, ft * 128:(ft + 1) * 128], rhs=xnT[:, kc, :],
                        start=(kc == 0), stop=(kc == 7)), reads=[wq_, xnT], writes=[pb[hf]])
            kb.op("act", lambda e, hf=hf: e.copy(out=qpT[:, 4 * hf:4 * hf + 4, :],
                                                 in_=pb[hf][:, 0:512].rearrange("p (f t) -> p f t", f=4)),
                  reads=[pb[hf]], writes=[qpT])
        sbanks = [pb[0], pb[1], pb[3], pb[4]]
        for h in range(8):
            bk = sbanks[h // 2]
            kb.op("pe", lambda e, bk=bk, h=h: e.matmul(bk[:, (h % 2) * 256:(h % 2) * 256 + 256], lhsT=qpT[:, h, :],
                                                      rhs=SK[:], start=True, stop=True),
                  reads=[qpT, SK], writes=[bk])
        for j4 in range(4):
            kb.op("act", lambda e, j4=j4: e.copy(out=ssc[:, j4 * 512:(j4 + 1) * 512], in_=sbanks[j4][:, 0:512]),
                  reads=[sbanks[j4]], writes=[Isc])
        tvv = tv[:].rearrange("p h s k -> p (h s) k")
        tiv = ti[:].rearrange("p h s k -> p (h s) k")
        for gi in range(16):
            sv = ssc[:, gi * 128:(gi + 1) * 128]
            sw = sscw[:, gi * 128:(gi + 1) * 128]
            kb.op("dve", lambda e, sv=sv, gi=gi: e.max(out=tvv[:, gi, 0:8], in_=sv), reads=[Isc], writes=[tv])
            kb.op("dve", lambda e, sv=sv, gi=gi: e.max_index(out=tiv[:, gi, 0:8], in_max=tvv[:, gi, 0:8], in_values=sv),
                  reads=[Isc, tv], writes=[ti])
            kb.op("dve", lambda e, sv=sv, sw=sw, gi=gi: e.match_replace(out=sw, in_to_replace=tvv[:, gi, 0:8],
                                                                       in_values=sv, imm_value=-1e30),
                  reads=[Isc, tv], writes=[Isc])
            kb.op("dve", lambda e, sw=sw, gi=gi: e.max(out=tvv[:, gi, 8:16], in_=sw), reads=[Isc], writes=[tv])
            kb.op("dve", lambda e, sw=sw, gi=gi: e.max_index(out=tiv[:, gi, 8:16], in_max=tvv[:, gi, 8:16], in_values=sw),
                  reads=[Isc, tv], writes=[ti])
        kb.op("dve", lambda e: e.tensor_copy(out=tif[:], in_=ti[:]), reads=[ti], writes=[tif])
        candv = cand.rearrange("p (h a b) -> p h a b", h=8, a=16)
        kb.op("dve", lambda e: e.tensor_tensor(out=candv, in0=tv[:, :, 0, :].unsqueeze(3).to_broadcast([128, 8, 16, 16]),
                                               in1=tv[:, :, 1, :].unsqueeze(2).to_broadcast([128, 8, 16, 16]), op=ALU.add),
              reads=[tv], writes=[Isc])
        for h in range(8):
            cv = cand[:, h * 256:(h + 1) * 256]
            cw = candw[:, h * 256:(h + 1) * 256]
            kb.op("dve", lambda e, cv=cv, h=h: e.max(out=tsv[:, h, 0:8], in_=cv), reads=[Isc], writes=[tsv])
            kb.op("dve", lambda e, cv=cv, h=h: e.max_index(out=tpos[:, h, 0:8], in_max=tsv[:, h, 0:8], in_values=cv),
                  reads=[Isc, tsv], writes=[tpos])
            kb.op("dve", lambda e, cv=cv, cw=cw, h=h: e.match_replace(out=cw, in_to_replace=tsv[:, h, 0:8],
                                                                     in_values=cv, imm_value=-1e30),
                  reads=[Isc, tsv], writes=[Isc])
            kb.op("dve", lambda e, cw=cw, h=h: e.max(out=tsv[:, h, 8:16], in_=cw), reads=[Isc], writes=[tsv])
            kb.op("dve", lambda e, cw=cw, h=h: e.max_index(out=tpos[:, h, 8:16], in_max=tsv[:, h, 8:16], in_values=cw),
                  reads=[Isc, tsv], writes=[tpos])
        kb.op("dve", lambda e: e.tensor_single_scalar(out=pa[:], in_=tpos[:], scalar=4, op=ALU.logical_shift_right),
              reads=[tpos], writes=[pa])
        kb.op("dve", lambda e: e.tensor_single_scalar(out=pbb[:], in_=tpos[:], scalar=15, op=ALU.bitwise_and),
              reads=[tpos], writes=[pbb])
        kb.op("dve", lambda e: e.tensor_copy(out=paf[:], in_=pa[:]), reads=[pa], writes=[paf])
        kb.op("dve", lambda e: e.tensor_copy(out=pbf[:], in_=pbb[:]), reads=[pbb], writes=[pbf])
        oh4 = ohv.rearrange("p (h k a) -> p h k a", h=8, k=16)
        oh42 = ohv2.rearrange("p (h k a) -> p h k a", h=8, k=16)
        io4 = iota16[:].unsqueeze(1).unsqueeze(1).to_broadcast([128, 8, 16, 16])
        for (pf, half, dst) in ((paf, 0, i1s), (pbf, 1, i2s)):
            kb.op("dve", lambda e, pf=pf: e.tensor_tensor(out=oh4, in0=pf[:].unsqueeze(3).to_broadcast([128, 8, 16, 16]),
                                                          in1=io4, op=ALU.is_equal),
                  reads=[pf, iota16], writes=[Isc])
            kb.op("dve", lambda e, half=half: e.tensor_tensor(
                out=oh42, in0=oh4, in1=tif[:, :, half, :].unsqueeze(2).to_broadcast([128, 8, 16, 16]), op=ALU.mult),
                reads=[Isc, tif], writes=[Isc])
            kb.op("dve", lambda e, dst=dst: e.tensor_reduce(out=dst[:], in_=oh42, axis=AX.X, op=ALU.add),
                  reads=[Isc], writes=[dst])
        kb.op("dve", lambda e: e.scalar_tensor_tensor(out=eidf[:].rearrange("p (h k) -> p h k", h=8), in0=i1s[:],
                                                      scalar=128.0, in1=i2s[:], op0=ALU.mult, op1=ALU.add),
              reads=[i1s, i2s], writes=[eidf])
        kb.op("dve", lambda e: e.tensor_copy(out=eidi[:], in_=eidf[:]), reads=[eidf], writes=[eidi])
        kb.op("dve", lambda e: e.tensor_reduce(out=gmx[:], in_=tsv[:], axis=AX.X, op=ALU.max), reads=[tsv], writes=[gmx])
        kb.op("dve", lambda e: e.tensor_tensor(out=gk[:], in0=tsv[:], in1=gmx[:].unsqueeze(2).to_broadcast([128, 8, 16]),
                                               op=ALU.subtract), reads=[tsv, gmx], writes=[gk])
        kb.op("act", lambda e: e.activation(out=gk[:], in_=gk[:], func=AF.Exp), reads=[gk], writes=[gk])
        kb.op("dve", lambda e: e.tensor_reduce(out=gmx[:], in_=gk[:], axis=AX.X, op=ALU.add), reads=[gk], writes=[gmx])
        kb.op("dve", lambda e: e.reciprocal(out=gmx[:], in_=gmx[:]), reads=[gmx], writes=[gmx])
        kb.op("dve", lambda e: e.tensor_tensor(out=gk[:], in0=gk[:], in1=gmx[:].unsqueeze(2).to_broadcast([128, 8, 16]),
                                               op=ALU.mult), reads=[gk, gmx], writes=[gk])
        def peer_steps():
            for hk in range(128):
                ub = Ub[hk % 3]
                kb.dma("pool", ub[:], peer_u[:, :], reads=[eidi], writes=[ub.owner],
                       indirect=bass.IndirectOffsetOnAxis(ap=eidi[:, hk:hk + 1], axis=0))
                kb.op("dve", lambda e, ub=ub, hk=hk: e.scalar_tensor_tensor(
                    out=ub[:], in0=ub[:], scalar=1.0, in1=xn[:], op0=ALU.mult, op1=ALU.mult,
                    accum_out=actv[:, hk:hk + 1]), reads=[ub.owner, xn], writes=[ub.owner, actv])
                yield
            kb.op("act", lambda e: e.activation(out=wgt[:], in_=actv[:], func=AF.Gelu), reads=[actv], writes=[wgt])
            kb.op("dve", lambda e: e.tensor_tensor(out=wgt[:], in0=wgt[:], in1=gk[:].rearrange("p h k -> p (h k)"),
                                                   op=ALU.mult), reads=[wgt, gk], writes=[wgt])
            for hk in range(128):
                ub = Ub[hk % 3]
                kb.dma("pool", ub[:], peer_v[:, :], reads=[eidi], writes=[ub.owner],
                       indirect=bass.IndirectOffsetOnAxis(ap=eidi[:, hk:hk + 1], axis=0))
                kb.op("dve", lambda e, ub=ub, hk=hk: e.scalar_tensor_tensor(
                    out=h2[:], in0=ub[:], scalar=wgt[:, hk:hk + 1], in1=h2[:], op0=ALU.mult, op1=ALU.add),
                    reads=[ub.owner, wgt, NBt], writes=[NBt])
                yield
            if sample:
                kb.dma("sp", y_s_out[:, :], h2[:], reads=[NBt], writes=[y_s_out])
            else:
                kb.dma("sp", y_own[i, :, :], h2[:], reads=[NBt], writes=[y_own])
            yield
        return peer_steps()

    pending = [None]
    nslots = [1]

    def advance(n=1):
        g = pending[0]
        if g is None:
            return
        for _ in range(n):
            try:
                next(g)
            except StopIteration:
                pending[0] = None
                return

    def drain():
        while pending[0] is not None:
            advance(64)

    for i in range(nq):
        xb = xt[i % 2]
        kb.dma("sp", xb[:], xown[i, :, :], writes=[xb])
        norm_transpose(xb, hnT)
        wsl = load_w(win_bf, *CHUNKS[CH_K])
        proj_tok(pb[0], wsl, 512)
        head_norm(pb[0], kg, ko)
        kb.dma("sp", k_own[i, :, :], ko[:], reads=[ko], writes=[k_own])
        wsl = load_w(win_bf, *CHUNKS[CH_V])
        proj_tok(pb[1], wsl, 512)
        kb.op("act", lambda e: e.copy(out=vo[:], in_=pb[1][:, 0:512]), reads=[pb[1]], writes=[vo])
        kb.dma("sp", v_own[i, :, :], vo[:], reads=[vo], writes=[v_own])
        wsl = load_w(win_bf, *CHUNKS[CH_KW])
        proj_tok(pb[0], wsl, 72)
        kb.op("dve", lambda e: e.tensor_copy(out=kio[:], in_=pb[0][:, 0:64]), reads=[pb[0]], writes=[kio])
        kb.dma("sp", ki_own[i, :, :], kio[:], reads=[kio], writes=[ki_own])
        kb.op("dve", lambda e: e.tensor_scalar(out=wis[:], in0=pb[0][:, 64:72], scalar1=0.125 * (8 ** -0.5),
                                               scalar2=None, op0=ALU.mult), reads=[pb[0]], writes=[wis])
        if not do_attn:
            continue
        tpv = tp_bank.t[:].bitcast(BF16)
        wsl = load_w(win_bf, *CHUNKS[CH_Q])
        proj_tok(pb[0], wsl, 512)
        head_norm(pb[0], qg, qf)
        kb.op("dve", lambda e: e.tensor_copy(out=qbf[:], in_=qf[:]), reads=[qf], writes=[qbf])
        for pr in range(4):
            kb.op("pe", lambda e, pr=pr: e.transpose(out=tpv[:, pr * 128:(pr + 1) * 128],
                                                     in_=qbf[:, pr * 128:(pr + 1) * 128], identity=ident_b[:]),
                  reads=[qbf, ident_b], writes=[tp_bank])
        kb.op("act", lambda e: e.copy(out=qT2[:], in_=tpv[:, 0:512].rearrange("p (a t) -> p a t", a=4)),
              reads=[tp_bank], writes=[qT2])
        wsl = load_w(win_bf, *CHUNKS[CH_QI])
        proj_tok(pb[1], wsl, 512)
        kb.op("act", lambda e: e.copy(out=qbf[:], in_=pb[1][:, 0:512]), reads=[pb[1]], writes=[qbf])
        for pr in range(4):
            kb.op("pe", lambda e, pr=pr: e.transpose(out=tpv[:, pr * 128:(pr + 1) * 128],
                                                     in_=qbf[:, pr * 128:(pr + 1) * 128], identity=ident_b[:]),
                  reads=[qbf, ident_b], writes=[tp_bank])
        kb.op("act", lambda e: e.copy(out=qiT2[:], in_=tpv[:, 0:512].rearrange("p (a t) -> p a t", a=4)),
              reads=[tp_bank], writes=[qiT2])
        nkb = min(4 * i + 4, nblk_a)
        nch = nkb // 4
        nk = nkb * 128
        ibanks = [pb[0], pb[1], pb[3], pb[4]]
        cnt_i = 0
        per_slot = -(-260 // (nch * 8 + nkb))
        for ch in range(nch):
            kc_ = kitc[ch % 2]
            kb.dma("sp", kc_[:], kit_s[:, ch * 512:(ch + 1) * 512], reads=[kit_s], writes=[kc_])
            Ic = Isc[:, ch * 512:(ch + 1) * 512]
            for h in range(8):
                a, half = h // 2, h % 2
                ps_ = slice(64 * half, 64 * half + 64)
                bk = ibanks[cnt_i % 4]
                rl_ = rl[cnt_i % 2]
                cnt_i += 1
                advance(per_slot)
                kb.op("pe", lambda e, bk=bk, a=a, ps_=ps_, kc_=kc_: e.matmul(
                    bk[:, 0:512], lhsT=qiT2[ps_, a, :], rhs=kc_[ps_, :], start=True, stop=True),
                    reads=[qiT2, kc_], writes=[bk])
                kb.op("act", lambda e, bk=bk, rl_=rl_: e.activation(out=rl_[:], in_=bk[:, 0:512], func=AF.Relu),
                      reads=[bk], writes=[rl_])
                if h == 0:
                    kb.op("dve", lambda e, rl_=rl_, Ic=Ic: e.tensor_scalar(
                        out=Ic, in0=rl_[:], scalar1=wis[:, 0:1], scalar2=None, op0=ALU.mult),
                        reads=[rl_, wis], writes=[Isc])
                else:
                    kb.op("dve", lambda e, rl_=rl_, Ic=Ic, h=h: e.scalar_tensor_tensor(
                        out=Ic, in0=rl_[:], scalar=wis[:, h:h + 1], in1=Ic, op0=ALU.mult, op1=ALU.add),
                        reads=[rl_, wis, Isc], writes=[Isc])
        kb.op("dve", lambda e: e.tensor_reduce(out=hi0[:], in_=Isc[:, 0:nk], axis=AX.X, op=ALU.max),
              reads=[Isc], writes=[hi0])
        kb.op("dve", lambda e: e.tensor_reduce(out=lo[:], in_=Isc[:, 0:nk], axis=AX.X, op=ALU.min),
              reads=[Isc], writes=[lo])
        kb.op("dve", lambda e: e.tensor_tensor(out=Isc[:, nk - 512:nk], in0=Isc[:, nk - 512:nk], in1=cmask[:],
                                               op=ALU.add), reads=[Isc, cmask], writes=[Isc])
        kb.op("dve", lambda e: e.tensor_tensor(out=hi0[:], in0=hi0[:], in1=lo[:], op=ALU.subtract),
              reads=[hi0, lo], writes=[hi0])
        kb.op("dve", lambda e: e.tensor_scalar(out=wh[:], in0=pw[:], scalar1=hi0[:, 0:1], scalar2=None,
                                               op0=ALU.mult), reads=[pw, hi0], writes=[wh])
        for n in range(NIT):
            kb.op("dve", lambda e, n=n: e.tensor_tensor(out=mid[:], in0=lo[:], in1=wh[:, n:n + 1], op=ALU.add),
                  reads=[lo, wh], writes=[mid])
            kb.op("dve", lambda e: e.memset(cnt4[:], 0.0), writes=[cnt4])
            for q4 in range((nk + 2175) // 2176):
                c0 = q4 * 2176
                c1 = min(nk, c0 + 2176)
                kb.op("dve", lambda e, c0=c0, c1=c1, q4=q4: e.tensor_scalar(
                    out=junkI[:, 0:c1 - c0], in0=Isc[:, c0:c1], scalar1=mid[:, 0:1],
                    scalar2=0.0, op0=ALU.is_ge, op1=ALU.add, accum_out=cnt4[:, q4:q4 + 1]),
                    reads=[Isc, mid], writes=[junkI, cnt4])
            kb.op("dve", lambda e: e.tensor_reduce(out=cntt[:], in_=cnt4[:], axis=AX.X, op=ALU.add),
                  reads=[cnt4], writes=[cntt])
            kb.op("dve", lambda e, n=n: e.tensor_scalar(out=dl[:], in0=cntt[:], scalar1=255.5,
                                                        scalar2=wh[:, n:n + 1], op0=ALU.is_ge, op1=ALU.mult),
                  reads=[cntt, wh], writes=[dl])
            kb.op("dve", lambda e: e.tensor_tensor(out=lo[:], in0=lo[:], in1=dl[:], op=ALU.add),
                  reads=[lo, dl], writes=[lo])
        kb.op("dve", lambda e: e.memset(pb[5][:, 0:260], 0.0), writes=[pb[5]])
        kb.op("dve", lambda e: e.memset(pb[6][:, 0:260], 0.0), writes=[pb[6]])
        def emit_S(kbi):
            ch, kloc = kbi // 4, kbi % 4
            ktc_ = ktc[ch % 2]
            vch_ = vch[ch % 2]
            if kloc == 0:
                kb.dma("sp", ktc_[:], kt_s[:, :, ch * 512:(ch + 1) * 512], reads=[kt_s], writes=[ktc_])
                kb.dma("sp", vch_[:], v_s.t[ch * 512:(ch + 1) * 512, :, :].rearrange("(b p) h e -> p b (h e)", p=128),
                       reads=[v_s], writes=[vch_])
            nm = negm[kbi % 2]
            kb.op("dve", lambda e, nm=nm, kbi=kbi: e.tensor_scalar(
                out=nm[:], in0=Isc[:, kbi * 128:(kbi + 1) * 128], scalar1=lo[:, 0:1], scalar2=-30000.0,
                op0=ALU.is_lt, op1=ALU.mult), reads=[Isc, lo], writes=[nm])
            r = kbi - 4 * i
            near = (-1 <= r <= 3)
            if near:
                kb.op("dve", lambda e, nm=nm, r=r: e.tensor_tensor(
                    out=addh[:], in0=NB[r + 1][:], in1=nm[:].unsqueeze(1).to_broadcast([128, 8, 128]), op=ALU.add),
                    reads=[NB[r + 1], nm], writes=[addh])
            pt_ = PT[kbi % 2]
            for g in range(2):
                sb_ = pb[3 + g]
                for hh in range(4):
                    h = 4 * g + hh
                    a, half = h // 2, h % 2
                    ps_ = slice(64 * half, 64 * half + 64)
                    kb.op("pe", lambda e, sb_=sb_, hh=hh, a=a, ps_=ps_, ktc_=ktc_, kloc=kloc: e.matmul(
                        sb_[:, hh * 128:(hh + 1) * 128], lhsT=ktc_[ps_, a, kloc * 128:(kloc + 1) * 128],
                        rhs=qT2[ps_, a, :], start=True, stop=False), reads=[ktc_, qT2], writes=[sb_])
                    if near:
                        kb.op("pe", lambda e, sb_=sb_, hh=hh, h=h: e.matmul(
                            sb_[:, hh * 128:(hh + 1) * 128], lhsT=addh[:, h, :], rhs=ident_b[:],
                            start=False, stop=True), reads=[addh, ident_b], writes=[sb_])
                    else:
                        kb.op("pe", lambda e, sb_=sb_, hh=hh, nm=nm: e.matmul(
                            sb_[:, hh * 128:(hh + 1) * 128], lhsT=nm[:], rhs=ident_b[:],
                            start=False, stop=True), reads=[nm, ident_b], writes=[sb_])
                kb.op("act", lambda e, sb_=sb_, pt_=pt_, g=g: e.activation(
                    out=pt_[:, 4 * g:4 * g + 4, :], in_=sb_[:, 0:512].rearrange("p (h q) -> p h q", h=4),
                    func=AF.Exp), reads=[sb_], writes=[pt_])

        def emit_PV(kbi):
            ch, kloc = kbi // 4, kbi % 4
            vch_ = vch[ch % 2]
            pt_ = PT[kbi % 2]
            for h in range(8):
                ob = pb[5 + h // 4]
                kb.op("pe", lambda e, ob=ob, h=h, pt_=pt_, vch_=vch_, kloc=kloc: e.matmul(
                    ob[:, (h % 4) * 65:(h % 4) * 65 + 65], lhsT=pt_[:, h, :], rhs=vch_[:, kloc, h * 65:(h + 1) * 65],
                    start=False, stop=False, skip_group_check=True), reads=[pt_, vch_], writes=[ob])

        for kbi in range(nkb):
            emit_S(kbi)
            advance(per_slot)
            if kbi > 0:
                emit_PV(kbi - 1)
        emit_PV(nkb - 1)
        for g in range(2):
            ob = pb[5 + g]
            obv = ob[:, 0:260].rearrange("p (h e) -> p h e", h=4)
            kb.op("dve", lambda e, obv=obv, g=g: e.reciprocal(out=rden[:, 4 * g:4 * g + 4], in_=obv[:, :, 64]),
                  reads=[ob], writes=[rden])
            kb.op("dve", lambda e, obv=obv, g=g: e.tensor_tensor(
                out=ao[:, 256 * g:256 * g + 256].rearrange("p (h d) -> p h d", h=4), in0=obv[:, :, 0:64],
                in1=rden[:, 4 * g:4 * g + 4].unsqueeze(2).to_broadcast([128, 4, 64]), op=ALU.mult),
                reads=[ob, rden], writes=[ao])
        if dbg_ao is not None:
            kb.op("dve", lambda e: e.tensor_copy(out=aof[:], in_=ao[:]), reads=[ao], writes=[aof])
            kb.dma("sp", dbg_ao[i, :, :], aof[:], reads=[aof], writes=[dbg_ao])

        if do_tail:
            drain()
            pending[0] = tail_block(i, xb)


    drain()
    if do_sample:
        LOB = _bucket_lo()
        wrep = kb.sb([128, 8], F32, "wrep")
        idx2 = kb.sb([128, 2], I32, "idx2")
        pti = kb.sb([128, 16], I32, "pti")
        ptf = kb.sb([128, 16], F32, "ptf")
        dh = kb.sb([128, 128], F32, "dh")
        dh2 = kb.sb([128, 128], F32, "dh2")
        scs = kb.sb([128, 2, 128], F32, "scs")
        scn = kb.sb([128, 1], F32, "scn")
        dn8 = kb.sb([128, 8], F32, "dn8")
        sm8 = kb.sb([128, 8], F32, "sm8")
        tA = View(Ubo[0], Ubo[0].t[:, 0:256])
        tE = View(Ubo[0], Ubo[0].t[:, 256:512])
        tF = View(Ubo[0], Ubo[0].t[:, 512:768])
        tG = View(Ubo[0], Ubo[0].t[:, 768:1024])
        idxs = View(Ubo[1], Ubo[1].t[:].bitcast(U32)[:, 0:256])
        tC = View(Ubo[1], Ubo[1].t[:].bitcast(U32)[:, 256:512])
        tD = View(Ubo[1], Ubo[1].t[:].bitcast(U32)[:, 512:768])
        rowT = kb.sb([128, 32], I32, "rowT")
        distT = kb.sb([128, 32], F32, "distT")
        negT = kb.sb([128, 32], F32, "negT")
        indT = kb.sb([128, 32], F32, "indT")
        biasT = kb.sb([128, 32, 8], F32, "biasT")
        tmp3 = kb.sb([128, 32, 8], F32, "tmp3")
        lg = kb.sb([128, 8], F32, "lg")
        pex = kb.sb([128, 8], F32, "pex")
        zsel = kb.sb([128, 31], F32, "zsel")
        numS = View(Ubo[2], Ubo[2].t[:, 0:512])
        denS = kb.sb([128, 8], F32, "denS")
        seln = kb.sb([128, 1], F32, "seln")
        nb0 = kb.sb([128, 8], F32, "nb0")
        sq, sk_, sv_, ski = qf, ko, vo, kio
        sqi = hsq
        su = tmpA
        qrep = tmpB
        IscV = Isc.t[:]
        kb.dma("sp", zsel[:], zsel_in[:, :], writes=[zsel])
        kb.op("dve", lambda e: e.memset(pti[:], 0), writes=[pti])
        kb.dma("sp", pti[0:16, :], pt_in[:, :], writes=[pti])
        kb.op("dve", lambda e: e.tensor_copy(out=ptf[:], in_=pti[:]), reads=[pti], writes=[ptf])
        with nc.allow_non_contiguous_dma(reason="page table slices"):
            for jg in range(8):
                kb.dma("sp", idx2[jg * 16:(jg + 1) * 16, :], pt_in[:, 2 * jg:2 * jg + 2], writes=[idx2])
        xb = xt[0]
        kb.dma("sp", xb[:], xs_in[:, :], writes=[xb])
        norm_transpose(xb, hnT)
        wsl = load_w(win_bf, *CHUNKS[CH_Q])
        proj_tok(pb[0], wsl, 512)
        head_norm(pb[0], qg, sq)
        wsl = load_w(win_bf, *CHUNKS[CH_K])
        proj_tok(pb[1], wsl, 512)
        head_norm(pb[1], kg, sk_)
        kb.dma("sp", k_s_out[:, :], sk_[:], reads=[sk_], writes=[k_s_out])
        wsl = load_w(win_bf, *CHUNKS[CH_V])
        proj_tok(pb[0], wsl, 512)
        kb.op("act", lambda e: e.copy(out=sv_[:], in_=pb[0][:, 0:512]), reads=[pb[0]], writes=[sv_])
        kb.dma("sp", v_s_out[:, :], sv_[:], reads=[sv_], writes=[v_s_out])
        wsl = load_w(win_bf, *CHUNKS[CH_QI])
        proj_tok(pb[1], wsl, 512)
        kb.op("act", lambda e: e.copy(out=sqi[:], in_=pb[1][:, 0:512]), reads=[pb[1]], writes=[sqi])
        wsl = load_w(win_bf, *CHUNKS[CH_KW])
        proj_tok(pb[0], wsl, 72)
        kb.op("dve", lambda e: e.tensor_copy(out=ski[:], in_=pb[0][:, 0:64]), reads=[pb[0]], writes=[ski])
        kb.dma("sp", ki_s_out[:, :], ski[:], reads=[ski], writes=[ki_s_out])
        kb.op("dve", lambda e: e.tensor_scalar(out=wis[:], in0=pb[0][:, 64:72], scalar1=0.125 * (8 ** -0.5),
                                               scalar2=None, op0=ALU.mult), reads=[pb[0]], writes=[wis])
        wsl = load_w(win_bf, *CHUNKS[CH_U])
        proj_tok(pb[1], wsl, 512)
        kb.op("act", lambda e: e.copy(out=su[:], in_=pb[1][:, 0:512]), reads=[pb[1]], writes=[su])
        kb.dma("sp", pool_s_out[:, 0:14, :], state_in[:, 1:15, :], reads=[state_in], writes=[pool_s_out])
        kb.dma("sp", pool_s_out[:, 14, :], su[0:16, :], reads=[su], writes=[pool_s_out])
        kb.dma("sp", qi_d[:, :], sqi[0:16, :], reads=[sqi], writes=[qi_d])
        kb.dma("sp", wi_d[:, :], wis[0:16, :], reads=[wis], writes=[wi_d])
        kb.dma("sp", q_d[:, :], sq[0:16, :], reads=[sq], writes=[q_d])
        for jg in range(8):
            kb.dma("sp", qrep[jg * 16:(jg + 1) * 16, :], qi_d[:, :], reads=[qi_d], writes=[qrep])
            kb.dma("sp", wrep[jg * 16:(jg + 1) * 16, :], wi_d[:, :], reads=[wi_d], writes=[wrep])
        KI = IscV[:, 0:8192]
        prodc = View(ktc[0], ktc[0].t[:].rearrange("p a b -> p (a b)").bitcast(F32)[:, 0:1024])
        for s in range(2):
            kb.dma("pool", KI, cache_ik[:, :], reads=[idx2], writes=[Isc],
                   indirect=bass.IndirectOffsetOnAxis(ap=idx2[:, s:s + 1], axis=0))
            for h in range(8):
                for c8 in range(8):
                    kb.op("dve", lambda e, h=h, c8=c8: e.tensor_tensor(
                        out=prodc[:].rearrange("p (k d) -> p k d", k=16),
                        in0=KI[:, c8 * 1024:(c8 + 1) * 1024].rearrange("p (k d) -> p k d", k=16),
                        in1=qrep[:, h * 64:(h + 1) * 64].unsqueeze(1).to_broadcast([128, 16, 64]), op=ALU.mult),
                        reads=[Isc, qrep], writes=[prodc.owner])
                    kb.op("dve", lambda e, c8=c8: e.tensor_reduce(
                        out=dh[:, c8 * 16:(c8 + 1) * 16], in_=prodc[:].rearrange("p (k d) -> p k d", k=16),
                        axis=AX.X, op=ALU.add), reads=[prodc.owner], writes=[dh])
                if h == 0:
                    kb.op("dve", lambda e, s=s: e.tensor_scalar(out=scs[:, s, :], in0=dh[:], scalar1=0.0,
                                                                scalar2=wrep[:, 0:1], op0=ALU.max, op1=ALU.mult),
                          reads=[dh, wrep], writes=[scs])
                else:
                    kb.op("dve", lambda e, h=h: e.tensor_scalar(out=dh2[:], in0=dh[:], scalar1=0.0,
                                                                scalar2=wrep[:, h:h + 1], op0=ALU.max, op1=ALU.mult),
                          reads=[dh, wrep], writes=[dh2])
                    kb.op("dve", lambda e, s=s: e.tensor_tensor(out=scs[:, s, :], in0=scs[:, s, :], in1=dh2[:],
                                                                op=ALU.add), reads=[scs, dh2], writes=[scs])
        for jg in range(8):
            kb.dma("sp", sc_d[:, jg * 256:(jg + 1) * 256], scs[jg * 16:(jg + 1) * 16, :, :].rearrange("p s k -> p (s k)"),
                   reads=[scs], writes=[sc_d])
        xtmp = xn[:, 512:1024]
        kb.op("dve", lambda e: e.tensor_tensor(out=xtmp.rearrange("p (h d) -> p h d", h=8),
                                               in0=sqi[:].rearrange("p (h d) -> p h d", h=8),
                                               in1=ski[:].unsqueeze(1).to_broadcast([128, 8, 64]), op=ALU.mult),
              reads=[sqi, ski], writes=[xn])
        kb.op("dve", lambda e: e.tensor_reduce(out=dn8[:], in_=xtmp.rearrange("p (h d) -> p h d", h=8), axis=AX.X,
                                               op=ALU.add), reads=[xn], writes=[dn8])
        kb.op("dve", lambda e: e.tensor_scalar(out=dn8[:], in0=dn8[:], scalar1=0.0, scalar2=None, op0=ALU.max),
              reads=[dn8], writes=[dn8])
        kb.op("dve", lambda e: e.tensor_tensor(out=dn8[:], in0=dn8[:], in1=wis[:], op=ALU.mult),
              reads=[dn8, wis], writes=[dn8])
        kb.op("dve", lambda e: e.tensor_reduce(out=scn[:], in_=dn8[:], axis=AX.X, op=ALU.add),
              reads=[dn8], writes=[scn])
        Is = IscV[:, 0:2049]
        kb.op("dve", lambda e: e.memset(IscV[:, 0:2304], 0.0), writes=[Isc])
        kb.dma("sp", IscV[0:16, 0:2048], sc_d[:, :], reads=[sc_d], writes=[Isc])
        kb.op("dve", lambda e: e.tensor_copy(out=IscV[:, 2048:2049], in_=scn[:]), reads=[scn], writes=[Isc])
        for rd in range(32):
            kb.op("dve", lambda e: e.max(out=sm8[:], in_=Is), reads=[Isc], writes=[sm8])
            kb.op("dve", lambda e, rd=rd: e.max_index(out=idxs[:, rd * 8:(rd + 1) * 8], in_max=sm8[:], in_values=Is),
                  reads=[Isc, sm8], writes=[idxs])
            kb.op("dve", lambda e: e.match_replace(out=Is, in_to_replace=sm8[:], in_values=Is, imm_value=-1e30),
                  reads=[Isc, sm8], writes=[Isc])
        kb.op("dve", lambda e: e.tensor_copy(out=tA[:], in_=idxs[:]), reads=[idxs], writes=[tA])
        kb.op("dve", lambda e: e.tensor_scalar(out=tC[:], in0=tA[:], scalar1=2047.0, scalar2=None, op0=ALU.min),
              reads=[tA], writes=[tC])
        kb.op("dve", lambda e: e.tensor_single_scalar(out=tD[:], in_=tC[:], scalar=7, op=ALU.logical_shift_right),
              reads=[tC], writes=[tD])
        kb.op("dve", lambda e: e.tensor_single_scalar(out=tC[:], in_=tC[:], scalar=127, op=ALU.bitwise_and),
              reads=[tC], writes=[tC])
        kb.op("dve", lambda e: e.tensor_copy(out=tE[:], in_=tD[:]), reads=[tD], writes=[tE])
        kb.op("dve", lambda e: e.tensor_copy(out=tF[:], in_=tC[:]), reads=[tC], writes=[tF])
        ohs = IscV[:, 4608:8704].rearrange("p (k j) -> p k j", k=256)
        kb.op("dve", lambda e: e.tensor_tensor(out=ohs, in0=tE[:].unsqueeze(2).to_broadcast([128, 256, 16]),
                                               in1=iota16[:].unsqueeze(1).to_broadcast([128, 256, 16]), op=ALU.is_equal),
              reads=[tE, iota16], writes=[Isc])
        kb.op("dve", lambda e: e.tensor_tensor(out=ohs, in0=ohs, in1=ptf[:].unsqueeze(1).to_broadcast([128, 256, 16]),
                                               op=ALU.mult), reads=[Isc, ptf], writes=[Isc])
        kb.op("dve", lambda e: e.tensor_reduce(out=tG[:], in_=ohs, axis=AX.X, op=ALU.add), reads=[Isc], writes=[tG])
        kb.op("dve", lambda e: e.scalar_tensor_tensor(out=tG[:], in0=tG[:], scalar=128.0, in1=tF[:], op0=ALU.mult,
                                                      op1=ALU.add), reads=[tG, tF], writes=[tG])
        kb.op("dve", lambda e: e.tensor_scalar(out=tE[:], in0=tA[:], scalar1=-1.0, scalar2=2048.0, op0=ALU.mult,
                                               op1=ALU.add), reads=[tA], writes=[tE])
        kb.op("dve", lambda e: e.tensor_scalar(out=tF[:], in0=tA[:], scalar1=2047.5, scalar2=-30000.0, op0=ALU.is_ge,
                                               op1=ALU.mult), reads=[tA], writes=[tF])
        kb.op("dve", lambda e: e.tensor_reduce(out=seln[:], in_=tF[:], axis=AX.X, op=ALU.min), reads=[tF], writes=[seln])
        kb.op("dve", lambda e: e.tensor_scalar(out=seln[:], in0=seln[:], scalar1=-1.0 / 30000.0, scalar2=None,
                                               op0=ALU.mult), reads=[seln], writes=[seln])
        for (srcb, dstb) in ((tG, rowT), (tE, distT), (tF, negT)):
            for gq in range(2):
                kb.op("pe", lambda e, srcb=srcb, gq=gq: e.transpose(out=pb[2][:, gq * 128:(gq + 1) * 128],
                                                                   in_=srcb[:, gq * 128:(gq + 1) * 128],
                                                                   identity=ident_f[:]),
                      reads=[srcb, ident_f], writes=[pb[2]])
            kb.op("dve", lambda e, dstb=dstb: e.tensor_copy(
                out=dstb[:].rearrange("p (g b) -> p g b", g=2),
                in_=pb[2][:, 0:256].rearrange("p (g b) -> p g b", g=2)[:, :, 0:16]), reads=[pb[2]], writes=[dstb])
        kb.op("dve", lambda e: e.memset(biasT[:], 0.0), writes=[biasT])
        for bkt in range(1, 32):
            kb.op("dve", lambda e, bkt=bkt: e.tensor_scalar(out=indT[:], in0=distT[:], scalar1=float(LOB[bkt - 1]),
                                                            scalar2=None, op0=ALU.is_lt), reads=[distT], writes=[indT])
            kb.op("dve", lambda e, bkt=bkt: e.tensor_tensor(
                out=tmp3[:], in0=indT[:].unsqueeze(2).to_broadcast([128, 32, 8]),
                in1=ndel[:, bkt * 8:(bkt + 1) * 8].unsqueeze(1).to_broadcast([128, 32, 8]), op=ALU.mult),
                reads=[indT, ndel], writes=[tmp3])
            kb.op("dve", lambda e: e.tensor_tensor(out=biasT[:], in0=biasT[:], in1=tmp3[:], op=ALU.add),
                  reads=[biasT, tmp3], writes=[biasT])
        kb.op("dve", lambda e: e.memset(pb[5][:, 0:512], 0.0), writes=[pb[5]])
        kb.op("dve", lambda e: e.memset(pb[6][:, 0:8], 0.0), writes=[pb[6]])
        qbv = xn[:, 0:512]
        pvv = xn[:, 512:1024]
        for b in range(16):
            kb.dma("sp", qbv, q_d.t[b:b + 1, :].partition_broadcast(128).rearrange("p o d -> p (o d)"),
                   reads=[q_d], writes=[xn])
            for gq in range(2):
                col = gq * 16 + b
                kg_ = UbS[gq]
                vg_ = UbS[2 + gq]
                kb.dma("pool", kg_[:, 0:512], cache_k[:, :], reads=[rowT], writes=[kg_.owner],
                       indirect=bass.IndirectOffsetOnAxis(ap=rowT[:, col:col + 1], axis=0))
                kb.dma("pool", vg_[:, 0:512], cache_v[:, :], reads=[rowT], writes=[vg_.owner],
                       indirect=bass.IndirectOffsetOnAxis(ap=rowT[:, col:col + 1], axis=0))
                kb.op("dve", lambda e, kg_=kg_: e.tensor_tensor(out=pvv, in0=kg_[:, 0:512], in1=qbv, op=ALU.mult),
                      reads=[kg_.owner, xn], writes=[xn])
                kb.op("dve", lambda e: e.tensor_reduce(out=lg[:], in_=pvv.rearrange("p (h d) -> p h d", h=8),
                                                       axis=AX.X, op=ALU.add), reads=[xn], writes=[lg])
                kb.op("dve", lambda e, col=col: e.scalar_tensor_tensor(
                    out=lg[:], in0=lg[:], scalar=negT[:, col:col + 1], in1=biasT[:, col, :], op0=ALU.add, op1=ALU.add),
                    reads=[lg, negT, biasT], writes=[lg])
                kb.op("act", lambda e: e.activation(out=pex[:], in_=lg[:], func=AF.Exp), reads=[lg], writes=[pex])
                kb.op("dve", lambda e, vg_=vg_: e.tensor_tensor(
                    out=pvv.rearrange("p (h d) -> p h d", h=8), in0=vg_[:, 0:512].rearrange("p (h d) -> p h d", h=8),
                    in1=pex[:].unsqueeze(2).to_broadcast([128, 8, 64]), op=ALU.mult),
                    reads=[vg_.owner, pex], writes=[xn])
                kb.op("pe", lambda e, b=b: e.matmul(pb[5][0:16, 0:512], lhsT=zsel[:, 15 - b:31 - b], rhs=pvv,
                                                    start=False, stop=False, skip_group_check=True),
                      reads=[zsel, xn], writes=[pb[5]])
                kb.op("pe", lambda e, b=b: e.matmul(pb[6][0:16, 0:8], lhsT=zsel[:, 15 - b:31 - b], rhs=pex[:],
                                                    start=False, stop=False, skip_group_check=True),
                      reads=[zsel, pex], writes=[pb[6]])
        kb.op("dve", lambda e: e.memset(numS[:], 0.0), writes=[numS])
        kb.op("dve", lambda e: e.memset(denS[:], 1.0), writes=[denS])
        kb.op("dve", lambda e: e.tensor_copy(out=numS[0:16, :], in_=pb[5][0:16, 0:512]), reads=[pb[5]], writes=[numS])
        kb.op("dve", lambda e: e.tensor_copy(out=denS[0:16, :], in_=pb[6][0:16, 0:8]), reads=[pb[6]], writes=[denS])
        kb.op("dve", lambda e: e.tensor_reduce(out=nb0[:], in_=ndel[:, 8:256].rearrange("p (b h) -> p h b", h=8),
                                               axis=AX.X, op=ALU.add), reads=[ndel], writes=[nb0])
        kb.op("dve", lambda e: e.tensor_tensor(out=pvv, in0=sq[:], in1=sk_[:], op=ALU.mult), reads=[sq, sk_], writes=[xn])
        kb.op("dve", lambda e: e.tensor_reduce(out=lg[:], in_=pvv.rearrange("p (h d) -> p h d", h=8), axis=AX.X,
                                               op=ALU.add), reads=[xn], writes=[lg])
        kb.op("dve", lambda e: e.tensor_tensor(out=lg[:], in0=lg[:], in1=nb0[:], op=ALU.add), reads=[lg, nb0], writes=[lg])
        kb.op("act", lambda e: e.activation(out=pex[:], in_=lg[:], func=AF.Exp), reads=[lg], writes=[pex])
        kb.op("dve", lambda e: e.tensor_scalar(out=pex[:], in0=pex[:], scalar1=seln[:, 0:1], scalar2=None, op0=ALU.mult),
              reads=[pex, seln], writes=[pex])
        kb.op("dve", lambda e: e.tensor_tensor(out=pvv.rearrange("p (h d) -> p h d", h=8),
                                               in0=sv_[:].rearrange("p (h d) -> p h d", h=8),
                                               in1=pex[:].unsqueeze(2).to_broadcast([128, 8, 64]), op=ALU.mult),
              reads=[sv_, pex], writes=[xn])
        kb.op("dve", lambda e: e.tensor_tensor(out=numS[:], in0=numS[:], in1=pvv, op=ALU.add), reads=[numS, xn], writes=[numS])
        kb.op("dve", lambda e: e.tensor_tensor(out=denS[:], in0=denS[:], in1=pex[:], op=ALU.add), reads=[denS, pex],
              writes=[denS])
        kb.op("dve", lambda e: e.reciprocal(out=denS[:], in_=denS[:]), reads=[denS], writes=[denS])
        kb.op("dve", lambda e: e.tensor_tensor(out=ao[:].rearrange("p (h d) -> p h d", h=8),
                                               in0=numS[:].rearrange("p (h d) -> p h d", h=8),
                                               in1=denS[:].unsqueeze(2).to_broadcast([128, 8, 64]), op=ALU.mult),
              reads=[numS, denS], writes=[ao])
        stv = IscV[:, 0:7680].rearrange("p (r c) -> p r c", r=15)
        kb.op("dve", lambda e: e.memset(IscV[:, 0:7680], 0.0), writes=[Isc])
        kb.dma("sp", IscV[0:16, 0:7680], state_in.t.rearrange("b r c -> b (r c)"), reads=[state_in], writes=[Isc])
        for g in range(4):
            w = 2 ** (g + 1)
            kb.op("dve", lambda e, g=g, w=w: e.tensor_reduce(
                out=pmf[:, g, :], in_=stv[:, 16 - w:15, g * 128:(g + 1) * 128].rearrange("p r c -> p c r"),
                axis=AX.X, op=ALU.add), reads=[Isc], writes=[pmf])
            kb.op("dve", lambda e, g=g: e.tensor_tensor(out=pmf[:, g, :], in0=pmf[:, g, :], in1=su[:, g * 128:(g + 1) * 128],
                                                        op=ALU.add), reads=[pmf, su], writes=[pmf])
            kb.op("dve", lambda e, g=g, w=w: e.scalar_tensor_tensor(
                out=pmf[:, g, :], in0=pmf[:, g, :], scalar=1.0 / w, in1=su[:, g * 128:(g + 1) * 128], op0=ALU.mult,
                op1=ALU.subtract), reads=[pmf, su], writes=[pmf])
        for g in range(4):
            kb.op("pe", lambda e, g=g: e.transpose(out=pb[2][:, g * 128:(g + 1) * 128], in_=pmf[:, g, :],
                                                   identity=ident_f[:]), reads=[pmf, ident_f], writes=[pb[2]])
        kb.op("dve", lambda e: e.tensor_copy(out=pmT[:], in_=pb[2][:, 0:512].rearrange("p (g t) -> p g t", g=4)),
              reads=[pb[2]], writes=[pmT])
        pending[0] = tail_block(0, xb, sample=True)
        drain()

    kb.finish()
    return nc, kb


def _prep_inputs(inp, cfg, cores):
    nblk_a = cfg.get("nblk_a", NBLK_A)
    nq = cfg.get("nq", NQ)
    nkeys = nblk_a * 128
    xp = np.asarray(inp["x_prompt"], np.float32)
    meta = np.asarray(inp["meta_tokens"], np.float32)
    maps = []
    for c in cores:
        b, cc = c // 4, c % 4
        full = np.zeros((max(nkeys, (4 * nq + 4) * 128), D), np.float32)
        T = 16 + xp.shape[1]
        cat = np.concatenate([meta, xp[b]], axis=0)
        n = min(T, full.shape[0])
        full[:n] = cat[:n]
        xown = np.zeros((nq, 128, D), np.float32)
        xprev = np.zeros((nq, 16, D), np.float32)
        for i in range(nq):
            j = 4 * i + cc
            xown[i] = full[j * 128:(j + 1) * 128]
            if j > 0:
                xprev[i] = full[j * 128 - 16:j * 128]
        m = {
            "xcat": np.ascontiguousarray(full[:nkeys]),
            "xown": xown,
            "xprev": xprev,
            "w_in": np.ascontiguousarray(np.asarray(inp["w_in"], np.float32)[0]),
            "norm1_g": np.ascontiguousarray(np.asarray(inp["norm1_g"], np.float32)[0]),
            "q_norm_g": np.ascontiguousarray(np.asarray(inp["q_norm_g"], np.float32)[0]),
            "k_norm_g": np.ascontiguousarray(np.asarray(inp["k_norm_g"], np.float32)[0]),
            "ident": np.eye(128, dtype=np.float32),
            "rel_bias": np.ascontiguousarray(np.asarray(inp["rel_bias"], np.float32)),
            "qs": (np.arange(128, dtype=np.float32)[:, None] - np.arange(128, dtype=np.float32)[None, :]),
            "thrtab": _thrtab(cc),
            "cmask": _cmask(cc),
            "w_ba": np.ascontiguousarray(np.asarray(inp["w_branch_attn"], np.float32)[0]),
            "w_bp": np.ascontiguousarray(np.asarray(inp["w_branch_pool"], np.float32)[0]),
            "w_out": np.ascontiguousarray(np.asarray(inp["w_out"], np.float32)[0]),
            "peer_wq": np.ascontiguousarray(np.asarray(inp["peer_wq"], np.float32)[0]),
            "w_pool": np.ascontiguousarray(np.asarray(inp["w_pool"], np.float32)[0]),
            "pool_scale": np.ascontiguousarray(np.asarray(inp["pool_scale"], np.float32)[0]),
            "norm2_g": np.ascontiguousarray(np.asarray(inp["norm2_g"], np.float32)[0]),
            "subkeys": np.ascontiguousarray(np.asarray(inp["peer_subkeys"], np.float32)[0]),
            "peer_u": np.ascontiguousarray(np.asarray(inp["peer_u"], np.float32)[0]),
            "peer_v": np.ascontiguousarray(np.asarray(inp["peer_v"], np.float32)[0]),
            "rcnt": _rcnt(cc),
            "iota16": np.broadcast_to(np.arange(16, dtype=np.float32)[None, :], (128, 16)).copy(),
            "pw": np.broadcast_to((0.5 ** np.arange(1, NIT + 1)).astype(np.float32)[None, :], (128, NIT)).copy(),
        }
        if cfg.get("sample", True):
            xs = np.zeros((128, D), np.float32)
            xs[:16] = np.asarray(inp["x_sample"], np.float32)[16 * c:16 * c + 16, 0]
            z = np.zeros((128, 31), np.float32)
            z[:, 15] = 1.0
            m.update({
                "xs_own": xs,
                "cache_k": np.asarray(inp["cache_k"], np.float32).reshape(2560 * 128, 512),
                "cache_v": np.asarray(inp["cache_v"], np.float32).reshape(2560 * 128, 512),
                "cache_ik": np.asarray(inp["cache_idx_k"], np.float32).reshape(2560, 8192),
                "state_own": np.ascontiguousarray(np.asarray(inp["state_pool"], np.float32)[0, 16 * c:16 * c + 16]),
                "pt_own": np.ascontiguousarray(np.asarray(inp["page_table"], np.int32)[16 * c:16 * c + 16]),
                "zsel": z,
            })
        maps.append(m)
    return maps


def _bucket_lo():
    n = np.arange(0, 256)
    nf = np.maximum(n, 16).astype(np.float32)
    large = 16 + (np.log(nf / np.float32(16)) / np.float32(np.log(128 / 16)) * np.float32(16)).astype(np.int32)
    large = np.minimum(large, 31)
    bkt = np.where(n < 16, n, large)
    return [int(np.min(n[bkt >= b])) for b in range(1, 32)]


def _thrtab(cc):
    lo_b = _bucket_lo()
    t = np.zeros((155,), np.float32)
    for r5 in range(5):
        r = r5 - 1
        for b in range(31):
            t[r5 * 31 + b] = lo_b[b] - 128 * (cc - r)
    return np.broadcast_to(t[None, :], (128, 155)).copy()


def _rcnt(cc):
    r = np.zeros((128, 4, 128), np.float32)
    t = np.arange(128)
    for g, w in enumerate((2, 4, 8, 16)):
        cnt = np.minimum(w, t + 1) if cc == 0 else np.full(128, w)
        r[:, g, :] = (1.0 / cnt.astype(np.float32))[None, :]
    return r


def _cmask(cc):
    m = np.zeros((128, 512), np.float32)
    q = np.arange(128)[:, None]
    s = np.arange(128)[None, :]
    for r in range(4):
        if r > cc:
            m[:, r * 128:(r + 1) * 128] = -1e30
        elif r == cc:
            m[:, r * 128:(r + 1) * 128] = np.where(s > q, -1e30, 0.0)
    return m


def kernel(**inputs):
    cfg = {}
    nc = build(cfg)
    cores = list(range(8))
    maps = _prep_inputs(inputs, cfg, cores)
    res = run_bass_kernel_spmd(nc, maps, core_ids=cores)
    rs = res.results
    B, S = 2, 8192
    T = S + 16
    y_prompt = np.zeros((B, S, D), np.float32)
    k_p = np.zeros((1, B, T, 8, 64), np.float32)
    v_p = np.zeros((1, B, T, 8, 64), np.float32)
    i_p = np.zeros((1, B, T, 64), np.float32)
    pool_p = np.zeros((1, B, 15, 512), np.float32)
    for c in cores:
        b, cc = c // 4, c % 4
        r = rs[c]
        for i in range(NQ):
            j = 4 * i + cc
            p0 = j * 128
            if p0 >= T:
                continue
            p1 = min(p0 + 128, T)
            n = p1 - p0
            k_p[0, b, p0:p1] = r["k_own"][i][:n].reshape(n, 8, 64)
            v_p[0, b, p0:p1] = r["v_own"][i][:n].reshape(n, 8, 64)
            i_p[0, b, p0:p1] = r["ki_own"][i][:n]
            lo = max(p0, 16)
            y_prompt[b, lo - 16:p1 - 16] = r["y_own"][i][lo - p0:n]
        if cc == 0:
            ul = r["u_last"]
            rows = ul[:, :, 17:32]
            pool_p[0, b] = np.transpose(rows, (2, 1, 0)).reshape(15, 512)
    y_s = np.zeros((128, 1, D), np.float32)
    k_s = np.zeros((1, 128, 1, 8, 64), np.float32)
    v_s = np.zeros((1, 128, 1, 8, 64), np.float32)
    i_s = np.zeros((1, 128, 1, 64), np.float32)
    pool_s = np.zeros((1, 128, 15, 512), np.float32)
    for c in cores:
        r = rs[c]
        sl = slice(16 * c, 16 * c + 16)
        y_s[sl, 0] = r["y_s"][:16]
        k_s[0, sl, 0] = r["ks_o"][:16].reshape(16, 8, 64)
        v_s[0, sl, 0] = r["vs_o"][:16].reshape(16, 8, 64)
        i_s[0, sl, 0] = r["kis_o"][:16]
        pool_s[0, sl] = r["pool_s"]
    return (y_prompt, y_s, k_p, v_p, i_p, pool_p, k_s, v_s, i_s, pool_s)
```

```python
import numpy as np
from contextlib import ExitStack
import concourse.bass as bass
import concourse.mybir as mybir
from concourse.bass_utils import run_bass_kernel_spmd

F32 = mybir.dt.float32
BF16 = mybir.dt.bfloat16
I32 = mybir.dt.int32
U32 = mybir.dt.uint32
ALU = mybir.AluOpType
AF = mybir.ActivationFunctionType
AX = mybir.AxisListType

D = 1024
NBLK_A = 68
NQ = 17
EPS = 1e-6
IN_W = 4680
CH_Q, CH_K, CH_V, CH_QI, CH_KW, CH_U, CH_GA0, CH_GA1, CH_GB0, CH_GB1 = range(10)
CHUNKS = [(0, 512), (512, 512), (1024, 512), (1536, 512), (2048, 72), (2120, 512),
          (2632, 512), (3144, 512), (3656, 512), (4168, 512)]
N_DMA_SEMS = 12
NIT = 16


class Buf:
    def __init__(self, t, name):
        self.t = t
        self.name = name
        self.w = None
        self.r = {}

    def __getitem__(self, idx):
        return self.t[idx]


class KB:
    def __init__(self, nc, es, plan=None):
        self.nc = nc
        self.es = es
        self.plan = plan
        self.targets = {e: set() for e in ("pe", "dve", "act", "pool", "sp")}
        self.rank = None
        if plan is not None:
            self.rank = {e: {idx: r + 1 for r, idx in enumerate(sorted(plan[e]))} for e in plan}
        self.eng = {"pe": nc.tensor, "dve": nc.vector, "act": nc.scalar, "pool": nc.gpsimd, "sp": nc.sync}
        self.sem = {e: es.enter_context(nc.semaphore("s_" + e)) for e in self.eng}
        self.cnt = {e: 0 for e in self.eng}
        self.seen = {e: {} for e in self.eng}
        self.dsem = [es.enter_context(nc.semaphore("d_%d" % i)) for i in range(N_DMA_SEMS + 3)]
        self.dcnt = [0] * (N_DMA_SEMS + 3)
        self.drr = 0
        self.nbuf = 0

    def sb(self, shape, dt, name=None):
        self.nbuf += 1
        name = "sb_" + (name or ("%d" % self.nbuf))
        return Buf(self.es.enter_context(self.nc.sbuf_tensor(name, list(shape), dt)), name)

    def ps(self, shape, dt, name=None):
        self.nbuf += 1
        name = name or ("ps%d" % self.nbuf)
        return Buf(self.es.enter_context(self.nc.psum_tensor(name, list(shape), dt)), name)

    def dram(self, name, shape, dt, kind="Internal"):
        return Buf(self.nc.dram_tensor(name, list(shape), dt, kind=kind).ap(), name)

    def _deps(self, e, reads, writes):
        need = {}

        def add(tok):
            if tok is None:
                return
            key, sem, val = tok
            if key == "pe" and e == "pe":
                return
            if need.get(key, (None, 0))[1] < val:
                need[key] = (sem, val)

        for b in reads:
            add(b.w)
        for b in writes:
            add(b.w)
            for tok in b.r.values():
                add(tok)
        eo = self.eng[e]
        for key, (sem, val) in need.items():
            if self.seen[e].get(key, 0) < val:
                self.seen[e][key] = val
                if key in self.targets:
                    if self.plan is None:
                        self.targets[key].add(val)
                    else:
                        eo.wait_ge(sem, self.rank[key][val])
                elif self.plan is not None:
                    eo.wait_ge(sem, val)

    def _record(self, tok, reads, writes):
        for b in reads:
            if b.r.get(tok[0], (None, None, 0))[2] < tok[2]:
                b.r[tok[0]] = tok
        for b in writes:
            b.w = tok
            b.r = {}

    def op(self, e, fn, reads=(), writes=()):
        self._deps(e, reads, writes)
        self.cnt[e] += 1
        if self.plan is not None:
            ins = fn(self.eng[e])
            if self.cnt[e] in self.plan[e]:
                ins.then_inc(self.sem[e], 1)
        self._record((e, self.sem[e], self.cnt[e]), reads, writes)

    def dma(self, q, out, in_, reads=(), writes=(), indirect=None, own_sem=None):
        if own_sem is None:
            i = self.drr
            self.drr = (i + 1) % N_DMA_SEMS
        else:
            i = N_DMA_SEMS + own_sem
        eo = self.eng[q]
        key = "d%d" % i
        if own_sem is None and self.dcnt[i] > 0 and self.seen[q].get(key, 0) < 16 * self.dcnt[i]:
            if self.plan is not None:
                eo.wait_ge(self.dsem[i], 16 * self.dcnt[i])
            self.seen[q][key] = 16 * self.dcnt[i]
        self._deps(q, reads, writes)
        self.dcnt[i] += 1
        if self.plan is not None:
            if indirect is None:
                ins = eo.dma_start(out=out, in_=in_)
            else:
                ins = eo.indirect_dma_start(out=out, out_offset=None, in_=in_, in_offset=indirect)
            ins.then_inc(self.dsem[i], 16)
        self._record((key, self.dsem[i], 16 * self.dcnt[i]), reads, writes)

    def finish(self):
        eo = self.eng["sp"]
        if self.plan is None:
            for e in ("pe", "dve", "act", "pool"):
                if self.cnt[e] > 0:
                    self.targets[e].add(self.cnt[e])
            return
        for i in range(N_DMA_SEMS + 3):
            if self.dcnt[i] > 0:
                eo.wait_ge(self.dsem[i], 16 * self.dcnt[i])
        for e in ("pe", "dve", "act", "pool"):
            if self.cnt[e] > 0:
                eo.wait_ge(self.sem[e], self.rank[e][self.cnt[e]])


def build(cfg):
    _, kb_dry = _build(cfg, None)
    nc, _ = _build(cfg, kb_dry.targets)
    return nc


def _build(cfg, plan):
    nblk_a = cfg.get("nblk_a", NBLK_A)
    nq = cfg.get("nq", NQ)
    do_attn = cfg.get("attn", True)
    do_tail = cfg.get("tail", True)
    nkeys = nblk_a * 128
    nc = bass.Bass("TRN2", target_bir_lowering=False)
    es = ExitStack()
    kb = KB(nc, es, plan)
    nc._es_keep = es

    def din(name, shape, dt=F32):
        return Buf(nc.dram_tensor(name, list(shape), dt, kind="ExternalInput").ap(), name)

    def dout(name, shape, dt=F32):
        return Buf(nc.dram_tensor(name, list(shape), dt, kind="ExternalOutput").ap(), name)

    xcat = din("xcat", [nkeys, D])
    xown = din("xown", [nq, 128, D])
    xprev = din("xprev", [nq, 16, D])
    w_in = din("w_in", [D, IN_W])
    norm1_g = din("norm1_g", [D])
    q_norm_g = din("q_norm_g", [64])
    k_norm_g = din("k_norm_g", [64])
    ident_in = din("ident", [128, 128])
    rel_bias = din("rel_bias", [32, 8])
    qs_in = din("qs", [128, 128])
    thrtab_in = din("thrtab", [128, 155])
    cmask_in = din("cmask", [128, 512])
    pw_in = din("pw", [128, NIT])
    w_ba = din("w_ba", [512, D])
    w_bp = din("w_bp", [512, D])
    w_out = din("w_out", [D, D])
    peer_wq = din("peer_wq", [D, D])
    w_pool = din("w_pool", [4, 128, 128])
    pool_scale = din("pool_scale", [512])
    norm2_g = din("norm2_g", [D])
    subkeys = din("subkeys", [2, 128, 64])
    peer_u = din("peer_u", [16384, D])
    peer_v = din("peer_v", [16384, D])
    rcnt_in = din("rcnt", [128, 4, 128])
    iota16_in = din("iota16", [128, 16])
    k_own = dout("k_own", [nq, 128, 512])
    v_own = dout("v_own", [nq, 128, 512])
    ki_own = dout("ki_own", [nq, 128, 64])
    y_own = dout("y_own", [nq, 128, D])
    u_last = dout("u_last", [128, 4, 144])
    y_s_out = dout("y_s", [128, D])
    do_sample = cfg.get("sample", True)
    if do_sample:
        xs_in = din("xs_own", [128, D])
        cache_k = din("cache_k", [2560 * 128, 512])
        cache_v = din("cache_v", [2560 * 128, 512])
        cache_ik = din("cache_ik", [2560, 8192])
        state_in = din("state_own", [16, 15, 512])
        pt_in = din("pt_own", [16, 16], I32)
        zsel_in = din("zsel", [128, 31])
        k_s_out = dout("ks_o", [128, 512])
        v_s_out = dout("vs_o", [128, 512])
        ki_s_out = dout("kis_o", [128, 64])
        pool_s_out = dout("pool_s", [16, 15, 512])
        qi_d = kb.dram("qi_d", [16, 512], F32)
        wi_d = kb.dram("wi_d", [16, 8], F32)
        q_d = kb.dram("q_d", [16, 512], F32)
        sc_d = kb.dram("sc_d", [16, 2048], F32)
    wba_bf = kb.dram("wba_bf", [128, 4, D], BF16)
    wbp_bf = kb.dram("wbp_bf", [128, 4, D], BF16)
    wout_bf = kb.dram("wout_bf", [128, 8, D], BF16)
    pwq_bf = kb.dram("pwq_bf", [128, 8, D], BF16)
    win_bf = kb.dram("win_bf", [128, 8, IN_W], BF16)
    kt_s = kb.dram("kt_s", [128, 4, nkeys], BF16)
    v_s = kb.dram("v_s", [nkeys, 8, 65], BF16)
    kit_s = kb.dram("kit_s", [128, nkeys], BF16)

    ident_f = kb.sb([128, 128], F32, "ident_f")
    ident_b = kb.sb([128, 128], BF16, "ident_b")
    g1col = kb.sb([128, 8], F32, "g1col")
    qg = kb.sb([128, 64], F32, "qg")
    kg = kb.sb([128, 64], F32, "kg")
    kb.dma("sp", ident_f[:], ident_in[:, :], writes=[ident_f])
    kb.op("dve", lambda e: e.tensor_copy(out=ident_b[:], in_=ident_f[:]), reads=[ident_f], writes=[ident_b])
    ident4 = kb.sb([128, 512], BF16, "ident4")
    for j4 in range(4):
        kb.op("dve", lambda e, j4=j4: e.tensor_copy(out=ident4[:, j4 * 128:(j4 + 1) * 128], in_=ident_f[:]),
              reads=[ident_f, ident4], writes=[ident4])
    with nc.allow_non_contiguous_dma(reason="tiny param loads"):
        kb.dma("sp", g1col[:], norm1_g.t.rearrange("(k p) -> p k", p=128), writes=[g1col])
        kb.dma("sp", qg[:], q_norm_g.t.partition_broadcast(128), writes=[qg])
        kb.dma("sp", kg[:], k_norm_g.t.partition_broadcast(128), writes=[kg])
    kb.op("dve", lambda e: e.tensor_scalar(out=qg[:], in0=qg[:], scalar1=0.125, scalar2=None, op0=ALU.mult),
          reads=[qg], writes=[qg])

    pb = [kb.ps([128, 512], F32, "bank%d" % i) for i in range(8)]

    ktc = [kb.sb([128, 4, 512], BF16, "ktc%d" % j) for j in range(2)]
    vch = [kb.sb([128, 4, 520], BF16, "vch%d" % j) for j in range(2)]

    class View:
        def __init__(self, owner, ap):
            self.owner = owner
            self.ap = ap

        def __getitem__(self, idx):
            return self.ap[idx]

        @property
        def w(self):
            return self.owner.w

        @w.setter
        def w(self, v):
            self.owner.w = v

        @property
        def r(self):
            return self.owner.r

        @r.setter
        def r(self, v):
            self.owner.r = v

    wst_v = [View(ktc[j], ktc[j].t[:].rearrange("p a b -> p (a b)").bitcast(F32)[:, 0:1024]) for j in range(2)]
    wsb_v = [View(vch[j], vch[j].t[:].rearrange("p a b -> p (a b)")[:, 0:1024]) for j in range(2)]
    pcount = [0]

    def conv_w(src_ap, dst_ap, ncol, scal):
        s = pcount[0] % 2
        pcount[0] += 1
        kb.dma("sp", wst_v[s][:, 0:ncol], src_ap, writes=[wst_v[s].owner])
        kb.op("dve", lambda e: e.tensor_scalar(out=wsb_v[s][:, 0:ncol], in0=wst_v[s][:, 0:ncol], scalar1=scal,
                                               scalar2=None, op0=ALU.mult),
              reads=[wst_v[s].owner, g1col], writes=[wsb_v[s].owner])
        kb.dma("sp", dst_ap, wsb_v[s][:, 0:ncol], reads=[wsb_v[s].owner], writes=[win_bf])

    for kc in range(8):
        for pc in range(5):
            conv_w(w_in[kc * 128:(kc + 1) * 128, pc * 936:(pc + 1) * 936], win_bf[:, kc, pc * 936:(pc + 1) * 936],
                   936, g1col[:, kc:kc + 1])
    if do_tail:
        for (wsrc, wdst, nkc) in ((w_ba, wba_bf, 4), (w_bp, wbp_bf, 4), (w_out, wout_bf, 8), (peer_wq, pwq_bf, 8)):
            for kc in range(nkc):
                conv_w(wsrc[kc * 128:(kc + 1) * 128, :], wdst[:, kc, :], 1024, 1.0)

    wslot = [kb.sb([128, 8, 512], BF16, "wslot%d" % i) for i in range(2)]
    wstate = {"i": 0}

    def load_w(src, c0, cw, nk=8):
        s = wslot[wstate["i"] % 2]
        wstate["i"] += 1
        kb.dma("sp", s[:, 0:nk, 0:cw], src[:, 0:nk, c0:c0 + cw], reads=[src], writes=[s])
        return s

    xt = [kb.sb([128, D], F32, "xt%d" % i) for i in range(2)]
    xn = kb.sb([128, D], F32, "xn")
    junk = kb.sb([128, D], BF16, "junk")
    ssq = kb.sb([128, 1], F32, "ssq")
    rstd = kb.sb([128, 1], F32, "rstd")
    xs = kb.sb([128, D], BF16, "xs")
    hnT = kb.sb([128, 8, 128], BF16, "hnT")
    tp_bank = pb[2]

    def norm_transpose(xb, dstT, ntok=128, gtile=None, keep=None, xowner=None):
        xo = xowner or xb
        kb.op("act", lambda e: e.activation(out=junk[0:ntok, :], in_=xb[0:ntok, :], func=AF.Square,
                                            accum_out=ssq[0:ntok, :]),
              reads=[xo], writes=[junk, ssq])
        kb.op("act", lambda e: e.activation(out=rstd[0:ntok, :], in_=ssq[0:ntok, :], func=AF.Sqrt,
                                            scale=1.0 / D, bias=EPS),
              reads=[ssq], writes=[rstd])
        kb.op("dve", lambda e: e.reciprocal(out=rstd[0:ntok, :], in_=rstd[0:ntok, :]), reads=[rstd], writes=[rstd])
        if gtile is None:
            kb.op("dve", lambda e: e.tensor_scalar(out=xs[0:ntok, :], in0=xb[0:ntok, :], scalar1=rstd[0:ntok, :],
                                                   scalar2=None, op0=ALU.mult),
                  reads=[xo, rstd], writes=[xs])
        else:
            kb.op("dve", lambda e: e.scalar_tensor_tensor(out=keep[0:ntok, :], in0=xb[0:ntok, :],
                                                          scalar=rstd[0:ntok, :], in1=gtile[0:ntok, :],
                                                          op0=ALU.mult, op1=ALU.mult),
                  reads=[xo, rstd, gtile], writes=[keep])
            kb.op("dve", lambda e: e.tensor_copy(out=xs[0:ntok, :], in_=keep[0:ntok, :]), reads=[keep], writes=[xs])
        tpv = tp_bank.t[:].bitcast(BF16)
        for kc in range(8):
            kb.op("pe", lambda e, kc=kc: e.transpose(out=tpv[:, kc * 128:kc * 128 + ntok],
                                                     in_=xs[0:ntok, kc * 128:(kc + 1) * 128],
                                                     identity=ident_b[0:ntok, 0:ntok]),
                  reads=[xs, ident_b], writes=[tp_bank])
        kb.op("dve", lambda e: e.tensor_copy(
            out=dstT[:, :, 0:ntok], in_=tpv.rearrange("p (k t) -> p k t", k=8)[:, :, 0:ntok]),
            reads=[tp_bank], writes=[dstT])

    def proj_tok(dst_bank, wsl, cw, srcT=None, ntok=128):
        srcT = srcT or hnT
        for kc in range(8):
            kb.op("pe", lambda e, kc=kc: e.matmul(dst_bank[0:ntok, 0:cw], lhsT=srcT[:, kc, 0:ntok],
                                                  rhs=wsl[:, kc, 0:cw], start=(kc == 0), stop=(kc == 7)),
                  reads=[srcT, wsl], writes=[dst_bank])

    hsq = kb.sb([128, 512], F32, "hsq")
    hss = kb.sb([128, 8], F32, "hss")
    hrs = kb.sb([128, 8], F32, "hrs")

    def head_norm(src_bank, gain, dst):
        kb.op("act", lambda e: e.activation(out=hsq[:], in_=src_bank[:, 0:512], func=AF.Square),
              reads=[src_bank], writes=[hsq])
        kb.op("dve", lambda e: e.tensor_reduce(out=hss[:], in_=hsq[:].rearrange("p (h d) -> p h d", h=8),
                                               axis=AX.X, op=ALU.add),
              reads=[hsq], writes=[hss])
        kb.op("act", lambda e: e.activation(out=hrs[:], in_=hss[:], func=AF.Sqrt, scale=1.0 / 64, bias=EPS),
              reads=[hss], writes=[hrs])
        kb.op("dve", lambda e: e.reciprocal(out=hrs[:], in_=hrs[:]), reads=[hrs], writes=[hrs])
        kb.op("dve", lambda e: e.tensor_tensor(out=hsq[:].rearrange("p (h d) -> p h d", h=8),
                                               in0=src_bank[:, 0:512].rearrange("p (h d) -> p h d", h=8),
                                               in1=hrs[:].unsqueeze(2).to_broadcast([128, 8, 64]), op=ALU.mult),
              reads=[src_bank, hrs], writes=[hsq])
        kb.op("dve", lambda e: e.tensor_tensor(out=dst[:].rearrange("p (h d) -> p h d", h=8),
                                               in0=hsq[:].rearrange("p (h d) -> p h d", h=8),
                                               in1=gain[:].unsqueeze(1).to_broadcast([128, 8, 64]), op=ALU.mult),
              reads=[hsq, gain], writes=[dst])

    wk = wslot[0]
    wv = wslot[1]
    wkw = kb.sb([128, 8, 72], BF16, "wkw")
    kb.dma("sp", wk[:], win_bf[:, :, 512:1024], reads=[win_bf], writes=[wk])
    kb.dma("sp", wv[:], win_bf[:, :, 1024:1536], reads=[win_bf], writes=[wv])
    kb.dma("sp", wkw[:], win_bf[:, :, 2048:2120], reads=[win_bf], writes=[wkw])
    kf = kb.sb([128, 512], F32, "kf")
    kbf = kb.sb([128, 512], BF16, "kbf")
    ktb = kb.sb([128, 4, 128], BF16, "ktb")
    vb = kb.sb([128, 8, 65], BF16, "vb")
    kib = kb.sb([128, 128], BF16, "kib")
    kitb = kb.sb([128, 128], BF16, "kitb")
    kb.op("dve", lambda e: e.memset(vb[:], 1.0), writes=[vb])
    for blk in range(nblk_a):
        xb = xt[blk % 2]
        kb.dma("sp", xb[:], xcat[blk * 128:(blk + 1) * 128, :], writes=[xb])
        norm_transpose(xb, hnT)
        proj_tok(pb[0], wk, 512)
        head_norm(pb[0], kg, kf)
        kb.op("dve", lambda e: e.tensor_copy(out=kbf[:], in_=kf[:]), reads=[kf], writes=[kbf])
        tpv = tp_bank.t[:].bitcast(BF16)
        for pr in range(4):
            kb.op("pe", lambda e, pr=pr: e.transpose(out=tpv[:, pr * 128:(pr + 1) * 128],
                                                     in_=kbf[:, pr * 128:(pr + 1) * 128], identity=ident_b[:]),
                  reads=[kbf, ident_b], writes=[tp_bank])
        kb.op("act", lambda e: e.copy(out=ktb[:], in_=tpv[:, 0:512].rearrange("p (a t) -> p a t", a=4)),
              reads=[tp_bank], writes=[ktb])
        kb.dma("sp", kt_s[:, :, blk * 128:(blk + 1) * 128], ktb[:], reads=[ktb], writes=[kt_s])
        proj_tok(pb[1], wv, 512)
        kb.op("act", lambda e: e.copy(out=vb[:, :, 0:64], in_=pb[1][:, 0:512].rearrange("p (h d) -> p h d", h=8)),
              reads=[pb[1]], writes=[vb])
        kb.dma("sp", v_s[blk * 128:(blk + 1) * 128, :, :], vb[:], reads=[vb], writes=[v_s])
        proj_tok(pb[0], wkw, 72)
        kb.op("dve", lambda e: e.tensor_copy(out=kib[:, 0:64], in_=pb[0][:, 0:64]), reads=[pb[0]], writes=[kib])
        kb.op("dve", lambda e: e.tensor_copy(out=kib[:, 64:128], in_=pb[0][:, 0:64]), reads=[pb[0]], writes=[kib])
        kb.op("pe", lambda e: e.transpose(out=tpv[:, 512:640], in_=kib[:], identity=ident_b[:]),
              reads=[kib, ident_b], writes=[tp_bank])
        kb.op("act", lambda e: e.copy(out=kitb[:], in_=tpv[:, 512:640]), reads=[tp_bank], writes=[kitb])
        kb.dma("sp", kit_s[:, blk * 128:(blk + 1) * 128], kitb[:], reads=[kitb], writes=[kit_s])

    ko = kb.sb([128, 512], F32, "ko")
    vo = kb.sb([128, 512], F32, "vo")
    kio = kb.sb([128, 64], F32, "kio")
    wis = kb.sb([128, 8], F32, "wis")
    dbg_ao = dout("dbg_ao", [nq, 128, 512]) if cfg.get("dbg") else None
    if do_attn:
        qf = kb.sb([128, 512], F32, "qf")
        qbf = kb.sb([128, 512], BF16, "qbf")
        qT2 = kb.sb([128, 4, 128], BF16, "qT2")
        qiT2 = kb.sb([128, 4, 128], BF16, "qiT2")
        kitc = [kb.sb([128, 512], BF16, "kitc%d" % j) for j in range(2)]
        rl = [kb.sb([128, 512], BF16, "rl%d" % j) for j in range(2)]
        Isc = kb.sb([128, max(nkeys, 8704)], F32, "Isc")
        junkI = kb.sb([128, 2176], BF16, "junkI")
        cnt4 = kb.sb([128, 4], F32, "cnt4")
        hi0 = kb.sb([128, 1], F32, "hi0")
        lo = kb.sb([128, 1], F32, "lo")
        mid = kb.sb([128, 1], F32, "mid")
        cntt = kb.sb([128, 1], F32, "cntt")
        dl = kb.sb([128, 1], F32, "dl")
        wh = kb.sb([128, NIT], F32, "wh")
        pw = kb.sb([128, NIT], F32, "pw")
        cmask = kb.sb([128, 512], F32, "cmask")
        negm = [kb.sb([128, 128], BF16, "negm%d" % j) for j in range(2)]
        addh = kb.sb([128, 8, 128], BF16, "addh")
        PT = [kb.sb([128, 8, 128], BF16, "PT%d" % j) for j in range(2)]
        rden = kb.sb([128, 8], F32, "rden")
        ao = kb.sb([128, 512], BF16, "ao")
        aof = kb.sb([128, 512], F32, "aof") if cfg.get("dbg") else None
        kb.dma("sp", pw[:], pw_in[:, :], writes=[pw])
        kb.dma("sp", cmask[:], cmask_in[:, :], writes=[cmask])
        qs = kb.sb([128, 128], F32, "qs")
        thrtab = kb.sb([128, 155], F32, "thrtab")
        rbb = kb.sb([128, 256], F32, "rbb")
        ndel = kb.sb([128, 256], F32, "ndel")
        ind = kb.sb([128, 128], F32, "ind")
        NB = [kb.sb([128, 8, 128], BF16, "NB%d" % j) for j in range(5)]
        NBt = kb.sb([128, 8, 128], F32, "h2")
        h2 = View(NBt, NBt.t[:].rearrange("p a b -> p (a b)"))
        kb.dma("sp", qs[:], qs_in[:, :], writes=[qs])
        kb.dma("sp", thrtab[:], thrtab_in[:, :], writes=[thrtab])
        with nc.allow_non_contiguous_dma(reason="tiny param loads"):
            kb.dma("sp", rbb[:], rel_bias.t.rearrange("b h -> (b h)").partition_broadcast(128), writes=[rbb])
        kb.op("dve", lambda e: e.tensor_tensor(out=ndel[:, 8:256], in0=rbb[:, 0:248], in1=rbb[:, 8:256],
                                               op=ALU.subtract), reads=[rbb], writes=[ndel])
        for r5 in range(5):
            kb.op("dve", lambda e: e.memset(NBt[:], 0.0), writes=[NBt])
            for b in range(1, 32):
                col = r5 * 31 + (b - 1)
                kb.op("dve", lambda e, col=col: e.tensor_scalar(out=ind[:], in0=qs[:], scalar1=thrtab[:, col:col + 1],
                                                                scalar2=None, op0=ALU.is_lt),
                      reads=[qs, thrtab], writes=[ind])
                for h in range(8):
                    kb.op("dve", lambda e, b=b, h=h: e.scalar_tensor_tensor(
                        out=NBt[:, h, :], in0=ind[:], scalar=ndel[:, b * 8 + h:b * 8 + h + 1], in1=NBt[:, h, :],
                        op0=ALU.mult, op1=ALU.add), reads=[ind, ndel, NBt], writes=[NBt])
            kb.op("dve", lambda e, r5=r5: e.tensor_copy(out=NB[r5][:], in_=NBt[:]), reads=[NBt], writes=[NB[r5]])
    if do_tail:
        aoT = kb.sb([128, 4, 128], BF16, "aoT")
        hnTp = kb.sb([128, 8, 16], BF16, "hnTp")
        xpv = kb.sb([16, D], F32, "xpv")
        uT = kb.sb([128, 4, 144], F32, "uT")
        s1 = kb.sb([128, 4, 144], F32, "s1")
        s2 = kb.sb([128, 4, 144], F32, "s2")
        s3 = kb.sb([128, 4, 144], F32, "s3")
        pmf = kb.sb([128, 4, 128], F32, "pmf")
        pmT = kb.sb([128, 4, 128], BF16, "pmT")
        poT = kb.sb([128, 4, 128], BF16, "poT")
        sga = kb.sb([128, 8, 128], BF16, "sga")
        sgb = kb.sb([128, 8, 128], BF16, "sgb")
        tmpA = kb.sb([128, 512], F32, "tmpA")
        tmpB = kb.sb([128, 512], F32, "tmpB")
        mT = kb.sb([128, 8, 128], BF16, "mT")
        xnT = sgb
        qpT = sga
        g2b = kb.sb([128, D], F32, "g2b")
        wpl = kb.sb([128, 4, 128], BF16, "wpl")
        wplf = kb.sb([128, 4, 128], F32, "wplf")
        pscol = kb.sb([128, 4], F32, "pscol")
        SKf = kb.sb([128, 256], F32, "SKf")
        sktmp = kb.sb([128, 128], F32, "sktmp")
        SK = kb.sb([128, 256], BF16, "SK")
        rcnt = kb.sb([128, 4, 128], F32, "rcnt")
        iota16 = kb.sb([128, 16], F32, "iota16")
        tv = kb.sb([128, 8, 2, 16], F32, "tv")
        ti = kb.sb([128, 8, 2, 16], U32, "ti")
        tif = kb.sb([128, 8, 2, 16], F32, "tif")
        tsv = kb.sb([128, 8, 16], F32, "tsv")
        tpos = kb.sb([128, 8, 16], U32, "tpos")
        pa = kb.sb([128, 8, 16], U32, "pa")
        pbb = kb.sb([128, 8, 16], U32, "pbb")
        paf = kb.sb([128, 8, 16], F32, "paf")
        pbf = kb.sb([128, 8, 16], F32, "pbf")
        i1s = kb.sb([128, 8, 16], F32, "i1s")
        i2s = kb.sb([128, 8, 16], F32, "i2s")
        eidf = kb.sb([128, 128], F32, "eidf")
        eidi = kb.sb([128, 128], I32, "eidi")
        gmx = kb.sb([128, 8], F32, "gmx")
        gk = kb.sb([128, 8, 16], F32, "gk")
        actv = kb.sb([128, 128], F32, "actv")
        wgt = kb.sb([128, 128], F32, "wgt")
        m8 = kb.sb([128, 8], F32, "m8")
        UbS = [View(o, o.t[:].rearrange("p a b -> p (a b)").bitcast(F32)[:, 0:1024]) for o in (ktc[0], ktc[1], vch[0], vch[1])]
        Ubo = [kb.sb([128, D], F32, "Ub%d" % j) for j in range(3)]
        Ub = [View(o, o.t[:]) for o in Ubo]
        kb.dma("sp", rcnt[:], rcnt_in[:, :, :], writes=[rcnt])
        kb.dma("sp", iota16[:], iota16_in[:, :], writes=[iota16])
        kb.op("dve", lambda e: e.memset(SKf[:], 0.0), writes=[SKf])
        with nc.allow_non_contiguous_dma(reason="small param loads"):
            kb.dma("sp", g2b[:], norm2_g.t.partition_broadcast(128), writes=[g2b])
            kb.dma("sp", pscol[:], pool_scale.t.rearrange("(g d) -> d g", d=128), writes=[pscol])
            kb.dma("sp", wplf[:], w_pool.t.rearrange("g c d -> c g d"), writes=[wplf])
            kb.dma("sp", sktmp[:, 0:64], subkeys.t[0], writes=[sktmp])
            kb.dma("sp", sktmp[:, 64:128], subkeys.t[1], writes=[sktmp])
        kb.op("pe", lambda e: e.transpose(out=pb[2][:, 0:128], in_=sktmp[:], identity=ident_f[:]),
              reads=[sktmp, ident_f], writes=[pb[2]])
        kb.op("dve", lambda e: e.tensor_copy(out=SKf[0:64, 0:128], in_=pb[2][0:64, 0:128]), reads=[pb[2], SKf], writes=[SKf])
        kb.op("dve", lambda e: e.tensor_copy(out=SKf[64:128, 128:256], in_=pb[2][64:128, 0:128]), reads=[pb[2], SKf],
              writes=[SKf])
        kb.op("dve", lambda e: e.tensor_copy(out=SK[:], in_=SKf[:]), reads=[SKf], writes=[SK])
        kb.op("dve", lambda e: e.tensor_copy(out=wpl[:], in_=wplf[:]), reads=[wplf], writes=[wpl])
        IscV = Isc.t[:]
        ssc = IscV[:, 0:2048]
        sscw = IscV[:, 2048:4096]
        cand = IscV[:, 4096:6144]
        candw = IscV[:, 6144:8192]
        ohv = IscV[:, 0:2048]
        ohv2 = IscV[:, 2048:4096]

    def tail_block(i, xb, sample=False):
        tpv = tp_bank.t[:].bitcast(BF16)
        for pr in range(4):
            kb.op("pe", lambda e, pr=pr: e.transpose(out=tpv[:, pr * 128:(pr + 1) * 128],
                                                     in_=ao[:, pr * 128:(pr + 1) * 128], identity=ident_b[:]),
                  reads=[ao, ident_b], writes=[tp_bank])
        kb.op("act", lambda e: e.copy(out=aoT[:], in_=tpv[:, 0:512].rearrange("p (a t) -> p a t", a=4)),
              reads=[tp_bank], writes=[aoT])
        if not sample:
            kb.dma("sp", xpv[:], xprev[i, :, :], writes=[xpv])
            norm_transpose(xpv, hnTp, ntok=16)
            wsl = load_w(win_bf, *CHUNKS[CH_U])
            for g in range(4):
                bk = pb[g // 2]
                c0 = (g % 2) * 144
                for kc in range(8):
                    kb.op("pe", lambda e, bk=bk, c0=c0, g=g, kc=kc, wsl=wsl: e.matmul(
                        bk[:, c0:c0 + 16], lhsT=wsl[:, kc, g * 128:(g + 1) * 128], rhs=hnTp[:, kc, :],
                        start=(kc == 0), stop=(kc == 7)), reads=[wsl, hnTp], writes=[bk])
                for kc in range(8):
                    kb.op("pe", lambda e, bk=bk, c0=c0, g=g, kc=kc, wsl=wsl: e.matmul(
                        bk[:, c0 + 16:c0 + 144], lhsT=wsl[:, kc, g * 128:(g + 1) * 128], rhs=hnT[:, kc, :],
                        start=(kc == 0), stop=(kc == 7)), reads=[wsl, hnT], writes=[bk])
            for hf in range(2):
                kb.op("act", lambda e, hf=hf: e.copy(out=uT[:, 2 * hf:2 * hf + 2, :],
                                                     in_=pb[hf][:, 0:288].rearrange("p (g t) -> p g t", g=2)),
                      reads=[pb[hf]], writes=[uT])
            if i == nq - 1:
                kb.dma("sp", u_last[:, :, :], uT[:], reads=[uT], writes=[u_last])
            kb.op("dve", lambda e: e.tensor_tensor(out=s1[:, :, 1:144], in0=uT[:, :, 1:144], in1=uT[:, :, 0:143],
                                                   op=ALU.add), reads=[uT], writes=[s1])
            kb.op("dve", lambda e: e.tensor_tensor(out=s2[:, 1:4, 3:144], in0=s1[:, 1:4, 3:144], in1=s1[:, 1:4, 1:142],
                                                   op=ALU.add), reads=[s1], writes=[s2])
            kb.op("dve", lambda e: e.tensor_tensor(out=s3[:, 2:4, 7:144], in0=s2[:, 2:4, 7:144], in1=s2[:, 2:4, 3:140],
                                                   op=ALU.add), reads=[s2], writes=[s3])
            kb.op("dve", lambda e: e.tensor_tensor(out=s1[:, 3, 15:144], in0=s3[:, 3, 15:144], in1=s3[:, 3, 7:136],
                                                   op=ALU.add), reads=[s3, s1], writes=[s1])
            wsum = [s1[:, 0, 16:144], s2[:, 1, 16:144], s3[:, 2, 16:144], s1[:, 3, 16:144]]
            wsrc = [s1, s2, s3, s1]
            for g in range(4):
                if i == 0:
                    kb.op("dve", lambda e, g=g: e.tensor_tensor(out=pmf[:, g, :], in0=wsum[g], in1=rcnt[:, g, :],
                                                                op=ALU.mult), reads=[wsrc[g], rcnt], writes=[pmf])
                    kb.op("dve", lambda e, g=g: e.tensor_tensor(out=pmT[:, g, :], in0=pmf[:, g, :], in1=uT[:, g, 16:144],
                                                                op=ALU.subtract), reads=[pmf, uT], writes=[pmT])
                else:
                    kb.op("dve", lambda e, g=g: e.scalar_tensor_tensor(
                        out=pmT[:, g, :], in0=wsum[g], scalar=1.0 / (2 ** (g + 1)), in1=uT[:, g, 16:144],
                        op0=ALU.mult, op1=ALU.subtract), reads=[wsrc[g], uT], writes=[pmT])
        for g in range(4):
            kb.op("pe", lambda e, g=g: e.matmul(pb[0][:, g * 128:(g + 1) * 128], lhsT=wpl[:, g, :], rhs=pmT[:, g, :],
                                                start=True, stop=True), reads=[wpl, pmT], writes=[pb[0]])
        kb.op("dve", lambda e: e.tensor_tensor(out=poT[:], in0=pb[0][:, 0:512].rearrange("p (g t) -> p g t", g=4),
                                               in1=pscol[:].unsqueeze(2).to_broadcast([128, 4, 128]), op=ALU.mult),
              reads=[pb[0], pscol], writes=[poT])
        for (chs, dst) in (((CH_GA0, CH_GA1), sga), ((CH_GB0, CH_GB1), sgb)):
            for hf, chn in enumerate(chs):
                wsl = load_w(win_bf, *CHUNKS[chn])
                bk = pb[hf]
                for ft in range(4):
                    for kc in range(8):
                        kb.op("pe", lambda e, bk=bk, ft=ft, kc=kc, wsl=wsl: e.matmul(
                            bk[:, ft * 128:(ft + 1) * 128], lhsT=wsl[:, kc, ft * 128:(ft + 1) * 128], rhs=hnT[:, kc, :],
                            start=(kc == 0), stop=(kc == 7)), reads=[wsl, hnT], writes=[bk])
                kb.op("act", lambda e, bk=bk, dst=dst, hf=hf: e.activation(
                    out=dst[:, 4 * hf:4 * hf + 4, :], in_=bk[:, 0:512].rearrange("p (f t) -> p f t", f=4),
                    func=AF.Sigmoid), reads=[bk], writes=[dst])
        for hf in range(2):
            wa = load_w(wba_bf, hf * 512, 512, nk=4)
            wb = load_w(wbp_bf, hf * 512, 512, nk=4)
            for ft in range(4):
                for kc in range(4):
                    kb.op("pe", lambda e, ft=ft, kc=kc, wa=wa: e.matmul(
                        pb[0][:, ft * 128:(ft + 1) * 128], lhsT=wa[:, kc, ft * 128:(ft + 1) * 128], rhs=aoT[:, kc, :],
                        start=(kc == 0), stop=(kc == 3)), reads=[wa, aoT], writes=[pb[0]])
                for kc in range(4):
                    kb.op("pe", lambda e, ft=ft, kc=kc, wb=wb: e.matmul(
                        pb[1][:, ft * 128:(ft + 1) * 128], lhsT=wb[:, kc, ft * 128:(ft + 1) * 128], rhs=poT[:, kc, :],
                        start=(kc == 0), stop=(kc == 3)), reads=[wb, poT], writes=[pb[1]])
            kb.op("dve", lambda e, hf=hf: e.tensor_tensor(
                out=tmpA[:], in0=pb[0][:, 0:512], in1=sga[:, 4 * hf:4 * hf + 4, :].rearrange("p f t -> p (f t)"),
                op=ALU.mult), reads=[pb[0], sga], writes=[tmpA])
            kb.op("dve", lambda e, hf=hf: e.tensor_tensor(
                out=tmpB[:], in0=pb[1][:, 0:512], in1=sgb[:, 4 * hf:4 * hf + 4, :].rearrange("p f t -> p (f t)"),
                op=ALU.mult), reads=[pb[1], sgb], writes=[tmpB])
            kb.op("dve", lambda e, hf=hf: e.tensor_tensor(
                out=mT[:, 4 * hf:4 * hf + 4, :].rearrange("p f t -> p (f t)"), in0=tmpA[:], in1=tmpB[:], op=ALU.add),
                reads=[tmpA, tmpB], writes=[mT])
        for hf in range(2):
            wo = load_w(wout_bf, hf * 512, 512, nk=8)
            for kc in range(8):
                kb.op("pe", lambda e, kc=kc, wo=wo, hf=hf: e.matmul(
                    pb[hf][:, 0:512], lhsT=mT[:, kc, :], rhs=wo[:, kc, :], start=(kc == 0), stop=(kc == 7)),
                    reads=[mT, wo], writes=[pb[hf]])
            kb.op("dve", lambda e, hf=hf: e.tensor_tensor(out=h2[:, hf * 512:(hf + 1) * 512], in0=pb[hf][:, 0:512],
                                                          in1=xb[:, hf * 512:(hf + 1) * 512], op=ALU.add),
                  reads=[pb[hf], xb], writes=[NBt])
        norm_transpose(h2, xnT, gtile=g2b, keep=xn, xowner=NBt)
        for hf in range(2):
            wq_ = load_w(pwq_bf, hf * 512, 512, nk=8)
            for ft in range(4):
                for kc in range(8):
                    kb.op("pe", lambda e, ft=ft, kc=kc, wq_=wq_, hf=hf: e.matmul(
                        pb[hf][:, ft * 128:(ft + 1) * 128], lhsT=wq_[:, kc, ft * 128:(ft + 1) * 128], rhs=xnT[:, kc, :],
                        start=(kc == 0), stop=(kc == 7)), reads=[wq_, xnT], writes=[pb[hf]])
            kb.op("act", lambda e, hf=hf: e.copy(out=qpT[:, 4 * hf:4 * hf + 4, :],
                                                 in_=pb[hf][:, 0:512].rearrange("p (f t) -> p f t", f=4)),
                  reads=[pb[hf]], writes=[qpT])
        sbanks = [pb[0], pb[1], pb[3], pb[4]]
        for h in range(8):
            bk = sbanks[h // 2]
            kb.op("pe", lambda e, bk=bk, h=h: e.matmul(bk[:, (h % 2) * 256:(h % 2) * 256 + 256], lhsT=qpT[:, h, :],
                                                      rhs=SK[:], start=True, stop=True),
                  reads=[qpT, SK], writes=[bk])
        for j4 in range(4):
            kb.op("act", lambda e, j4=j4: e.copy(out=ssc[:, j4 * 512:(j4 + 1) * 512], in_=sbanks[j4][:, 0:512]),
                  reads=[sbanks[j4]], writes=[Isc])
        tvv = tv[:].rearrange("p h s k -> p (h s) k")
        tiv = ti[:].rearrange("p h s k -> p (h s) k")
        for gi in range(16):
            sv = ssc[:, gi * 128:(gi + 1) * 128]
            sw = sscw[:, gi * 128:(gi + 1) * 128]
            kb.op("dve", lambda e, sv=sv, gi=gi: e.max(out=tvv[:, gi, 0:8], in_=sv), reads=[Isc], writes=[tv])
            kb.op("dve", lambda e, sv=sv, gi=gi: e.max_index(out=tiv[:, gi, 0:8], in_max=tvv[:, gi, 0:8], in_values=sv),
                  reads=[Isc, tv], writes=[ti])
            kb.op("dve", lambda e, sv=sv, sw=sw, gi=gi: e.match_replace(out=sw, in_to_replace=tvv[:, gi, 0:8],
                                                                       in_values=sv, imm_value=-1e30),
                  reads=[Isc, tv], writes=[Isc])
            kb.op("dve", lambda e, sw=sw, gi=gi: e.max(out=tvv[:, gi, 8:16], in_=sw), reads=[Isc], writes=[tv])
            kb.op("dve", lambda e, sw=sw, gi=gi: e.max_index(out=tiv[:, gi, 8:16], in_max=tvv[:, gi, 8:16], in_values=sw),
                  reads=[Isc, tv], writes=[ti])
        kb.op("dve", lambda e: e.tensor_copy(out=tif[:], in_=ti[:]), reads=[ti], writes=[tif])
        candv = cand.rearrange("p (h a b) -> p h a b", h=8, a=16)
        kb.op("dve", lambda e: e.tensor_tensor(out=candv, in0=tv[:, :, 0, :].unsqueeze(3).to_broadcast([128, 8, 16, 16]),
                                               in1=tv[:, :, 1, :].unsqueeze(2).to_broadcast([128, 8, 16, 16]), op=ALU.add),
              reads=[tv], writes=[Isc])
        for h in range(8):
            cv = cand[:, h * 256:(h + 1) * 256]
            cw = candw[:, h * 256:(h + 1) * 256]
            kb.op("dve", lambda e, cv=cv, h=h: e.max(out=tsv[:, h, 0:8], in_=cv), reads=[Isc], writes=[tsv])
            kb.op("dve", lambda e, cv=cv, h=h: e.max_index(out=tpos[:, h, 0:8], in_max=tsv[:, h, 0:8], in_values=cv),
                  reads=[Isc, tsv], writes=[tpos])
            kb.op("dve", lambda e, cv=cv, cw=cw, h=h: e.match_replace(out=cw, in_to_replace=tsv[:, h, 0:8],
                                                                     in_values=cv, imm_value=-1e30),
                  reads=[Isc, tsv], writes=[Isc])
            kb.op("dve", lambda e, cw=cw, h=h: e.max(out=tsv[:, h, 8:16], in_=cw), reads=[Isc], writes=[tsv])
            kb.op("dve", lambda e, cw=cw, h=h: e.max_index(out=tpos[:, h, 8:16], in_max=tsv[:, h, 8:16], in_values=cw),
                  reads=[Isc, tsv], writes=[tpos])
        kb.op("dve", lambda e: e.tensor_single_scalar(out=pa[:], in_=tpos[:], scalar=4, op=ALU.logical_shift_right),
              reads=[tpos], writes=[pa])
        kb.op("dve", lambda e: e.tensor_single_scalar(out=pbb[:], in_=tpos[:], scalar=15, op=ALU.bitwise_and),
              reads=[tpos], writes=[pbb])
        kb.op("dve", lambda e: e.tensor_copy(out=paf[:], in_=pa[:]), reads=[pa], writes=[paf])
        kb.op("dve", lambda e: e.tensor_copy(out=pbf[:], in_=pbb[:]), reads=[pbb], writes=[pbf])
        oh4 = ohv.rearrange("p (h k a) -> p h k a", h=8, k=16)
        oh42 = ohv2.rearrange("p (h k a) -> p h k a", h=8, k=16)
        io4 = iota16[:].unsqueeze(1).unsqueeze(1).to_broadcast([128, 8, 16, 16])
        for (pf, half, dst) in ((paf, 0, i1s), (pbf, 1, i2s)):
            kb.op("dve", lambda e, pf=pf: e.tensor_tensor(out=oh4, in0=pf[:].unsqueeze(3).to_broadcast([128, 8, 16, 16]),
                                                          in1=io4, op=ALU.is_equal),
                  reads=[pf, iota16], writes=[Isc])
            kb.op("dve", lambda e, half=half: e.tensor_tensor(
                out=oh42, in0=oh4, in1=tif[:, :, half, :].unsqueeze(2).to_broadcast([128, 8, 16, 16]), op=ALU.mult),
                reads=[Isc, tif], writes=[Isc])
            kb.op("dve", lambda e, dst=dst: e.tensor_reduce(out=dst[:], in_=oh42, axis=AX.X, op=ALU.add),
                  reads=[Isc], writes=[dst])
        kb.op("dve", lambda e: e.scalar_tensor_tensor(out=eidf[:].rearrange("p (h k) -> p h k", h=8), in0=i1s[:],
                                                      scalar=128.0, in1=i2s[:], op0=ALU.mult, op1=ALU.add),
              reads=[i1s, i2s], writes=[eidf])
        kb.op("dve", lambda e: e.tensor_copy(out=eidi[:], in_=eidf[:]), reads=[eidf], writes=[eidi])
        kb.op("dve", lambda e: e.tensor_reduce(out=gmx[:], in_=tsv[:], axis=AX.X, op=ALU.max), reads=[tsv], writes=[gmx])
        kb.op("dve", lambda e: e.tensor_tensor(out=gk[:], in0=tsv[:], in1=gmx[:].unsqueeze(2).to_broadcast([128, 8, 16]),
                                               op=ALU.subtract), reads=[tsv, gmx], writes=[gk])
        kb.op("act", lambda e: e.activation(out=gk[:], in_=gk[:], func=AF.Exp), reads=[gk], writes=[gk])
        kb.op("dve", lambda e: e.tensor_reduce(out=gmx[:], in_=gk[:], axis=AX.X, op=ALU.add), reads=[gk], writes=[gmx])
        kb.op("dve", lambda e: e.reciprocal(out=gmx[:], in_=gmx[:]), reads=[gmx], writes=[gmx])
        kb.op("dve", lambda e: e.tensor_tensor(out=gk[:], in0=gk[:], in1=gmx[:].unsqueeze(2).to_broadcast([128, 8, 16]),
                                               op=ALU.mult), reads=[gk, gmx], writes=[gk])
        def peer_steps():
            for hk in range(128):
                ub = Ub[hk % 3]
                kb.dma("pool", ub[:], peer_u[:, :], reads=[eidi], writes=[ub.owner],
                       indirect=bass.IndirectOffsetOnAxis(ap=eidi[:, hk:hk + 1], axis=0), own_sem=hk % 3)
                kb.op("dve", lambda e, ub=ub, hk=hk: e.scalar_tensor_tensor(
                    out=ub[:], in0=ub[:], scalar=1.0, in1=xn[:], op0=ALU.mult, op1=ALU.mult,
                    accum_out=actv[:, hk:hk + 1]), reads=[ub.owner, xn], writes=[ub.owner, actv])
                yield
            kb.op("act", lambda e: e.activation(out=wgt[:], in_=actv[:], func=AF.Gelu), reads=[actv], writes=[wgt])
            kb.op("dve", lambda e: e.tensor_tensor(out=wgt[:], in0=wgt[:], in1=gk[:].rearrange("p h k -> p (h k)"),
                                                   op=ALU.mult), reads=[wgt, gk], writes=[wgt])
            for hk in range(128):
                ub = Ub[hk % 3]
                kb.dma("pool", ub[:], peer_v[:, :], reads=[eidi], writes=[ub.owner],
                       indirect=bass.IndirectOffsetOnAxis(ap=eidi[:, hk:hk + 1], axis=0), own_sem=hk % 3)
                kb.op("dve", lambda e, ub=ub, hk=hk: e.scalar_tensor_tensor(
                    out=h2[:], in0=ub[:], scalar=wgt[:, hk:hk + 1], in1=h2[:], op0=ALU.mult, op1=ALU.add),
                    reads=[ub.owner, wgt, NBt], writes=[NBt])
                yield
            if sample:
                kb.dma("sp", y_s_out[:, :], h2[:], reads=[NBt], writes=[y_s_out])
            else:
                kb.dma("sp", y_own[i, :, :], h2[:], reads=[NBt], writes=[y_own])
            yield
        return peer_steps()

    pending = [None]
    nslots = [1]

    def advance(n=1):
        g = pending[0]
        if g is None:
            return
        for _ in range(n):
            try:
                next(g)
            except StopIteration:
                pending[0] = None
                return

    def drain():
        while pending[0] is not None:
            advance(64)

    for i in range(nq):
        xb = xt[i % 2]
        kb.dma("sp", xb[:], xown[i, :, :], writes=[xb])
        norm_transpose(xb, hnT)
        wsl = load_w(win_bf, *CHUNKS[CH_K])
        proj_tok(pb[0], wsl, 512)
        head_norm(pb[0], kg, ko)
        kb.dma("sp", k_own[i, :, :], ko[:], reads=[ko], writes=[k_own])
        wsl = load_w(win_bf, *CHUNKS[CH_V])
        proj_tok(pb[1], wsl, 512)
        kb.op("act", lambda e: e.copy(out=vo[:], in_=pb[1][:, 0:512]), reads=[pb[1]], writes=[vo])
        kb.dma("sp", v_own[i, :, :], vo[:], reads=[vo], writes=[v_own])
        wsl = load_w(win_bf, *CHUNKS[CH_KW])
        proj_tok(pb[0], wsl, 72)
        kb.op("dve", lambda e: e.tensor_copy(out=kio[:], in_=pb[0][:, 0:64]), reads=[pb[0]], writes=[kio])
        kb.dma("sp", ki_own[i, :, :], kio[:], reads=[kio], writes=[ki_own])
        kb.op("dve", lambda e: e.tensor_scalar(out=wis[:], in0=pb[0][:, 64:72], scalar1=0.125 * (8 ** -0.5),
                                               scalar2=None, op0=ALU.mult), reads=[pb[0]], writes=[wis])
        if not do_attn:
            continue
        tpv = tp_bank.t[:].bitcast(BF16)
        wsl = load_w(win_bf, *CHUNKS[CH_Q])
        proj_tok(pb[0], wsl, 512)
        head_norm(pb[0], qg, qf)
        kb.op("dve", lambda e: e.tensor_copy(out=qbf[:], in_=qf[:]), reads=[qf], writes=[qbf])
        for pr in range(4):
            kb.op("pe", lambda e, pr=pr: e.transpose(out=tpv[:, pr * 128:(pr + 1) * 128],
                                                     in_=qbf[:, pr * 128:(pr + 1) * 128], identity=ident_b[:]),
                  reads=[qbf, ident_b], writes=[tp_bank])
        kb.op("act", lambda e: e.copy(out=qT2[:], in_=tpv[:, 0:512].rearrange("p (a t) -> p a t", a=4)),
              reads=[tp_bank], writes=[qT2])
        wsl = load_w(win_bf, *CHUNKS[CH_QI])
        proj_tok(pb[1], wsl, 512)
        kb.op("act", lambda e: e.copy(out=qbf[:], in_=pb[1][:, 0:512]), reads=[pb[1]], writes=[qbf])
        for pr in range(4):
            kb.op("pe", lambda e, pr=pr: e.transpose(out=tpv[:, pr * 128:(pr + 1) * 128],
                                                     in_=qbf[:, pr * 128:(pr + 1) * 128], identity=ident_b[:]),
                  reads=[qbf, ident_b], writes=[tp_bank])
        kb.op("act", lambda e: e.copy(out=qiT2[:], in_=tpv[:, 0:512].rearrange("p (a t) -> p a t", a=4)),
              reads=[tp_bank], writes=[qiT2])
        nkb = min(4 * i + 4, nblk_a)
        nch = nkb // 4
        nk = nkb * 128
        ibanks = [pb[0], pb[1], pb[3], pb[4]]
        cnt_i = 0
        per_slot = -(-260 // (nch * 8 + nkb))
        for ch in range(nch):
            kc_ = kitc[ch % 2]
            kb.dma("sp", kc_[:], kit_s[:, ch * 512:(ch + 1) * 512], reads=[kit_s], writes=[kc_])
            Ic = Isc[:, ch * 512:(ch + 1) * 512]
            for h in range(8):
                a, half = h // 2, h % 2
                ps_ = slice(64 * half, 64 * half + 64)
                bk = ibanks[cnt_i % 4]
                rl_ = rl[cnt_i % 2]
                cnt_i += 1
                advance(per_slot)
                kb.op("pe", lambda e, bk=bk, a=a, ps_=ps_, kc_=kc_: e.matmul(
                    bk[:, 0:512], lhsT=qiT2[ps_, a, :], rhs=kc_[ps_, :], start=True, stop=True),
                    reads=[qiT2, kc_], writes=[bk])
                kb.op("act", lambda e, bk=bk, rl_=rl_: e.activation(out=rl_[:], in_=bk[:, 0:512], func=AF.Relu),
                      reads=[bk], writes=[rl_])
                if h == 0:
                    kb.op("dve", lambda e, rl_=rl_, Ic=Ic: e.tensor_scalar(
                        out=Ic, in0=rl_[:], scalar1=wis[:, 0:1], scalar2=None, op0=ALU.mult),
                        reads=[rl_, wis], writes=[Isc])
                else:
                    kb.op("dve", lambda e, rl_=rl_, Ic=Ic, h=h: e.scalar_tensor_tensor(
                        out=Ic, in0=rl_[:], scalar=wis[:, h:h + 1], in1=Ic, op0=ALU.mult, op1=ALU.add),
                        reads=[rl_, wis, Isc], writes=[Isc])
        kb.op("dve", lambda e: e.tensor_reduce(out=hi0[:], in_=Isc[:, 0:nk], axis=AX.X, op=ALU.max),
              reads=[Isc], writes=[hi0])
        kb.op("dve", lambda e: e.tensor_reduce(out=lo[:], in_=Isc[:, 0:nk], axis=AX.X, op=ALU.min),
              reads=[Isc], writes=[lo])
        kb.op("dve", lambda e: e.tensor_tensor(out=Isc[:, nk - 512:nk], in0=Isc[:, nk - 512:nk], in1=cmask[:],
                                               op=ALU.add), reads=[Isc, cmask], writes=[Isc])
        kb.op("dve", lambda e: e.tensor_tensor(out=hi0[:], in0=hi0[:], in1=lo[:], op=ALU.subtract),
              reads=[hi0, lo], writes=[hi0])
        kb.op("dve", lambda e: e.tensor_scalar(out=wh[:], in0=pw[:], scalar1=hi0[:, 0:1], scalar2=None,
                                               op0=ALU.mult), reads=[pw, hi0], writes=[wh])
        for n in range(NIT):
            kb.op("dve", lambda e, n=n: e.tensor_tensor(out=mid[:], in0=lo[:], in1=wh[:, n:n + 1], op=ALU.add),
                  reads=[lo, wh], writes=[mid])
            kb.op("dve", lambda e: e.memset(cnt4[:], 0.0), writes=[cnt4])
            for q4 in range((nk + 2175) // 2176):
                c0 = q4 * 2176
                c1 = min(nk, c0 + 2176)
                kb.op("dve", lambda e, c0=c0, c1=c1, q4=q4: e.tensor_scalar(
                    out=junkI[:, 0:c1 - c0], in0=Isc[:, c0:c1], scalar1=mid[:, 0:1],
                    scalar2=0.0, op0=ALU.is_ge, op1=ALU.add, accum_out=cnt4[:, q4:q4 + 1]),
                    reads=[Isc, mid], writes=[junkI, cnt4])
            kb.op("dve", lambda e: e.tensor_reduce(out=cntt[:], in_=cnt4[:], axis=AX.X, op=ALU.add),
                  reads=[cnt4], writes=[cntt])
            kb.op("dve", lambda e, n=n: e.tensor_scalar(out=dl[:], in0=cntt[:], scalar1=255.5,
                                                        scalar2=wh[:, n:n + 1], op0=ALU.is_ge, op1=ALU.mult),
                  reads=[cntt, wh], writes=[dl])
            kb.op("dve", lambda e: e.tensor_tensor(out=lo[:], in0=lo[:], in1=dl[:], op=ALU.add),
                  reads=[lo, dl], writes=[lo])
        kb.op("dve", lambda e: e.memset(pb[5][:, 0:260], 0.0), writes=[pb[5]])
        kb.op("dve", lambda e: e.memset(pb[6][:, 0:260], 0.0), writes=[pb[6]])
        def emit_S(kbi):
            ch, kloc = kbi // 4, kbi % 4
            ktc_ = ktc[ch % 2]
            vch_ = vch[ch % 2]
            if kloc == 0:
                kb.dma("sp", ktc_[:], kt_s[:, :, ch * 512:(ch + 1) * 512], reads=[kt_s], writes=[ktc_])
                kb.dma("sp", vch_[:], v_s.t[ch * 512:(ch + 1) * 512, :, :].rearrange("(b p) h e -> p b (h e)", p=128),
                       reads=[v_s], writes=[vch_])
            nm = negm[kbi % 2]
            kb.op("dve", lambda e, nm=nm, kbi=kbi: e.tensor_scalar(
                out=nm[:], in0=Isc[:, kbi * 128:(kbi + 1) * 128], scalar1=lo[:, 0:1], scalar2=-30000.0,
                op0=ALU.is_lt, op1=ALU.mult), reads=[Isc, lo], writes=[nm])
            r = kbi - 4 * i
            near = (-1 <= r <= 3)
            if near:
                kb.op("dve", lambda e, nm=nm, r=r: e.tensor_tensor(
                    out=addh[:], in0=NB[r + 1][:], in1=nm[:].unsqueeze(1).to_broadcast([128, 8, 128]), op=ALU.add),
                    reads=[NB[r + 1], nm], writes=[addh])
            pt_ = PT[kbi % 2]
            for g in range(2):
                sb_ = pb[3 + g]
                for hh in range(4):
                    h = 4 * g + hh
                    a, half = h // 2, h % 2
                    ps_ = slice(64 * half, 64 * half + 64)
                    kb.op("pe", lambda e, sb_=sb_, hh=hh, a=a, ps_=ps_, ktc_=ktc_, kloc=kloc: e.matmul(
                        sb_[:, hh * 128:(hh + 1) * 128], lhsT=ktc_[ps_, a, kloc * 128:(kloc + 1) * 128],
                        rhs=qT2[ps_, a, :], start=True, stop=False), reads=[ktc_, qT2], writes=[sb_])
                    if near:
                        kb.op("pe", lambda e, sb_=sb_, hh=hh, h=h: e.matmul(
                            sb_[:, hh * 128:(hh + 1) * 128], lhsT=addh[:, h, :], rhs=ident_b[:],
                            start=False, stop=True), reads=[addh, ident_b], writes=[sb_])
                    else:
                        kb.op("pe", lambda e, sb_=sb_, hh=hh, nm=nm: e.matmul(
                            sb_[:, hh * 128:(hh + 1) * 128], lhsT=nm[:], rhs=ident_b[:],
                            start=False, stop=True), reads=[nm, ident_b], writes=[sb_])
                kb.op("act", lambda e, sb_=sb_, pt_=pt_, g=g: e.activation(
                    out=pt_[:, 4 * g:4 * g + 4, :], in_=sb_[:, 0:512].rearrange("p (h q) -> p h q", h=4),
                    func=AF.Exp), reads=[sb_], writes=[pt_])

        def emit_PV(kbi):
            ch, kloc = kbi // 4, kbi % 4
            vch_ = vch[ch % 2]
            pt_ = PT[kbi % 2]
            for h in range(8):
                ob = pb[5 + h // 4]
                kb.op("pe", lambda e, ob=ob, h=h, pt_=pt_, vch_=vch_, kloc=kloc: e.matmul(
                    ob[:, (h % 4) * 65:(h % 4) * 65 + 65], lhsT=pt_[:, h, :], rhs=vch_[:, kloc, h * 65:(h + 1) * 65],
                    start=False, stop=False, skip_group_check=True), reads=[pt_, vch_], writes=[ob])

        for kbi in range(nkb):
            emit_S(kbi)
            advance(per_slot)
            if kbi > 0:
                emit_PV(kbi - 1)
        emit_PV(nkb - 1)
        for g in range(2):
            ob = pb[5 + g]
            obv = ob[:, 0:260].rearrange("p (h e) -> p h e", h=4)
            kb.op("dve", lambda e, obv=obv, g=g: e.reciprocal(out=rden[:, 4 * g:4 * g + 4], in_=obv[:, :, 64]),
                  reads=[ob], writes=[rden])
            kb.op("dve", lambda e, obv=obv, g=g: e.tensor_tensor(
                out=ao[:, 256 * g:256 * g + 256].rearrange("p (h d) -> p h d", h=4), in0=obv[:, :, 0:64],
                in1=rden[:, 4 * g:4 * g + 4].unsqueeze(2).to_broadcast([128, 4, 64]), op=ALU.mult),
                reads=[ob, rden], writes=[ao])
        if dbg_ao is not None:
            kb.op("dve", lambda e: e.tensor_copy(out=aof[:], in_=ao[:]), reads=[ao], writes=[aof])
            kb.dma("sp", dbg_ao[i, :, :], aof[:], reads=[aof], writes=[dbg_ao])

        if do_tail:
            drain()
            pending[0] = tail_block(i, xb)


    drain()
    if do_sample:
        LOB = _bucket_lo()
        wrep = kb.sb([128, 8], F32, "wrep")
        idx2 = kb.sb([128, 2], I32, "idx2")
        pti = kb.sb([128, 16], I32, "pti")
        ptf = kb.sb([128, 16], F32, "ptf")
        dh = kb.sb([128, 128], F32, "dh")
        dh2 = kb.sb([128, 128], F32, "dh2")
        scs = kb.sb([128, 2, 128], F32, "scs")
        scn = kb.sb([128, 1], F32, "scn")
        dn8 = kb.sb([128, 8], F32, "dn8")
        sm8 = kb.sb([128, 8], F32, "sm8")
        tA = View(Ubo[0], Ubo[0].t[:, 0:256])
        tE = View(Ubo[0], Ubo[0].t[:, 256:512])
        tF = View(Ubo[0], Ubo[0].t[:, 512:768])
        tG = View(Ubo[0], Ubo[0].t[:, 768:1024])
        idxs = View(Ubo[1], Ubo[1].t[:].bitcast(U32)[:, 0:256])
        tC = View(Ubo[1], Ubo[1].t[:].bitcast(U32)[:, 256:512])
        tD = View(Ubo[1], Ubo[1].t[:].bitcast(U32)[:, 512:768])
        rowT = kb.sb([128, 32], I32, "rowT")
        distT = kb.sb([128, 32], F32, "distT")
        negT = kb.sb([128, 32], F32, "negT")
        indT = kb.sb([128, 32], F32, "indT")
        biasT = kb.sb([128, 32, 8], F32, "biasT")
        tmp3 = kb.sb([128, 32, 8], F32, "tmp3")
        lg = kb.sb([128, 8], F32, "lg")
        pex = kb.sb([128, 8], F32, "pex")
        zsel = kb.sb([128, 31], F32, "zsel")
        numS = View(Ubo[2], Ubo[2].t[:, 0:512])
        denS = kb.sb([128, 8], F32, "denS")
        seln = kb.sb([128, 1], F32, "seln")
        nb0 = kb.sb([128, 8], F32, "nb0")
        sq, sk_, sv_, ski = qf, ko, vo, kio
        sqi = hsq
        su = tmpA
        qrep = tmpB
        IscV = Isc.t[:]
        kb.dma("sp", zsel[:], zsel_in[:, :], writes=[zsel])
        kb.op("dve", lambda e: e.memset(pti[:], 0), writes=[pti])
        kb.dma("sp", pti[0:16, :], pt_in[:, :], writes=[pti])
        kb.op("dve", lambda e: e.tensor_copy(out=ptf[:], in_=pti[:]), reads=[pti], writes=[ptf])
        with nc.allow_non_contiguous_dma(reason="page table slices"):
            for jg in range(8):
                kb.dma("sp", idx2[jg * 16:(jg + 1) * 16, :], pt_in[:, 2 * jg:2 * jg + 2], writes=[idx2])
        xb = xt[0]
        kb.dma("sp", xb[:], xs_in[:, :], writes=[xb])
        norm_transpose(xb, hnT)
        wsl = load_w(win_bf, *CHUNKS[CH_Q])
        proj_tok(pb[0], wsl, 512)
        head_norm(pb[0], qg, sq)
        wsl = load_w(win_bf, *CHUNKS[CH_K])
        proj_tok(pb[1], wsl, 512)
        head_norm(pb[1], kg, sk_)
        kb.dma("sp", k_s_out[:, :], sk_[:], reads=[sk_], writes=[k_s_out])
        wsl = load_w(win_bf, *CHUNKS[CH_V])
        proj_tok(pb[0], wsl, 512)
        kb.op("act", lambda e: e.copy(out=sv_[:], in_=pb[0][:, 0:512]), reads=[pb[0]], writes=[sv_])
        kb.dma("sp", v_s_out[:, :], sv_[:], reads=[sv_], writes=[v_s_out])
        wsl = load_w(win_bf, *CHUNKS[CH_QI])
        proj_tok(pb[1], wsl, 512)
        kb.op("act", lambda e: e.copy(out=sqi[:], in_=pb[1][:, 0:512]), reads=[pb[1]], writes=[sqi])
        wsl = load_w(win_bf, *CHUNKS[CH_KW])
        proj_tok(pb[0], wsl, 72)
        kb.op("dve", lambda e: e.tensor_copy(out=ski[:], in_=pb[0][:, 0:64]), reads=[pb[0]], writes=[ski])
        kb.dma("sp", ki_s_out[:, :], ski[:], reads=[ski], writes=[ki_s_out])
        kb.op("dve", lambda e: e.tensor_scalar(out=wis[:], in0=pb[0][:, 64:72], scalar1=0.125 * (8 ** -0.5),
                                               scalar2=None, op0=ALU.mult), reads=[pb[0]], writes=[wis])
        wsl = load_w(win_bf, *CHUNKS[CH_U])
        proj_tok(pb[1], wsl, 512)
        kb.op("act", lambda e: e.copy(out=su[:], in_=pb[1][:, 0:512]), reads=[pb[1]], writes=[su])
        kb.dma("sp", pool_s_out[:, 0:14, :], state_in[:, 1:15, :], reads=[state_in], writes=[pool_s_out])
        kb.dma("sp", pool_s_out[:, 14, :], su[0:16, :], reads=[su], writes=[pool_s_out])
        kb.dma("sp", qi_d[:, :], sqi[0:16, :], reads=[sqi], writes=[qi_d])
        kb.dma("sp", wi_d[:, :], wis[0:16, :], reads=[wis], writes=[wi_d])
        kb.dma("sp", q_d[:, :], sq[0:16, :], reads=[sq], writes=[q_d])
        for jg in range(8):
            kb.dma("sp", qrep[jg * 16:(jg + 1) * 16, :], qi_d[:, :], reads=[qi_d], writes=[qrep])
            kb.dma("sp", wrep[jg * 16:(jg + 1) * 16, :], wi_d[:, :], reads=[wi_d], writes=[wrep])
        KI = IscV[:, 0:8192]
        prodc = View(ktc[0], ktc[0].t[:].rearrange("p a b -> p (a b)").bitcast(F32)[:, 0:1024])
        for s in range(2):
            kb.dma("pool", KI, cache_ik[:, :], reads=[idx2], writes=[Isc],
                   indirect=bass.IndirectOffsetOnAxis(ap=idx2[:, s:s + 1], axis=0))
            for h in range(8):
                for c8 in range(8):
                    kb.op("dve", lambda e, h=h, c8=c8: e.tensor_tensor(
                        out=prodc[:].rearrange("p (k d) -> p k d", k=16),
                        in0=KI[:, c8 * 1024:(c8 + 1) * 1024].rearrange("p (k d) -> p k d", k=16),
                        in1=qrep[:, h * 64:(h + 1) * 64].unsqueeze(1).to_broadcast([128, 16, 64]), op=ALU.mult),
                        reads=[Isc, qrep], writes=[prodc.owner])
                    kb.op("dve", lambda e, c8=c8: e.tensor_reduce(
                        out=dh[:, c8 * 16:(c8 + 1) * 16], in_=prodc[:].rearrange("p (k d) -> p k d", k=16),
                        axis=AX.X, op=ALU.add), reads=[prodc.owner], writes=[dh])
                if h == 0:
                    kb.op("dve", lambda e, s=s: e.tensor_scalar(out=scs[:, s, :], in0=dh[:], scalar1=0.0,
                                                                scalar2=wrep[:, 0:1], op0=ALU.max, op1=ALU.mult),
                          reads=[dh, wrep], writes=[scs])
                else:
                    kb.op("dve", lambda e, h=h: e.tensor_scalar(out=dh2[:], in0=dh[:], scalar1=0.0,
                                                                scalar2=wrep[:, h:h + 1], op0=ALU.max, op1=ALU.mult),
                          reads=[dh, wrep], writes=[dh2])
                    kb.op("dve", lambda e, s=s: e.tensor_tensor(out=scs[:, s, :], in0=scs[:, s, :], in1=dh2[:],
                                                                op=ALU.add), reads=[scs, dh2], writes=[scs])
        for jg in range(8):
            kb.dma("sp", sc_d[:, jg * 256:(jg + 1) * 256], scs[jg * 16:(jg + 1) * 16, :, :].rearrange("p s k -> p (s k)"),
                   reads=[scs], writes=[sc_d])
        xtmp = xn[:, 512:1024]
        kb.op("dve", lambda e: e.tensor_tensor(out=xtmp.rearrange("p (h d) -> p h d", h=8),
                                               in0=sqi[:].rearrange("p (h d) -> p h d", h=8),
                                               in1=ski[:].unsqueeze(1).to_broadcast([128, 8, 64]), op=ALU.mult),
              reads=[sqi, ski], writes=[xn])
        kb.op("dve", lambda e: e.tensor_reduce(out=dn8[:], in_=xtmp.rearrange("p (h d) -> p h d", h=8), axis=AX.X,
                                               op=ALU.add), reads=[xn], writes=[dn8])
        kb.op("dve", lambda e: e.tensor_scalar(out=dn8[:], in0=dn8[:], scalar1=0.0, scalar2=None, op0=ALU.max),
              reads=[dn8], writes=[dn8])
        kb.op("dve", lambda e: e.tensor_tensor(out=dn8[:], in0=dn8[:], in1=wis[:], op=ALU.mult),
              reads=[dn8, wis], writes=[dn8])
        kb.op("dve", lambda e: e.tensor_reduce(out=scn[:], in_=dn8[:], axis=AX.X, op=ALU.add),
              reads=[dn8], writes=[scn])
        Is = IscV[:, 0:2049]
        kb.op("dve", lambda e: e.memset(IscV[:, 0:2304], 0.0), writes=[Isc])
        kb.dma("sp", IscV[0:16, 0:2048], sc_d[:, :], reads=[sc_d], writes=[Isc])
        kb.op("dve", lambda e: e.tensor_copy(out=IscV[:, 2048:2049], in_=scn[:]), reads=[scn], writes=[Isc])
        for rd in range(32):
            kb.op("dve", lambda e: e.max(out=sm8[:], in_=Is), reads=[Isc], writes=[sm8])
            kb.op("dve", lambda e, rd=rd: e.max_index(out=idxs[:, rd * 8:(rd + 1) * 8], in_max=sm8[:], in_values=Is),
                  reads=[Isc, sm8], writes=[idxs])
            kb.op("dve", lambda e: e.match_replace(out=Is, in_to_replace=sm8[:], in_values=Is, imm_value=-1e30),
                  reads=[Isc, sm8], writes=[Isc])
        kb.op("dve", lambda e: e.tensor_copy(out=tA[:], in_=idxs[:]), reads=[idxs], writes=[tA])
        kb.op("dve", lambda e: e.tensor_scalar(out=tC[:], in0=tA[:], scalar1=2047.0, scalar2=None, op0=ALU.min),
              reads=[tA], writes=[tC])
        kb.op("dve", lambda e: e.tensor_single_scalar(out=tD[:], in_=tC[:], scalar=7, op=ALU.logical_shift_right),
              reads=[tC], writes=[tD])
        kb.op("dve", lambda e: e.tensor_single_scalar(out=tC[:], in_=tC[:], scalar=127, op=ALU.bitwise_and),
              reads=[tC], writes=[tC])
        kb.op("dve", lambda e: e.tensor_copy(out=tE[:], in_=tD[:]), reads=[tD], writes=[tE])
        kb.op("dve", lambda e: e.tensor_copy(out=tF[:], in_=tC[:]), reads=[tC], writes=[tF])
        ohs = IscV[:, 4608:8704].rearrange("p (k j) -> p k j", k=256)
        kb.op("dve", lambda e: e.tensor_tensor(out=ohs, in0=tE[:].unsqueeze(2).to_broadcast([128, 256, 16]),
                                               in1=iota16[:].unsqueeze(1).to_broadcast([128, 256, 16]), op=ALU.is_equal),
              reads=[tE, iota16], writes=[Isc])
        kb.op("dve", lambda e: e.tensor_tensor(out=ohs, in0=ohs, in1=ptf[:].unsqueeze(1).to_broadcast([128, 256, 16]),
                                               op=ALU.mult), reads=[Isc, ptf], writes=[Isc])
        kb.op("dve", lambda e: e.tensor_reduce(out=tG[:], in_=ohs, axis=AX.X, op=ALU.add), reads=[Isc], writes=[tG])
        kb.op("dve", lambda e: e.scalar_tensor_tensor(out=tG[:], in0=tG[:], scalar=128.0, in1=tF[:], op0=ALU.mult,
                                                      op1=ALU.add), reads=[tG, tF], writes=[tG])
        kb.op("dve", lambda e: e.tensor_scalar(out=tE[:], in0=tA[:], scalar1=-1.0, scalar2=2048.0, op0=ALU.mult,
                                               op1=ALU.add), reads=[tA], writes=[tE])
        kb.op("dve", lambda e: e.tensor_scalar(out=tF[:], in0=tA[:], scalar1=2047.5, scalar2=-30000.0, op0=ALU.is_ge,
                                               op1=ALU.mult), reads=[tA], writes=[tF])
        kb.op("dve", lambda e: e.tensor_reduce(out=seln[:], in_=tF[:], axis=AX.X, op=ALU.min), reads=[tF], writes=[seln])
        kb.op("dve", lambda e: e.tensor_scalar(out=seln[:], in0=seln[:], scalar1=-1.0 / 30000.0, scalar2=None,
                                               op0=ALU.mult), reads=[seln], writes=[seln])
        for (srcb, dstb) in ((tG, rowT), (tE, distT), (tF, negT)):
            for gq in range(2):
                kb.op("pe", lambda e, srcb=srcb, gq=gq: e.transpose(out=pb[2][:, gq * 128:(gq + 1) * 128],
                                                                   in_=srcb[:, gq * 128:(gq + 1) * 128],
                                                                   identity=ident_f[:]),
                      reads=[srcb, ident_f], writes=[pb[2]])
            kb.op("dve", lambda e, dstb=dstb: e.tensor_copy(
                out=dstb[:].rearrange("p (g b) -> p g b", g=2),
                in_=pb[2][:, 0:256].rearrange("p (g b) -> p g b", g=2)[:, :, 0:16]), reads=[pb[2]], writes=[dstb])
        kb.op("dve", lambda e: e.memset(biasT[:], 0.0), writes=[biasT])
        for bkt in range(1, 32):
            kb.op("dve", lambda e, bkt=bkt: e.tensor_scalar(out=indT[:], in0=distT[:], scalar1=float(LOB[bkt - 1]),
                                                            scalar2=None, op0=ALU.is_lt), reads=[distT], writes=[indT])
            kb.op("dve", lambda e, bkt=bkt: e.tensor_tensor(
                out=tmp3[:], in0=indT[:].unsqueeze(2).to_broadcast([128, 32, 8]),
                in1=ndel[:, bkt * 8:(bkt + 1) * 8].unsqueeze(1).to_broadcast([128, 32, 8]), op=ALU.mult),
                reads=[indT, ndel], writes=[tmp3])
            kb.op("dve", lambda e: e.tensor_tensor(out=biasT[:], in0=biasT[:], in1=tmp3[:], op=ALU.add),
                  reads=[biasT, tmp3], writes=[biasT])
        kb.op("dve", lambda e: e.memset(pb[5][:, 0:512], 0.0), writes=[pb[5]])
        kb.op("dve", lambda e: e.memset(pb[6][:, 0:8], 0.0), writes=[pb[6]])
        qbv = xn[:, 0:512]
        pvv = xn[:, 512:1024]
        for b in range(16):
            kb.dma("sp", qbv, q_d.t[b:b + 1, :].partition_broadcast(128).rearrange("p o d -> p (o d)"),
                   reads=[q_d], writes=[xn])
            for gq in range(2):
                col = gq * 16 + b
                kg_ = UbS[gq]
                vg_ = UbS[2 + gq]
                kb.dma("pool", kg_[:, 0:512], cache_k[:, :], reads=[rowT], writes=[kg_.owner],
                       indirect=bass.IndirectOffsetOnAxis(ap=rowT[:, col:col + 1], axis=0))
                kb.dma("pool", vg_[:, 0:512], cache_v[:, :], reads=[rowT], writes=[vg_.owner],
                       indirect=bass.IndirectOffsetOnAxis(ap=rowT[:, col:col + 1], axis=0))
                kb.op("dve", lambda e, kg_=kg_: e.tensor_tensor(out=pvv, in0=kg_[:, 0:512], in1=qbv, op=ALU.mult),
                      reads=[kg_.owner, xn], writes=[xn])
                kb.op("dve", lambda e: e.tensor_reduce(out=lg[:], in_=pvv.rearrange("p (h d) -> p h d", h=8),
                                                       axis=AX.X, op=ALU.add), reads=[xn], writes=[lg])
                kb.op("dve", lambda e, col=col: e.scalar_tensor_tensor(
                    out=lg[:], in0=lg[:], scalar=negT[:, col:col + 1], in1=biasT[:, col, :], op0=ALU.add, op1=ALU.add),
                    reads=[lg, negT, biasT], writes=[lg])
                kb.op("act", lambda e: e.activation(out=pex[:], in_=lg[:], func=AF.Exp), reads=[lg], writes=[pex])
                kb.op("dve", lambda e, vg_=vg_: e.tensor_tensor(
                    out=pvv.rearrange("p (h d) -> p h d", h=8), in0=vg_[:, 0:512].rearrange("p (h d) -> p h d", h=8),
                    in1=pex[:].unsqueeze(2).to_broadcast([128, 8, 64]), op=ALU.mult),
                    reads=[vg_.owner, pex], writes=[xn])
                kb.op("pe", lambda e, b=b: e.matmul(pb[5][0:16, 0:512], lhsT=zsel[:, 15 - b:31 - b], rhs=pvv,
                                                    start=False, stop=False, skip_group_check=True),
                      reads=[zsel, xn], writes=[pb[5]])
                kb.op("pe", lambda e, b=b: e.matmul(pb[6][0:16, 0:8], lhsT=zsel[:, 15 - b:31 - b], rhs=pex[:],
                                                    start=False, stop=False, skip_group_check=True),
                      reads=[zsel, pex], writes=[pb[6]])
        kb.op("dve", lambda e: e.memset(numS[:], 0.0), writes=[numS])
        kb.op("dve", lambda e: e.memset(denS[:], 1.0), writes=[denS])
        kb.op("dve", lambda e: e.tensor_copy(out=numS[0:16, :], in_=pb[5][0:16, 0:512]), reads=[pb[5]], writes=[numS])
        kb.op("dve", lambda e: e.tensor_copy(out=denS[0:16, :], in_=pb[6][0:16, 0:8]), reads=[pb[6]], writes=[denS])
        kb.op("dve", lambda e: e.tensor_reduce(out=nb0[:], in_=ndel[:, 8:256].rearrange("p (b h) -> p h b", h=8),
                                               axis=AX.X, op=ALU.add), reads=[ndel], writes=[nb0])
        kb.op("dve", lambda e: e.tensor_tensor(out=pvv, in0=sq[:], in1=sk_[:], op=ALU.mult), reads=[sq, sk_], writes=[xn])
        kb.op("dve", lambda e: e.tensor_reduce(out=lg[:], in_=pvv.rearrange("p (h d) -> p h d", h=8), axis=AX.X,
                                               op=ALU.add), reads=[xn], writes=[lg])
        kb.op("dve", lambda e: e.tensor_tensor(out=lg[:], in0=lg[:], in1=nb0[:], op=ALU.add), reads=[lg, nb0], writes=[lg])
        kb.op("act", lambda e: e.activation(out=pex[:], in_=lg[:], func=AF.Exp), reads=[lg], writes=[pex])
        kb.op("dve", lambda e: e.tensor_scalar(out=pex[:], in0=pex[:], scalar1=seln[:, 0:1], scalar2=None, op0=ALU.mult),
              reads=[pex, seln], writes=[pex])
        kb.op("dve", lambda e: e.tensor_tensor(out=pvv.rearrange("p (h d) -> p h d", h=8),
                                               in0=sv_[:].rearrange("p (h d) -> p h d", h=8),
                                               in1=pex[:].unsqueeze(2).to_broadcast([128, 8, 64]), op=ALU.mult),
              reads=[sv_, pex], writes=[xn])
        kb.op("dve", lambda e: e.tensor_tensor(out=numS[:], in0=numS[:], in1=pvv, op=ALU.add), reads=[numS, xn], writes=[numS])
        kb.op("dve", lambda e: e.tensor_tensor(out=denS[:], in0=denS[:], in1=pex[:], op=ALU.add), reads=[denS, pex],
              writes=[denS])
        kb.op("dve", lambda e: e.reciprocal(out=denS[:], in_=denS[:]), reads=[denS], writes=[denS])
        kb.op("dve", lambda e: e.tensor_tensor(out=ao[:].rearrange("p (h d) -> p h d", h=8),
                                               in0=numS[:].rearrange("p (h d) -> p h d", h=8),
                                               in1=denS[:].unsqueeze(2).to_broadcast([128, 8, 64]), op=ALU.mult),
              reads=[numS, denS], writes=[ao])
        stv = IscV[:, 0:7680].rearrange("p (r c) -> p r c", r=15)
        kb.op("dve", lambda e: e.memset(IscV[:, 0:7680], 0.0), writes=[Isc])
        kb.dma("sp", IscV[0:16, 0:7680], state_in.t.rearrange("b r c -> b (r c)"), reads=[state_in], writes=[Isc])
        for g in range(4):
            w = 2 ** (g + 1)
            kb.op("dve", lambda e, g=g, w=w: e.tensor_reduce(
                out=pmf[:, g, :], in_=stv[:, 16 - w:15, g * 128:(g + 1) * 128].rearrange("p r c -> p c r"),
                axis=AX.X, op=ALU.add), reads=[Isc], writes=[pmf])
            kb.op("dve", lambda e, g=g: e.tensor_tensor(out=pmf[:, g, :], in0=pmf[:, g, :], in1=su[:, g * 128:(g + 1) * 128],
                                                        op=ALU.add), reads=[pmf, su], writes=[pmf])
            kb.op("dve", lambda e, g=g, w=w: e.scalar_tensor_tensor(
                out=pmf[:, g, :], in0=pmf[:, g, :], scalar=1.0 / w, in1=su[:, g * 128:(g + 1) * 128], op0=ALU.mult,
                op1=ALU.subtract), reads=[pmf, su], writes=[pmf])
        for g in range(4):
            kb.op("pe", lambda e, g=g: e.transpose(out=pb[2][:, g * 128:(g + 1) * 128], in_=pmf[:, g, :],
                                                   identity=ident_f[:]), reads=[pmf, ident_f], writes=[pb[2]])
        kb.op("dve", lambda e: e.tensor_copy(out=pmT[:], in_=pb[2][:, 0:512].rearrange("p (g t) -> p g t", g=4)),
              reads=[pb[2]], writes=[pmT])
        pending[0] = tail_block(0, xb, sample=True)
        drain()

    kb.finish()
    return nc, kb


def _prep_inputs(inp, cfg, cores):
    nblk_a = cfg.get("nblk_a", NBLK_A)
    nq = cfg.get("nq", NQ)
    nkeys = nblk_a * 128
    xp = np.asarray(inp["x_prompt"], np.float32)
    meta = np.asarray(inp["meta_tokens"], np.float32)
    maps = []
    for c in cores:
        b, cc = c // 4, c % 4
        full = np.zeros((max(nkeys, (4 * nq + 4) * 128), D), np.float32)
        T = 16 + xp.shape[1]
        cat = np.concatenate([meta, xp[b]], axis=0)
        n = min(T, full.shape[0])
        full[:n] = cat[:n]
        xown = np.zeros((nq, 128, D), np.float32)
        xprev = np.zeros((nq, 16, D), np.float32)
        for i in range(nq):
            j = 4 * i + cc
            xown[i] = full[j * 128:(j + 1) * 128]
            if j > 0:
                xprev[i] = full[j * 128 - 16:j * 128]
        m = {
            "xcat": np.ascontiguousarray(full[:nkeys]),
            "xown": xown,
            "xprev": xprev,
            "w_in": np.ascontiguousarray(np.asarray(inp["w_in"], np.float32)[0]),
            "norm1_g": np.ascontiguousarray(np.asarray(inp["norm1_g"], np.float32)[0]),
            "q_norm_g": np.ascontiguousarray(np.asarray(inp["q_norm_g"], np.float32)[0]),
            "k_norm_g": np.ascontiguousarray(np.asarray(inp["k_norm_g"], np.float32)[0]),
            "ident": np.eye(128, dtype=np.float32),
            "rel_bias": np.ascontiguousarray(np.asarray(inp["rel_bias"], np.float32)),
            "qs": (np.arange(128, dtype=np.float32)[:, None] - np.arange(128, dtype=np.float32)[None, :]),
            "thrtab": _thrtab(cc),
            "cmask": _cmask(cc),
            "w_ba": np.ascontiguousarray(np.asarray(inp["w_branch_attn"], np.float32)[0]),
            "w_bp": np.ascontiguousarray(np.asarray(inp["w_branch_pool"], np.float32)[0]),
            "w_out": np.ascontiguousarray(np.asarray(inp["w_out"], np.float32)[0]),
            "peer_wq": np.ascontiguousarray(np.asarray(inp["peer_wq"], np.float32)[0]),
            "w_pool": np.ascontiguousarray(np.asarray(inp["w_pool"], np.float32)[0]),
            "pool_scale": np.ascontiguousarray(np.asarray(inp["pool_scale"], np.float32)[0]),
            "norm2_g": np.ascontiguousarray(np.asarray(inp["norm2_g"], np.float32)[0]),
            "subkeys": np.ascontiguousarray(np.asarray(inp["peer_subkeys"], np.float32)[0]),
            "peer_u": np.ascontiguousarray(np.asarray(inp["peer_u"], np.float32)[0]),
            "peer_v": np.ascontiguousarray(np.asarray(inp["peer_v"], np.float32)[0]),
            "rcnt": _rcnt(cc),
            "iota16": np.broadcast_to(np.arange(16, dtype=np.float32)[None, :], (128, 16)).copy(),
            "pw": np.broadcast_to((0.5 ** np.arange(1, NIT + 1)).astype(np.float32)[None, :], (128, NIT)).copy(),
        }
        if cfg.get("sample", True):
            xs = np.zeros((128, D), np.float32)
            xs[:16] = np.asarray(inp["x_sample"], np.float32)[16 * c:16 * c + 16, 0]
            z = np.zeros((128, 31), np.float32)
            z[:, 15] = 1.0
            m.update({
                "xs_own": xs,
                "cache_k": np.asarray(inp["cache_k"], np.float32).reshape(2560 * 128, 512),
                "cache_v": np.asarray(inp["cache_v"], np.float32).reshape(2560 * 128, 512),
                "cache_ik": np.asarray(inp["cache_idx_k"], np.float32).reshape(2560, 8192),
                "state_own": np.ascontiguousarray(np.asarray(inp["state_pool"], np.float32)[0, 16 * c:16 * c + 16]),
                "pt_own": np.ascontiguousarray(np.asarray(inp["page_table"], np.int32)[16 * c:16 * c + 16]),
                "zsel": z,
            })
        maps.append(m)
    return maps


def _bucket_lo():
    n = np.arange(0, 256)
    nf = np.maximum(n, 16).astype(np.float32)
    large = 16 + (np.log(nf / np.float32(16)) / np.float32(np.log(128 / 16)) * np.float32(16)).astype(np.int32)
    large = np.minimum(large, 31)
    bkt = np.where(n < 16, n, large)
    return [int(np.min(n[bkt >= b])) for b in range(1, 32)]


def _thrtab(cc):
    lo_b = _bucket_lo()
    t = np.zeros((155,), np.float32)
    for r5 in range(5):
        r = r5 - 1
        for b in range(31):
            t[r5 * 31 + b] = lo_b[b] - 128 * (cc - r)
    return np.broadcast_to(t[None, :], (128, 155)).copy()


def _rcnt(cc):
    r = np.zeros((128, 4, 128), np.float32)
    t = np.arange(128)
    for g, w in enumerate((2, 4, 8, 16)):
        cnt = np.minimum(w, t + 1) if cc == 0 else np.full(128, w)
        r[:, g, :] = (1.0 / cnt.astype(np.float32))[None, :]
    return r


def _cmask(cc):
    m = np.zeros((128, 512), np.float32)
    q = np.arange(128)[:, None]
    s = np.arange(128)[None, :]
    for r in range(4):
        if r > cc:
            m[:, r * 128:(r + 1) * 128] = -1e30
        elif r == cc:
            m[:, r * 128:(r + 1) * 128] = np.where(s > q, -1e30, 0.0)
    return m


def kernel(**inputs):
    cfg = {}
    nc = build(cfg)
    cores = list(range(8))
    maps = _prep_inputs(inputs, cfg, cores)
    res = run_bass_kernel_spmd(nc, maps, core_ids=cores)
    rs = res.results
    B, S = 2, 8192
    T = S + 16
    y_prompt = np.zeros((B, S, D), np.float32)
    k_p = np.zeros((1, B, T, 8, 64), np.float32)
    v_p = np.zeros((1, B, T, 8, 64), np.float32)
    i_p = np.zeros((1, B, T, 64), np.float32)
    pool_p = np.zeros((1, B, 15, 512), np.float32)
    for c in cores:
        b, cc = c // 4, c % 4
        r = rs[c]
        for i in range(NQ):
            j = 4 * i + cc
            p0 = j * 128
            if p0 >= T:
                continue
            p1 = min(p0 + 128, T)
            n = p1 - p0
            k_p[0, b, p0:p1] = r["k_own"][i][:n].reshape(n, 8, 64)
            v_p[0, b, p0:p1] = r["v_own"][i][:n].reshape(n, 8, 64)
            i_p[0, b, p0:p1] = r["ki_own"][i][:n]
            lo = max(p0, 16)
            y_prompt[b, lo - 16:p1 - 16] = r["y_own"][i][lo - p0:n]
        if cc == 0:
            ul = r["u_last"]
            rows = ul[:, :, 17:32]
            pool_p[0, b] = np.transpose(rows, (2, 1, 0)).reshape(15, 512)
    y_s = np.zeros((128, 1, D), np.float32)
    k_s = np.zeros((1, 128, 1, 8, 64), np.float32)
    v_s = np.zeros((1, 128, 1, 8, 64), np.float32)
    i_s = np.zeros((1, 128, 1, 64), np.float32)
    pool_s = np.zeros((1, 128, 15, 512), np.float32)
    for c in cores:
        r = rs[c]
        sl = slice(16 * c, 16 * c + 16)
        y_s[sl, 0] = r["y_s"][:16]
        k_s[0, sl, 0] = r["ks_o"][:16].reshape(16, 8, 64)
        v_s[0, sl, 0] = r["vs_o"][:16].reshape(16, 8, 64)
        i_s[0, sl, 0] = r["kis_o"][:16]
        pool_s[0, sl] = r["pool_s"]
    return (y_prompt, y_s, k_p, v_p, i_p, pool_p, k_s, v_s, i_s, pool_s)
```

```python
import numpy as np
from contextlib import ExitStack
import concourse.bass as bass
import concourse.mybir as mybir
from concourse.bass_utils import run_bass_kernel_spmd

F32 = mybir.dt.float32
BF16 = mybir.dt.bfloat16
I32 = mybir.dt.int32
U32 = mybir.dt.uint32
ALU = mybir.AluOpType
AF = mybir.ActivationFunctionType
AX = mybir.AxisListType

D = 1024
NBLK_A = 68
NQ = 17
EPS = 1e-6
IN_W = 4680
CH_Q, CH_K, CH_V, CH_QI, CH_KW, CH_U, CH_GA0, CH_GA1, CH_GB0, CH_GB1 = range(10)
CHUNKS = [(0, 512), (512, 512), (1024, 512), (1536, 512), (2048, 72), (2120, 512),
          (2632, 512), (3144, 512), (3656, 512), (4168, 512)]
N_DMA_SEMS = 12
NIT = 16


class Buf:
    def __init__(self, t, name):
        self.t = t
        self.name = name
        self.w = None
        self.r = {}

    def __getitem__(self, idx):
        return self.t[idx]


class KB:
    def __init__(self, nc, es, plan=None):
        self.nc = nc
        self.es = es
        self.plan = plan
        self.targets = {e: set() for e in ("pe", "dve", "act", "pool", "sp")}
        self.rank = None
        if plan is not None:
            self.rank = {e: {idx: r + 1 for r, idx in enumerate(sorted(plan[e]))} for e in plan}
        self.eng = {"pe": nc.tensor, "dve": nc.vector, "act": nc.scalar, "pool": nc.gpsimd, "sp": nc.sync}
        self.sem = {e: es.enter_context(nc.semaphore("s_" + e)) for e in self.eng}
        self.cnt = {e: 0 for e in self.eng}
        self.seen = {e: {} for e in self.eng}
        self.dsem = [es.enter_context(nc.semaphore("d_%d" % i)) for i in range(N_DMA_SEMS + 6)]
        self.dcnt = [0] * (N_DMA_SEMS + 6)
        self.drr = 0
        self.nbuf = 0

    def sb(self, shape, dt, name=None):
        self.nbuf += 1
        name = "sb_" + (name or ("%d" % self.nbuf))
        return Buf(self.es.enter_context(self.nc.sbuf_tensor(name, list(shape), dt)), name)

    def ps(self, shape, dt, name=None):
        self.nbuf += 1
        name = name or ("ps%d" % self.nbuf)
        return Buf(self.es.enter_context(self.nc.psum_tensor(name, list(shape), dt)), name)

    def dram(self, name, shape, dt, kind="Internal"):
        return Buf(self.nc.dram_tensor(name, list(shape), dt, kind=kind).ap(), name)

    def _deps(self, e, reads, writes):
        need = {}

        def add(tok):
            if tok is None:
                return
            key, sem, val = tok
            if key == "pe" and e == "pe":
                return
            if need.get(key, (None, 0))[1] < val:
                need[key] = (sem, val)

        for b in reads:
            add(b.w)
        for b in writes:
            add(b.w)
            for tok in b.r.values():
                add(tok)
        eo = self.eng[e]
        for key, (sem, val) in need.items():
            if self.seen[e].get(key, 0) < val:
                self.seen[e][key] = val
                if key in self.targets:
                    if self.plan is None:
                        self.targets[key].add(val)
                    else:
                        eo.wait_ge(sem, self.rank[key][val])
                elif self.plan is not None:
                    eo.wait_ge(sem, val)

    def _record(self, tok, reads, writes):
        for b in reads:
            if b.r.get(tok[0], (None, None, 0))[2] < tok[2]:
                b.r[tok[0]] = tok
        for b in writes:
            b.w = tok
            b.r = {}

    def op(self, e, fn, reads=(), writes=()):
        self._deps(e, reads, writes)
        self.cnt[e] += 1
        if self.plan is not None:
            ins = fn(self.eng[e])
            if self.cnt[e] in self.plan[e]:
                ins.then_inc(self.sem[e], 1)
        self._record((e, self.sem[e], self.cnt[e]), reads, writes)

    def dma(self, q, out, in_, reads=(), writes=(), indirect=None, own_sem=None):
        if own_sem is None:
            i = self.drr
            self.drr = (i + 1) % N_DMA_SEMS
        else:
            i = N_DMA_SEMS + own_sem
        eo = self.eng[q]
        key = "d%d" % i
        if own_sem is None and self.dcnt[i] > 0 and self.seen[q].get(key, 0) < 16 * self.dcnt[i]:
            if self.plan is not None:
                eo.wait_ge(self.dsem[i], 16 * self.dcnt[i])
            self.seen[q][key] = 16 * self.dcnt[i]
        self._deps(q, reads, writes)
        self.dcnt[i] += 1
        if self.plan is not None:
            if indirect is None:
                ins = eo.dma_start(out=out, in_=in_)
            else:
                ins = eo.indirect_dma_start(out=out, out_offset=None, in_=in_, in_offset=indirect)
            ins.then_inc(self.dsem[i], 16)
        self._record((key, self.dsem[i], 16 * self.dcnt[i]), reads, writes)

    def finish(self):
        eo = self.eng["sp"]
        if self.plan is None:
            for e in ("pe", "dve", "act", "pool"):
                if self.cnt[e] > 0:
                    self.targets[e].add(self.cnt[e])
            return
        for i in range(N_DMA_SEMS + 6):
            if self.dcnt[i] > 0:
                eo.wait_ge(self.dsem[i], 16 * self.dcnt[i])
        for e in ("pe", "dve", "act", "pool"):
            if self.cnt[e] > 0:
                eo.wait_ge(self.sem[e], self.rank[e][self.cnt[e]])


def build(cfg):
    _, kb_dry = _build(cfg, None)
    nc, _ = _build(cfg, kb_dry.targets)
    return nc


def _build(cfg, plan):
    nblk_a = cfg.get("nblk_a", NBLK_A)
    nq = cfg.get("nq", NQ)
    do_attn = cfg.get("attn", True)
    do_tail = cfg.get("tail", True)
    nkeys = nblk_a * 128
    nc = bass.Bass("TRN2", target_bir_lowering=False)
    es = ExitStack()
    kb = KB(nc, es, plan)
    nc._es_keep = es

    def din(name, shape, dt=F32):
        return Buf(nc.dram_tensor(name, list(shape), dt, kind="ExternalInput").ap(), name)

    def dout(name, shape, dt=F32):
        return Buf(nc.dram_tensor(name, list(shape), dt, kind="ExternalOutput").ap(), name)

    xcat = din("xcat", [nkeys, D])
    xown = din("xown", [nq, 128, D])
    xprev = din("xprev", [nq, 16, D])
    w_in = din("w_in", [D, IN_W])
    norm1_g = din("norm1_g", [D])
    q_norm_g = din("q_norm_g", [64])
    k_norm_g = din("k_norm_g", [64])
    ident_in = din("ident", [128, 128])
    rel_bias = din("rel_bias", [32, 8])
    qs_in = din("qs", [128, 128])
    thrtab_in = din("thrtab", [128, 155])
    cmask_in = din("cmask", [128, 512])
    pw_in = din("pw", [128, NIT])
    w_ba = din("w_ba", [512, D])
    w_bp = din("w_bp", [512, D])
    w_out = din("w_out", [D, D])
    peer_wq = din("peer_wq", [D, D])
    w_pool = din("w_pool", [4, 128, 128])
    pool_scale = din("pool_scale", [512])
    norm2_g = din("norm2_g", [D])
    subkeys = din("subkeys", [2, 128, 64])
    peer_u = din("peer_u", [16384, D])
    peer_v = din("peer_v", [16384, D])
    rcnt_in = din("rcnt", [128, 4, 128])
    iota16_in = din("iota16", [128, 16])
    k_own = dout("k_own", [nq, 128, 512])
    v_own = dout("v_own", [nq, 128, 512])
    ki_own = dout("ki_own", [nq, 128, 64])
    y_own = dout("y_own", [nq, 128, D])
    u_last = dout("u_last", [128, 4, 144])
    y_s_out = dout("y_s", [128, D])
    do_sample = cfg.get("sample", True)
    if do_sample:
        xs_in = din("xs_own", [128, D])
        cache_k = din("cache_k", [2560 * 128, 512])
        cache_v = din("cache_v", [2560 * 128, 512])
        cache_ik = din("cache_ik", [2560, 8192])
        state_in = din("state_own", [16, 15, 512])
        pt_in = din("pt_own", [16, 16], I32)
        zsel_in = din("zsel", [128, 31])
        k_s_out = dout("ks_o", [128, 512])
        v_s_out = dout("vs_o", [128, 512])
        ki_s_out = dout("kis_o", [128, 64])
        pool_s_out = dout("pool_s", [16, 15, 512])
        qi_d = kb.dram("qi_d", [16, 512], F32)
        wi_d = kb.dram("wi_d", [16, 8], F32)
        q_d = kb.dram("q_d", [16, 512], F32)
        sc_d = kb.dram("sc_d", [16, 2048], F32)
    wba_bf = kb.dram("wba_bf", [128, 4, D], BF16)
    wbp_bf = kb.dram("wbp_bf", [128, 4, D], BF16)
    wout_bf = kb.dram("wout_bf", [128, 8, D], BF16)
    pwq_bf = kb.dram("pwq_bf", [128, 8, D], BF16)
    pu_bf = kb.dram("pu_bf", [16384, D], BF16)
    pv_bf = kb.dram("pv_bf", [16384, D], BF16)
    win_bf = kb.dram("win_bf", [128, 8, IN_W], BF16)
    kt_s = kb.dram("kt_s", [128, 4, nkeys], BF16)
    v_s = kb.dram("v_s", [nkeys, 8, 65], BF16)
    kit_s = kb.dram("kit_s", [128, nkeys], BF16)

    ident_f = kb.sb([128, 128], F32, "ident_f")
    ident_b = kb.sb([128, 128], BF16, "ident_b")
    g1col = kb.sb([128, 8], F32, "g1col")
    qg = kb.sb([128, 64], F32, "qg")
    kg = kb.sb([128, 64], F32, "kg")
    kb.dma("sp", ident_f[:], ident_in[:, :], writes=[ident_f])
    kb.op("dve", lambda e: e.tensor_copy(out=ident_b[:], in_=ident_f[:]), reads=[ident_f], writes=[ident_b])
    ident4 = kb.sb([128, 512], BF16, "ident4")
    for j4 in range(4):
        kb.op("dve", lambda e, j4=j4: e.tensor_copy(out=ident4[:, j4 * 128:(j4 + 1) * 128], in_=ident_f[:]),
              reads=[ident_f, ident4], writes=[ident4])
    with nc.allow_non_contiguous_dma(reason="tiny param loads"):
        kb.dma("sp", g1col[:], norm1_g.t.rearrange("(k p) -> p k", p=128), writes=[g1col])
        kb.dma("sp", qg[:], q_norm_g.t.partition_broadcast(128), writes=[qg])
        kb.dma("sp", kg[:], k_norm_g.t.partition_broadcast(128), writes=[kg])
    kb.op("dve", lambda e: e.tensor_scalar(out=qg[:], in0=qg[:], scalar1=0.125, scalar2=None, op0=ALU.mult),
          reads=[qg], writes=[qg])

    pb = [kb.ps([128, 512], F32, "bank%d" % i) for i in range(8)]

    ktc = [kb.sb([128, 4, 512], BF16, "ktc%d" % j) for j in range(2)]
    vch = [kb.sb([128, 4, 520], BF16, "vch%d" % j) for j in range(2)]
    Isc = kb.sb([128, max(nkeys, 8704)], F32, "Isc")

    class View:
        def __init__(self, owner, ap):
            self.owner = owner
            self.ap = ap

        def __getitem__(self, idx):
            return self.ap[idx]

        @property
        def w(self):
            return self.owner.w

        @w.setter
        def w(self, v):
            self.owner.w = v

        @property
        def r(self):
            return self.owner.r

        @r.setter
        def r(self, v):
            self.owner.r = v

    wst_v = [View(ktc[j], ktc[j].t[:].rearrange("p a b -> p (a b)").bitcast(F32)[:, 0:1024]) for j in range(2)]
    wsb_v = [View(vch[j], vch[j].t[:].rearrange("p a b -> p (a b)")[:, 0:1024]) for j in range(2)]
    pcount = [0]

    def conv_w(src_ap, dst_ap, ncol, scal):
        s = pcount[0] % 2
        pcount[0] += 1
        kb.dma("sp", wst_v[s][:, 0:ncol], src_ap, writes=[wst_v[s].owner])
        kb.op("dve", lambda e: e.tensor_scalar(out=wsb_v[s][:, 0:ncol], in0=wst_v[s][:, 0:ncol], scalar1=scal,
                                               scalar2=None, op0=ALU.mult),
              reads=[wst_v[s].owner, g1col], writes=[wsb_v[s].owner])
        kb.dma("sp", dst_ap, wsb_v[s][:, 0:ncol], reads=[wsb_v[s].owner], writes=[win_bf])

    for kc in range(8):
        for pc in range(5):
            conv_w(w_in[kc * 128:(kc + 1) * 128, pc * 936:(pc + 1) * 936], win_bf[:, kc, pc * 936:(pc + 1) * 936],
                   936, g1col[:, kc:kc + 1])
    if do_tail:
        for (wsrc, wdst, nkc) in ((w_ba, wba_bf, 4), (w_bp, wbp_bf, 4), (w_out, wout_bf, 8), (peer_wq, pwq_bf, 8)):
            for kc in range(nkc):
                conv_w(wsrc[kc * 128:(kc + 1) * 128, :], wdst[:, kc, :], 1024, 1.0)

    wslot = [kb.sb([128, 8, 512], BF16, "wslot%d" % i) for i in range(2)]
    wstate = {"i": 0}

    def load_w(src, c0, cw, nk=8):
        s = wslot[wstate["i"] % 2]
        wstate["i"] += 1
        kb.dma("sp", s[:, 0:nk, 0:cw], src[:, 0:nk, c0:c0 + cw], reads=[src], writes=[s])
        return s

    xt = [kb.sb([128, D], F32, "xt%d" % i) for i in range(2)]
    xn = kb.sb([128, D], F32, "xn")
    junk = kb.sb([128, D], BF16, "junk")
    ssq = kb.sb([128, 1], F32, "ssq")
    rstd = kb.sb([128, 1], F32, "rstd")
    xs = kb.sb([128, D], BF16, "xs")
    hnT = kb.sb([128, 8, 128], BF16, "hnT")
    tp_bank = pb[2]

    def norm_transpose(xb, dstT, ntok=128, gtile=None, keep=None, xowner=None):
        xo = xowner or xb
        kb.op("act", lambda e: e.activation(out=junk[0:ntok, :], in_=xb[0:ntok, :], func=AF.Square,
                                            accum_out=ssq[0:ntok, :]),
              reads=[xo], writes=[junk, ssq])
        kb.op("act", lambda e: e.activation(out=rstd[0:ntok, :], in_=ssq[0:ntok, :], func=AF.Sqrt,
                                            scale=1.0 / D, bias=EPS),
              reads=[ssq], writes=[rstd])
        kb.op("dve", lambda e: e.reciprocal(out=rstd[0:ntok, :], in_=rstd[0:ntok, :]), reads=[rstd], writes=[rstd])
        if gtile is None:
            kb.op("dve", lambda e: e.tensor_scalar(out=xs[0:ntok, :], in0=xb[0:ntok, :], scalar1=rstd[0:ntok, :],
                                                   scalar2=None, op0=ALU.mult),
                  reads=[xo, rstd], writes=[xs])
        else:
            kb.op("dve", lambda e: e.scalar_tensor_tensor(out=keep[0:ntok, :], in0=xb[0:ntok, :],
                                                          scalar=rstd[0:ntok, :], in1=gtile[0:ntok, :],
                                                          op0=ALU.mult, op1=ALU.mult),
                  reads=[xo, rstd, gtile], writes=[keep])
            kb.op("dve", lambda e: e.tensor_copy(out=xs[0:ntok, :], in_=keep[0:ntok, :]), reads=[keep], writes=[xs])
        tpv = tp_bank.t[:].bitcast(BF16)
        for kc in range(8):
            kb.op("pe", lambda e, kc=kc: e.transpose(out=tpv[:, kc * 128:kc * 128 + ntok],
                                                     in_=xs[0:ntok, kc * 128:(kc + 1) * 128],
                                                     identity=ident_b[0:ntok, 0:ntok]),
                  reads=[xs, ident_b], writes=[tp_bank])
        kb.op("dve", lambda e: e.tensor_copy(
            out=dstT[:, :, 0:ntok], in_=tpv.rearrange("p (k t) -> p k t", k=8)[:, :, 0:ntok]),
            reads=[tp_bank], writes=[dstT])

    def proj_tok(dst_bank, wsl, cw, srcT=None, ntok=128):
        srcT = srcT or hnT
        for kc in range(8):
            kb.op("pe", lambda e, kc=kc: e.matmul(dst_bank[0:ntok, 0:cw], lhsT=srcT[:, kc, 0:ntok],
                                                  rhs=wsl[:, kc, 0:cw], start=(kc == 0), stop=(kc == 7)),
                  reads=[srcT, wsl], writes=[dst_bank])

    hsq = kb.sb([128, 512], F32, "hsq")
    hss = kb.sb([128, 8], F32, "hss")
    hrs = kb.sb([128, 8], F32, "hrs")

    def head_norm(src_bank, gain, dst):
        kb.op("act", lambda e: e.activation(out=hsq[:], in_=src_bank[:, 0:512], func=AF.Square),
              reads=[src_bank], writes=[hsq])
        kb.op("dve", lambda e: e.tensor_reduce(out=hss[:], in_=hsq[:].rearrange("p (h d) -> p h d", h=8),
                                               axis=AX.X, op=ALU.add),
              reads=[hsq], writes=[hss])
        kb.op("act", lambda e: e.activation(out=hrs[:], in_=hss[:], func=AF.Sqrt, scale=1.0 / 64, bias=EPS),
              reads=[hss], writes=[hrs])
        kb.op("dve", lambda e: e.reciprocal(out=hrs[:], in_=hrs[:]), reads=[hrs], writes=[hrs])
        kb.op("dve", lambda e: e.tensor_tensor(out=hsq[:].rearrange("p (h d) -> p h d", h=8),
                                               in0=src_bank[:, 0:512].rearrange("p (h d) -> p h d", h=8),
                                               in1=hrs[:].unsqueeze(2).to_broadcast([128, 8, 64]), op=ALU.mult),
              reads=[src_bank, hrs], writes=[hsq])
        kb.op("dve", lambda e: e.tensor_tensor(out=dst[:].rearrange("p (h d) -> p h d", h=8),
                                               in0=hsq[:].rearrange("p (h d) -> p h d", h=8),
                                               in1=gain[:].unsqueeze(1).to_broadcast([128, 8, 64]), op=ALU.mult),
              reads=[hsq, gain], writes=[dst])

    wk = wslot[0]
    wv = wslot[1]
    wkw = kb.sb([128, 8, 72], BF16, "wkw")
    kb.dma("sp", wk[:], win_bf[:, :, 512:1024], reads=[win_bf], writes=[wk])
    kb.dma("sp", wv[:], win_bf[:, :, 1024:1536], reads=[win_bf], writes=[wv])
    kb.dma("sp", wkw[:], win_bf[:, :, 2048:2120], reads=[win_bf], writes=[wkw])
    kf = kb.sb([128, 512], F32, "kf")
    kbf = kb.sb([128, 512], BF16, "kbf")
    ktb = kb.sb([128, 4, 128], BF16, "ktb")
    vb = kb.sb([128, 8, 65], BF16, "vb")
    kib = kb.sb([128, 128], BF16, "kib")
    kitb = kb.sb([128, 128], BF16, "kitb")
    kb.op("dve", lambda e: e.memset(vb[:], 1.0), writes=[vb])
    def table_conv_steps():
        it = 0
        for (tsrc, tdst) in ((peer_u, pu_bf), (peer_v, pv_bf)):
            for c in range(32):
                half = it % 2
                it += 1
                fst = Isc.t[:, half * 4096:(half + 1) * 4096]
                bst = (ktc if half == 0 else vch)
                kb.dma("sp", fst, tsrc[c * 512:(c + 1) * 512, :].rearrange("(p r) d -> p (r d)", p=128),
                       writes=[Isc])
                for j2 in range(2):
                    bv = bst[j2].t[:].rearrange("p a b -> p (a b)")[:, 0:2048]
                    kb.op("act", lambda e, bv=bv, fst=fst, j2=j2: e.copy(out=bv, in_=fst[:, j2 * 2048:(j2 + 1) * 2048]),
                          reads=[Isc], writes=[bst[j2]])
                    kb.dma("sp", tdst[c * 512:(c + 1) * 512, :].rearrange("(p r) d -> p r d", p=128)[:, 2 * j2:2 * j2 + 2, :],
                           bv.rearrange("p (r d) -> p r d", r=2), reads=[bst[j2]], writes=[tdst])
                yield

    tconv = table_conv_steps() if (do_tail and do_attn) else iter(())
    for blk in range(nblk_a):
        next(tconv, None)
        xb = xt[blk % 2]
        kb.dma("sp", xb[:], xcat[blk * 128:(blk + 1) * 128, :], writes=[xb])
        norm_transpose(xb, hnT)
        proj_tok(pb[0], wk, 512)
        head_norm(pb[0], kg, kf)
        kb.op("dve", lambda e: e.tensor_copy(out=kbf[:], in_=kf[:]), reads=[kf], writes=[kbf])
        tpv = tp_bank.t[:].bitcast(BF16)
        for pr in range(4):
            kb.op("pe", lambda e, pr=pr: e.transpose(out=tpv[:, pr * 128:(pr + 1) * 128],
                                                     in_=kbf[:, pr * 128:(pr + 1) * 128], identity=ident_b[:]),
                  reads=[kbf, ident_b], writes=[tp_bank])
        kb.op("act", lambda e: e.copy(out=ktb[:], in_=tpv[:, 0:512].rearrange("p (a t) -> p a t", a=4)),
              reads=[tp_bank], writes=[ktb])
        kb.dma("sp", kt_s[:, :, blk * 128:(blk + 1) * 128], ktb[:], reads=[ktb], writes=[kt_s])
        proj_tok(pb[1], wv, 512)
        kb.op("act", lambda e: e.copy(out=vb[:, :, 0:64], in_=pb[1][:, 0:512].rearrange("p (h d) -> p h d", h=8)),
              reads=[pb[1]], writes=[vb])
        kb.dma("sp", v_s[blk * 128:(blk + 1) * 128, :, :], vb[:], reads=[vb], writes=[v_s])
        proj_tok(pb[0], wkw, 72)
        kb.op("dve", lambda e: e.tensor_copy(out=kib[:, 0:64], in_=pb[0][:, 0:64]), reads=[pb[0]], writes=[kib])
        kb.op("dve", lambda e: e.tensor_copy(out=kib[:, 64:128], in_=pb[0][:, 0:64]), reads=[pb[0]], writes=[kib])
        kb.op("pe", lambda e: e.transpose(out=tpv[:, 512:640], in_=kib[:], identity=ident_b[:]),
              reads=[kib, ident_b], writes=[tp_bank])
        kb.op("act", lambda e: e.copy(out=kitb[:], in_=tpv[:, 512:640]), reads=[tp_bank], writes=[kitb])
        kb.dma("sp", kit_s[:, blk * 128:(blk + 1) * 128], kitb[:], reads=[kitb], writes=[kit_s])

    for _ in tconv:
        pass
    ko = kb.sb([128, 512], F32, "ko")
    vo = kb.sb([128, 512], F32, "vo")
    kio = kb.sb([128, 64], F32, "kio")
    wis = kb.sb([128, 8], F32, "wis")
    dbg_ao = dout("dbg_ao", [nq, 128, 512]) if cfg.get("dbg") else None
    if do_attn:
        qf = kb.sb([128, 512], F32, "qf")
        qbf = kb.sb([128, 512], BF16, "qbf")
        qT2 = kb.sb([128, 4, 128], BF16, "qT2")
        qiT2 = kb.sb([128, 4, 128], BF16, "qiT2")
        kitc = [kb.sb([128, 512], BF16, "kitc%d" % j) for j in range(2)]
        rl = [kb.sb([128, 512], BF16, "rl%d" % j) for j in range(2)]
        junkI = kb.sb([128, 2176], BF16, "junkI")
        cnt4 = kb.sb([128, 4], F32, "cnt4")
        hi0 = kb.sb([128, 1], F32, "hi0")
        lo = kb.sb([128, 1], F32, "lo")
        mid = kb.sb([128, 1], F32, "mid")
        cntt = kb.sb([128, 1], F32, "cntt")
        dl = kb.sb([128, 1], F32, "dl")
        wh = kb.sb([128, NIT], F32, "wh")
        pw = kb.sb([128, NIT], F32, "pw")
        cmask = kb.sb([128, 512], F32, "cmask")
        negm = [kb.sb([128, 128], BF16, "negm%d" % j) for j in range(2)]
        addh = kb.sb([128, 8, 128], BF16, "addh")
        PT = [kb.sb([128, 8, 128], BF16, "PT%d" % j) for j in range(2)]
        rden = kb.sb([128, 8], F32, "rden")
        ao = kb.sb([128, 512], BF16, "ao")
        aof = kb.sb([128, 512], F32, "aof") if cfg.get("dbg") else None
        kb.dma("sp", pw[:], pw_in[:, :], writes=[pw])
        kb.dma("sp", cmask[:], cmask_in[:, :], writes=[cmask])
        qs = kb.sb([128, 128], F32, "qs")
        thrtab = kb.sb([128, 155], F32, "thrtab")
        rbb = kb.sb([128, 256], F32, "rbb")
        ndel = kb.sb([128, 256], F32, "ndel")
        ind = kb.sb([128, 128], F32, "ind")
        NB = [kb.sb([128, 8, 128], BF16, "NB%d" % j) for j in range(5)]
        NBt = kb.sb([128, 8, 128], F32, "h2")
        h2 = View(NBt, NBt.t[:].rearrange("p a b -> p (a b)"))
        kb.dma("sp", qs[:], qs_in[:, :], writes=[qs])
        kb.dma("sp", thrtab[:], thrtab_in[:, :], writes=[thrtab])
        with nc.allow_non_contiguous_dma(reason="tiny param loads"):
            kb.dma("sp", rbb[:], rel_bias.t.rearrange("b h -> (b h)").partition_broadcast(128), writes=[rbb])
        kb.op("dve", lambda e: e.tensor_tensor(out=ndel[:, 8:256], in0=rbb[:, 0:248], in1=rbb[:, 8:256],
                                               op=ALU.subtract), reads=[rbb], writes=[ndel])
        for r5 in range(5):
            kb.op("dve", lambda e: e.memset(NBt[:], 0.0), writes=[NBt])
            for b in range(1, 32):
                col = r5 * 31 + (b - 1)
                kb.op("dve", lambda e, col=col: e.tensor_scalar(out=ind[:], in0=qs[:], scalar1=thrtab[:, col:col + 1],
                                                                scalar2=None, op0=ALU.is_lt),
                      reads=[qs, thrtab], writes=[ind])
                for h in range(8):
                    kb.op("dve", lambda e, b=b, h=h: e.scalar_tensor_tensor(
                        out=NBt[:, h, :], in0=ind[:], scalar=ndel[:, b * 8 + h:b * 8 + h + 1], in1=NBt[:, h, :],
                        op0=ALU.mult, op1=ALU.add), reads=[ind, ndel, NBt], writes=[NBt])
            kb.op("dve", lambda e, r5=r5: e.tensor_copy(out=NB[r5][:], in_=NBt[:]), reads=[NBt], writes=[NB[r5]])
    if do_tail:
        aoT = kb.sb([128, 4, 128], BF16, "aoT")
        hnTp = kb.sb([128, 8, 16], BF16, "hnTp")
        xpv = kb.sb([16, D], F32, "xpv")
        uT = kb.sb([128, 4, 144], F32, "uT")
        s1 = kb.sb([128, 4, 144], F32, "s1")
        s2 = kb.sb([128, 4, 144], F32, "s2")
        s3 = kb.sb([128, 4, 144], F32, "s3")
        pmf = kb.sb([128, 4, 128], F32, "pmf")
        pmT = kb.sb([128, 4, 128], BF16, "pmT")
        poT = kb.sb([128, 4, 128], BF16, "poT")
        sga = kb.sb([128, 8, 128], BF16, "sga")
        sgb = kb.sb([128, 8, 128], BF16, "sgb")
        tmpA = kb.sb([128, 512], F32, "tmpA")
        tmpB = kb.sb([128, 512], F32, "tmpB")
        mT = kb.sb([128, 8, 128], BF16, "mT")
        xnT = sgb
        qpT = sga
        g2b = kb.sb([128, D], F32, "g2b")
        wpl = kb.sb([128, 4, 128], BF16, "wpl")
        wplf = kb.sb([128, 4, 128], F32, "wplf")
        pscol = kb.sb([128, 4], F32, "pscol")
        SKf = kb.sb([128, 256], F32, "SKf")
        sktmp = kb.sb([128, 128], F32, "sktmp")
        SK = kb.sb([128, 256], BF16, "SK")
        rcnt = kb.sb([128, 4, 128], F32, "rcnt")
        iota16 = kb.sb([128, 16], F32, "iota16")
        tv = kb.sb([128, 8, 2, 16], F32, "tv")
        ti = kb.sb([128, 8, 2, 16], U32, "ti")
        tif = kb.sb([128, 8, 2, 16], F32, "tif")
        tsv = kb.sb([128, 8, 16], F32, "tsv")
        tpos = kb.sb([128, 8, 16], U32, "tpos")
        pa = kb.sb([128, 8, 16], U32, "pa")
        pbb = kb.sb([128, 8, 16], U32, "pbb")
        paf = kb.sb([128, 8, 16], F32, "paf")
        pbf = kb.sb([128, 8, 16], F32, "pbf")
        i1s = kb.sb([128, 8, 16], F32, "i1s")
        i2s = kb.sb([128, 8, 16], F32, "i2s")
        eidf = kb.sb([128, 128], F32, "eidf")
        eidi = kb.sb([128, 128], I32, "eidi")
        gmx = kb.sb([128, 8], F32, "gmx")
        gk = kb.sb([128, 8, 16], F32, "gk")
        actv = kb.sb([128, 128], F32, "actv")
        wgt = kb.sb([128, 128], F32, "wgt")
        m8 = kb.sb([128, 8], F32, "m8")
        UbS = [View(o, o.t[:].rearrange("p a b -> p (a b)").bitcast(F32)[:, 0:1024]) for o in (ktc[0], ktc[1], vch[0], vch[1])]
        Ubo = [kb.sb([128, D], BF16, "Ub%d" % j) for j in range(6)]
        Ub = [View(o, o.t[:]) for o in Ubo]
        kb.dma("sp", rcnt[:], rcnt_in[:, :, :], writes=[rcnt])
        kb.dma("sp", iota16[:], iota16_in[:, :], writes=[iota16])
        kb.op("dve", lambda e: e.memset(SKf[:], 0.0), writes=[SKf])
        with nc.allow_non_contiguous_dma(reason="small param loads"):
            kb.dma("sp", g2b[:], norm2_g.t.partition_broadcast(128), writes=[g2b])
            kb.dma("sp", pscol[:], pool_scale.t.rearrange("(g d) -> d g", d=128), writes=[pscol])
            kb.dma("sp", wplf[:], w_pool.t.rearrange("g c d -> c g d"), writes=[wplf])
            kb.dma("sp", sktmp[:, 0:64], subkeys.t[0], writes=[sktmp])
            kb.dma("sp", sktmp[:, 64:128], subkeys.t[1], writes=[sktmp])
        kb.op("pe", lambda e: e.transpose(out=pb[2][:, 0:128], in_=sktmp[:], identity=ident_f[:]),
              reads=[sktmp, ident_f], writes=[pb[2]])
        kb.op("dve", lambda e: e.tensor_copy(out=SKf[0:64, 0:128], in_=pb[2][0:64, 0:128]), reads=[pb[2], SKf], writes=[SKf])
        kb.op("dve", lambda e: e.tensor_copy(out=SKf[64:128, 128:256], in_=pb[2][64:128, 0:128]), reads=[pb[2], SKf],
              writes=[SKf])
        kb.op("dve", lambda e: e.tensor_copy(out=SK[:], in_=SKf[:]), reads=[SKf], writes=[SK])
        kb.op("dve", lambda e: e.tensor_copy(out=wpl[:], in_=wplf[:]), reads=[wplf], writes=[wpl])
        IscV = Isc.t[:]
        ssc = IscV[:, 0:2048]
        sscw = IscV[:, 2048:4096]
        cand = IscV[:, 4096:6144]
        candw = IscV[:, 6144:8192]
        ohv = IscV[:, 0:2048]
        ohv2 = IscV[:, 2048:4096]

    def tail_block(i, xb, sample=False):
        tpv = tp_bank.t[:].bitcast(BF16)
        for pr in range(4):
            kb.op("pe", lambda e, pr=pr: e.transpose(out=tpv[:, pr * 128:(pr + 1) * 128],
                                                     in_=ao[:, pr * 128:(pr + 1) * 128], identity=ident_b[:]),
                  reads=[ao, ident_b], writes=[tp_bank])
        kb.op("act", lambda e: e.copy(out=aoT[:], in_=tpv[:, 0:512].rearrange("p (a t) -> p a t", a=4)),
              reads=[tp_bank], writes=[aoT])
        if not sample:
            kb.dma("sp", xpv[:], xprev[i, :, :], writes=[xpv])
            norm_transpose(xpv, hnTp, ntok=16)
            wsl = load_w(win_bf, *CHUNKS[CH_U])
            for g in range(4):
                bk = pb[g // 2]
                c0 = (g % 2) * 144
                for kc in range(8):
                    kb.op("pe", lambda e, bk=bk, c0=c0, g=g, kc=kc, wsl=wsl: e.matmul(
                        bk[:, c0:c0 + 16], lhsT=wsl[:, kc, g * 128:(g + 1) * 128], rhs=hnTp[:, kc, :],
                        start=(kc == 0), stop=(kc == 7)), reads=[wsl, hnTp], writes=[bk])
                for kc in range(8):
                    kb.op("pe", lambda e, bk=bk, c0=c0, g=g, kc=kc, wsl=wsl: e.matmul(
                        bk[:, c0 + 16:c0 + 144], lhsT=wsl[:, kc, g * 128:(g + 1) * 128], rhs=hnT[:, kc, :],
                        start=(kc == 0), stop=(kc == 7)), reads=[wsl, hnT], writes=[bk])
            for hf in range(2):
                kb.op("act", lambda e, hf=hf: e.copy(out=uT[:, 2 * hf:2 * hf + 2, :],
                                                     in_=pb[hf][:, 0:288].rearrange("p (g t) -> p g t", g=2)),
                      reads=[pb[hf]], writes=[uT])
            if i == nq - 1:
                kb.dma("sp", u_last[:, :, :], uT[:], reads=[uT], writes=[u_last])
            kb.op("dve", lambda e: e.tensor_tensor(out=s1[:, :, 1:144], in0=uT[:, :, 1:144], in1=uT[:, :, 0:143],
                                                   op=ALU.add), reads=[uT], writes=[s1])
            kb.op("dve", lambda e: e.tensor_tensor(out=s2[:, 1:4, 3:144], in0=s1[:, 1:4, 3:144], in1=s1[:, 1:4, 1:142],
                                                   op=ALU.add), reads=[s1], writes=[s2])
            kb.op("dve", lambda e: e.tensor_tensor(out=s3[:, 2:4, 7:144], in0=s2[:, 2:4, 7:144], in1=s2[:, 2:4, 3:140],
                                                   op=ALU.add), reads=[s2], writes=[s3])
            kb.op("dve", lambda e: e.tensor_tensor(out=s1[:, 3, 15:144], in0=s3[:, 3, 15:144], in1=s3[:, 3, 7:136],
                                                   op=ALU.add), reads=[s3, s1], writes=[s1])
            wsum = [s1[:, 0, 16:144], s2[:, 1, 16:144], s3[:, 2, 16:144], s1[:, 3, 16:144]]
            wsrc = [s1, s2, s3, s1]
            for g in range(4):
                if i == 0:
                    kb.op("dve", lambda e, g=g: e.tensor_tensor(out=pmf[:, g, :], in0=wsum[g], in1=rcnt[:, g, :],
                                                                op=ALU.mult), reads=[wsrc[g], rcnt], writes=[pmf])
                    kb.op("dve", lambda e, g=g: e.tensor_tensor(out=pmT[:, g, :], in0=pmf[:, g, :], in1=uT[:, g, 16:144],
                                                                op=ALU.subtract), reads=[pmf, uT], writes=[pmT])
                else:
                    kb.op("dve", lambda e, g=g: e.scalar_tensor_tensor(
                        out=pmT[:, g, :], in0=wsum[g], scalar=1.0 / (2 ** (g + 1)), in1=uT[:, g, 16:144],
                        op0=ALU.mult, op1=ALU.subtract), reads=[wsrc[g], uT], writes=[pmT])
        for g in range(4):
            kb.op("pe", lambda e, g=g: e.matmul(pb[0][:, g * 128:(g + 1) * 128], lhsT=wpl[:, g, :], rhs=pmT[:, g, :],
                                                start=True, stop=True), reads=[wpl, pmT], writes=[pb[0]])
        kb.op("dve", lambda e: e.tensor_tensor(out=poT[:], in0=pb[0][:, 0:512].rearrange("p (g t) -> p g t", g=4),
                                               in1=pscol[:].unsqueeze(2).to_broadcast([128, 4, 128]), op=ALU.mult),
              reads=[pb[0], pscol], writes=[poT])
        for (chs, dst) in (((CH_GA0, CH_GA1), sga), ((CH_GB0, CH_GB1), sgb)):
            for hf, chn in enumerate(chs):
                wsl = load_w(win_bf, *CHUNKS[chn])
                bk = pb[hf]
                for ft in range(4):
                    for kc in range(8):
                        kb.op("pe", lambda e, bk=bk, ft=ft, kc=kc, wsl=wsl: e.matmul(
                            bk[:, ft * 128:(ft + 1) * 128], lhsT=wsl[:, kc, ft * 128:(ft + 1) * 128], rhs=hnT[:, kc, :],
                            start=(kc == 0), stop=(kc == 7)), reads=[wsl, hnT], writes=[bk])
                kb.op("act", lambda e, bk=bk, dst=dst, hf=hf: e.activation(
                    out=dst[:, 4 * hf:4 * hf + 4, :], in_=bk[:, 0:512].rearrange("p (f t) -> p f t", f=4),
                    func=AF.Sigmoid), reads=[bk], writes=[dst])
        for hf in range(2):
            wa = load_w(wba_bf, hf * 512, 512, nk=4)
            wb = load_w(wbp_bf, hf * 512, 512, nk=4)
            for ft in range(4):
                for kc in range(4):
                    kb.op("pe", lambda e, ft=ft, kc=kc, wa=wa: e.matmul(
                        pb[0][:, ft * 128:(ft + 1) * 128], lhsT=wa[:, kc, ft * 128:(ft + 1) * 128], rhs=aoT[:, kc, :],
                        start=(kc == 0), stop=(kc == 3)), reads=[wa, aoT], writes=[pb[0]])
                for kc in range(4):
                    kb.op("pe", lambda e, ft=ft, kc=kc, wb=wb: e.matmul(
                        pb[1][:, ft * 128:(ft + 1) * 128], lhsT=wb[:, kc, ft * 128:(ft + 1) * 128], rhs=poT[:, kc, :],
                        start=(kc == 0), stop=(kc == 3)), reads=[wb, poT], writes=[pb[1]])
            kb.op("dve", lambda e, hf=hf: e.tensor_tensor(
                out=tmpA[:], in0=pb[0][:, 0:512], in1=sga[:, 4 * hf:4 * hf + 4, :].rearrange("p f t -> p (f t)"),
                op=ALU.mult), reads=[pb[0], sga], writes=[tmpA])
            kb.op("dve", lambda e, hf=hf: e.tensor_tensor(
                out=tmpB[:], in0=pb[1][:, 0:512], in1=sgb[:, 4 * hf:4 * hf + 4, :].rearrange("p f t -> p (f t)"),
                op=ALU.mult), reads=[pb[1], sgb], writes=[tmpB])
            kb.op("dve", lambda e, hf=hf: e.tensor_tensor(
                out=mT[:, 4 * hf:4 * hf + 4, :].rearrange("p f t -> p (f t)"), in0=tmpA[:], in1=tmpB[:], op=ALU.add),
                reads=[tmpA, tmpB], writes=[mT])
        for hf in range(2):
            wo = load_w(wout_bf, hf * 512, 512, nk=8)
            for kc in range(8):
                kb.op("pe", lambda e, kc=kc, wo=wo, hf=hf: e.matmul(
                    pb[hf][:, 0:512], lhsT=mT[:, kc, :], rhs=wo[:, kc, :], start=(kc == 0), stop=(kc == 7)),
                    reads=[mT, wo], writes=[pb[hf]])
            kb.op("dve", lambda e, hf=hf: e.tensor_tensor(out=h2[:, hf * 512:(hf + 1) * 512], in0=pb[hf][:, 0:512],
                                                          in1=xb[:, hf * 512:(hf + 1) * 512], op=ALU.add),
                  reads=[pb[hf], xb], writes=[NBt])
        norm_transpose(h2, xnT, gtile=g2b, keep=xn, xowner=NBt)
        for hf in range(2):
            wq_ = load_w(pwq_bf, hf * 512, 512, nk=8)
            for ft in range(4):
                for kc in range(8):
                    kb.op("pe", lambda e, ft=ft, kc=kc, wq_=wq_, hf=hf: e.matmul(
                        pb[hf][:, ft * 128:(ft + 1) * 128], lhsT=wq_[:, kc, ft * 128:(ft + 1) * 128], rhs=xnT[:, kc, :],
                        start=(kc == 0), stop=(kc == 7)), reads=[wq_, xnT], writes=[pb[hf]])
            kb.op("act", lambda e, hf=hf: e.copy(out=qpT[:, 4 * hf:4 * hf + 4, :],
                                                 in_=pb[hf][:, 0:512].rearrange("p (f t) -> p f t", f=4)),
                  reads=[pb[hf]], writes=[qpT])
        sbanks = [pb[0], pb[1], pb[3], pb[4]]
        for h in range(8):
            bk = sbanks[h // 2]
            kb.op("pe", lambda e, bk=bk, h=h: e.matmul(bk[:, (h % 2) * 256:(h % 2) * 256 + 256], lhsT=qpT[:, h, :],
                                                      rhs=SK[:], start=True, stop=True),
                  reads=[qpT, SK], writes=[bk])
        for j4 in range(4):
            kb.op("act", lambda e, j4=j4: e.copy(out=ssc[:, j4 * 512:(j4 + 1) * 512], in_=sbanks[j4][:, 0:512]),
                  reads=[sbanks[j4]], writes=[Isc])
        tvv = tv[:].rearrange("p h s k -> p (h s) k")
        tiv = ti[:].rearrange("p h s k -> p (h s) k")
        for gi in range(16):
            sv = ssc[:, gi * 128:(gi + 1) * 128]
            sw = sscw[:, gi * 128:(gi + 1) * 128]
            kb.op("dve", lambda e, sv=sv, gi=gi: e.max(out=tvv[:, gi, 0:8], in_=sv), reads=[Isc], writes=[tv])
            kb.op("dve", lambda e, sv=sv, gi=gi: e.max_index(out=tiv[:, gi, 0:8], in_max=tvv[:, gi, 0:8], in_values=sv),
                  reads=[Isc, tv], writes=[ti])
            kb.op("dve", lambda e, sv=sv, sw=sw, gi=gi: e.match_replace(out=sw, in_to_replace=tvv[:, gi, 0:8],
                                                                       in_values=sv, imm_value=-1e30),
                  reads=[Isc, tv], writes=[Isc])
            kb.op("dve", lambda e, sw=sw, gi=gi: e.max(out=tvv[:, gi, 8:16], in_=sw), reads=[Isc], writes=[tv])
            kb.op("dve", lambda e, sw=sw, gi=gi: e.max_index(out=tiv[:, gi, 8:16], in_max=tvv[:, gi, 8:16], in_values=sw),
                  reads=[Isc, tv], writes=[ti])
        kb.op("dve", lambda e: e.tensor_copy(out=tif[:], in_=ti[:]), reads=[ti], writes=[tif])
        candv = cand.rearrange("p (h a b) -> p h a b", h=8, a=16)
        kb.op("dve", lambda e: e.tensor_tensor(out=candv, in0=tv[:, :, 0, :].unsqueeze(3).to_broadcast([128, 8, 16, 16]),
                                               in1=tv[:, :, 1, :].unsqueeze(2).to_broadcast([128, 8, 16, 16]), op=ALU.add),
              reads=[tv], writes=[Isc])
        for h in range(8):
            cv = cand[:, h * 256:(h + 1) * 256]
            cw = candw[:, h * 256:(h + 1) * 256]
            kb.op("dve", lambda e, cv=cv, h=h: e.max(out=tsv[:, h, 0:8], in_=cv), reads=[Isc], writes=[tsv])
            kb.op("dve", lambda e, cv=cv, h=h: e.max_index(out=tpos[:, h, 0:8], in_max=tsv[:, h, 0:8], in_values=cv),
                  reads=[Isc, tsv], writes=[tpos])
            kb.op("dve", lambda e, cv=cv, cw=cw, h=h: e.match_replace(out=cw, in_to_replace=tsv[:, h, 0:8],
                                                                     in_values=cv, imm_value=-1e30),
                  reads=[Isc, tsv], writes=[Isc])
            kb.op("dve", lambda e, cw=cw, h=h: e.max(out=tsv[:, h, 8:16], in_=cw), reads=[Isc], writes=[tsv])
            kb.op("dve", lambda e, cw=cw, h=h: e.max_index(out=tpos[:, h, 8:16], in_max=tsv[:, h, 8:16], in_values=cw),
                  reads=[Isc, tsv], writes=[tpos])
        kb.op("dve", lambda e: e.tensor_single_scalar(out=pa[:], in_=tpos[:], scalar=4, op=ALU.logical_shift_right),
              reads=[tpos], writes=[pa])
        kb.op("dve", lambda e: e.tensor_single_scalar(out=pbb[:], in_=tpos[:], scalar=15, op=ALU.bitwise_and),
              reads=[tpos], writes=[pbb])
        kb.op("dve", lambda e: e.tensor_copy(out=paf[:], in_=pa[:]), reads=[pa], writes=[paf])
        kb.op("dve", lambda e: e.tensor_copy(out=pbf[:], in_=pbb[:]), reads=[pbb], writes=[pbf])
        oh4 = ohv.rearrange("p (h k a) -> p h k a", h=8, k=16)
        oh42 = ohv2.rearrange("p (h k a) -> p h k a", h=8, k=16)
        io4 = iota16[:].unsqueeze(1).unsqueeze(1).to_broadcast([128, 8, 16, 16])
        for (pf, half, dst) in ((paf, 0, i1s), (pbf, 1, i2s)):
            kb.op("dve", lambda e, pf=pf: e.tensor_tensor(out=oh4, in0=pf[:].unsqueeze(3).to_broadcast([128, 8, 16, 16]),
                                                          in1=io4, op=ALU.is_equal),
                  reads=[pf, iota16], writes=[Isc])
            kb.op("dve", lambda e, half=half: e.tensor_tensor(
                out=oh42, in0=oh4, in1=tif[:, :, half, :].unsqueeze(2).to_broadcast([128, 8, 16, 16]), op=ALU.mult),
                reads=[Isc, tif], writes=[Isc])
            kb.op("dve", lambda e, dst=dst: e.tensor_reduce(out=dst[:], in_=oh42, axis=AX.X, op=ALU.add),
                  reads=[Isc], writes=[dst])
        kb.op("dve", lambda e: e.scalar_tensor_tensor(out=eidf[:].rearrange("p (h k) -> p h k", h=8), in0=i1s[:],
                                                      scalar=128.0, in1=i2s[:], op0=ALU.mult, op1=ALU.add),
              reads=[i1s, i2s], writes=[eidf])
        kb.op("dve", lambda e: e.tensor_copy(out=eidi[:], in_=eidf[:]), reads=[eidf], writes=[eidi])
        kb.op("dve", lambda e: e.tensor_reduce(out=gmx[:], in_=tsv[:], axis=AX.X, op=ALU.max), reads=[tsv], writes=[gmx])
        kb.op("dve", lambda e: e.tensor_tensor(out=gk[:], in0=tsv[:], in1=gmx[:].unsqueeze(2).to_broadcast([128, 8, 16]),
                                               op=ALU.subtract), reads=[tsv, gmx], writes=[gk])
        kb.op("act", lambda e: e.activation(out=gk[:], in_=gk[:], func=AF.Exp), reads=[gk], writes=[gk])
        kb.op("dve", lambda e: e.tensor_reduce(out=gmx[:], in_=gk[:], axis=AX.X, op=ALU.add), reads=[gk], writes=[gmx])
        kb.op("dve", lambda e: e.reciprocal(out=gmx[:], in_=gmx[:]), reads=[gmx], writes=[gmx])
        kb.op("dve", lambda e: e.tensor_tensor(out=gk[:], in0=gk[:], in1=gmx[:].unsqueeze(2).to_broadcast([128, 8, 16]),
                                               op=ALU.mult), reads=[gk, gmx], writes=[gk])
        def peer_steps():
            for hk in range(128):
                ub = Ub[hk % 6]
                kb.dma("pool", ub[:], pu_bf[:, :], reads=[eidi, pu_bf], writes=[ub.owner],
                       indirect=bass.IndirectOffsetOnAxis(ap=eidi[:, hk:hk + 1], axis=0), own_sem=hk % 6)
                kb.op("dve", lambda e, ub=ub, hk=hk: e.scalar_tensor_tensor(
                    out=ub[:], in0=ub[:], scalar=1.0, in1=xn[:], op0=ALU.mult, op1=ALU.mult,
                    accum_out=actv[:, hk:hk + 1]), reads=[ub.owner, xn], writes=[ub.owner, actv])
                yield
            kb.op("act", lambda e: e.activation(out=wgt[:], in_=actv[:], func=AF.Gelu), reads=[actv], writes=[wgt])
            kb.op("dve", lambda e: e.tensor_tensor(out=wgt[:], in0=wgt[:], in1=gk[:].rearrange("p h k -> p (h k)"),
                                                   op=ALU.mult), reads=[wgt, gk], writes=[wgt])
            for hk in range(128):
                ub = Ub[(2 + hk) % 6]
                kb.dma("pool", ub[:], pv_bf[:, :], reads=[eidi, pv_bf], writes=[ub.owner],
                       indirect=bass.IndirectOffsetOnAxis(ap=eidi[:, hk:hk + 1], axis=0), own_sem=(2 + hk) % 6)
                kb.op("dve", lambda e, ub=ub, hk=hk: e.scalar_tensor_tensor(
                    out=h2[:], in0=ub[:], scalar=wgt[:, hk:hk + 1], in1=h2[:], op0=ALU.mult, op1=ALU.add),
                    reads=[ub.owner, wgt, NBt], writes=[NBt])
                yield
            if sample:
                kb.dma("sp", y_s_out[:, :], h2[:], reads=[NBt], writes=[y_s_out])
            else:
                kb.dma("sp", y_own[i, :, :], h2[:], reads=[NBt], writes=[y_own])
            yield
        return peer_steps()

    pending = [None]
    nslots = [1]

    def advance(n=1):
        g = pending[0]
        if g is None:
            return
        for _ in range(n):
            try:
                next(g)
            except StopIteration:
                pending[0] = None
                return

    def drain():
        while pending[0] is not None:
            advance(64)

    for i in range(nq):
        xb = xt[i % 2]
        kb.dma("sp", xb[:], xown[i, :, :], writes=[xb])
        norm_transpose(xb, hnT)
        wsl = load_w(win_bf, *CHUNKS[CH_K])
        proj_tok(pb[0], wsl, 512)
        head_norm(pb[0], kg, ko)
        kb.dma("sp", k_own[i, :, :], ko[:], reads=[ko], writes=[k_own])
        wsl = load_w(win_bf, *CHUNKS[CH_V])
        proj_tok(pb[1], wsl, 512)
        kb.op("act", lambda e: e.copy(out=vo[:], in_=pb[1][:, 0:512]), reads=[pb[1]], writes=[vo])
        kb.dma("sp", v_own[i, :, :], vo[:], reads=[vo], writes=[v_own])
        wsl = load_w(win_bf, *CHUNKS[CH_KW])
        proj_tok(pb[0], wsl, 72)
        kb.op("dve", lambda e: e.tensor_copy(out=kio[:], in_=pb[0][:, 0:64]), reads=[pb[0]], writes=[kio])
        kb.dma("sp", ki_own[i, :, :], kio[:], reads=[kio], writes=[ki_own])
        kb.op("dve", lambda e: e.tensor_scalar(out=wis[:], in0=pb[0][:, 64:72], scalar1=0.125 * (8 ** -0.5),
                                               scalar2=None, op0=ALU.mult), reads=[pb[0]], writes=[wis])
        if not do_attn:
            continue
        tpv = tp_bank.t[:].bitcast(BF16)
        wsl = load_w(win_bf, *CHUNKS[CH_Q])
        proj_tok(pb[0], wsl, 512)
        head_norm(pb[0], qg, qf)
        kb.op("dve", lambda e: e.tensor_copy(out=qbf[:], in_=qf[:]), reads=[qf], writes=[qbf])
        for pr in range(4):
            kb.op("pe", lambda e, pr=pr: e.transpose(out=tpv[:, pr * 128:(pr + 1) * 128],
                                                     in_=qbf[:, pr * 128:(pr + 1) * 128], identity=ident_b[:]),
                  reads=[qbf, ident_b], writes=[tp_bank])
        kb.op("act", lambda e: e.copy(out=qT2[:], in_=tpv[:, 0:512].rearrange("p (a t) -> p a t", a=4)),
              reads=[tp_bank], writes=[qT2])
        wsl = load_w(win_bf, *CHUNKS[CH_QI])
        proj_tok(pb[1], wsl, 512)
        kb.op("act", lambda e: e.copy(out=qbf[:], in_=pb[1][:, 0:512]), reads=[pb[1]], writes=[qbf])
        for pr in range(4):
            kb.op("pe", lambda e, pr=pr: e.transpose(out=tpv[:, pr * 128:(pr + 1) * 128],
                                                     in_=qbf[:, pr * 128:(pr + 1) * 128], identity=ident_b[:]),
                  reads=[qbf, ident_b], writes=[tp_bank])
        kb.op("act", lambda e: e.copy(out=qiT2[:], in_=tpv[:, 0:512].rearrange("p (a t) -> p a t", a=4)),
              reads=[tp_bank], writes=[qiT2])
        nkb = min(4 * i + 4, nblk_a)
        nch = nkb // 4
        nk = nkb * 128
        ibanks = [pb[0], pb[1], pb[3], pb[4]]
        cnt_i = 0
        per_slot = -(-260 // (nch * 8 + nkb))
        for ch in range(nch):
            kc_ = kitc[ch % 2]
            kb.dma("sp", kc_[:], kit_s[:, ch * 512:(ch + 1) * 512], reads=[kit_s], writes=[kc_])
            Ic = Isc[:, ch * 512:(ch + 1) * 512]
            for h in range(8):
                a, half = h // 2, h % 2
                ps_ = slice(64 * half, 64 * half + 64)
                bk = ibanks[cnt_i % 4]
                rl_ = rl[cnt_i % 2]
                cnt_i += 1
                advance(per_slot)
                kb.op("pe", lambda e, bk=bk, a=a, ps_=ps_, kc_=kc_: e.matmul(
                    bk[:, 0:512], lhsT=qiT2[ps_, a, :], rhs=kc_[ps_, :], start=True, stop=True),
                    reads=[qiT2, kc_], writes=[bk])
                kb.op("act", lambda e, bk=bk, rl_=rl_: e.activation(out=rl_[:], in_=bk[:, 0:512], func=AF.Relu),
                      reads=[bk], writes=[rl_])
                if h == 0:
                    kb.op("dve", lambda e, rl_=rl_, Ic=Ic: e.tensor_scalar(
                        out=Ic, in0=rl_[:], scalar1=wis[:, 0:1], scalar2=None, op0=ALU.mult),
                        reads=[rl_, wis], writes=[Isc])
                else:
                    kb.op("dve", lambda e, rl_=rl_, Ic=Ic, h=h: e.scalar_tensor_tensor(
                        out=Ic, in0=rl_[:], scalar=wis[:, h:h + 1], in1=Ic, op0=ALU.mult, op1=ALU.add),
                        reads=[rl_, wis, Isc], writes=[Isc])
        kb.op("dve", lambda e: e.tensor_reduce(out=hi0[:], in_=Isc[:, 0:nk], axis=AX.X, op=ALU.max),
              reads=[Isc], writes=[hi0])
        kb.op("dve", lambda e: e.tensor_reduce(out=lo[:], in_=Isc[:, 0:nk], axis=AX.X, op=ALU.min),
              reads=[Isc], writes=[lo])
        kb.op("dve", lambda e: e.tensor_tensor(out=Isc[:, nk - 512:nk], in0=Isc[:, nk - 512:nk], in1=cmask[:],
                                               op=ALU.add), reads=[Isc, cmask], writes=[Isc])
        kb.op("dve", lambda e: e.tensor_tensor(out=hi0[:], in0=hi0[:], in1=lo[:], op=ALU.subtract),
              reads=[hi0, lo], writes=[hi0])
        kb.op("dve", lambda e: e.tensor_scalar(out=wh[:], in0=pw[:], scalar1=hi0[:, 0:1], scalar2=None,
                                               op0=ALU.mult), reads=[pw, hi0], writes=[wh])
        for n in range(NIT):
            kb.op("dve", lambda e, n=n: e.tensor_tensor(out=mid[:], in0=lo[:], in1=wh[:, n:n + 1], op=ALU.add),
                  reads=[lo, wh], writes=[mid])
            kb.op("dve", lambda e: e.memset(cnt4[:], 0.0), writes=[cnt4])
            for q4 in range((nk + 2175) // 2176):
                c0 = q4 * 2176
                c1 = min(nk, c0 + 2176)
                kb.op("dve", lambda e, c0=c0, c1=c1, q4=q4: e.tensor_scalar(
                    out=junkI[:, 0:c1 - c0], in0=Isc[:, c0:c1], scalar1=mid[:, 0:1],
                    scalar2=0.0, op0=ALU.is_ge, op1=ALU.add, accum_out=cnt4[:, q4:q4 + 1]),
                    reads=[Isc, mid], writes=[junkI, cnt4])
            kb.op("dve", lambda e: e.tensor_reduce(out=cntt[:], in_=cnt4[:], axis=AX.X, op=ALU.add),
                  reads=[cnt4], writes=[cntt])
            kb.op("dve", lambda e, n=n: e.tensor_scalar(out=dl[:], in0=cntt[:], scalar1=255.5,
                                                        scalar2=wh[:, n:n + 1], op0=ALU.is_ge, op1=ALU.mult),
                  reads=[cntt, wh], writes=[dl])
            kb.op("dve", lambda e: e.tensor_tensor(out=lo[:], in0=lo[:], in1=dl[:], op=ALU.add),
                  reads=[lo, dl], writes=[lo])
        kb.op("dve", lambda e: e.memset(pb[5][:, 0:260], 0.0), writes=[pb[5]])
        kb.op("dve", lambda e: e.memset(pb[6][:, 0:260], 0.0), writes=[pb[6]])
        def emit_S(kbi):
            ch, kloc = kbi // 4, kbi % 4
            ktc_ = ktc[ch % 2]
            vch_ = vch[ch % 2]
            if kloc == 0:
                kb.dma("sp", ktc_[:], kt_s[:, :, ch * 512:(ch + 1) * 512], reads=[kt_s], writes=[ktc_])
                kb.dma("sp", vch_[:], v_s.t[ch * 512:(ch + 1) * 512, :, :].rearrange("(b p) h e -> p b (h e)", p=128),
                       reads=[v_s], writes=[vch_])
            nm = negm[kbi % 2]
            kb.op("dve", lambda e, nm=nm, kbi=kbi: e.tensor_scalar(
                out=nm[:], in0=Isc[:, kbi * 128:(kbi + 1) * 128], scalar1=lo[:, 0:1], scalar2=-30000.0,
                op0=ALU.is_lt, op1=ALU.mult), reads=[Isc, lo], writes=[nm])
            r = kbi - 4 * i
            near = (-1 <= r <= 3)
            if near:
                kb.op("dve", lambda e, nm=nm, r=r: e.tensor_tensor(
                    out=addh[:], in0=NB[r + 1][:], in1=nm[:].unsqueeze(1).to_broadcast([128, 8, 128]), op=ALU.add),
                    reads=[NB[r + 1], nm], writes=[addh])
            pt_ = PT[kbi % 2]
            for g in range(2):
                sb_ = pb[3 + g]
                for hh in range(4):
                    h = 4 * g + hh
                    a, half = h // 2, h % 2
                    ps_ = slice(64 * half, 64 * half + 64)
                    kb.op("pe", lambda e, sb_=sb_, hh=hh, a=a, ps_=ps_, ktc_=ktc_, kloc=kloc: e.matmul(
                        sb_[:, hh * 128:(hh + 1) * 128], lhsT=ktc_[ps_, a, kloc * 128:(kloc + 1) * 128],
                        rhs=qT2[ps_, a, :], start=True, stop=False), reads=[ktc_, qT2], writes=[sb_])
                    if near:
                        kb.op("pe", lambda e, sb_=sb_, hh=hh, h=h: e.matmul(
                            sb_[:, hh * 128:(hh + 1) * 128], lhsT=addh[:, h, :], rhs=ident_b[:],
                            start=False, stop=True), reads=[addh, ident_b], writes=[sb_])
                    else:
                        kb.op("pe", lambda e, sb_=sb_, hh=hh, nm=nm: e.matmul(
                            sb_[:, hh * 128:(hh + 1) * 128], lhsT=nm[:], rhs=ident_b[:],
                            start=False, stop=True), reads=[nm, ident_b], writes=[sb_])
                kb.op("act", lambda e, sb_=sb_, pt_=pt_, g=g: e.activation(
                    out=pt_[:, 4 * g:4 * g + 4, :], in_=sb_[:, 0:512].rearrange("p (h q) -> p h q", h=4),
                    func=AF.Exp), reads=[sb_], writes=[pt_])

        def emit_PV(kbi):
            ch, kloc = kbi // 4, kbi % 4
            vch_ = vch[ch % 2]
            pt_ = PT[kbi % 2]
            for h in range(8):
                ob = pb[5 + h // 4]
                kb.op("pe", lambda e, ob=ob, h=h, pt_=pt_, vch_=vch_, kloc=kloc: e.matmul(
                    ob[:, (h % 4) * 65:(h % 4) * 65 + 65], lhsT=pt_[:, h, :], rhs=vch_[:, kloc, h * 65:(h + 1) * 65],
                    start=False, stop=False, skip_group_check=True), reads=[pt_, vch_], writes=[ob])

        for kbi in range(nkb):
            emit_S(kbi)
            advance(per_slot)
            if kbi > 0:
                emit_PV(kbi - 1)
        emit_PV(nkb - 1)
        for g in range(2):
            ob = pb[5 + g]
            obv = ob[:, 0:260].rearrange("p (h e) -> p h e", h=4)
            kb.op("dve", lambda e, obv=obv, g=g: e.reciprocal(out=rden[:, 4 * g:4 * g + 4], in_=obv[:, :, 64]),
                  reads=[ob], writes=[rden])
            kb.op("dve", lambda e, obv=obv, g=g: e.tensor_tensor(
                out=ao[:, 256 * g:256 * g + 256].rearrange("p (h d) -> p h d", h=4), in0=obv[:, :, 0:64],
                in1=rden[:, 4 * g:4 * g + 4].unsqueeze(2).to_broadcast([128, 4, 64]), op=ALU.mult),
                reads=[ob, rden], writes=[ao])
        if dbg_ao is not None:
            kb.op("dve", lambda e: e.tensor_copy(out=aof[:], in_=ao[:]), reads=[ao], writes=[aof])
            kb.dma("sp", dbg_ao[i, :, :], aof[:], reads=[aof], writes=[dbg_ao])

        if do_tail:
            drain()
            pending[0] = tail_block(i, xb)


    drain()
    if do_sample:
        LOB = _bucket_lo()
        wrep = kb.sb([128, 8], F32, "wrep")
        idx2 = kb.sb([128, 2], I32, "idx2")
        pti = kb.sb([128, 16], I32, "pti")
        ptf = kb.sb([128, 16], F32, "ptf")
        dh = kb.sb([128, 128], F32, "dh")
        dh2 = kb.sb([128, 128], F32, "dh2")
        scs = kb.sb([128, 2, 128], F32, "scs")
        scn = kb.sb([128, 1], F32, "scn")
        dn8 = kb.sb([128, 8], F32, "dn8")
        sm8 = kb.sb([128, 8], F32, "sm8")
        tA = View(Ubo[0], Ubo[0].t[:].bitcast(F32)[:, 0:256])
        tE = View(Ubo[0], Ubo[0].t[:].bitcast(F32)[:, 256:512])
        tF = View(Ubo[1], Ubo[1].t[:].bitcast(F32)[:, 0:256])
        tG = View(Ubo[1], Ubo[1].t[:].bitcast(F32)[:, 256:512])
        idxs = View(Ubo[2], Ubo[2].t[:].bitcast(U32)[:, 0:256])
        tC = View(Ubo[2], Ubo[2].t[:].bitcast(U32)[:, 256:512])
        tD = View(Ubo[3], Ubo[3].t[:].bitcast(U32)[:, 0:256])
        rowT = kb.sb([128, 32], I32, "rowT")
        distT = kb.sb([128, 32], F32, "distT")
        negT = kb.sb([128, 32], F32, "negT")
        indT = kb.sb([128, 32], F32, "indT")
        biasT = kb.sb([128, 32, 8], F32, "biasT")
        tmp3 = kb.sb([128, 32, 8], F32, "tmp3")
        lg = kb.sb([128, 8], F32, "lg")
        pex = kb.sb([128, 8], F32, "pex")
        zsel = kb.sb([128, 31], F32, "zsel")
        numS = View(Ubo[4], Ubo[4].t[:].bitcast(F32)[:, 0:512])
        denS = kb.sb([128, 8], F32, "denS")
        seln = kb.sb([128, 1], F32, "seln")
        nb0 = kb.sb([128, 8], F32, "nb0")
        sq, sk_, sv_, ski = qf, ko, vo, kio
        sqi = hsq
        su = tmpA
        qrep = tmpB
        IscV = Isc.t[:]
        kb.dma("sp", zsel[:], zsel_in[:, :], writes=[zsel])
        kb.op("dve", lambda e: e.memset(pti[:], 0), writes=[pti])
        kb.dma("sp", pti[0:16, :], pt_in[:, :], writes=[pti])
        kb.op("dve", lambda e: e.tensor_copy(out=ptf[:], in_=pti[:]), reads=[pti], writes=[ptf])
        with nc.allow_non_contiguous_dma(reason="page table slices"):
            for jg in range(8):
                kb.dma("sp", idx2[jg * 16:(jg + 1) * 16, :], pt_in[:, 2 * jg:2 * jg + 2], writes=[idx2])
        xb = xt[0]
        kb.dma("sp", xb[:], xs_in[:, :], writes=[xb])
        norm_transpose(xb, hnT)
        wsl = load_w(win_bf, *CHUNKS[CH_Q])
        proj_tok(pb[0], wsl, 512)
        head_norm(pb[0], qg, sq)
        wsl = load_w(win_bf, *CHUNKS[CH_K])
        proj_tok(pb[1], wsl, 512)
        head_norm(pb[1], kg, sk_)
        kb.dma("sp", k_s_out[:, :], sk_[:], reads=[sk_], writes=[k_s_out])
        wsl = load_w(win_bf, *CHUNKS[CH_V])
        proj_tok(pb[0], wsl, 512)
        kb.op("act", lambda e: e.copy(out=sv_[:], in_=pb[0][:, 0:512]), reads=[pb[0]], writes=[sv_])
        kb.dma("sp", v_s_out[:, :], sv_[:], reads=[sv_], writes=[v_s_out])
        wsl = load_w(win_bf, *CHUNKS[CH_QI])
        proj_tok(pb[1], wsl, 512)
        kb.op("act", lambda e: e.copy(out=sqi[:], in_=pb[1][:, 0:512]), reads=[pb[1]], writes=[sqi])
        wsl = load_w(win_bf, *CHUNKS[CH_KW])
        proj_tok(pb[0], wsl, 72)
        kb.op("dve", lambda e: e.tensor_copy(out=ski[:], in_=pb[0][:, 0:64]), reads=[pb[0]], writes=[ski])
        kb.dma("sp", ki_s_out[:, :], ski[:], reads=[ski], writes=[ki_s_out])
        kb.op("dve", lambda e: e.tensor_scalar(out=wis[:], in0=pb[0][:, 64:72], scalar1=0.125 * (8 ** -0.5),
                                               scalar2=None, op0=ALU.mult), reads=[pb[0]], writes=[wis])
        wsl = load_w(win_bf, *CHUNKS[CH_U])
        proj_tok(pb[1], wsl, 512)
        kb.op("act", lambda e: e.copy(out=su[:], in_=pb[1][:, 0:512]), reads=[pb[1]], writes=[su])
        kb.dma("sp", pool_s_out[:, 0:14, :], state_in[:, 1:15, :], reads=[state_in], writes=[pool_s_out])
        kb.dma("sp", pool_s_out[:, 14, :], su[0:16, :], reads=[su], writes=[pool_s_out])
        kb.dma("sp", qi_d[:, :], sqi[0:16, :], reads=[sqi], writes=[qi_d])
        kb.dma("sp", wi_d[:, :], wis[0:16, :], reads=[wis], writes=[wi_d])
        kb.dma("sp", q_d[:, :], sq[0:16, :], reads=[sq], writes=[q_d])
        for jg in range(8):
            kb.dma("sp", qrep[jg * 16:(jg + 1) * 16, :], qi_d[:, :], reads=[qi_d], writes=[qrep])
            kb.dma("sp", wrep[jg * 16:(jg + 1) * 16, :], wi_d[:, :], reads=[wi_d], writes=[wrep])
        KI = IscV[:, 0:8192]
        prodc = View(ktc[0], ktc[0].t[:].rearrange("p a b -> p (a b)").bitcast(F32)[:, 0:1024])
        for s in range(2):
            kb.dma("pool", KI, cache_ik[:, :], reads=[idx2], writes=[Isc],
                   indirect=bass.IndirectOffsetOnAxis(ap=idx2[:, s:s + 1], axis=0))
            for h in range(8):
                for c8 in range(8):
                    kb.op("dve", lambda e, h=h, c8=c8: e.tensor_tensor(
                        out=prodc[:].rearrange("p (k d) -> p k d", k=16),
                        in0=KI[:, c8 * 1024:(c8 + 1) * 1024].rearrange("p (k d) -> p k d", k=16),
                        in1=qrep[:, h * 64:(h + 1) * 64].unsqueeze(1).to_broadcast([128, 16, 64]), op=ALU.mult),
                        reads=[Isc, qrep], writes=[prodc.owner])
                    kb.op("dve", lambda e, c8=c8: e.tensor_reduce(
                        out=dh[:, c8 * 16:(c8 + 1) * 16], in_=prodc[:].rearrange("p (k d) -> p k d", k=16),
                        axis=AX.X, op=ALU.add), reads=[prodc.owner], writes=[dh])
                if h == 0:
                    kb.op("dve", lambda e, s=s: e.tensor_scalar(out=scs[:, s, :], in0=dh[:], scalar1=0.0,
                                                                scalar2=wrep[:, 0:1], op0=ALU.max, op1=ALU.mult),
                          reads=[dh, wrep], writes=[scs])
                else:
                    kb.op("dve", lambda e, h=h: e.tensor_scalar(out=dh2[:], in0=dh[:], scalar1=0.0,
                                                                scalar2=wrep[:, h:h + 1], op0=ALU.max, op1=ALU.mult),
                          reads=[dh, wrep], writes=[dh2])
                    kb.op("dve", lambda e, s=s: e.tensor_tensor(out=scs[:, s, :], in0=scs[:, s, :], in1=dh2[:],
                                                                op=ALU.add), reads=[scs, dh2], writes=[scs])
        for jg in range(8):
            kb.dma("sp", sc_d[:, jg * 256:(jg + 1) * 256], scs[jg * 16:(jg + 1) * 16, :, :].rearrange("p s k -> p (s k)"),
                   reads=[scs], writes=[sc_d])
        xtmp = xn[:, 512:1024]
        kb.op("dve", lambda e: e.tensor_tensor(out=xtmp.rearrange("p (h d) -> p h d", h=8),
                                               in0=sqi[:].rearrange("p (h d) -> p h d", h=8),
                                               in1=ski[:].unsqueeze(1).to_broadcast([128, 8, 64]), op=ALU.mult),
              reads=[sqi, ski], writes=[xn])
        kb.op("dve", lambda e: e.tensor_reduce(out=dn8[:], in_=xtmp.rearrange("p (h d) -> p h d", h=8), axis=AX.X,
                                               op=ALU.add), reads=[xn], writes=[dn8])
        kb.op("dve", lambda e: e.tensor_scalar(out=dn8[:], in0=dn8[:], scalar1=0.0, scalar2=None, op0=ALU.max),
              reads=[dn8], writes=[dn8])
        kb.op("dve", lambda e: e.tensor_tensor(out=dn8[:], in0=dn8[:], in1=wis[:], op=ALU.mult),
              reads=[dn8, wis], writes=[dn8])
        kb.op("dve", lambda e: e.tensor_reduce(out=scn[:], in_=dn8[:], axis=AX.X, op=ALU.add),
              reads=[dn8], writes=[scn])
        Is = IscV[:, 0:2049]
        kb.op("dve", lambda e: e.memset(IscV[:, 0:2304], 0.0), writes=[Isc])
        kb.dma("sp", IscV[0:16, 0:2048], sc_d[:, :], reads=[sc_d], writes=[Isc])
        kb.op("dve", lambda e: e.tensor_copy(out=IscV[:, 2048:2049], in_=scn[:]), reads=[scn], writes=[Isc])
        for rd in range(32):
            kb.op("dve", lambda e: e.max(out=sm8[:], in_=Is), reads=[Isc], writes=[sm8])
            kb.op("dve", lambda e, rd=rd: e.max_index(out=idxs[:, rd * 8:(rd + 1) * 8], in_max=sm8[:], in_values=Is),
                  reads=[Isc, sm8], writes=[idxs])
            kb.op("dve", lambda e: e.match_replace(out=Is, in_to_replace=sm8[:], in_values=Is, imm_value=-1e30),
                  reads=[Isc, sm8], writes=[Isc])
        kb.op("dve", lambda e: e.tensor_copy(out=tA[:], in_=idxs[:]), reads=[idxs], writes=[tA])
        kb.op("dve", lambda e: e.tensor_scalar(out=tC[:], in0=tA[:], scalar1=2047.0, scalar2=None, op0=ALU.min),
              reads=[tA], writes=[tC])
        kb.op("dve", lambda e: e.tensor_single_scalar(out=tD[:], in_=tC[:], scalar=7, op=ALU.logical_shift_right),
              reads=[tC], writes=[tD])
        kb.op("dve", lambda e: e.tensor_single_scalar(out=tC[:], in_=tC[:], scalar=127, op=ALU.bitwise_and),
              reads=[tC], writes=[tC])
        kb.op("dve", lambda e: e.tensor_copy(out=tE[:], in_=tD[:]), reads=[tD], writes=[tE])
        kb.op("dve", lambda e: e.tensor_copy(out=tF[:], in_=tC[:]), reads=[tC], writes=[tF])
        ohs = IscV[:, 4608:8704].rearrange("p (k j) -> p k j", k=256)
        kb.op("dve", lambda e: e.tensor_tensor(out=ohs, in0=tE[:].unsqueeze(2).to_broadcast([128, 256, 16]),
                                               in1=iota16[:].unsqueeze(1).to_broadcast([128, 256, 16]), op=ALU.is_equal),
              reads=[tE, iota16], writes=[Isc])
        kb.op("dve", lambda e: e.tensor_tensor(out=ohs, in0=ohs, in1=ptf[:].unsqueeze(1).to_broadcast([128, 256, 16]),
                                               op=ALU.mult), reads=[Isc, ptf], writes=[Isc])
        kb.op("dve", lambda e: e.tensor_reduce(out=tG[:], in_=ohs, axis=AX.X, op=ALU.add), reads=[Isc], writes=[tG])
        kb.op("dve", lambda e: e.scalar_tensor_tensor(out=tG[:], in0=tG[:], scalar=128.0, in1=tF[:], op0=ALU.mult,
                                                      op1=ALU.add), reads=[tG, tF], writes=[tG])
        kb.op("dve", lambda e: e.tensor_scalar(out=tE[:], in0=tA[:], scalar1=-1.0, scalar2=2048.0, op0=ALU.mult,
                                               op1=ALU.add), reads=[tA], writes=[tE])
        kb.op("dve", lambda e: e.tensor_scalar(out=tF[:], in0=tA[:], scalar1=2047.5, scalar2=-30000.0, op0=ALU.is_ge,
                                               op1=ALU.mult), reads=[tA], writes=[tF])
        kb.op("dve", lambda e: e.tensor_reduce(out=seln[:], in_=tF[:], axis=AX.X, op=ALU.min), reads=[tF], writes=[seln])
        kb.op("dve", lambda e: e.tensor_scalar(out=seln[:], in0=seln[:], scalar1=-1.0 / 30000.0, scalar2=None,
                                               op0=ALU.mult), reads=[seln], writes=[seln])
        for (srcb, dstb) in ((tG, rowT), (tE, distT), (tF, negT)):
            for gq in range(2):
                kb.op("pe", lambda e, srcb=srcb, gq=gq: e.transpose(out=pb[2][:, gq * 128:(gq + 1) * 128],
                                                                   in_=srcb[:, gq * 128:(gq + 1) * 128],
                                                                   identity=ident_f[:]),
                      reads=[srcb, ident_f], writes=[pb[2]])
            kb.op("dve", lambda e, dstb=dstb: e.tensor_copy(
                out=dstb[:].rearrange("p (g b) -> p g b", g=2),
                in_=pb[2][:, 0:256].rearrange("p (g b) -> p g b", g=2)[:, :, 0:16]), reads=[pb[2]], writes=[dstb])
        kb.op("dve", lambda e: e.memset(biasT[:], 0.0), writes=[biasT])
        for bkt in range(1, 32):
            kb.op("dve", lambda e, bkt=bkt: e.tensor_scalar(out=indT[:], in0=distT[:], scalar1=float(LOB[bkt - 1]),
                                                            scalar2=None, op0=ALU.is_lt), reads=[distT], writes=[indT])
            kb.op("dve", lambda e, bkt=bkt: e.tensor_tensor(
                out=tmp3[:], in0=indT[:].unsqueeze(2).to_broadcast([128, 32, 8]),
                in1=ndel[:, bkt * 8:(bkt + 1) * 8].unsqueeze(1).to_broadcast([128, 32, 8]), op=ALU.mult),
                reads=[indT, ndel], writes=[tmp3])
            kb.op("dve", lambda e: e.tensor_tensor(out=biasT[:], in0=biasT[:], in1=tmp3[:], op=ALU.add),
                  reads=[biasT, tmp3], writes=[biasT])
        kb.op("dve", lambda e: e.memset(pb[5][:, 0:512], 0.0), writes=[pb[5]])
        kb.op("dve", lambda e: e.memset(pb[6][:, 0:8], 0.0), writes=[pb[6]])
        qbv = xn[:, 0:512]
        pvv = xn[:, 512:1024]
        for b in range(16):
            kb.dma("sp", qbv, q_d.t[b:b + 1, :].partition_broadcast(128).rearrange("p o d -> p (o d)"),
                   reads=[q_d], writes=[xn])
            for gq in range(2):
                col = gq * 16 + b
                kg_ = UbS[gq]
                vg_ = UbS[2 + gq]
                kb.dma("pool", kg_[:, 0:512], cache_k[:, :], reads=[rowT], writes=[kg_.owner],
                       indirect=bass.IndirectOffsetOnAxis(ap=rowT[:, col:col + 1], axis=0))
                kb.dma("pool", vg_[:, 0:512], cache_v[:, :], reads=[rowT], writes=[vg_.owner],
                       indirect=bass.IndirectOffsetOnAxis(ap=rowT[:, col:col + 1], axis=0))
                kb.op("dve", lambda e, kg_=kg_: e.tensor_tensor(out=pvv, in0=kg_[:, 0:512], in1=qbv, op=ALU.mult),
                      reads=[kg_.owner, xn], writes=[xn])
                kb.op("dve", lambda e: e.tensor_reduce(out=lg[:], in_=pvv.rearrange("p (h d) -> p h d", h=8),
                                                       axis=AX.X, op=ALU.add), reads=[xn], writes=[lg])
                kb.op("dve", lambda e, col=col: e.scalar_tensor_tensor(
                    out=lg[:], in0=lg[:], scalar=negT[:, col:col + 1], in1=biasT[:, col, :], op0=ALU.add, op1=ALU.add),
                    reads=[lg, negT, biasT], writes=[lg])
                kb.op("act", lambda e: e.activation(out=pex[:], in_=lg[:], func=AF.Exp), reads=[lg], writes=[pex])
                kb.op("dve", lambda e, vg_=vg_: e.tensor_tensor(
                    out=pvv.rearrange("p (h d) -> p h d", h=8), in0=vg_[:, 0:512].rearrange("p (h d) -> p h d", h=8),
                    in1=pex[:].unsqueeze(2).to_broadcast([128, 8, 64]), op=ALU.mult),
                    reads=[vg_.owner, pex], writes=[xn])
                kb.op("pe", lambda e, b=b: e.matmul(pb[5][0:16, 0:512], lhsT=zsel[:, 15 - b:31 - b], rhs=pvv,
                                                    start=False, stop=False, skip_group_check=True),
                      reads=[zsel, xn], writes=[pb[5]])
                kb.op("pe", lambda e, b=b: e.matmul(pb[6][0:16, 0:8], lhsT=zsel[:, 15 - b:31 - b], rhs=pex[:],
                                                    start=False, stop=False, skip_group_check=True),
                      reads=[zsel, pex], writes=[pb[6]])
        kb.op("dve", lambda e: e.memset(numS[:], 0.0), writes=[numS])
        kb.op("dve", lambda e: e.memset(denS[:], 1.0), writes=[denS])
        kb.op("dve", lambda e: e.tensor_copy(out=numS[0:16, :], in_=pb[5][0:16, 0:512]), reads=[pb[5]], writes=[numS])
        kb.op("dve", lambda e: e.tensor_copy(out=denS[0:16, :], in_=pb[6][0:16, 0:8]), reads=[pb[6]], writes=[denS])
        kb.op("dve", lambda e: e.tensor_reduce(out=nb0[:], in_=ndel[:, 8:256].rearrange("p (b h) -> p h b", h=8),
                                               axis=AX.X, op=ALU.add), reads=[ndel], writes=[nb0])
        kb.op("dve", lambda e: e.tensor_tensor(out=pvv, in0=sq[:], in1=sk_[:], op=ALU.mult), reads=[sq, sk_], writes=[xn])
        kb.op("dve", lambda e: e.tensor_reduce(out=lg[:], in_=pvv.rearrange("p (h d) -> p h d", h=8), axis=AX.X,
                                               op=ALU.add), reads=[xn], writes=[lg])
        kb.op("dve", lambda e: e.tensor_tensor(out=lg[:], in0=lg[:], in1=nb0[:], op=ALU.add), reads=[lg, nb0], writes=[lg])
        kb.op("act", lambda e: e.activation(out=pex[:], in_=lg[:], func=AF.Exp), reads=[lg], writes=[pex])
        kb.op("dve", lambda e: e.tensor_scalar(out=pex[:], in0=pex[:], scalar1=seln[:, 0:1], scalar2=None, op0=ALU.mult),
              reads=[pex, seln], writes=[pex])
        kb.op("dve", lambda e: e.tensor_tensor(out=pvv.rearrange("p (h d) -> p h d", h=8),
                                               in0=sv_[:].rearrange("p (h d) -> p h d", h=8),
                                               in1=pex[:].unsqueeze(2).to_broadcast([128, 8, 64]), op=ALU.mult),
              reads=[sv_, pex], writes=[xn])
        kb.op("dve", lambda e: e.tensor_tensor(out=numS[:], in0=numS[:], in1=pvv, op=ALU.add), reads=[numS, xn], writes=[numS])
        kb.op("dve", lambda e: e.tensor_tensor(out=denS[:], in0=denS[:], in1=pex[:], op=ALU.add), reads=[denS, pex],
              writes=[denS])
        kb.op("dve", lambda e: e.reciprocal(out=denS[:], in_=denS[:]), reads=[denS], writes=[denS])
        kb.op("dve", lambda e: e.tensor_tensor(out=ao[:].rearrange("p (h d) -> p h d", h=8),
                                               in0=numS[:].rearrange("p (h d) -> p h d", h=8),
                                               in1=denS[:].unsqueeze(2).to_broadcast([128, 8, 64]), op=ALU.mult),
              reads=[numS, denS], writes=[ao])
        stv = IscV[:, 0:7680].rearrange("p (r c) -> p r c", r=15)
        kb.op("dve", lambda e: e.memset(IscV[:, 0:7680], 0.0), writes=[Isc])
        kb.dma("sp", IscV[0:16, 0:7680], state_in.t.rearrange("b r c -> b (r c)"), reads=[state_in], writes=[Isc])
        for g in range(4):
            w = 2 ** (g + 1)
            kb.op("dve", lambda e, g=g, w=w: e.tensor_reduce(
                out=pmf[:, g, :], in_=stv[:, 16 - w:15, g * 128:(g + 1) * 128].rearrange("p r c -> p c r"),
                axis=AX.X, op=ALU.add), reads=[Isc], writes=[pmf])
            kb.op("dve", lambda e, g=g: e.tensor_tensor(out=pmf[:, g, :], in0=pmf[:, g, :], in1=su[:, g * 128:(g + 1) * 128],
                                                        op=ALU.add), reads=[pmf, su], writes=[pmf])
            kb.op("dve", lambda e, g=g, w=w: e.scalar_tensor_tensor(
                out=pmf[:, g, :], in0=pmf[:, g, :], scalar=1.0 / w, in1=su[:, g * 128:(g + 1) * 128], op0=ALU.mult,
                op1=ALU.subtract), reads=[pmf, su], writes=[pmf])
        for g in range(4):
            kb.op("pe", lambda e, g=g: e.transpose(out=pb[2][:, g * 128:(g + 1) * 128], in_=pmf[:, g, :],
                                                   identity=ident_f[:]), reads=[pmf, ident_f], writes=[pb[2]])
        kb.op("dve", lambda e: e.tensor_copy(out=pmT[:], in_=pb[2][:, 0:512].rearrange("p (g t) -> p g t", g=4)),
              reads=[pb[2]], writes=[pmT])
        pending[0] = tail_block(0, xb, sample=True)
        drain()

    kb.finish()
    return nc, kb


def _prep_inputs(inp, cfg, cores):
    nblk_a = cfg.get("nblk_a", NBLK_A)
    nq = cfg.get("nq", NQ)
    nkeys = nblk_a * 128
    xp = np.asarray(inp["x_prompt"], np.float32)
    meta = np.asarray(inp["meta_tokens"], np.float32)
    maps = []
    for c in cores:
        b, cc = c // 4, c % 4
        full = np.zeros((max(nkeys, (4 * nq + 4) * 128), D), np.float32)
        T = 16 + xp.shape[1]
        cat = np.concatenate([meta, xp[b]], axis=0)
        n = min(T, full.shape[0])
        full[:n] = cat[:n]
        xown = np.zeros((nq, 128, D), np.float32)
        xprev = np.zeros((nq, 16, D), np.float32)
        for i in range(nq):
            j = 4 * i + cc
            xown[i] = full[j * 128:(j + 1) * 128]
            if j > 0:
                xprev[i] = full[j * 128 - 16:j * 128]
        m = {
            "xcat": np.ascontiguousarray(full[:nkeys]),
            "xown": xown,
            "xprev": xprev,
            "w_in": np.ascontiguousarray(np.asarray(inp["w_in"], np.float32)[0]),
            "norm1_g": np.ascontiguousarray(np.asarray(inp["norm1_g"], np.float32)[0]),
            "q_norm_g": np.ascontiguousarray(np.asarray(inp["q_norm_g"], np.float32)[0]),
            "k_norm_g": np.ascontiguousarray(np.asarray(inp["k_norm_g"], np.float32)[0]),
            "ident": np.eye(128, dtype=np.float32),
            "rel_bias": np.ascontiguousarray(np.asarray(inp["rel_bias"], np.float32)),
            "qs": (np.arange(128, dtype=np.float32)[:, None] - np.arange(128, dtype=np.float32)[None, :]),
            "thrtab": _thrtab(cc),
            "cmask": _cmask(cc),
            "w_ba": np.ascontiguousarray(np.asarray(inp["w_branch_attn"], np.float32)[0]),
            "w_bp": np.ascontiguousarray(np.asarray(inp["w_branch_pool"], np.float32)[0]),
            "w_out": np.ascontiguousarray(np.asarray(inp["w_out"], np.float32)[0]),
            "peer_wq": np.ascontiguousarray(np.asarray(inp["peer_wq"], np.float32)[0]),
            "w_pool": np.ascontiguousarray(np.asarray(inp["w_pool"], np.float32)[0]),
            "pool_scale": np.ascontiguousarray(np.asarray(inp["pool_scale"], np.float32)[0]),
            "norm2_g": np.ascontiguousarray(np.asarray(inp["norm2_g"], np.float32)[0]),
            "subkeys": np.ascontiguousarray(np.asarray(inp["peer_subkeys"], np.float32)[0]),
            "peer_u": np.ascontiguousarray(np.asarray(inp["peer_u"], np.float32)[0]),
            "peer_v": np.ascontiguousarray(np.asarray(inp["peer_v"], np.float32)[0]),
            "rcnt": _rcnt(cc),
            "iota16": np.broadcast_to(np.arange(16, dtype=np.float32)[None, :], (128, 16)).copy(),
            "pw": np.broadcast_to((0.5 ** np.arange(1, NIT + 1)).astype(np.float32)[None, :], (128, NIT)).copy(),
        }
        if cfg.get("sample", True):
            xs = np.zeros((128, D), np.float32)
            xs[:16] = np.asarray(inp["x_sample"], np.float32)[16 * c:16 * c + 16, 0]
            z = np.zeros((128, 31), np.float32)
            z[:, 15] = 1.0
            m.update({
                "xs_own": xs,
                "cache_k": np.asarray(inp["cache_k"], np.float32).reshape(2560 * 128, 512),
                "cache_v": np.asarray(inp["cache_v"], np.float32).reshape(2560 * 128, 512),
                "cache_ik": np.asarray(inp["cache_idx_k"], np.float32).reshape(2560, 8192),
                "state_own": np.ascontiguousarray(np.asarray(inp["state_pool"], np.float32)[0, 16 * c:16 * c + 16]),
                "pt_own": np.ascontiguousarray(np.asarray(inp["page_table"], np.int32)[16 * c:16 * c + 16]),
                "zsel": z,
            })
        maps.append(m)
    return maps


def _bucket_lo():
    n = np.arange(0, 256)
    nf = np.maximum(n, 16).astype(np.float32)
    large = 16 + (np.log(nf / np.float32(16)) / np.float32(np.log(128 / 16)) * np.float32(16)).astype(np.int32)
    large = np.minimum(large, 31)
    bkt = np.where(n < 16, n, large)
    return [int(np.min(n[bkt >= b])) for b in range(1, 32)]


def _thrtab(cc):
    lo_b = _bucket_lo()
    t = np.zeros((155,), np.float32)
    for r5 in range(5):
        r = r5 - 1
        for b in range(31):
            t[r5 * 31 + b] = lo_b[b] - 128 * (cc - r)
    return np.broadcast_to(t[None, :], (128, 155)).copy()


def _rcnt(cc):
    r = np.zeros((128, 4, 128), np.float32)
    t = np.arange(128)
    for g, w in enumerate((2, 4, 8, 16)):
        cnt = np.minimum(w, t + 1) if cc == 0 else np.full(128, w)
        r[:, g, :] = (1.0 / cnt.astype(np.float32))[None, :]
    return r


def _cmask(cc):
    m = np.zeros((128, 512), np.float32)
    q = np.arange(128)[:, None]
    s = np.arange(128)[None, :]
    for r in range(4):
        if r > cc:
            m[:, r * 128:(r + 1) * 128] = -1e30
        elif r == cc:
            m[:, r * 128:(r + 1) * 128] = np.where(s > q, -1e30, 0.0)
    return m


def kernel(**inputs):
    cfg = {}
    nc = build(cfg)
    cores = list(range(8))
    maps = _prep_inputs(inputs, cfg, cores)
    res = run_bass_kernel_spmd(nc, maps, core_ids=cores)
    rs = res.results
    B, S = 2, 8192
    T = S + 16
    y_prompt = np.zeros((B, S, D), np.float32)
    k_p = np.zeros((1, B, T, 8, 64), np.float32)
    v_p = np.zeros((1, B, T, 8, 64), np.float32)
    i_p = np.zeros((1, B, T, 64), np.float32)
    pool_p = np.zeros((1, B, 15, 512), np.float32)
    for c in cores:
        b, cc = c // 4, c % 4
        r = rs[c]
        for i in range(NQ):
            j = 4 * i + cc
            p0 = j * 128
            if p0 >= T:
                continue
            p1 = min(p0 + 128, T)
            n = p1 - p0
            k_p[0, b, p0:p1] = r["k_own"][i][:n].reshape(n, 8, 64)
            v_p[0, b, p0:p1] = r["v_own"][i][:n].reshape(n, 8, 64)
            i_p[0, b, p0:p1] = r["ki_own"][i][:n]
            lo = max(p0, 16)
            y_prompt[b, lo - 16:p1 - 16] = r["y_own"][i][lo - p0:n]
        if cc == 0:
            ul = r["u_last"]
            rows = ul[:, :, 17:32]
            pool_p[0, b] = np.transpose(rows, (2, 1, 0)).reshape(15, 512)
    y_s = np.zeros((128, 1, D), np.float32)
    k_s = np.zeros((1, 128, 1, 8, 64), np.float32)
    v_s = np.zeros((1, 128, 1, 8, 64), np.float32)
    i_s = np.zeros((1, 128, 1, 64), np.float32)
    pool_s = np.zeros((1, 128, 15, 512), np.float32)
    for c in cores:
        r = rs[c]
        sl = slice(16 * c, 16 * c + 16)
        y_s[sl, 0] = r["y_s"][:16]
        k_s[0, sl, 0] = r["ks_o"][:16].reshape(16, 8, 64)
        v_s[0, sl, 0] = r["vs_o"][:16].reshape(16, 8, 64)
        i_s[0, sl, 0] = r["kis_o"][:16]
        pool_s[0, sl] = r["pool_s"]
    return (y_prompt, y_s, k_p, v_p, i_p, pool_p, k_s, v_s, i_s, pool_s)
```

```python
import numpy as np
from contextlib import ExitStack
import concourse.bass as bass
import concourse.mybir as mybir
from concourse.bass_utils import run_bass_kernel_spmd

F32 = mybir.dt.float32
BF16 = mybir.dt.bfloat16
I32 = mybir.dt.int32
U32 = mybir.dt.uint32
ALU = mybir.AluOpType
AF = mybir.ActivationFunctionType
AX = mybir.AxisListType

D = 1024
NBLK_A = 68
NQ = 17
EPS = 1e-6
IN_W = 4680
CH_Q, CH_K, CH_V, CH_QI, CH_KW, CH_U, CH_GA0, CH_GA1, CH_GB0, CH_GB1 = range(10)
CHUNKS = [(0, 512), (512, 512), (1024, 512), (1536, 512), (2048, 72), (2120, 512),
          (2632, 512), (3144, 512), (3656, 512), (4168, 512)]
N_DMA_SEMS = 12
NIT = 16


class Buf:
    def __init__(self, t, name):
        self.t = t
        self.name = name
        self.w = None
        self.r = {}

    def __getitem__(self, idx):
        return self.t[idx]


class KB:
    def __init__(self, nc, es, plan=None):
        self.nc = nc
        self.es = es
        self.plan = plan
        self.targets = {e: set() for e in ("pe", "dve", "act", "pool", "sp")}
        self.rank = None
        if plan is not None:
            self.rank = {e: {idx: r + 1 for r, idx in enumerate(sorted(plan[e]))} for e in plan}
        self.eng = {"pe": nc.tensor, "dve": nc.vector, "act": nc.scalar, "pool": nc.gpsimd, "sp": nc.sync}
        self.sem = {e: es.enter_context(nc.semaphore("s_" + e)) for e in self.eng}
        self.cnt = {e: 0 for e in self.eng}
        self.seen = {e: {} for e in self.eng}
        self.dsem = [es.enter_context(nc.semaphore("d_%d" % i)) for i in range(N_DMA_SEMS + 6)]
        self.dcnt = [0] * (N_DMA_SEMS + 6)
        self.drr = 0
        self.nbuf = 0

    def sb(self, shape, dt, name=None):
        self.nbuf += 1
        name = "sb_" + (name or ("%d" % self.nbuf))
        return Buf(self.es.enter_context(self.nc.sbuf_tensor(name, list(shape), dt)), name)

    def ps(self, shape, dt, name=None):
        self.nbuf += 1
        name = name or ("ps%d" % self.nbuf)
        return Buf(self.es.enter_context(self.nc.psum_tensor(name, list(shape), dt)), name)

    def dram(self, name, shape, dt, kind="Internal"):
        return Buf(self.nc.dram_tensor(name, list(shape), dt, kind=kind).ap(), name)

    def _deps(self, e, reads, writes):
        need = {}

        def add(tok):
            if tok is None:
                return
            key, sem, val = tok
            if key == "pe" and e == "pe":
                return
            if need.get(key, (None, 0))[1] < val:
                need[key] = (sem, val)

        for b in reads:
            add(b.w)
        for b in writes:
            add(b.w)
            for tok in b.r.values():
                add(tok)
        eo = self.eng[e]
        for key, (sem, val) in need.items():
            if self.seen[e].get(key, 0) < val:
                self.seen[e][key] = val
                if key in self.targets:
                    if self.plan is None:
                        self.targets[key].add(val)
                    else:
                        eo.wait_ge(sem, self.rank[key][val])
                elif self.plan is not None:
                    eo.wait_ge(sem, val)

    def _record(self, tok, reads, writes):
        for b in reads:
            if b.r.get(tok[0], (None, None, 0))[2] < tok[2]:
                b.r[tok[0]] = tok
        for b in writes:
            b.w = tok
            b.r = {}

    def op(self, e, fn, reads=(), writes=()):
        self._deps(e, reads, writes)
        self.cnt[e] += 1
        if self.plan is not None:
            ins = fn(self.eng[e])
            if self.cnt[e] in self.plan[e]:
                ins.then_inc(self.sem[e], 1)
        self._record((e, self.sem[e], self.cnt[e]), reads, writes)

    def dma(self, q, out, in_, reads=(), writes=(), indirect=None, own_sem=None):
        if own_sem is None:
            i = self.drr
            self.drr = (i + 1) % N_DMA_SEMS
        else:
            i = N_DMA_SEMS + own_sem
        eo = self.eng[q]
        key = "d%d" % i
        if own_sem is None and self.dcnt[i] > 0 and self.seen[q].get(key, 0) < 16 * self.dcnt[i]:
            if self.plan is not None:
                eo.wait_ge(self.dsem[i], 16 * self.dcnt[i])
            self.seen[q][key] = 16 * self.dcnt[i]
        self._deps(q, reads, writes)
        self.dcnt[i] += 1
        if self.plan is not None:
            if indirect is None:
                ins = eo.dma_start(out=out, in_=in_)
            else:
                ins = eo.indirect_dma_start(out=out, out_offset=None, in_=in_, in_offset=indirect)
            ins.then_inc(self.dsem[i], 16)
        self._record((key, self.dsem[i], 16 * self.dcnt[i]), reads, writes)

    def finish(self):
        eo = self.eng["sp"]
        if self.plan is None:
            for e in ("pe", "dve", "act", "pool"):
                if self.cnt[e] > 0:
                    self.targets[e].add(self.cnt[e])
            return
        for i in range(N_DMA_SEMS + 6):
            if self.dcnt[i] > 0:
                eo.wait_ge(self.dsem[i], 16 * self.dcnt[i])
        for e in ("pe", "dve", "act", "pool"):
            if self.cnt[e] > 0:
                eo.wait_ge(self.sem[e], self.rank[e][self.cnt[e]])


def build(cfg):
    _, kb_dry = _build(cfg, None)
    nc, _ = _build(cfg, kb_dry.targets)
    return nc


def _build(cfg, plan):
    nblk_a = cfg.get("nblk_a", NBLK_A)
    nq = cfg.get("nq", NQ)
    do_attn = cfg.get("attn", True)
    do_tail = cfg.get("tail", True)
    nkeys = nblk_a * 128
    nc = bass.Bass("TRN2", target_bir_lowering=False)
    es = ExitStack()
    kb = KB(nc, es, plan)
    nc._es_keep = es

    def din(name, shape, dt=F32):
        return Buf(nc.dram_tensor(name, list(shape), dt, kind="ExternalInput").ap(), name)

    def dout(name, shape, dt=F32):
        return Buf(nc.dram_tensor(name, list(shape), dt, kind="ExternalOutput").ap(), name)

    xcat = din("xcat", [nkeys, D])
    xown = din("xown", [nq, 128, D])
    xprev = din("xprev", [nq, 16, D])
    w_in = din("w_in", [D, IN_W])
    norm1_g = din("norm1_g", [D])
    q_norm_g = din("q_norm_g", [64])
    k_norm_g = din("k_norm_g", [64])
    ident_in = din("ident", [128, 128])
    rel_bias = din("rel_bias", [32, 8])
    qs_in = din("qs", [128, 128])
    thrtab_in = din("thrtab", [128, 155])
    cmask_in = din("cmask", [128, 512])
    pw_in = din("pw", [128, NIT])
    w_ba = din("w_ba", [512, D])
    w_bp = din("w_bp", [512, D])
    w_out = din("w_out", [D, D])
    peer_wq = din("peer_wq", [D, D])
    w_pool = din("w_pool", [4, 128, 128])
    pool_scale = din("pool_scale", [512])
    norm2_g = din("norm2_g", [D])
    subkeys = din("subkeys", [2, 128, 64])
    peer_u = din("peer_u", [16384, D])
    peer_v = din("peer_v", [16384, D])
    rcnt_in = din("rcnt", [128, 4, 128])
    iota16_in = din("iota16", [128, 16])
    k_own = dout("k_own", [nq, 128, 512])
    v_own = dout("v_own", [nq, 128, 512])
    ki_own = dout("ki_own", [nq, 128, 64])
    y_own = dout("y_own", [nq, 128, D])
    u_last = dout("u_last", [128, 4, 144])
    y_s_out = dout("y_s", [128, D])
    do_sample = cfg.get("sample", True)
    if do_sample:
        xs_in = din("xs_own", [128, D])
        cache_k = din("cache_k", [2560 * 128, 512])
        cache_v = din("cache_v", [2560 * 128, 512])
        cache_ik = din("cache_ik", [2560, 8192])
        state_in = din("state_own", [16, 15, 512])
        pt_in = din("pt_own", [16, 16], I32)
        zsel_in = din("zsel", [128, 31])
        k_s_out = dout("ks_o", [128, 512])
        v_s_out = dout("vs_o", [128, 512])
        ki_s_out = dout("kis_o", [128, 64])
        pool_s_out = dout("pool_s", [16, 15, 512])
        qi_d = kb.dram("qi_d", [16, 512], F32)
        wi_d = kb.dram("wi_d", [16, 8], F32)
        q_d = kb.dram("q_d", [16, 512], F32)
        sc_d = kb.dram("sc_d", [16, 2048], F32)
    wba_bf = kb.dram("wba_bf", [128, 4, D], BF16)
    wbp_bf = kb.dram("wbp_bf", [128, 4, D], BF16)
    wout_bf = kb.dram("wout_bf", [128, 8, D], BF16)
    pwq_bf = kb.dram("pwq_bf", [128, 8, D], BF16)
    pu_bf = kb.dram("pu_bf", [16384, D], BF16)
    pv_bf = kb.dram("pv_bf", [16384, D], BF16)
    win_bf = kb.dram("win_bf", [128, 8, IN_W], BF16)
    kt_s = kb.dram("kt_s", [128, 4, nkeys], BF16)
    v_s = kb.dram("v_s", [nkeys, 8, 65], BF16)
    kit_s = kb.dram("kit_s", [128, nkeys], BF16)

    ident_f = kb.sb([128, 128], F32, "ident_f")
    ident_b = kb.sb([128, 128], BF16, "ident_b")
    g1col = kb.sb([128, 8], F32, "g1col")
    qg = kb.sb([128, 64], F32, "qg")
    kg = kb.sb([128, 64], F32, "kg")
    kb.dma("sp", ident_f[:], ident_in[:, :], writes=[ident_f])
    kb.op("dve", lambda e: e.tensor_copy(out=ident_b[:], in_=ident_f[:]), reads=[ident_f], writes=[ident_b])
    ident4 = kb.sb([128, 512], BF16, "ident4")
    for j4 in range(4):
        kb.op("dve", lambda e, j4=j4: e.tensor_copy(out=ident4[:, j4 * 128:(j4 + 1) * 128], in_=ident_f[:]),
              reads=[ident_f, ident4], writes=[ident4])
    with nc.allow_non_contiguous_dma(reason="tiny param loads"):
        kb.dma("sp", g1col[:], norm1_g.t.rearrange("(k p) -> p k", p=128), writes=[g1col])
        kb.dma("sp", qg[:], q_norm_g.t.partition_broadcast(128), writes=[qg])
        kb.dma("sp", kg[:], k_norm_g.t.partition_broadcast(128), writes=[kg])
    kb.op("dve", lambda e: e.tensor_scalar(out=qg[:], in0=qg[:], scalar1=0.125, scalar2=None, op0=ALU.mult),
          reads=[qg], writes=[qg])

    pb = [kb.ps([128, 512], F32, "bank%d" % i) for i in range(8)]

    ktc = [kb.sb([128, 4, 512], BF16, "ktc%d" % j) for j in range(2)]
    vch = [kb.sb([128, 4, 520], BF16, "vch%d" % j) for j in range(2)]
    Isc = kb.sb([128, max(nkeys, 8704)], F32, "Isc")

    class View:
        def __init__(self, owner, ap):
            self.owner = owner
            self.ap = ap

        def __getitem__(self, idx):
            return self.ap[idx]

        @property
        def w(self):
            return self.owner.w

        @w.setter
        def w(self, v):
            self.owner.w = v

        @property
        def r(self):
            return self.owner.r

        @r.setter
        def r(self, v):
            self.owner.r = v

    wst_v = [View(ktc[j], ktc[j].t[:].rearrange("p a b -> p (a b)").bitcast(F32)[:, 0:1024]) for j in range(2)]
    wsb_v = [View(vch[j], vch[j].t[:].rearrange("p a b -> p (a b)")[:, 0:1024]) for j in range(2)]
    pcount = [0]

    def conv_w(src_ap, dst_ap, ncol, scal):
        s = pcount[0] % 2
        pcount[0] += 1
        kb.dma("sp", wst_v[s][:, 0:ncol], src_ap, writes=[wst_v[s].owner])
        kb.op("dve", lambda e: e.tensor_scalar(out=wsb_v[s][:, 0:ncol], in0=wst_v[s][:, 0:ncol], scalar1=scal,
                                               scalar2=None, op0=ALU.mult),
              reads=[wst_v[s].owner, g1col], writes=[wsb_v[s].owner])
        kb.dma("sp", dst_ap, wsb_v[s][:, 0:ncol], reads=[wsb_v[s].owner], writes=[win_bf])

    for kc in range(8):
        for pc in range(5):
            conv_w(w_in[kc * 128:(kc + 1) * 128, pc * 936:(pc + 1) * 936], win_bf[:, kc, pc * 936:(pc + 1) * 936],
                   936, g1col[:, kc:kc + 1])
    if do_tail:
        for (wsrc, wdst, nkc) in ((w_ba, wba_bf, 4), (w_bp, wbp_bf, 4), (w_out, wout_bf, 8), (peer_wq, pwq_bf, 8)):
            for kc in range(nkc):
                conv_w(wsrc[kc * 128:(kc + 1) * 128, :], wdst[:, kc, :], 1024, 1.0)

    wslot = [kb.sb([128, 8, 512], BF16, "wslot%d" % i) for i in range(2)]
    wstate = {"i": 0}

    def load_w(src, c0, cw, nk=8):
        s = wslot[wstate["i"] % 2]
        wstate["i"] += 1
        kb.dma("sp", s[:, 0:nk, 0:cw], src[:, 0:nk, c0:c0 + cw], reads=[src], writes=[s])
        return s

    xt = [kb.sb([128, D], F32, "xt%d" % i) for i in range(2)]
    xn = kb.sb([128, D], F32, "xn")
    junk = kb.sb([128, D], BF16, "junk")
    ssq = kb.sb([128, 1], F32, "ssq")
    rstd = kb.sb([128, 1], F32, "rstd")
    xs = kb.sb([128, D], BF16, "xs")
    hnT = kb.sb([128, 8, 128], BF16, "hnT")
    tp_bank = pb[2]

    def norm_transpose(xb, dstT, ntok=128, gtile=None, keep=None, xowner=None):
        xo = xowner or xb
        kb.op("act", lambda e: e.activation(out=junk[0:ntok, :], in_=xb[0:ntok, :], func=AF.Square,
                                            accum_out=ssq[0:ntok, :]),
              reads=[xo], writes=[junk, ssq])
        kb.op("act", lambda e: e.activation(out=rstd[0:ntok, :], in_=ssq[0:ntok, :], func=AF.Sqrt,
                                            scale=1.0 / D, bias=EPS),
              reads=[ssq], writes=[rstd])
        kb.op("dve", lambda e: e.reciprocal(out=rstd[0:ntok, :], in_=rstd[0:ntok, :]), reads=[rstd], writes=[rstd])
        if gtile is None:
            kb.op("dve", lambda e: e.tensor_scalar(out=xs[0:ntok, :], in0=xb[0:ntok, :], scalar1=rstd[0:ntok, :],
                                                   scalar2=None, op0=ALU.mult),
                  reads=[xo, rstd], writes=[xs])
        else:
            kb.op("dve", lambda e: e.scalar_tensor_tensor(out=keep[0:ntok, :], in0=xb[0:ntok, :],
                                                          scalar=rstd[0:ntok, :], in1=gtile[0:ntok, :],
                                                          op0=ALU.mult, op1=ALU.mult),
                  reads=[xo, rstd, gtile], writes=[keep])
            kb.op("dve", lambda e: e.tensor_copy(out=xs[0:ntok, :], in_=keep[0:ntok, :]), reads=[keep], writes=[xs])
        tpv = tp_bank.t[:].bitcast(BF16)
        for kc in range(8):
            kb.op("pe", lambda e, kc=kc: e.transpose(out=tpv[:, kc * 128:kc * 128 + ntok],
                                                     in_=xs[0:ntok, kc * 128:(kc + 1) * 128],
                                                     identity=ident_b[0:ntok, 0:ntok]),
                  reads=[xs, ident_b], writes=[tp_bank])
        kb.op("dve", lambda e: e.tensor_copy(
            out=dstT[:, :, 0:ntok], in_=tpv.rearrange("p (k t) -> p k t", k=8)[:, :, 0:ntok]),
            reads=[tp_bank], writes=[dstT])

    def proj_tok(dst_bank, wsl, cw, srcT=None, ntok=128):
        srcT = srcT or hnT
        for kc in range(8):
            kb.op("pe", lambda e, kc=kc: e.matmul(dst_bank[0:ntok, 0:cw], lhsT=srcT[:, kc, 0:ntok],
                                                  rhs=wsl[:, kc, 0:cw], start=(kc == 0), stop=(kc == 7)),
                  reads=[srcT, wsl], writes=[dst_bank])

    hsq = kb.sb([128, 512], F32, "hsq")
    hss = kb.sb([128, 8], F32, "hss")
    hrs = kb.sb([128, 8], F32, "hrs")

    def head_norm(src_bank, gain, dst):
        kb.op("act", lambda e: e.activation(out=hsq[:], in_=src_bank[:, 0:512], func=AF.Square),
              reads=[src_bank], writes=[hsq])
        kb.op("dve", lambda e: e.tensor_reduce(out=hss[:], in_=hsq[:].rearrange("p (h d) -> p h d", h=8),
                                               axis=AX.X, op=ALU.add),
              reads=[hsq], writes=[hss])
        kb.op("act", lambda e: e.activation(out=hrs[:], in_=hss[:], func=AF.Sqrt, scale=1.0 / 64, bias=EPS),
              reads=[hss], writes=[hrs])
        kb.op("dve", lambda e: e.reciprocal(out=hrs[:], in_=hrs[:]), reads=[hrs], writes=[hrs])
        kb.op("dve", lambda e: e.tensor_tensor(out=hsq[:].rearrange("p (h d) -> p h d", h=8),
                                               in0=src_bank[:, 0:512].rearrange("p (h d) -> p h d", h=8),
                                               in1=hrs[:].unsqueeze(2).to_broadcast([128, 8, 64]), op=ALU.mult),
              reads=[src_bank, hrs], writes=[hsq])
        kb.op("dve", lambda e: e.tensor_tensor(out=dst[:].rearrange("p (h d) -> p h d", h=8),
                                               in0=hsq[:].rearrange("p (h d) -> p h d", h=8),
                                               in1=gain[:].unsqueeze(1).to_broadcast([128, 8, 64]), op=ALU.mult),
              reads=[hsq, gain], writes=[dst])

    wk = wslot[0]
    wv = wslot[1]
    wkw = kb.sb([128, 8, 72], BF16, "wkw")
    kb.dma("sp", wk[:], win_bf[:, :, 512:1024], reads=[win_bf], writes=[wk])
    kb.dma("sp", wv[:], win_bf[:, :, 1024:1536], reads=[win_bf], writes=[wv])
    kb.dma("sp", wkw[:], win_bf[:, :, 2048:2120], reads=[win_bf], writes=[wkw])
    kf = kb.sb([128, 512], F32, "kf")
    kbf = kb.sb([128, 512], BF16, "kbf")
    ktb = kb.sb([128, 4, 128], BF16, "ktb")
    vb = kb.sb([128, 8, 65], BF16, "vb")
    kib = kb.sb([128, 128], BF16, "kib")
    kitb = kb.sb([128, 128], BF16, "kitb")
    kb.op("dve", lambda e: e.memset(vb[:], 1.0), writes=[vb])
    def table_conv_steps():
        it = 0
        for (tsrc, tdst) in ((peer_u, pu_bf), (peer_v, pv_bf)):
            for c in range(32):
                half = it % 2
                it += 1
                fst = Isc.t[:, half * 4096:(half + 1) * 4096]
                bst = (ktc if half == 0 else vch)
                kb.dma("sp", fst, tsrc[c * 512:(c + 1) * 512, :].rearrange("(p r) d -> p (r d)", p=128),
                       writes=[Isc])
                for j2 in range(2):
                    bv = bst[j2].t[:].rearrange("p a b -> p (a b)")[:, 0:2048]
                    kb.op("act", lambda e, bv=bv, fst=fst, j2=j2: e.copy(out=bv, in_=fst[:, j2 * 2048:(j2 + 1) * 2048]),
                          reads=[Isc], writes=[bst[j2]])
                    kb.dma("sp", tdst[c * 512:(c + 1) * 512, :].rearrange("(p r) d -> p r d", p=128)[:, 2 * j2:2 * j2 + 2, :],
                           bv.rearrange("p (r d) -> p r d", r=2), reads=[bst[j2]], writes=[tdst])
                yield

    tconv = table_conv_steps() if (do_tail and do_attn) else iter(())
    for blk in range(nblk_a):
        next(tconv, None)
        xb = xt[blk % 2]
        kb.dma("sp", xb[:], xcat[blk * 128:(blk + 1) * 128, :], writes=[xb])
        norm_transpose(xb, hnT)
        proj_tok(pb[0], wk, 512)
        head_norm(pb[0], kg, kf)
        kb.op("dve", lambda e: e.tensor_copy(out=kbf[:], in_=kf[:]), reads=[kf], writes=[kbf])
        tpv = tp_bank.t[:].bitcast(BF16)
        for pr in range(4):
            kb.op("pe", lambda e, pr=pr: e.transpose(out=tpv[:, pr * 128:(pr + 1) * 128],
                                                     in_=kbf[:, pr * 128:(pr + 1) * 128], identity=ident_b[:]),
                  reads=[kbf, ident_b], writes=[tp_bank])
        kb.op("act", lambda e: e.copy(out=ktb[:], in_=tpv[:, 0:512].rearrange("p (a t) -> p a t", a=4)),
              reads=[tp_bank], writes=[ktb])
        kb.dma("sp", kt_s[:, :, blk * 128:(blk + 1) * 128], ktb[:], reads=[ktb], writes=[kt_s])
        proj_tok(pb[1], wv, 512)
        kb.op("act", lambda e: e.copy(out=vb[:, :, 0:64], in_=pb[1][:, 0:512].rearrange("p (h d) -> p h d", h=8)),
              reads=[pb[1]], writes=[vb])
        kb.dma("sp", v_s[blk * 128:(blk + 1) * 128, :, :], vb[:], reads=[vb], writes=[v_s])
        proj_tok(pb[0], wkw, 72)
        kb.op("dve", lambda e: e.tensor_copy(out=kib[:, 0:64], in_=pb[0][:, 0:64]), reads=[pb[0]], writes=[kib])
        kb.op("dve", lambda e: e.tensor_copy(out=kib[:, 64:128], in_=pb[0][:, 0:64]), reads=[pb[0]], writes=[kib])
        kb.op("pe", lambda e: e.transpose(out=tpv[:, 512:640], in_=kib[:], identity=ident_b[:]),
              reads=[kib, ident_b], writes=[tp_bank])
        kb.op("act", lambda e: e.copy(out=kitb[:], in_=tpv[:, 512:640]), reads=[tp_bank], writes=[kitb])
        kb.dma("sp", kit_s[:, blk * 128:(blk + 1) * 128], kitb[:], reads=[kitb], writes=[kit_s])

    for _ in tconv:
        pass
    ko = kb.sb([128, 512], F32, "ko")
    vo = kb.sb([128, 512], F32, "vo")
    kio = kb.sb([128, 64], F32, "kio")
    wis = kb.sb([128, 8], F32, "wis")
    dbg_ao = dout("dbg_ao", [nq, 128, 512]) if cfg.get("dbg") else None
    if do_attn:
        qf = kb.sb([128, 512], F32, "qf")
        qbf = kb.sb([128, 512], BF16, "qbf")
        qT2 = kb.sb([128, 4, 128], BF16, "qT2")
        qiT2 = kb.sb([128, 4, 128], BF16, "qiT2")
        kitc = [kb.sb([128, 512], BF16, "kitc%d" % j) for j in range(2)]
        rl = [kb.sb([128, 512], BF16, "rl%d" % j) for j in range(2)]
        junkI = kb.sb([128, 2176], BF16, "junkI")
        cnt4 = kb.sb([128, 4], F32, "cnt4")
        nmid = kb.sb([128, 1], F32, "nmid")
        hi0 = kb.sb([128, 1], F32, "hi0")
        lo = kb.sb([128, 1], F32, "lo")
        mid = kb.sb([128, 1], F32, "mid")
        cntt = kb.sb([128, 1], F32, "cntt")
        dl = kb.sb([128, 1], F32, "dl")
        wh = kb.sb([128, NIT], F32, "wh")
        pw = kb.sb([128, NIT], F32, "pw")
        cmask = kb.sb([128, 512], F32, "cmask")
        negm = [kb.sb([128, 128], BF16, "negm%d" % j) for j in range(2)]
        addh = kb.sb([128, 8, 128], BF16, "addh")
        PT = [kb.sb([128, 8, 128], BF16, "PT%d" % j) for j in range(2)]
        rden = kb.sb([128, 8], F32, "rden")
        ao = kb.sb([128, 512], BF16, "ao")
        aof = kb.sb([128, 512], F32, "aof") if cfg.get("dbg") else None
        kb.dma("sp", pw[:], pw_in[:, :], writes=[pw])
        kb.dma("sp", cmask[:], cmask_in[:, :], writes=[cmask])
        qs = kb.sb([128, 128], F32, "qs")
        thrtab = kb.sb([128, 155], F32, "thrtab")
        rbb = kb.sb([128, 256], F32, "rbb")
        ndel = kb.sb([128, 256], F32, "ndel")
        ind = kb.sb([128, 128], F32, "ind")
        NB = [kb.sb([128, 8, 128], BF16, "NB%d" % j) for j in range(5)]
        NBt = kb.sb([128, 8, 128], F32, "h2")
        h2 = View(NBt, NBt.t[:].rearrange("p a b -> p (a b)"))
        kb.dma("sp", qs[:], qs_in[:, :], writes=[qs])
        kb.dma("sp", thrtab[:], thrtab_in[:, :], writes=[thrtab])
        with nc.allow_non_contiguous_dma(reason="tiny param loads"):
            kb.dma("sp", rbb[:], rel_bias.t.rearrange("b h -> (b h)").partition_broadcast(128), writes=[rbb])
        kb.op("dve", lambda e: e.tensor_tensor(out=ndel[:, 8:256], in0=rbb[:, 0:248], in1=rbb[:, 8:256],
                                               op=ALU.subtract), reads=[rbb], writes=[ndel])
        for r5 in range(5):
            kb.op("dve", lambda e: e.memset(NBt[:], 0.0), writes=[NBt])
            for b in range(1, 32):
                col = r5 * 31 + (b - 1)
                kb.op("dve", lambda e, col=col: e.tensor_scalar(out=ind[:], in0=qs[:], scalar1=thrtab[:, col:col + 1],
                                                                scalar2=None, op0=ALU.is_lt),
                      reads=[qs, thrtab], writes=[ind])
                for h in range(8):
                    kb.op("dve", lambda e, b=b, h=h: e.scalar_tensor_tensor(
                        out=NBt[:, h, :], in0=ind[:], scalar=ndel[:, b * 8 + h:b * 8 + h + 1], in1=NBt[:, h, :],
                        op0=ALU.mult, op1=ALU.add), reads=[ind, ndel, NBt], writes=[NBt])
            kb.op("dve", lambda e, r5=r5: e.tensor_copy(out=NB[r5][:], in_=NBt[:]), reads=[NBt], writes=[NB[r5]])
    if do_tail:
        aoT = kb.sb([128, 4, 128], BF16, "aoT")
        hnTp = kb.sb([128, 8, 16], BF16, "hnTp")
        xpv = kb.sb([16, D], F32, "xpv")
        uT = kb.sb([128, 4, 144], F32, "uT")
        s1 = kb.sb([128, 4, 144], F32, "s1")
        s2 = kb.sb([128, 4, 144], F32, "s2")
        s3 = kb.sb([128, 4, 144], F32, "s3")
        pmf = kb.sb([128, 4, 128], F32, "pmf")
        pmT = kb.sb([128, 4, 128], BF16, "pmT")
        poT = kb.sb([128, 4, 128], BF16, "poT")
        sga = kb.sb([128, 8, 128], BF16, "sga")
        sgb = kb.sb([128, 8, 128], BF16, "sgb")
        tmpA = kb.sb([128, 512], F32, "tmpA")
        tmpB = kb.sb([128, 512], F32, "tmpB")
        mT = kb.sb([128, 8, 128], BF16, "mT")
        xnT = sgb
        qpT = sga
        g2b = kb.sb([128, D], F32, "g2b")
        wpl = kb.sb([128, 4, 128], BF16, "wpl")
        wplf = kb.sb([128, 4, 128], F32, "wplf")
        pscol = kb.sb([128, 4], F32, "pscol")
        SKf = kb.sb([128, 256], F32, "SKf")
        sktmp = kb.sb([128, 128], F32, "sktmp")
        SK = kb.sb([128, 256], BF16, "SK")
        rcnt = kb.sb([128, 4, 128], F32, "rcnt")
        iota16 = kb.sb([128, 16], F32, "iota16")
        tv = kb.sb([128, 8, 2, 16], F32, "tv")
        ti = kb.sb([128, 8, 2, 16], U32, "ti")
        tif = kb.sb([128, 8, 2, 16], F32, "tif")
        tsv = kb.sb([128, 8, 16], F32, "tsv")
        tpos = kb.sb([128, 8, 16], U32, "tpos")
        pa = kb.sb([128, 8, 16], U32, "pa")
        pbb = kb.sb([128, 8, 16], U32, "pbb")
        paf = kb.sb([128, 8, 16], F32, "paf")
        pbf = kb.sb([128, 8, 16], F32, "pbf")
        i1s = kb.sb([128, 8, 16], F32, "i1s")
        i2s = kb.sb([128, 8, 16], F32, "i2s")
        eidf = kb.sb([128, 128], F32, "eidf")
        eidi = kb.sb([128, 128], I32, "eidi")
        gmx = kb.sb([128, 8], F32, "gmx")
        gk = kb.sb([128, 8, 16], F32, "gk")
        actv = kb.sb([128, 128], F32, "actv")
        wgt = kb.sb([128, 128], F32, "wgt")
        m8 = kb.sb([128, 8], F32, "m8")
        UbS = [View(o, o.t[:].rearrange("p a b -> p (a b)").bitcast(F32)[:, 0:1024]) for o in (ktc[0], ktc[1], vch[0], vch[1])]
        Ubo = [kb.sb([128, D], BF16, "Ub%d" % j) for j in range(6)]
        Ub = [View(o, o.t[:]) for o in Ubo]
        kb.dma("sp", rcnt[:], rcnt_in[:, :, :], writes=[rcnt])
        kb.dma("sp", iota16[:], iota16_in[:, :], writes=[iota16])
        kb.op("dve", lambda e: e.memset(SKf[:], 0.0), writes=[SKf])
        with nc.allow_non_contiguous_dma(reason="small param loads"):
            kb.dma("sp", g2b[:], norm2_g.t.partition_broadcast(128), writes=[g2b])
            kb.dma("sp", pscol[:], pool_scale.t.rearrange("(g d) -> d g", d=128), writes=[pscol])
            kb.dma("sp", wplf[:], w_pool.t.rearrange("g c d -> c g d"), writes=[wplf])
            kb.dma("sp", sktmp[:, 0:64], subkeys.t[0], writes=[sktmp])
            kb.dma("sp", sktmp[:, 64:128], subkeys.t[1], writes=[sktmp])
        kb.op("pe", lambda e: e.transpose(out=pb[2][:, 0:128], in_=sktmp[:], identity=ident_f[:]),
              reads=[sktmp, ident_f], writes=[pb[2]])
        kb.op("dve", lambda e: e.tensor_copy(out=SKf[0:64, 0:128], in_=pb[2][0:64, 0:128]), reads=[pb[2], SKf], writes=[SKf])
        kb.op("dve", lambda e: e.tensor_copy(out=SKf[64:128, 128:256], in_=pb[2][64:128, 0:128]), reads=[pb[2], SKf],
              writes=[SKf])
        kb.op("dve", lambda e: e.tensor_copy(out=SK[:], in_=SKf[:]), reads=[SKf], writes=[SK])
        kb.op("dve", lambda e: e.tensor_copy(out=wpl[:], in_=wplf[:]), reads=[wplf], writes=[wpl])
        IscV = Isc.t[:]
        ssc = IscV[:, 0:2048]
        sscw = IscV[:, 2048:4096]
        cand = IscV[:, 4096:6144]
        candw = IscV[:, 6144:8192]
        ohv = IscV[:, 0:2048]
        ohv2 = IscV[:, 2048:4096]

    def tail_block(i, xb, sample=False):
        tpv = tp_bank.t[:].bitcast(BF16)
        for pr in range(4):
            kb.op("pe", lambda e, pr=pr: e.transpose(out=tpv[:, pr * 128:(pr + 1) * 128],
                                                     in_=ao[:, pr * 128:(pr + 1) * 128], identity=ident_b[:]),
                  reads=[ao, ident_b], writes=[tp_bank])
        kb.op("act", lambda e: e.copy(out=aoT[:], in_=tpv[:, 0:512].rearrange("p (a t) -> p a t", a=4)),
              reads=[tp_bank], writes=[aoT])
        if not sample:
            kb.dma("sp", xpv[:], xprev[i, :, :], writes=[xpv])
            norm_transpose(xpv, hnTp, ntok=16)
            wsl = load_w(win_bf, *CHUNKS[CH_U])
            for g in range(4):
                bk = pb[g // 2]
                c0 = (g % 2) * 144
                for kc in range(8):
                    kb.op("pe", lambda e, bk=bk, c0=c0, g=g, kc=kc, wsl=wsl: e.matmul(
                        bk[:, c0:c0 + 16], lhsT=wsl[:, kc, g * 128:(g + 1) * 128], rhs=hnTp[:, kc, :],
                        start=(kc == 0), stop=(kc == 7)), reads=[wsl, hnTp], writes=[bk])
                for kc in range(8):
                    kb.op("pe", lambda e, bk=bk, c0=c0, g=g, kc=kc, wsl=wsl: e.matmul(
                        bk[:, c0 + 16:c0 + 144], lhsT=wsl[:, kc, g * 128:(g + 1) * 128], rhs=hnT[:, kc, :],
                        start=(kc == 0), stop=(kc == 7)), reads=[wsl, hnT], writes=[bk])
            for hf in range(2):
                kb.op("act", lambda e, hf=hf: e.copy(out=uT[:, 2 * hf:2 * hf + 2, :],
                                                     in_=pb[hf][:, 0:288].rearrange("p (g t) -> p g t", g=2)),
                      reads=[pb[hf]], writes=[uT])
            if i == nq - 1:
                kb.dma("sp", u_last[:, :, :], uT[:], reads=[uT], writes=[u_last])
            kb.op("dve", lambda e: e.tensor_tensor(out=s1[:, :, 1:144], in0=uT[:, :, 1:144], in1=uT[:, :, 0:143],
                                                   op=ALU.add), reads=[uT], writes=[s1])
            kb.op("dve", lambda e: e.tensor_tensor(out=s2[:, 1:4, 3:144], in0=s1[:, 1:4, 3:144], in1=s1[:, 1:4, 1:142],
                                                   op=ALU.add), reads=[s1], writes=[s2])
            kb.op("dve", lambda e: e.tensor_tensor(out=s3[:, 2:4, 7:144], in0=s2[:, 2:4, 7:144], in1=s2[:, 2:4, 3:140],
                                                   op=ALU.add), reads=[s2], writes=[s3])
            kb.op("dve", lambda e: e.tensor_tensor(out=s1[:, 3, 15:144], in0=s3[:, 3, 15:144], in1=s3[:, 3, 7:136],
                                                   op=ALU.add), reads=[s3, s1], writes=[s1])
            wsum = [s1[:, 0, 16:144], s2[:, 1, 16:144], s3[:, 2, 16:144], s1[:, 3, 16:144]]
            wsrc = [s1, s2, s3, s1]
            for g in range(4):
                if i == 0:
                    kb.op("dve", lambda e, g=g: e.tensor_tensor(out=pmf[:, g, :], in0=wsum[g], in1=rcnt[:, g, :],
                                                                op=ALU.mult), reads=[wsrc[g], rcnt], writes=[pmf])
                    kb.op("dve", lambda e, g=g: e.tensor_tensor(out=pmT[:, g, :], in0=pmf[:, g, :], in1=uT[:, g, 16:144],
                                                                op=ALU.subtract), reads=[pmf, uT], writes=[pmT])
                else:
                    kb.op("dve", lambda e, g=g: e.scalar_tensor_tensor(
                        out=pmT[:, g, :], in0=wsum[g], scalar=1.0 / (2 ** (g + 1)), in1=uT[:, g, 16:144],
                        op0=ALU.mult, op1=ALU.subtract), reads=[wsrc[g], uT], writes=[pmT])
        for g in range(4):
            kb.op("pe", lambda e, g=g: e.matmul(pb[0][:, g * 128:(g + 1) * 128], lhsT=wpl[:, g, :], rhs=pmT[:, g, :],
                                                start=True, stop=True), reads=[wpl, pmT], writes=[pb[0]])
        kb.op("dve", lambda e: e.tensor_tensor(out=poT[:], in0=pb[0][:, 0:512].rearrange("p (g t) -> p g t", g=4),
                                               in1=pscol[:].unsqueeze(2).to_broadcast([128, 4, 128]), op=ALU.mult),
              reads=[pb[0], pscol], writes=[poT])
        for (chs, dst) in (((CH_GA0, CH_GA1), sga), ((CH_GB0, CH_GB1), sgb)):
            for hf, chn in enumerate(chs):
                wsl = load_w(win_bf, *CHUNKS[chn])
                bk = pb[hf]
                for ft in range(4):
                    for kc in range(8):
                        kb.op("pe", lambda e, bk=bk, ft=ft, kc=kc, wsl=wsl: e.matmul(
                            bk[:, ft * 128:(ft + 1) * 128], lhsT=wsl[:, kc, ft * 128:(ft + 1) * 128], rhs=hnT[:, kc, :],
                            start=(kc == 0), stop=(kc == 7)), reads=[wsl, hnT], writes=[bk])
                kb.op("act", lambda e, bk=bk, dst=dst, hf=hf: e.activation(
                    out=dst[:, 4 * hf:4 * hf + 4, :], in_=bk[:, 0:512].rearrange("p (f t) -> p f t", f=4),
                    func=AF.Sigmoid), reads=[bk], writes=[dst])
        for hf in range(2):
            wa = load_w(wba_bf, hf * 512, 512, nk=4)
            wb = load_w(wbp_bf, hf * 512, 512, nk=4)
            for ft in range(4):
                for kc in range(4):
                    kb.op("pe", lambda e, ft=ft, kc=kc, wa=wa: e.matmul(
                        pb[0][:, ft * 128:(ft + 1) * 128], lhsT=wa[:, kc, ft * 128:(ft + 1) * 128], rhs=aoT[:, kc, :],
                        start=(kc == 0), stop=(kc == 3)), reads=[wa, aoT], writes=[pb[0]])
                for kc in range(4):
                    kb.op("pe", lambda e, ft=ft, kc=kc, wb=wb: e.matmul(
                        pb[1][:, ft * 128:(ft + 1) * 128], lhsT=wb[:, kc, ft * 128:(ft + 1) * 128], rhs=poT[:, kc, :],
                        start=(kc == 0), stop=(kc == 3)), reads=[wb, poT], writes=[pb[1]])
            kb.op("dve", lambda e, hf=hf: e.tensor_tensor(
                out=tmpA[:], in0=pb[0][:, 0:512], in1=sga[:, 4 * hf:4 * hf + 4, :].rearrange("p f t -> p (f t)"),
                op=ALU.mult), reads=[pb[0], sga], writes=[tmpA])
            kb.op("dve", lambda e, hf=hf: e.tensor_tensor(
                out=tmpB[:], in0=pb[1][:, 0:512], in1=sgb[:, 4 * hf:4 * hf + 4, :].rearrange("p f t -> p (f t)"),
                op=ALU.mult), reads=[pb[1], sgb], writes=[tmpB])
            kb.op("dve", lambda e, hf=hf: e.tensor_tensor(
                out=mT[:, 4 * hf:4 * hf + 4, :].rearrange("p f t -> p (f t)"), in0=tmpA[:], in1=tmpB[:], op=ALU.add),
                reads=[tmpA, tmpB], writes=[mT])
        for hf in range(2):
            wo = load_w(wout_bf, hf * 512, 512, nk=8)
            for kc in range(8):
                kb.op("pe", lambda e, kc=kc, wo=wo, hf=hf: e.matmul(
                    pb[hf][:, 0:512], lhsT=mT[:, kc, :], rhs=wo[:, kc, :], start=(kc == 0), stop=(kc == 7)),
                    reads=[mT, wo], writes=[pb[hf]])
            kb.op("dve", lambda e, hf=hf: e.tensor_tensor(out=h2[:, hf * 512:(hf + 1) * 512], in0=pb[hf][:, 0:512],
                                                          in1=xb[:, hf * 512:(hf + 1) * 512], op=ALU.add),
                  reads=[pb[hf], xb], writes=[NBt])
        norm_transpose(h2, xnT, gtile=g2b, keep=xn, xowner=NBt)
        for hf in range(2):
            wq_ = load_w(pwq_bf, hf * 512, 512, nk=8)
            for ft in range(4):
                for kc in range(8):
                    kb.op("pe", lambda e, ft=ft, kc=kc, wq_=wq_, hf=hf: e.matmul(
                        pb[hf][:, ft * 128:(ft + 1) * 128], lhsT=wq_[:, kc, ft * 128:(ft + 1) * 128], rhs=xnT[:, kc, :],
                        start=(kc == 0), stop=(kc == 7)), reads=[wq_, xnT], writes=[pb[hf]])
            kb.op("act", lambda e, hf=hf: e.copy(out=qpT[:, 4 * hf:4 * hf + 4, :],
                                                 in_=pb[hf][:, 0:512].rearrange("p (f t) -> p f t", f=4)),
                  reads=[pb[hf]], writes=[qpT])
        sbanks = [pb[0], pb[1], pb[3], pb[4]]
        for h in range(8):
            bk = sbanks[h // 2]
            kb.op("pe", lambda e, bk=bk, h=h: e.matmul(bk[:, (h % 2) * 256:(h % 2) * 256 + 256], lhsT=qpT[:, h, :],
                                                      rhs=SK[:], start=True, stop=True),
                  reads=[qpT, SK], writes=[bk])
        for j4 in range(4):
            kb.op("act", lambda e, j4=j4: e.copy(out=ssc[:, j4 * 512:(j4 + 1) * 512], in_=sbanks[j4][:, 0:512]),
                  reads=[sbanks[j4]], writes=[Isc])
        tvv = tv[:].rearrange("p h s k -> p (h s) k")
        tiv = ti[:].rearrange("p h s k -> p (h s) k")
        for gi in range(16):
            sv = ssc[:, gi * 128:(gi + 1) * 128]
            sw = sscw[:, gi * 128:(gi + 1) * 128]
            kb.op("dve", lambda e, sv=sv, gi=gi: e.max(out=tvv[:, gi, 0:8], in_=sv), reads=[Isc], writes=[tv])
            kb.op("dve", lambda e, sv=sv, gi=gi: e.max_index(out=tiv[:, gi, 0:8], in_max=tvv[:, gi, 0:8], in_values=sv),
                  reads=[Isc, tv], writes=[ti])
            kb.op("dve", lambda e, sv=sv, sw=sw, gi=gi: e.match_replace(out=sw, in_to_replace=tvv[:, gi, 0:8],
                                                                       in_values=sv, imm_value=-1e30),
                  reads=[Isc, tv], writes=[Isc])
            kb.op("dve", lambda e, sw=sw, gi=gi: e.max(out=tvv[:, gi, 8:16], in_=sw), reads=[Isc], writes=[tv])
            kb.op("dve", lambda e, sw=sw, gi=gi: e.max_index(out=tiv[:, gi, 8:16], in_max=tvv[:, gi, 8:16], in_values=sw),
                  reads=[Isc, tv], writes=[ti])
        kb.op("dve", lambda e: e.tensor_copy(out=tif[:], in_=ti[:]), reads=[ti], writes=[tif])
        candv = cand.rearrange("p (h a b) -> p h a b", h=8, a=16)
        kb.op("dve", lambda e: e.tensor_tensor(out=candv, in0=tv[:, :, 0, :].unsqueeze(3).to_broadcast([128, 8, 16, 16]),
                                               in1=tv[:, :, 1, :].unsqueeze(2).to_broadcast([128, 8, 16, 16]), op=ALU.add),
              reads=[tv], writes=[Isc])
        for h in range(8):
            cv = cand[:, h * 256:(h + 1) * 256]
            cw = candw[:, h * 256:(h + 1) * 256]
            kb.op("dve", lambda e, cv=cv, h=h: e.max(out=tsv[:, h, 0:8], in_=cv), reads=[Isc], writes=[tsv])
            kb.op("dve", lambda e, cv=cv, h=h: e.max_index(out=tpos[:, h, 0:8], in_max=tsv[:, h, 0:8], in_values=cv),
                  reads=[Isc, tsv], writes=[tpos])
            kb.op("dve", lambda e, cv=cv, cw=cw, h=h: e.match_replace(out=cw, in_to_replace=tsv[:, h, 0:8],
                                                                     in_values=cv, imm_value=-1e30),
                  reads=[Isc, tsv], writes=[Isc])
            kb.op("dve", lambda e, cw=cw, h=h: e.max(out=tsv[:, h, 8:16], in_=cw), reads=[Isc], writes=[tsv])
            kb.op("dve", lambda e, cw=cw, h=h: e.max_index(out=tpos[:, h, 8:16], in_max=tsv[:, h, 8:16], in_values=cw),
                  reads=[Isc, tsv], writes=[tpos])
        kb.op("dve", lambda e: e.tensor_single_scalar(out=pa[:], in_=tpos[:], scalar=4, op=ALU.logical_shift_right),
              reads=[tpos], writes=[pa])
        kb.op("dve", lambda e: e.tensor_single_scalar(out=pbb[:], in_=tpos[:], scalar=15, op=ALU.bitwise_and),
              reads=[tpos], writes=[pbb])
        kb.op("dve", lambda e: e.tensor_copy(out=paf[:], in_=pa[:]), reads=[pa], writes=[paf])
        kb.op("dve", lambda e: e.tensor_copy(out=pbf[:], in_=pbb[:]), reads=[pbb], writes=[pbf])
        oh4 = ohv.rearrange("p (h k a) -> p h k a", h=8, k=16)
        oh42 = ohv2.rearrange("p (h k a) -> p h k a", h=8, k=16)
        io4 = iota16[:].unsqueeze(1).unsqueeze(1).to_broadcast([128, 8, 16, 16])
        for (pf, half, dst) in ((paf, 0, i1s), (pbf, 1, i2s)):
            kb.op("dve", lambda e, pf=pf: e.tensor_tensor(out=oh4, in0=pf[:].unsqueeze(3).to_broadcast([128, 8, 16, 16]),
                                                          in1=io4, op=ALU.is_equal),
                  reads=[pf, iota16], writes=[Isc])
            kb.op("dve", lambda e, half=half: e.tensor_tensor(
                out=oh42, in0=oh4, in1=tif[:, :, half, :].unsqueeze(2).to_broadcast([128, 8, 16, 16]), op=ALU.mult),
                reads=[Isc, tif], writes=[Isc])
            kb.op("dve", lambda e, dst=dst: e.tensor_reduce(out=dst[:], in_=oh42, axis=AX.X, op=ALU.add),
                  reads=[Isc], writes=[dst])
        kb.op("dve", lambda e: e.scalar_tensor_tensor(out=eidf[:].rearrange("p (h k) -> p h k", h=8), in0=i1s[:],
                                                      scalar=128.0, in1=i2s[:], op0=ALU.mult, op1=ALU.add),
              reads=[i1s, i2s], writes=[eidf])
        kb.op("dve", lambda e: e.tensor_copy(out=eidi[:], in_=eidf[:]), reads=[eidf], writes=[eidi])
        kb.op("dve", lambda e: e.tensor_reduce(out=gmx[:], in_=tsv[:], axis=AX.X, op=ALU.max), reads=[tsv], writes=[gmx])
        kb.op("dve", lambda e: e.tensor_tensor(out=gk[:], in0=tsv[:], in1=gmx[:].unsqueeze(2).to_broadcast([128, 8, 16]),
                                               op=ALU.subtract), reads=[tsv, gmx], writes=[gk])
        kb.op("act", lambda e: e.activation(out=gk[:], in_=gk[:], func=AF.Exp), reads=[gk], writes=[gk])
        kb.op("dve", lambda e: e.tensor_reduce(out=gmx[:], in_=gk[:], axis=AX.X, op=ALU.add), reads=[gk], writes=[gmx])
        kb.op("dve", lambda e: e.reciprocal(out=gmx[:], in_=gmx[:]), reads=[gmx], writes=[gmx])
        kb.op("dve", lambda e: e.tensor_tensor(out=gk[:], in0=gk[:], in1=gmx[:].unsqueeze(2).to_broadcast([128, 8, 16]),
                                               op=ALU.mult), reads=[gk, gmx], writes=[gk])
        def peer_steps():
            for hk in range(128):
                ub = Ub[hk % 6]
                kb.dma("pool", ub[:], pu_bf[:, :], reads=[eidi, pu_bf], writes=[ub.owner],
                       indirect=bass.IndirectOffsetOnAxis(ap=eidi[:, hk:hk + 1], axis=0), own_sem=hk % 6)
                kb.op("dve", lambda e, ub=ub, hk=hk: e.scalar_tensor_tensor(
                    out=ub[:], in0=ub[:], scalar=1.0, in1=xn[:], op0=ALU.mult, op1=ALU.mult,
                    accum_out=actv[:, hk:hk + 1]), reads=[ub.owner, xn], writes=[ub.owner, actv])
                yield
            kb.op("act", lambda e: e.activation(out=wgt[:], in_=actv[:], func=AF.Gelu), reads=[actv], writes=[wgt])
            kb.op("dve", lambda e: e.tensor_tensor(out=wgt[:], in0=wgt[:], in1=gk[:].rearrange("p h k -> p (h k)"),
                                                   op=ALU.mult), reads=[wgt, gk], writes=[wgt])
            for hk in range(128):
                ub = Ub[(2 + hk) % 6]
                kb.dma("pool", ub[:], pv_bf[:, :], reads=[eidi, pv_bf], writes=[ub.owner],
                       indirect=bass.IndirectOffsetOnAxis(ap=eidi[:, hk:hk + 1], axis=0), own_sem=(2 + hk) % 6)
                kb.op("dve", lambda e, ub=ub, hk=hk: e.scalar_tensor_tensor(
                    out=h2[:], in0=ub[:], scalar=wgt[:, hk:hk + 1], in1=h2[:], op0=ALU.mult, op1=ALU.add),
                    reads=[ub.owner, wgt, NBt], writes=[NBt])
                yield
            if sample:
                kb.dma("sp", y_s_out[:, :], h2[:], reads=[NBt], writes=[y_s_out])
            else:
                kb.dma("sp", y_own[i, :, :], h2[:], reads=[NBt], writes=[y_own])
            yield
        return peer_steps()

    pending = [None]
    nslots = [1]

    def advance(n=1):
        g = pending[0]
        if g is None:
            return
        for _ in range(n):
            try:
                next(g)
            except StopIteration:
                pending[0] = None
                return

    def drain():
        while pending[0] is not None:
            advance(64)

    for i in range(nq):
        xb = xt[i % 2]
        kb.dma("sp", xb[:], xown[i, :, :], writes=[xb])
        norm_transpose(xb, hnT)
        wsl = load_w(win_bf, *CHUNKS[CH_K])
        proj_tok(pb[0], wsl, 512)
        head_norm(pb[0], kg, ko)
        kb.dma("sp", k_own[i, :, :], ko[:], reads=[ko], writes=[k_own])
        wsl = load_w(win_bf, *CHUNKS[CH_V])
        proj_tok(pb[1], wsl, 512)
        kb.op("act", lambda e: e.copy(out=vo[:], in_=pb[1][:, 0:512]), reads=[pb[1]], writes=[vo])
        kb.dma("sp", v_own[i, :, :], vo[:], reads=[vo], writes=[v_own])
        wsl = load_w(win_bf, *CHUNKS[CH_KW])
        proj_tok(pb[0], wsl, 72)
        kb.op("dve", lambda e: e.tensor_copy(out=kio[:], in_=pb[0][:, 0:64]), reads=[pb[0]], writes=[kio])
        kb.dma("sp", ki_own[i, :, :], kio[:], reads=[kio], writes=[ki_own])
        kb.op("dve", lambda e: e.tensor_scalar(out=wis[:], in0=pb[0][:, 64:72], scalar1=0.125 * (8 ** -0.5),
                                               scalar2=None, op0=ALU.mult), reads=[pb[0]], writes=[wis])
        if not do_attn:
            continue
        tpv = tp_bank.t[:].bitcast(BF16)
        wsl = load_w(win_bf, *CHUNKS[CH_Q])
        proj_tok(pb[0], wsl, 512)
        head_norm(pb[0], qg, qf)
        kb.op("dve", lambda e: e.tensor_copy(out=qbf[:], in_=qf[:]), reads=[qf], writes=[qbf])
        for pr in range(4):
            kb.op("pe", lambda e, pr=pr: e.transpose(out=tpv[:, pr * 128:(pr + 1) * 128],
                                                     in_=qbf[:, pr * 128:(pr + 1) * 128], identity=ident_b[:]),
                  reads=[qbf, ident_b], writes=[tp_bank])
        kb.op("act", lambda e: e.copy(out=qT2[:], in_=tpv[:, 0:512].rearrange("p (a t) -> p a t", a=4)),
              reads=[tp_bank], writes=[qT2])
        wsl = load_w(win_bf, *CHUNKS[CH_QI])
        proj_tok(pb[1], wsl, 512)
        kb.op("act", lambda e: e.copy(out=qbf[:], in_=pb[1][:, 0:512]), reads=[pb[1]], writes=[qbf])
        for pr in range(4):
            kb.op("pe", lambda e, pr=pr: e.transpose(out=tpv[:, pr * 128:(pr + 1) * 128],
                                                     in_=qbf[:, pr * 128:(pr + 1) * 128], identity=ident_b[:]),
                  reads=[qbf, ident_b], writes=[tp_bank])
        kb.op("act", lambda e: e.copy(out=qiT2[:], in_=tpv[:, 0:512].rearrange("p (a t) -> p a t", a=4)),
              reads=[tp_bank], writes=[qiT2])
        nkb = min(4 * i + 4, nblk_a)
        nch = nkb // 4
        nk = nkb * 128
        ibanks = [pb[0], pb[1], pb[3], pb[4]]
        cnt_i = 0
        per_slot = -(-(260 - NIT) // (nch * 8 + nkb))
        for ch in range(nch):
            kc_ = kitc[ch % 2]
            kb.dma("sp", kc_[:], kit_s[:, ch * 512:(ch + 1) * 512], reads=[kit_s], writes=[kc_])
            Ic = Isc[:, ch * 512:(ch + 1) * 512]
            for h in range(8):
                a, half = h // 2, h % 2
                ps_ = slice(64 * half, 64 * half + 64)
                bk = ibanks[cnt_i % 4]
                rl_ = rl[cnt_i % 2]
                cnt_i += 1
                advance(per_slot)
                kb.op("pe", lambda e, bk=bk, a=a, ps_=ps_, kc_=kc_: e.matmul(
                    bk[:, 0:512], lhsT=qiT2[ps_, a, :], rhs=kc_[ps_, :], start=True, stop=True),
                    reads=[qiT2, kc_], writes=[bk])
                kb.op("act", lambda e, bk=bk, rl_=rl_: e.activation(out=rl_[:], in_=bk[:, 0:512], func=AF.Relu),
                      reads=[bk], writes=[rl_])
                if h == 0:
                    kb.op("dve", lambda e, rl_=rl_, Ic=Ic: e.tensor_scalar(
                        out=Ic, in0=rl_[:], scalar1=wis[:, 0:1], scalar2=None, op0=ALU.mult),
                        reads=[rl_, wis], writes=[Isc])
                else:
                    kb.op("dve", lambda e, rl_=rl_, Ic=Ic, h=h: e.scalar_tensor_tensor(
                        out=Ic, in0=rl_[:], scalar=wis[:, h:h + 1], in1=Ic, op0=ALU.mult, op1=ALU.add),
                        reads=[rl_, wis, Isc], writes=[Isc])
        kb.op("dve", lambda e: e.tensor_reduce(out=hi0[:], in_=Isc[:, 0:nk], axis=AX.X, op=ALU.max),
              reads=[Isc], writes=[hi0])
        kb.op("dve", lambda e: e.tensor_reduce(out=lo[:], in_=Isc[:, 0:nk], axis=AX.X, op=ALU.min),
              reads=[Isc], writes=[lo])
        kb.op("dve", lambda e: e.tensor_tensor(out=Isc[:, nk - 512:nk], in0=Isc[:, nk - 512:nk], in1=cmask[:],
                                               op=ALU.add), reads=[Isc, cmask], writes=[Isc])
        kb.op("dve", lambda e: e.tensor_tensor(out=hi0[:], in0=hi0[:], in1=lo[:], op=ALU.subtract),
              reads=[hi0, lo], writes=[hi0])
        kb.op("dve", lambda e: e.tensor_scalar(out=wh[:], in0=pw[:], scalar1=hi0[:, 0:1], scalar2=None,
                                               op0=ALU.mult), reads=[pw, hi0], writes=[wh])
        for n in range(NIT):
            kb.op("dve", lambda e, n=n: e.tensor_tensor(out=mid[:], in0=lo[:], in1=wh[:, n:n + 1], op=ALU.add),
                  reads=[lo, wh], writes=[mid])
            kb.op("dve", lambda e: e.tensor_scalar(out=nmid[:], in0=mid[:], scalar1=-1.0, scalar2=None, op0=ALU.mult),
                  reads=[mid], writes=[nmid])
            kb.op("dve", lambda e: e.memset(cnt4[:], 0.0), writes=[cnt4])
            for q4 in range((nk + 2175) // 2176):
                c0 = q4 * 2176
                c1 = min(nk, c0 + 2176)
                kb.op("act", lambda e, c0=c0, c1=c1, q4=q4: e.activation(
                    out=junkI[:, 0:c1 - c0], in_=Isc[:, c0:c1], func=AF.Sign, bias=nmid[:, 0:1], scale=1.0,
                    accum_out=cnt4[:, q4:q4 + 1]), reads=[Isc, nmid], writes=[junkI, cnt4])
            advance(1)
            kb.op("dve", lambda e: e.tensor_reduce(out=cntt[:], in_=cnt4[:], axis=AX.X, op=ALU.add),
                  reads=[cnt4], writes=[cntt])
            kb.op("dve", lambda e, n=n: e.tensor_scalar(out=dl[:], in0=cntt[:], scalar1=511.5 - nk,
                                                        scalar2=wh[:, n:n + 1], op0=ALU.is_ge, op1=ALU.mult),
                  reads=[cntt, wh], writes=[dl])
            kb.op("dve", lambda e: e.tensor_tensor(out=lo[:], in0=lo[:], in1=dl[:], op=ALU.add),
                  reads=[lo, dl], writes=[lo])
        kb.op("dve", lambda e: e.memset(pb[5][:, 0:260], 0.0), writes=[pb[5]])
        kb.op("dve", lambda e: e.memset(pb[6][:, 0:260], 0.0), writes=[pb[6]])
        def emit_S(kbi):
            ch, kloc = kbi // 4, kbi % 4
            ktc_ = ktc[ch % 2]
            vch_ = vch[ch % 2]
            if kloc == 0:
                kb.dma("sp", ktc_[:], kt_s[:, :, ch * 512:(ch + 1) * 512], reads=[kt_s], writes=[ktc_])
                kb.dma("sp", vch_[:], v_s.t[ch * 512:(ch + 1) * 512, :, :].rearrange("(b p) h e -> p b (h e)", p=128),
                       reads=[v_s], writes=[vch_])
            nm = negm[kbi % 2]
            kb.op("dve", lambda e, nm=nm, kbi=kbi: e.tensor_scalar(
                out=nm[:], in0=Isc[:, kbi * 128:(kbi + 1) * 128], scalar1=lo[:, 0:1], scalar2=-30000.0,
                op0=ALU.is_lt, op1=ALU.mult), reads=[Isc, lo], writes=[nm])
            r = kbi - 4 * i
            near = (-1 <= r <= 3)
            if near:
                kb.op("dve", lambda e, nm=nm, r=r: e.tensor_tensor(
                    out=addh[:], in0=NB[r + 1][:], in1=nm[:].unsqueeze(1).to_broadcast([128, 8, 128]), op=ALU.add),
                    reads=[NB[r + 1], nm], writes=[addh])
            pt_ = PT[kbi % 2]
            for g in range(2):
                sb_ = pb[3 + g]
                for hh in range(4):
                    h = 4 * g + hh
                    a, half = h // 2, h % 2
                    ps_ = slice(64 * half, 64 * half + 64)
                    kb.op("pe", lambda e, sb_=sb_, hh=hh, a=a, ps_=ps_, ktc_=ktc_, kloc=kloc: e.matmul(
                        sb_[:, hh * 128:(hh + 1) * 128], lhsT=ktc_[ps_, a, kloc * 128:(kloc + 1) * 128],
                        rhs=qT2[ps_, a, :], start=True, stop=False), reads=[ktc_, qT2], writes=[sb_])
                    if near:
                        kb.op("pe", lambda e, sb_=sb_, hh=hh, h=h: e.matmul(
                            sb_[:, hh * 128:(hh + 1) * 128], lhsT=addh[:, h, :], rhs=ident_b[:],
                            start=False, stop=True), reads=[addh, ident_b], writes=[sb_])
                    else:
                        kb.op("pe", lambda e, sb_=sb_, hh=hh, nm=nm: e.matmul(
                            sb_[:, hh * 128:(hh + 1) * 128], lhsT=nm[:], rhs=ident_b[:],
                            start=False, stop=True), reads=[nm, ident_b], writes=[sb_])
                kb.op("act", lambda e, sb_=sb_, pt_=pt_, g=g: e.activation(
                    out=pt_[:, 4 * g:4 * g + 4, :], in_=sb_[:, 0:512].rearrange("p (h q) -> p h q", h=4),
                    func=AF.Exp), reads=[sb_], writes=[pt_])

        def emit_PV(kbi):
            ch, kloc = kbi // 4, kbi % 4
            vch_ = vch[ch % 2]
            pt_ = PT[kbi % 2]
            for h in range(8):
                ob = pb[5 + h // 4]
                kb.op("pe", lambda e, ob=ob, h=h, pt_=pt_, vch_=vch_, kloc=kloc: e.matmul(
                    ob[:, (h % 4) * 65:(h % 4) * 65 + 65], lhsT=pt_[:, h, :], rhs=vch_[:, kloc, h * 65:(h + 1) * 65],
                    start=False, stop=False, skip_group_check=True), reads=[pt_, vch_], writes=[ob])

        for kbi in range(nkb):
            emit_S(kbi)
            advance(per_slot)
            if kbi > 0:
                emit_PV(kbi - 1)
        emit_PV(nkb - 1)
        for g in range(2):
            ob = pb[5 + g]
            obv = ob[:, 0:260].rearrange("p (h e) -> p h e", h=4)
            kb.op("dve", lambda e, obv=obv, g=g: e.reciprocal(out=rden[:, 4 * g:4 * g + 4], in_=obv[:, :, 64]),
                  reads=[ob], writes=[rden])
            kb.op("dve", lambda e, obv=obv, g=g: e.tensor_tensor(
                out=ao[:, 256 * g:256 * g + 256].rearrange("p (h d) -> p h d", h=4), in0=obv[:, :, 0:64],
                in1=rden[:, 4 * g:4 * g + 4].unsqueeze(2).to_broadcast([128, 4, 64]), op=ALU.mult),
                reads=[ob, rden], writes=[ao])
        if dbg_ao is not None:
            kb.op("dve", lambda e: e.tensor_copy(out=aof[:], in_=ao[:]), reads=[ao], writes=[aof])
            kb.dma("sp", dbg_ao[i, :, :], aof[:], reads=[aof], writes=[dbg_ao])

        if do_tail:
            drain()
            pending[0] = tail_block(i, xb)


    drain()
    if do_sample:
        LOB = _bucket_lo()
        wrep = kb.sb([128, 8], F32, "wrep")
        idx2 = kb.sb([128, 2], I32, "idx2")
        pti = kb.sb([128, 16], I32, "pti")
        ptf = kb.sb([128, 16], F32, "ptf")
        dh = kb.sb([128, 128], F32, "dh")
        dh2 = kb.sb([128, 128], F32, "dh2")
        scs = kb.sb([128, 2, 128], F32, "scs")
        scn = kb.sb([128, 1], F32, "scn")
        dn8 = kb.sb([128, 8], F32, "dn8")
        sm8 = kb.sb([128, 8], F32, "sm8")
        tA = View(Ubo[0], Ubo[0].t[:].bitcast(F32)[:, 0:256])
        tE = View(Ubo[0], Ubo[0].t[:].bitcast(F32)[:, 256:512])
        tF = View(Ubo[1], Ubo[1].t[:].bitcast(F32)[:, 0:256])
        tG = View(Ubo[1], Ubo[1].t[:].bitcast(F32)[:, 256:512])
        idxs = View(Ubo[2], Ubo[2].t[:].bitcast(U32)[:, 0:256])
        tC = View(Ubo[2], Ubo[2].t[:].bitcast(U32)[:, 256:512])
        tD = View(Ubo[3], Ubo[3].t[:].bitcast(U32)[:, 0:256])
        rowT = kb.sb([128, 32], I32, "rowT")
        distT = kb.sb([128, 32], F32, "distT")
        negT = kb.sb([128, 32], F32, "negT")
        indT = kb.sb([128, 32], F32, "indT")
        biasT = kb.sb([128, 32, 8], F32, "biasT")
        tmp3 = kb.sb([128, 32, 8], F32, "tmp3")
        lg = kb.sb([128, 8], F32, "lg")
        pex = kb.sb([128, 8], F32, "pex")
        zsel = kb.sb([128, 31], F32, "zsel")
        numS = View(Ubo[4], Ubo[4].t[:].bitcast(F32)[:, 0:512])
        denS = kb.sb([128, 8], F32, "denS")
        seln = kb.sb([128, 1], F32, "seln")
        nb0 = kb.sb([128, 8], F32, "nb0")
        sq, sk_, sv_, ski = qf, ko, vo, kio
        sqi = hsq
        su = tmpA
        qrep = tmpB
        IscV = Isc.t[:]
        kb.dma("sp", zsel[:], zsel_in[:, :], writes=[zsel])
        kb.op("dve", lambda e: e.memset(pti[:], 0), writes=[pti])
        kb.dma("sp", pti[0:16, :], pt_in[:, :], writes=[pti])
        kb.op("dve", lambda e: e.tensor_copy(out=ptf[:], in_=pti[:]), reads=[pti], writes=[ptf])
        with nc.allow_non_contiguous_dma(reason="page table slices"):
            for jg in range(8):
                kb.dma("sp", idx2[jg * 16:(jg + 1) * 16, :], pt_in[:, 2 * jg:2 * jg + 2], writes=[idx2])
        xb = xt[0]
        kb.dma("sp", xb[:], xs_in[:, :], writes=[xb])
        norm_transpose(xb, hnT)
        wsl = load_w(win_bf, *CHUNKS[CH_Q])
        proj_tok(pb[0], wsl, 512)
        head_norm(pb[0], qg, sq)
        wsl = load_w(win_bf, *CHUNKS[CH_K])
        proj_tok(pb[1], wsl, 512)
        head_norm(pb[1], kg, sk_)
        kb.dma("sp", k_s_out[:, :], sk_[:], reads=[sk_], writes=[k_s_out])
        wsl = load_w(win_bf, *CHUNKS[CH_V])
        proj_tok(pb[0], wsl, 512)
        kb.op("act", lambda e: e.copy(out=sv_[:], in_=pb[0][:, 0:512]), reads=[pb[0]], writes=[sv_])
        kb.dma("sp", v_s_out[:, :], sv_[:], reads=[sv_], writes=[v_s_out])
        wsl = load_w(win_bf, *CHUNKS[CH_QI])
        proj_tok(pb[1], wsl, 512)
        kb.op("act", lambda e: e.copy(out=sqi[:], in_=pb[1][:, 0:512]), reads=[pb[1]], writes=[sqi])
        wsl = load_w(win_bf, *CHUNKS[CH_KW])
        proj_tok(pb[0], wsl, 72)
        kb.op("dve", lambda e: e.tensor_copy(out=ski[:], in_=pb[0][:, 0:64]), reads=[pb[0]], writes=[ski])
        kb.dma("sp", ki_s_out[:, :], ski[:], reads=[ski], writes=[ki_s_out])
        kb.op("dve", lambda e: e.tensor_scalar(out=wis[:], in0=pb[0][:, 64:72], scalar1=0.125 * (8 ** -0.5),
                                               scalar2=None, op0=ALU.mult), reads=[pb[0]], writes=[wis])
        wsl = load_w(win_bf, *CHUNKS[CH_U])
        proj_tok(pb[1], wsl, 512)
        kb.op("act", lambda e: e.copy(out=su[:], in_=pb[1][:, 0:512]), reads=[pb[1]], writes=[su])
        kb.dma("sp", pool_s_out[:, 0:14, :], state_in[:, 1:15, :], reads=[state_in], writes=[pool_s_out])
        kb.dma("sp", pool_s_out[:, 14, :], su[0:16, :], reads=[su], writes=[pool_s_out])
        kb.dma("sp", qi_d[:, :], sqi[0:16, :], reads=[sqi], writes=[qi_d])
        kb.dma("sp", wi_d[:, :], wis[0:16, :], reads=[wis], writes=[wi_d])
        kb.dma("sp", q_d[:, :], sq[0:16, :], reads=[sq], writes=[q_d])
        for jg in range(8):
            kb.dma("sp", qrep[jg * 16:(jg + 1) * 16, :], qi_d[:, :], reads=[qi_d], writes=[qrep])
            kb.dma("sp", wrep[jg * 16:(jg + 1) * 16, :], wi_d[:, :], reads=[wi_d], writes=[wrep])
        KI = IscV[:, 0:8192]
        prodc = View(ktc[0], ktc[0].t[:].rearrange("p a b -> p (a b)").bitcast(F32)[:, 0:1024])
        for s in range(2):
            kb.dma("pool", KI, cache_ik[:, :], reads=[idx2], writes=[Isc],
                   indirect=bass.IndirectOffsetOnAxis(ap=idx2[:, s:s + 1], axis=0))
            for h in range(8):
                for c8 in range(8):
                    kb.op("dve", lambda e, h=h, c8=c8: e.tensor_tensor(
                        out=prodc[:].rearrange("p (k d) -> p k d", k=16),
                        in0=KI[:, c8 * 1024:(c8 + 1) * 1024].rearrange("p (k d) -> p k d", k=16),
                        in1=qrep[:, h * 64:(h + 1) * 64].unsqueeze(1).to_broadcast([128, 16, 64]), op=ALU.mult),
                        reads=[Isc, qrep], writes=[prodc.owner])
                    kb.op("dve", lambda e, c8=c8: e.tensor_reduce(
                        out=dh[:, c8 * 16:(c8 + 1) * 16], in_=prodc[:].rearrange("p (k d) -> p k d", k=16),
                        axis=AX.X, op=ALU.add), reads=[prodc.owner], writes=[dh])
                if h == 0:
                    kb.op("dve", lambda e, s=s: e.tensor_scalar(out=scs[:, s, :], in0=dh[:], scalar1=0.0,
                                                                scalar2=wrep[:, 0:1], op0=ALU.max, op1=ALU.mult),
                          reads=[dh, wrep], writes=[scs])
                else:
                    kb.op("dve", lambda e, h=h: e.tensor_scalar(out=dh2[:], in0=dh[:], scalar1=0.0,
                                                                scalar2=wrep[:, h:h + 1], op0=ALU.max, op1=ALU.mult),
                          reads=[dh, wrep], writes=[dh2])
                    kb.op("dve", lambda e, s=s: e.tensor_tensor(out=scs[:, s, :], in0=scs[:, s, :], in1=dh2[:],
                                                                op=ALU.add), reads=[scs, dh2], writes=[scs])
        for jg in range(8):
            kb.dma("sp", sc_d[:, jg * 256:(jg + 1) * 256], scs[jg * 16:(jg + 1) * 16, :, :].rearrange("p s k -> p (s k)"),
                   reads=[scs], writes=[sc_d])
        xtmp = xn[:, 512:1024]
        kb.op("dve", lambda e: e.tensor_tensor(out=xtmp.rearrange("p (h d) -> p h d", h=8),
                                               in0=sqi[:].rearrange("p (h d) -> p h d", h=8),
                                               in1=ski[:].unsqueeze(1).to_broadcast([128, 8, 64]), op=ALU.mult),
              reads=[sqi, ski], writes=[xn])
        kb.op("dve", lambda e: e.tensor_reduce(out=dn8[:], in_=xtmp.rearrange("p (h d) -> p h d", h=8), axis=AX.X,
                                               op=ALU.add), reads=[xn], writes=[dn8])
        kb.op("dve", lambda e: e.tensor_scalar(out=dn8[:], in0=dn8[:], scalar1=0.0, scalar2=None, op0=ALU.max),
              reads=[dn8], writes=[dn8])
        kb.op("dve", lambda e: e.tensor_tensor(out=dn8[:], in0=dn8[:], in1=wis[:], op=ALU.mult),
              reads=[dn8, wis], writes=[dn8])
        kb.op("dve", lambda e: e.tensor_reduce(out=scn[:], in_=dn8[:], axis=AX.X, op=ALU.add),
              reads=[dn8], writes=[scn])
        Is = IscV[:, 0:2049]
        kb.op("dve", lambda e: e.memset(IscV[:, 0:2304], 0.0), writes=[Isc])
        kb.dma("sp", IscV[0:16, 0:2048], sc_d[:, :], reads=[sc_d], writes=[Isc])
        kb.op("dve", lambda e: e.tensor_copy(out=IscV[:, 2048:2049], in_=scn[:]), reads=[scn], writes=[Isc])
        for rd in range(32):
            kb.op("dve", lambda e: e.max(out=sm8[:], in_=Is), reads=[Isc], writes=[sm8])
            kb.op("dve", lambda e, rd=rd: e.max_index(out=idxs[:, rd * 8:(rd + 1) * 8], in_max=sm8[:], in_values=Is),
                  reads=[Isc, sm8], writes=[idxs])
            kb.op("dve", lambda e: e.match_replace(out=Is, in_to_replace=sm8[:], in_values=Is, imm_value=-1e30),
                  reads=[Isc, sm8], writes=[Isc])
        kb.op("dve", lambda e: e.tensor_copy(out=tA[:], in_=idxs[:]), reads=[idxs], writes=[tA])
        kb.op("dve", lambda e: e.tensor_scalar(out=tC[:], in0=tA[:], scalar1=2047.0, scalar2=None, op0=ALU.min),
              reads=[tA], writes=[tC])
        kb.op("dve", lambda e: e.tensor_single_scalar(out=tD[:], in_=tC[:], scalar=7, op=ALU.logical_shift_right),
              reads=[tC], writes=[tD])
        kb.op("dve", lambda e: e.tensor_single_scalar(out=tC[:], in_=tC[:], scalar=127, op=ALU.bitwise_and),
              reads=[tC], writes=[tC])
        kb.op("dve", lambda e: e.tensor_copy(out=tE[:], in_=tD[:]), reads=[tD], writes=[tE])
        kb.op("dve", lambda e: e.tensor_copy(out=tF[:], in_=tC[:]), reads=[tC], writes=[tF])
        ohs = IscV[:, 4608:8704].rearrange("p (k j) -> p k j", k=256)
        kb.op("dve", lambda e: e.tensor_tensor(out=ohs, in0=tE[:].unsqueeze(2).to_broadcast([128, 256, 16]),
                                               in1=iota16[:].unsqueeze(1).to_broadcast([128, 256, 16]), op=ALU.is_equal),
              reads=[tE, iota16], writes=[Isc])
        kb.op("dve", lambda e: e.tensor_tensor(out=ohs, in0=ohs, in1=ptf[:].unsqueeze(1).to_broadcast([128, 256, 16]),
                                               op=ALU.mult), reads=[Isc, ptf], writes=[Isc])
        kb.op("dve", lambda e: e.tensor_reduce(out=tG[:], in_=ohs, axis=AX.X, op=ALU.add), reads=[Isc], writes=[tG])
        kb.op("dve", lambda e: e.scalar_tensor_tensor(out=tG[:], in0=tG[:], scalar=128.0, in1=tF[:], op0=ALU.mult,
                                                      op1=ALU.add), reads=[tG, tF], writes=[tG])
        kb.op("dve", lambda e: e.tensor_scalar(out=tE[:], in0=tA[:], scalar1=-1.0, scalar2=2048.0, op0=ALU.mult,
                                               op1=ALU.add), reads=[tA], writes=[tE])
        kb.op("dve", lambda e: e.tensor_scalar(out=tF[:], in0=tA[:], scalar1=2047.5, scalar2=-30000.0, op0=ALU.is_ge,
                                               op1=ALU.mult), reads=[tA], writes=[tF])
        kb.op("dve", lambda e: e.tensor_reduce(out=seln[:], in_=tF[:], axis=AX.X, op=ALU.min), reads=[tF], writes=[seln])
        kb.op("dve", lambda e: e.tensor_scalar(out=seln[:], in0=seln[:], scalar1=-1.0 / 30000.0, scalar2=None,
                                               op0=ALU.mult), reads=[seln], writes=[seln])
        for (srcb, dstb) in ((tG, rowT), (tE, distT), (tF, negT)):
            for gq in range(2):
                kb.op("pe", lambda e, srcb=srcb, gq=gq: e.transpose(out=pb[2][:, gq * 128:(gq + 1) * 128],
                                                                   in_=srcb[:, gq * 128:(gq + 1) * 128],
                                                                   identity=ident_f[:]),
                      reads=[srcb, ident_f], writes=[pb[2]])
            kb.op("dve", lambda e, dstb=dstb: e.tensor_copy(
                out=dstb[:].rearrange("p (g b) -> p g b", g=2),
                in_=pb[2][:, 0:256].rearrange("p (g b) -> p g b", g=2)[:, :, 0:16]), reads=[pb[2]], writes=[dstb])
        kb.op("dve", lambda e: e.memset(biasT[:], 0.0), writes=[biasT])
        for bkt in range(1, 32):
            kb.op("dve", lambda e, bkt=bkt: e.tensor_scalar(out=indT[:], in0=distT[:], scalar1=float(LOB[bkt - 1]),
                                                            scalar2=None, op0=ALU.is_lt), reads=[distT], writes=[indT])
            kb.op("dve", lambda e, bkt=bkt: e.tensor_tensor(
                out=tmp3[:], in0=indT[:].unsqueeze(2).to_broadcast([128, 32, 8]),
                in1=ndel[:, bkt * 8:(bkt + 1) * 8].unsqueeze(1).to_broadcast([128, 32, 8]), op=ALU.mult),
                reads=[indT, ndel], writes=[tmp3])
            kb.op("dve", lambda e: e.tensor_tensor(out=biasT[:], in0=biasT[:], in1=tmp3[:], op=ALU.add),
                  reads=[biasT, tmp3], writes=[biasT])
        kb.op("dve", lambda e: e.memset(pb[5][:, 0:512], 0.0), writes=[pb[5]])
        kb.op("dve", lambda e: e.memset(pb[6][:, 0:8], 0.0), writes=[pb[6]])
        qbv = xn[:, 0:512]
        pvv = xn[:, 512:1024]
        for b in range(16):
            kb.dma("sp", qbv, q_d.t[b:b + 1, :].partition_broadcast(128).rearrange("p o d -> p (o d)"),
                   reads=[q_d], writes=[xn])
            for gq in range(2):
                col = gq * 16 + b
                kg_ = UbS[gq]
                vg_ = UbS[2 + gq]
                kb.dma("pool", kg_[:, 0:512], cache_k[:, :], reads=[rowT], writes=[kg_.owner],
                       indirect=bass.IndirectOffsetOnAxis(ap=rowT[:, col:col + 1], axis=0))
                kb.dma("pool", vg_[:, 0:512], cache_v[:, :], reads=[rowT], writes=[vg_.owner],
                       indirect=bass.IndirectOffsetOnAxis(ap=rowT[:, col:col + 1], axis=0))
                kb.op("dve", lambda e, kg_=kg_: e.tensor_tensor(out=pvv, in0=kg_[:, 0:512], in1=qbv, op=ALU.mult),
                      reads=[kg_.owner, xn], writes=[xn])
                kb.op("dve", lambda e: e.tensor_reduce(out=lg[:], in_=pvv.rearrange("p (h d) -> p h d", h=8),
                                                       axis=AX.X, op=ALU.add), reads=[xn], writes=[lg])
                kb.op("dve", lambda e, col=col: e.scalar_tensor_tensor(
                    out=lg[:], in0=lg[:], scalar=negT[:, col:col + 1], in1=biasT[:, col, :], op0=ALU.add, op1=ALU.add),
                    reads=[lg, negT, biasT], writes=[lg])
                kb.op("act", lambda e: e.activation(out=pex[:], in_=lg[:], func=AF.Exp), reads=[lg], writes=[pex])
                kb.op("dve", lambda e, vg_=vg_: e.tensor_tensor(
                    out=pvv.rearrange("p (h d) -> p h d", h=8), in0=vg_[:, 0:512].rearrange("p (h d) -> p h d", h=8),
                    in1=pex[:].unsqueeze(2).to_broadcast([128, 8, 64]), op=ALU.mult),
                    reads=[vg_.owner, pex], writes=[xn])
                kb.op("pe", lambda e, b=b: e.matmul(pb[5][0:16, 0:512], lhsT=zsel[:, 15 - b:31 - b], rhs=pvv,
                                                    start=False, stop=False, skip_group_check=True),
                      reads=[zsel, xn], writes=[pb[5]])
                kb.op("pe", lambda e, b=b: e.matmul(pb[6][0:16, 0:8], lhsT=zsel[:, 15 - b:31 - b], rhs=pex[:],
                                                    start=False, stop=False, skip_group_check=True),
                      reads=[zsel, pex], writes=[pb[6]])
        kb.op("dve", lambda e: e.memset(numS[:], 0.0), writes=[numS])
        kb.op("dve", lambda e: e.memset(denS[:], 1.0), writes=[denS])
        kb.op("dve", lambda e: e.tensor_copy(out=numS[0:16, :], in_=pb[5][0:16, 0:512]), reads=[pb[5]], writes=[numS])
        kb.op("dve", lambda e: e.tensor_copy(out=denS[0:16, :], in_=pb[6][0:16, 0:8]), reads=[pb[6]], writes=[denS])
        kb.op("dve", lambda e: e.tensor_reduce(out=nb0[:], in_=ndel[:, 8:256].rearrange("p (b h) -> p h b", h=8),
                                               axis=AX.X, op=ALU.add), reads=[ndel], writes=[nb0])
        kb.op("dve", lambda e: e.tensor_tensor(out=pvv, in0=sq[:], in1=sk_[:], op=ALU.mult), reads=[sq, sk_], writes=[xn])
        kb.op("dve", lambda e: e.tensor_reduce(out=lg[:], in_=pvv.rearrange("p (h d) -> p h d", h=8), axis=AX.X,
                                               op=ALU.add), reads=[xn], writes=[lg])
        kb.op("dve", lambda e: e.tensor_tensor(out=lg[:], in0=lg[:], in1=nb0[:], op=ALU.add), reads=[lg, nb0], writes=[lg])
        kb.op("act", lambda e: e.activation(out=pex[:], in_=lg[:], func=AF.Exp), reads=[lg], writes=[pex])
        kb.op("dve", lambda e: e.tensor_scalar(out=pex[:], in0=pex[:], scalar1=seln[:, 0:1], scalar2=None, op0=ALU.mult),
              reads=[pex, seln], writes=[pex])
        kb.op("dve", lambda e: e.tensor_tensor(out=pvv.rearrange("p (h d) -> p h d", h=8),
                                               in0=sv_[:].rearrange("p (h d) -> p h d", h=8),
                                               in1=pex[:].unsqueeze(2).to_broadcast([128, 8, 64]), op=ALU.mult),
              reads=[sv_, pex], writes=[xn])
        kb.op("dve", lambda e: e.tensor_tensor(out=numS[:], in0=numS[:], in1=pvv, op=ALU.add), reads=[numS, xn], writes=[numS])
        kb.op("dve", lambda e: e.tensor_tensor(out=denS[:], in0=denS[:], in1=pex[:], op=ALU.add), reads=[denS, pex],
              writes=[denS])
        kb.op("dve", lambda e: e.reciprocal(out=denS[:], in_=denS[:]), reads=[denS], writes=[denS])
        kb.op("dve", lambda e: e.tensor_tensor(out=ao[:].rearrange("p (h d) -> p h d", h=8),
                                               in0=numS[:].rearrange("p (h d) -> p h d", h=8),
                                               in1=denS[:].unsqueeze(2).to_broadcast([128, 8, 64]), op=ALU.mult),
              reads=[numS, denS], writes=[ao])
        stv = IscV[:, 0:7680].rearrange("p (r c) -> p r c", r=15)
        kb.op("dve", lambda e: e.memset(IscV[:, 0:7680], 0.0), writes=[Isc])
        kb.dma("sp", IscV[0:16, 0:7680], state_in.t.rearrange("b r c -> b (r c)"), reads=[state_in], writes=[Isc])
        for g in range(4):
            w = 2 ** (g + 1)
            kb.op("dve", lambda e, g=g, w=w: e.tensor_reduce(
                out=pmf[:, g, :], in_=stv[:, 16 - w:15, g * 128:(g + 1) * 128].rearrange("p r c -> p c r"),
                axis=AX.X, op=ALU.add), reads=[Isc], writes=[pmf])
            kb.op("dve", lambda e, g=g: e.tensor_tensor(out=pmf[:, g, :], in0=pmf[:, g, :], in1=su[:, g * 128:(g + 1) * 128],
                                                        op=ALU.add), reads=[pmf, su], writes=[pmf])
            kb.op("dve", lambda e, g=g, w=w: e.scalar_tensor_tensor(
                out=pmf[:, g, :], in0=pmf[:, g, :], scalar=1.0 / w, in1=su[:, g * 128:(g + 1) * 128], op0=ALU.mult,
                op1=ALU.subtract), reads=[pmf, su], writes=[pmf])
        for g in range(4):
            kb.op("pe", lambda e, g=g: e.transpose(out=pb[2][:, g * 128:(g + 1) * 128], in_=pmf[:, g, :],
                                                   identity=ident_f[:]), reads=[pmf, ident_f], writes=[pb[2]])
        kb.op("dve", lambda e: e.tensor_copy(out=pmT[:], in_=pb[2][:, 0:512].rearrange("p (g t) -> p g t", g=4)),
              reads=[pb[2]], writes=[pmT])
        pending[0] = tail_block(0, xb, sample=True)
        drain()

    kb.finish()
    return nc, kb


def _prep_inputs(inp, cfg, cores):
    nblk_a = cfg.get("nblk_a", NBLK_A)
    nq = cfg.get("nq", NQ)
    nkeys = nblk_a * 128
    xp = np.asarray(inp["x_prompt"], np.float32)
    meta = np.asarray(inp["meta_tokens"], np.float32)
    maps = []
    for c in cores:
        b, cc = c // 4, c % 4
        full = np.zeros((max(nkeys, (4 * nq + 4) * 128), D), np.float32)
        T = 16 + xp.shape[1]
        cat = np.concatenate([meta, xp[b]], axis=0)
        n = min(T, full.shape[0])
        full[:n] = cat[:n]
        xown = np.zeros((nq, 128, D), np.float32)
        xprev = np.zeros((nq, 16, D), np.float32)
        for i in range(nq):
            j = 4 * i + cc
            xown[i] = full[j * 128:(j + 1) * 128]
            if j > 0:
                xprev[i] = full[j * 128 - 16:j * 128]
        m = {
            "xcat": np.ascontiguousarray(full[:nkeys]),
            "xown": xown,
            "xprev": xprev,
            "w_in": np.ascontiguousarray(np.asarray(inp["w_in"], np.float32)[0]),
            "norm1_g": np.ascontiguousarray(np.asarray(inp["norm1_g"], np.float32)[0]),
            "q_norm_g": np.ascontiguousarray(np.asarray(inp["q_norm_g"], np.float32)[0]),
            "k_norm_g": np.ascontiguousarray(np.asarray(inp["k_norm_g"], np.float32)[0]),
            "ident": np.eye(128, dtype=np.float32),
            "rel_bias": np.ascontiguousarray(np.asarray(inp["rel_bias"], np.float32)),
            "qs": (np.arange(128, dtype=np.float32)[:, None] - np.arange(128, dtype=np.float32)[None, :]),
            "thrtab": _thrtab(cc),
            "cmask": _cmask(cc),
            "w_ba": np.ascontiguousarray(np.asarray(inp["w_branch_attn"], np.float32)[0]),
            "w_bp": np.ascontiguousarray(np.asarray(inp["w_branch_pool"], np.float32)[0]),
            "w_out": np.ascontiguousarray(np.asarray(inp["w_out"], np.float32)[0]),
            "peer_wq": np.ascontiguousarray(np.asarray(inp["peer_wq"], np.float32)[0]),
            "w_pool": np.ascontiguousarray(np.asarray(inp["w_pool"], np.float32)[0]),
            "pool_scale": np.ascontiguousarray(np.asarray(inp["pool_scale"], np.float32)[0]),
            "norm2_g": np.ascontiguousarray(np.asarray(inp["norm2_g"], np.float32)[0]),
            "subkeys": np.ascontiguousarray(np.asarray(inp["peer_subkeys"], np.float32)[0]),
            "peer_u": np.ascontiguousarray(np.asarray(inp["peer_u"], np.float32)[0]),
            "peer_v": np.ascontiguousarray(np.asarray(inp["peer_v"], np.float32)[0]),
            "rcnt": _rcnt(cc),
            "iota16": np.broadcast_to(np.arange(16, dtype=np.float32)[None, :], (128, 16)).copy(),
            "pw": np.broadcast_to((0.5 ** np.arange(1, NIT + 1)).astype(np.float32)[None, :], (128, NIT)).copy(),
        }
        if cfg.get("sample", True):
            xs = np.zeros((128, D), np.float32)
            xs[:16] = np.asarray(inp["x_sample"], np.float32)[16 * c:16 * c + 16, 0]
            z = np.zeros((128, 31), np.float32)
            z[:, 15] = 1.0
            m.update({
                "xs_own": xs,
                "cache_k": np.asarray(inp["cache_k"], np.float32).reshape(2560 * 128, 512),
                "cache_v": np.asarray(inp["cache_v"], np.float32).reshape(2560 * 128, 512),
                "cache_ik": np.asarray(inp["cache_idx_k"], np.float32).reshape(2560, 8192),
                "state_own": np.ascontiguousarray(np.asarray(inp["state_pool"], np.float32)[0, 16 * c:16 * c + 16]),
                "pt_own": np.ascontiguousarray(np.asarray(inp["page_table"], np.int32)[16 * c:16 * c + 16]),
                "zsel": z,
            })
        maps.append(m)
    return maps


def _bucket_lo():
    n = np.arange(0, 256)
    nf = np.maximum(n, 16).astype(np.float32)
    large = 16 + (np.log(nf / np.float32(16)) / np.float32(np.log(128 / 16)) * np.float32(16)).astype(np.int32)
    large = np.minimum(large, 31)
    bkt = np.where(n < 16, n, large)
    return [int(np.min(n[bkt >= b])) for b in range(1, 32)]


def _thrtab(cc):
    lo_b = _bucket_lo()
    t = np.zeros((155,), np.float32)
    for r5 in range(5):
        r = r5 - 1
        for b in range(31):
            t[r5 * 31 + b] = lo_b[b] - 128 * (cc - r)
    return np.broadcast_to(t[None, :], (128, 155)).copy()


def _rcnt(cc):
    r = np.zeros((128, 4, 128), np.float32)
    t = np.arange(128)
    for g, w in enumerate((2, 4, 8, 16)):
        cnt = np.minimum(w, t + 1) if cc == 0 else np.full(128, w)
        r[:, g, :] = (1.0 / cnt.astype(np.float32))[None, :]
    return r


def _cmask(cc):
    m = np.zeros((128, 512), np.float32)
    q = np.arange(128)[:, None]
    s = np.arange(128)[None, :]
    for r in range(4):
        if r > cc:
            m[:, r * 128:(r + 1) * 128] = -1e30
        elif r == cc:
            m[:, r * 128:(r + 1) * 128] = np.where(s > q, -1e30, 0.0)
    return m


def kernel(**inputs):
    cfg = {}
    nc = build(cfg)
    cores = list(range(8))
    maps = _prep_inputs(inputs, cfg, cores)
    res = run_bass_kernel_spmd(nc, maps, core_ids=cores)
    rs = res.results
    B, S = 2, 8192
    T = S + 16
    y_prompt = np.zeros((B, S, D), np.float32)
    k_p = np.zeros((1, B, T, 8, 64), np.float32)
    v_p = np.zeros((1, B, T, 8, 64), np.float32)
    i_p = np.zeros((1, B, T, 64), np.float32)
    pool_p = np.zeros((1, B, 15, 512), np.float32)
    for c in cores:
        b, cc = c // 4, c % 4
        r = rs[c]
        for i in range(NQ):
            j = 4 * i + cc
            p0 = j * 128
            if p0 >= T:
                continue
            p1 = min(p0 + 128, T)
            n = p1 - p0
            k_p[0, b, p0:p1] = r["k_own"][i][:n].reshape(n, 8, 64)
            v_p[0, b, p0:p1] = r["v_own"][i][:n].reshape(n, 8, 64)
            i_p[0, b, p0:p1] = r["ki_own"][i][:n]
            lo = max(p0, 16)
            y_prompt[b, lo - 16:p1 - 16] = r["y_own"][i][lo - p0:n]
        if cc == 0:
            ul = r["u_last"]
            rows = ul[:, :, 17:32]
            pool_p[0, b] = np.transpose(rows, (2, 1, 0)).reshape(15, 512)
    y_s = np.zeros((128, 1, D), np.float32)
    k_s = np.zeros((1, 128, 1, 8, 64), np.float32)
    v_s = np.zeros((1, 128, 1, 8, 64), np.float32)
    i_s = np.zeros((1, 128, 1, 64), np.float32)
    pool_s = np.zeros((1, 128, 15, 512), np.float32)
    for c in cores:
        r = rs[c]
        sl = slice(16 * c, 16 * c + 16)
        y_s[sl, 0] = r["y_s"][:16]
        k_s[0, sl, 0] = r["ks_o"][:16].reshape(16, 8, 64)
        v_s[0, sl, 0] = r["vs_o"][:16].reshape(16, 8, 64)
        i_s[0, sl, 0] = r["kis_o"][:16]
        pool_s[0, sl] = r["pool_s"]
    return (y_prompt, y_s, k_p, v_p, i_p, pool_p, k_s, v_s, i_s, pool_s)
```

```python
import numpy as np
from contextlib import ExitStack
import concourse.bass as bass
import concourse.mybir as mybir
from concourse.bass_utils import run_bass_kernel_spmd

F32 = mybir.dt.float32
BF16 = mybir.dt.bfloat16
I32 = mybir.dt.int32
U32 = mybir.dt.uint32
ALU = mybir.AluOpType
AF = mybir.ActivationFunctionType
AX = mybir.AxisListType

D = 1024
NBLK_A = 68
NQ = 17
EPS = 1e-6
IN_W = 4680
CH_Q, CH_K, CH_V, CH_QI, CH_KW, CH_U, CH_GA0, CH_GA1, CH_GB0, CH_GB1 = range(10)
CHUNKS = [(0, 512), (512, 512), (1024, 512), (1536, 512), (2048, 72), (2120, 512),
          (2632, 512), (3144, 512), (3656, 512), (4168, 512)]
N_DMA_SEMS = 12
NIT = 16


class Buf:
    def __init__(self, t, name):
        self.t = t
        self.name = name
        self.w = None
        self.r = {}

    def __getitem__(self, idx):
        return self.t[idx]


class KB:
    def __init__(self, nc, es, plan=None):
        self.nc = nc
        self.es = es
        self.plan = plan
        self.targets = {e: set() for e in ("pe", "dve", "act", "pool", "sp")}
        self.rank = None
        if plan is not None:
            self.rank = {e: {idx: r + 1 for r, idx in enumerate(sorted(plan[e]))} for e in plan}
        self.eng = {"pe": nc.tensor, "dve": nc.vector, "act": nc.scalar, "pool": nc.gpsimd, "sp": nc.sync}
        self.sem = {e: es.enter_context(nc.semaphore("s_" + e)) for e in self.eng}
        self.cnt = {e: 0 for e in self.eng}
        self.seen = {e: {} for e in self.eng}
        self.dsem = [es.enter_context(nc.semaphore("d_%d" % i)) for i in range(N_DMA_SEMS + 6)]
        self.dcnt = [0] * (N_DMA_SEMS + 6)
        self.drr = 0
        self.nbuf = 0

    def sb(self, shape, dt, name=None):
        self.nbuf += 1
        name = "sb_" + (name or ("%d" % self.nbuf))
        return Buf(self.es.enter_context(self.nc.sbuf_tensor(name, list(shape), dt)), name)

    def ps(self, shape, dt, name=None):
        self.nbuf += 1
        name = name or ("ps%d" % self.nbuf)
        return Buf(self.es.enter_context(self.nc.psum_tensor(name, list(shape), dt)), name)

    def dram(self, name, shape, dt, kind="Internal"):
        return Buf(self.nc.dram_tensor(name, list(shape), dt, kind=kind).ap(), name)

    def _deps(self, e, reads, writes):
        need = {}

        def add(tok):
            if tok is None:
                return
            key, sem, val = tok
            if key == "pe" and e == "pe":
                return
            if need.get(key, (None, 0))[1] < val:
                need[key] = (sem, val)

        for b in reads:
            add(b.w)
        for b in writes:
            add(b.w)
            for tok in b.r.values():
                add(tok)
        eo = self.eng[e]
        for key, (sem, val) in need.items():
            if self.seen[e].get(key, 0) < val:
                self.seen[e][key] = val
                if key in self.targets:
                    if self.plan is None:
                        self.targets[key].add(val)
                    else:
                        eo.wait_ge(sem, self.rank[key][val])
                elif self.plan is not None:
                    eo.wait_ge(sem, val)

    def _record(self, tok, reads, writes):
        for b in reads:
            if b.r.get(tok[0], (None, None, 0))[2] < tok[2]:
                b.r[tok[0]] = tok
        for b in writes:
            b.w = tok
            b.r = {}

    def op(self, e, fn, reads=(), writes=()):
        self._deps(e, reads, writes)
        self.cnt[e] += 1
        if self.plan is not None:
            ins = fn(self.eng[e])
            if self.cnt[e] in self.plan[e]:
                ins.then_inc(self.sem[e], 1)
        self._record((e, self.sem[e], self.cnt[e]), reads, writes)

    def dma(self, q, out, in_, reads=(), writes=(), indirect=None, own_sem=None):
        if own_sem is None:
            i = self.drr
            self.drr = (i + 1) % N_DMA_SEMS
        else:
            i = N_DMA_SEMS + own_sem
        eo = self.eng[q]
        key = "d%d" % i
        if own_sem is None and self.dcnt[i] > 0 and self.seen[q].get(key, 0) < 16 * self.dcnt[i]:
            if self.plan is not None:
                eo.wait_ge(self.dsem[i], 16 * self.dcnt[i])
            self.seen[q][key] = 16 * self.dcnt[i]
        self._deps(q, reads, writes)
        self.dcnt[i] += 1
        if self.plan is not None:
            if indirect is None:
                ins = eo.dma_start(out=out, in_=in_)
            else:
                ins = eo.indirect_dma_start(out=out, out_offset=None, in_=in_, in_offset=indirect)
            ins.then_inc(self.dsem[i], 16)
        self._record((key, self.dsem[i], 16 * self.dcnt[i]), reads, writes)

    def finish(self):
        eo = self.eng["sp"]
        if self.plan is None:
            for e in ("pe", "dve", "act", "pool"):
                if self.cnt[e] > 0:
                    self.targets[e].add(self.cnt[e])
            return
        for i in range(N_DMA_SEMS + 6):
            if self.dcnt[i] > 0:
                eo.wait_ge(self.dsem[i], 16 * self.dcnt[i])
        for e in ("pe", "dve", "act", "pool"):
            if self.cnt[e] > 0:
                eo.wait_ge(self.sem[e], self.rank[e][self.cnt[e]])


def build(cfg):
    _, kb_dry = _build(cfg, None)
    nc, _ = _build(cfg, kb_dry.targets)
    return nc


def _build(cfg, plan):
    nblk_a = cfg.get("nblk_a", NBLK_A)
    nq = cfg.get("nq", NQ)
    do_attn = cfg.get("attn", True)
    do_tail = cfg.get("tail", True)
    nkeys = nblk_a * 128
    nc = bass.Bass("TRN2", target_bir_lowering=False)
    es = ExitStack()
    kb = KB(nc, es, plan)
    nc._es_keep = es

    def din(name, shape, dt=F32):
        return Buf(nc.dram_tensor(name, list(shape), dt, kind="ExternalInput").ap(), name)

    def dout(name, shape, dt=F32):
        return Buf(nc.dram_tensor(name, list(shape), dt, kind="ExternalOutput").ap(), name)

    xcat = din("xcat", [nkeys, D])
    xown = din("xown", [nq, 128, D])
    xprev = din("xprev", [nq, 16, D])
    w_in = din("w_in", [D, IN_W])
    norm1_g = din("norm1_g", [D])
    q_norm_g = din("q_norm_g", [64])
    k_norm_g = din("k_norm_g", [64])
    ident_in = din("ident", [128, 128])
    rel_bias = din("rel_bias", [32, 8])
    qs_in = din("qs", [128, 128])
    thrtab_in = din("thrtab", [128, 155])
    cmask_in = din("cmask", [128, 512])
    pw_in = din("pw", [128, NIT])
    w_ba = din("w_ba", [512, D])
    w_bp = din("w_bp", [512, D])
    w_out = din("w_out", [D, D])
    peer_wq = din("peer_wq", [D, D])
    w_pool = din("w_pool", [4, 128, 128])
    pool_scale = din("pool_scale", [512])
    norm2_g = din("norm2_g", [D])
    subkeys = din("subkeys", [2, 128, 64])
    peer_u = din("peer_u", [16384, D])
    peer_v = din("peer_v", [16384, D])
    rcnt_in = din("rcnt", [128, 4, 128])
    iota16_in = din("iota16", [128, 16])
    k_own = dout("k_own", [nq, 128, 512])
    v_own = dout("v_own", [nq, 128, 512])
    ki_own = dout("ki_own", [nq, 128, 64])
    y_own = dout("y_own", [nq, 128, D])
    u_last = dout("u_last", [128, 4, 144])
    y_s_out = dout("y_s", [128, D])
    do_sample = cfg.get("sample", True)
    if do_sample:
        xs_in = din("xs_own", [128, D])
        cache_k = din("cache_k", [2560 * 128, 512])
        cache_v = din("cache_v", [2560 * 128, 512])
        cache_ik = din("cache_ik", [2560, 8192])
        state_in = din("state_own", [16, 15, 512])
        pt_in = din("pt_own", [16, 16], I32)
        zsel_in = din("zsel", [128, 31])
        k_s_out = dout("ks_o", [128, 512])
        v_s_out = dout("vs_o", [128, 512])
        ki_s_out = dout("kis_o", [128, 64])
        pool_s_out = dout("pool_s", [16, 15, 512])
        qi_d = kb.dram("qi_d", [16, 512], F32)
        wi_d = kb.dram("wi_d", [16, 8], F32)
        q_d = kb.dram("q_d", [16, 512], F32)
        sc_d = kb.dram("sc_d", [16, 2048], F32)
    wba_bf = kb.dram("wba_bf", [128, 4, D], BF16)
    wbp_bf = kb.dram("wbp_bf", [128, 4, D], BF16)
    wout_bf = kb.dram("wout_bf", [128, 8, D], BF16)
    pwq_bf = kb.dram("pwq_bf", [128, 8, D], BF16)
    pu_bf = kb.dram("pu_bf", [16384, D], BF16)
    pv_bf = kb.dram("pv_bf", [16384, D], BF16)
    win_bf = kb.dram("win_bf", [128, 8, IN_W], BF16)
    kt_s = kb.dram("kt_s", [128, 4, nkeys], BF16)
    v_s = kb.dram("v_s", [nkeys, 8, 65], BF16)
    kit_s = kb.dram("kit_s", [128, nkeys], BF16)

    ident_f = kb.sb([128, 128], F32, "ident_f")
    ident_b = kb.sb([128, 128], BF16, "ident_b")
    g1col = kb.sb([128, 8], F32, "g1col")
    qg = kb.sb([128, 64], F32, "qg")
    kg = kb.sb([128, 64], F32, "kg")
    kb.dma("sp", ident_f[:], ident_in[:, :], writes=[ident_f])
    kb.op("dve", lambda e: e.tensor_copy(out=ident_b[:], in_=ident_f[:]), reads=[ident_f], writes=[ident_b])
    ident4 = kb.sb([128, 512], BF16, "ident4")
    for j4 in range(4):
        kb.op("dve", lambda e, j4=j4: e.tensor_copy(out=ident4[:, j4 * 128:(j4 + 1) * 128], in_=ident_f[:]),
              reads=[ident_f, ident4], writes=[ident4])
    with nc.allow_non_contiguous_dma(reason="tiny param loads"):
        kb.dma("sp", g1col[:], norm1_g.t.rearrange("(k p) -> p k", p=128), writes=[g1col])
        kb.dma("sp", qg[:], q_norm_g.t.partition_broadcast(128), writes=[qg])
        kb.dma("sp", kg[:], k_norm_g.t.partition_broadcast(128), writes=[kg])
    kb.op("dve", lambda e: e.tensor_scalar(out=qg[:], in0=qg[:], scalar1=0.125, scalar2=None, op0=ALU.mult),
          reads=[qg], writes=[qg])

    pb = [kb.ps([128, 512], F32, "bank%d" % i) for i in range(8)]

    ktc = [kb.sb([128, 4, 512], BF16, "ktc%d" % j) for j in range(2)]
    vch = [kb.sb([128, 4, 520], BF16, "vch%d" % j) for j in range(2)]
    Isc = kb.sb([128, max(nkeys, 8704)], F32, "Isc")

    class View:
        def __init__(self, owner, ap):
            self.owner = owner
            self.ap = ap

        def __getitem__(self, idx):
            return self.ap[idx]

        @property
        def w(self):
            return self.owner.w

        @w.setter
        def w(self, v):
            self.owner.w = v

        @property
        def r(self):
            return self.owner.r

        @r.setter
        def r(self, v):
            self.owner.r = v

    wst_v = [View(ktc[j], ktc[j].t[:].rearrange("p a b -> p (a b)").bitcast(F32)[:, 0:1024]) for j in range(2)]
    wsb_v = [View(vch[j], vch[j].t[:].rearrange("p a b -> p (a b)")[:, 0:1024]) for j in range(2)]
    pcount = [0]

    def conv_w(src_ap, dst_ap, ncol, scal):
        s = pcount[0] % 2
        pcount[0] += 1
        kb.dma("sp", wst_v[s][:, 0:ncol], src_ap, writes=[wst_v[s].owner])
        kb.op("dve", lambda e: e.tensor_scalar(out=wsb_v[s][:, 0:ncol], in0=wst_v[s][:, 0:ncol], scalar1=scal,
                                               scalar2=None, op0=ALU.mult),
              reads=[wst_v[s].owner, g1col], writes=[wsb_v[s].owner])
        kb.dma("sp", dst_ap, wsb_v[s][:, 0:ncol], reads=[wsb_v[s].owner], writes=[win_bf])

    for kc in range(8):
        for pc in range(5):
            conv_w(w_in[kc * 128:(kc + 1) * 128, pc * 936:(pc + 1) * 936], win_bf[:, kc, pc * 936:(pc + 1) * 936],
                   936, g1col[:, kc:kc + 1])
    if do_tail:
        for (wsrc, wdst, nkc) in ((w_ba, wba_bf, 4), (w_bp, wbp_bf, 4), (w_out, wout_bf, 8), (peer_wq, pwq_bf, 8)):
            for kc in range(nkc):
                conv_w(wsrc[kc * 128:(kc + 1) * 128, :], wdst[:, kc, :], 1024, 1.0)

    wslot = [kb.sb([128, 8, 512], BF16, "wslot%d" % i) for i in range(2)]
    wstate = {"i": 0}

    def load_w(src, c0, cw, nk=8):
        s = wslot[wstate["i"] % 2]
        wstate["i"] += 1
        kb.dma("sp", s[:, 0:nk, 0:cw], src[:, 0:nk, c0:c0 + cw], reads=[src], writes=[s])
        return s

    xt = [kb.sb([128, D], F32, "xt%d" % i) for i in range(2)]
    xn = kb.sb([128, D], F32, "xn")
    junk = kb.sb([128, D], BF16, "junk")
    ssq = kb.sb([128, 1], F32, "ssq")
    rstd = kb.sb([128, 1], F32, "rstd")
    xs = kb.sb([128, D], BF16, "xs")
    hnT = kb.sb([128, 8, 128], BF16, "hnT")
    tp_bank = pb[2]

    def norm_transpose(xb, dstT, ntok=128, gtile=None, keep=None, xowner=None):
        xo = xowner or xb
        kb.op("act", lambda e: e.activation(out=junk[0:ntok, :], in_=xb[0:ntok, :], func=AF.Square,
                                            accum_out=ssq[0:ntok, :]),
              reads=[xo], writes=[junk, ssq])
        kb.op("act", lambda e: e.activation(out=rstd[0:ntok, :], in_=ssq[0:ntok, :], func=AF.Sqrt,
                                            scale=1.0 / D, bias=EPS),
              reads=[ssq], writes=[rstd])
        kb.op("dve", lambda e: e.reciprocal(out=rstd[0:ntok, :], in_=rstd[0:ntok, :]), reads=[rstd], writes=[rstd])
        if gtile is None:
            kb.op("dve", lambda e: e.tensor_scalar(out=xs[0:ntok, :], in0=xb[0:ntok, :], scalar1=rstd[0:ntok, :],
                                                   scalar2=None, op0=ALU.mult),
                  reads=[xo, rstd], writes=[xs])
        else:
            kb.op("dve", lambda e: e.scalar_tensor_tensor(out=keep[0:ntok, :], in0=xb[0:ntok, :],
                                                          scalar=rstd[0:ntok, :], in1=gtile[0:ntok, :],
                                                          op0=ALU.mult, op1=ALU.mult),
                  reads=[xo, rstd, gtile], writes=[keep])
            kb.op("dve", lambda e: e.tensor_copy(out=xs[0:ntok, :], in_=keep[0:ntok, :]), reads=[keep], writes=[xs])
        tpv = tp_bank.t[:].bitcast(BF16)
        for kc in range(8):
            kb.op("pe", lambda e, kc=kc: e.transpose(out=tpv[:, kc * 128:kc * 128 + ntok],
                                                     in_=xs[0:ntok, kc * 128:(kc + 1) * 128],
                                                     identity=ident_b[0:ntok, 0:ntok]),
                  reads=[xs, ident_b], writes=[tp_bank])
        kb.op("dve", lambda e: e.tensor_copy(
            out=dstT[:, :, 0:ntok], in_=tpv.rearrange("p (k t) -> p k t", k=8)[:, :, 0:ntok]),
            reads=[tp_bank], writes=[dstT])

    def proj_tok(dst_bank, wsl, cw, srcT=None, ntok=128):
        srcT = srcT or hnT
        for kc in range(8):
            kb.op("pe", lambda e, kc=kc: e.matmul(dst_bank[0:ntok, 0:cw], lhsT=srcT[:, kc, 0:ntok],
                                                  rhs=wsl[:, kc, 0:cw], start=(kc == 0), stop=(kc == 7)),
                  reads=[srcT, wsl], writes=[dst_bank])

    hsq = kb.sb([128, 512], F32, "hsq")
    hss = kb.sb([128, 8], F32, "hss")
    hrs = kb.sb([128, 8], F32, "hrs")

    def head_norm(src_bank, gain, dst):
        kb.op("act", lambda e: e.activation(out=hsq[:], in_=src_bank[:, 0:512], func=AF.Square),
              reads=[src_bank], writes=[hsq])
        kb.op("dve", lambda e: e.tensor_reduce(out=hss[:], in_=hsq[:].rearrange("p (h d) -> p h d", h=8),
                                               axis=AX.X, op=ALU.add),
              reads=[hsq], writes=[hss])
        kb.op("act", lambda e: e.activation(out=hrs[:], in_=hss[:], func=AF.Sqrt, scale=1.0 / 64, bias=EPS),
              reads=[hss], writes=[hrs])
        kb.op("dve", lambda e: e.reciprocal(out=hrs[:], in_=hrs[:]), reads=[hrs], writes=[hrs])
        kb.op("dve", lambda e: e.tensor_tensor(out=hsq[:].rearrange("p (h d) -> p h d", h=8),
                                               in0=src_bank[:, 0:512].rearrange("p (h d) -> p h d", h=8),
                                               in1=hrs[:].unsqueeze(2).to_broadcast([128, 8, 64]), op=ALU.mult),
              reads=[src_bank, hrs], writes=[hsq])
        kb.op("dve", lambda e: e.tensor_tensor(out=dst[:].rearrange("p (h d) -> p h d", h=8),
                                               in0=hsq[:].rearrange("p (h d) -> p h d", h=8),
                                               in1=gain[:].unsqueeze(1).to_broadcast([128, 8, 64]), op=ALU.mult),
              reads=[hsq, gain], writes=[dst])

    wk = wslot[0]
    wv = wslot[1]
    wkw = kb.sb([128, 8, 72], BF16, "wkw")
    kb.dma("sp", wk[:], win_bf[:, :, 512:1024], reads=[win_bf], writes=[wk])
    kb.dma("sp", wv[:], win_bf[:, :, 1024:1536], reads=[win_bf], writes=[wv])
    kb.dma("sp", wkw[:], win_bf[:, :, 2048:2120], reads=[win_bf], writes=[wkw])
    kf = kb.sb([128, 512], F32, "kf")
    kbf = kb.sb([128, 512], BF16, "kbf")
    ktb = kb.sb([128, 4, 128], BF16, "ktb")
    vb = kb.sb([128, 8, 65], BF16, "vb")
    kib = kb.sb([128, 128], BF16, "kib")
    kitb = kb.sb([128, 128], BF16, "kitb")
    kb.op("dve", lambda e: e.memset(vb[:], 1.0), writes=[vb])
    def table_conv_steps():
        it = 0
        for (tsrc, tdst) in ((peer_u, pu_bf), (peer_v, pv_bf)):
            for c in range(32):
                half = it % 2
                it += 1
                fst = Isc.t[:, half * 4096:(half + 1) * 4096]
                bst = (ktc if half == 0 else vch)
                kb.dma("sp", fst, tsrc[c * 512:(c + 1) * 512, :].rearrange("(p r) d -> p (r d)", p=128),
                       writes=[Isc])
                for j2 in range(2):
                    bv = bst[j2].t[:].rearrange("p a b -> p (a b)")[:, 0:2048]
                    kb.op("act", lambda e, bv=bv, fst=fst, j2=j2: e.copy(out=bv, in_=fst[:, j2 * 2048:(j2 + 1) * 2048]),
                          reads=[Isc], writes=[bst[j2]])
                    kb.dma("sp", tdst[c * 512:(c + 1) * 512, :].rearrange("(p r) d -> p r d", p=128)[:, 2 * j2:2 * j2 + 2, :],
                           bv.rearrange("p (r d) -> p r d", r=2), reads=[bst[j2]], writes=[tdst])
                yield

    tconv = table_conv_steps() if (do_tail and do_attn) else iter(())
    for blk in range(nblk_a):
        next(tconv, None)
        xb = xt[blk % 2]
        kb.dma("sp", xb[:], xcat[blk * 128:(blk + 1) * 128, :], writes=[xb])
        norm_transpose(xb, hnT)
        proj_tok(pb[0], wk, 512)
        head_norm(pb[0], kg, kf)
        kb.op("dve", lambda e: e.tensor_copy(out=kbf[:], in_=kf[:]), reads=[kf], writes=[kbf])
        tpv = tp_bank.t[:].bitcast(BF16)
        for pr in range(4):
            kb.op("pe", lambda e, pr=pr: e.transpose(out=tpv[:, pr * 128:(pr + 1) * 128],
                                                     in_=kbf[:, pr * 128:(pr + 1) * 128], identity=ident_b[:]),
                  reads=[kbf, ident_b], writes=[tp_bank])
        kb.op("act", lambda e: e.copy(out=ktb[:], in_=tpv[:, 0:512].rearrange("p (a t) -> p a t", a=4)),
              reads=[tp_bank], writes=[ktb])
        kb.dma("sp", kt_s[:, :, blk * 128:(blk + 1) * 128], ktb[:], reads=[ktb], writes=[kt_s])
        proj_tok(pb[1], wv, 512)
        kb.op("act", lambda e: e.copy(out=vb[:, :, 0:64], in_=pb[1][:, 0:512].rearrange("p (h d) -> p h d", h=8)),
              reads=[pb[1]], writes=[vb])
        kb.dma("sp", v_s[blk * 128:(blk + 1) * 128, :, :], vb[:], reads=[vb], writes=[v_s])
        proj_tok(pb[0], wkw, 72)
        kb.op("dve", lambda e: e.tensor_copy(out=kib[:, 0:64], in_=pb[0][:, 0:64]), reads=[pb[0]], writes=[kib])
        kb.op("dve", lambda e: e.tensor_copy(out=kib[:, 64:128], in_=pb[0][:, 0:64]), reads=[pb[0]], writes=[kib])
        kb.op("pe", lambda e: e.transpose(out=tpv[:, 512:640], in_=kib[:], identity=ident_b[:]),
              reads=[kib, ident_b], writes=[tp_bank])
        kb.op("act", lambda e: e.copy(out=kitb[:], in_=tpv[:, 512:640]), reads=[tp_bank], writes=[kitb])
        kb.dma("sp", kit_s[:, blk * 128:(blk + 1) * 128], kitb[:], reads=[kitb], writes=[kit_s])

    for _ in tconv:
        pass
    ko = kb.sb([128, 512], F32, "ko")
    vo = kb.sb([128, 512], F32, "vo")
    kio = kb.sb([128, 64], F32, "kio")
    wis = kb.sb([128, 8], F32, "wis")
    dbg_ao = dout("dbg_ao", [nq, 128, 512]) if cfg.get("dbg") else None
    if do_attn:
        qf = kb.sb([128, 512], F32, "qf")
        qbf = kb.sb([128, 512], BF16, "qbf")
        qT2 = kb.sb([128, 4, 128], BF16, "qT2")
        qiT2 = kb.sb([128, 4, 128], BF16, "qiT2")
        kitc = [kb.sb([128, 512], BF16, "kitc%d" % j) for j in range(2)]
        rl = [kb.sb([128, 512], BF16, "rl%d" % j) for j in range(2)]
        junkI = kb.sb([128, 2176], BF16, "junkI")
        cnt4 = kb.sb([128, 4], F32, "cnt4")
        nmid = kb.sb([128, 1], F32, "nmid")
        hi0 = kb.sb([128, 1], F32, "hi0")
        lo = kb.sb([128, 1], F32, "lo")
        mid = kb.sb([128, 1], F32, "mid")
        cntt = kb.sb([128, 1], F32, "cntt")
        dl = kb.sb([128, 1], F32, "dl")
        wh = kb.sb([128, NIT], F32, "wh")
        pw = kb.sb([128, NIT], F32, "pw")
        cmask = kb.sb([128, 512], F32, "cmask")
        negm = [kb.sb([128, 128], BF16, "negm%d" % j) for j in range(2)]
        addh = kb.sb([128, 8, 128], BF16, "addh")
        PT = [kb.sb([128, 8, 128], BF16, "PT%d" % j) for j in range(2)]
        rden = kb.sb([128, 8], F32, "rden")
        ao = kb.sb([128, 512], BF16, "ao")
        aof = kb.sb([128, 512], F32, "aof") if cfg.get("dbg") else None
        kb.dma("sp", pw[:], pw_in[:, :], writes=[pw])
        kb.dma("sp", cmask[:], cmask_in[:, :], writes=[cmask])
        qs = kb.sb([128, 128], F32, "qs")
        thrtab = kb.sb([128, 155], F32, "thrtab")
        rbb = kb.sb([128, 256], F32, "rbb")
        ndel = kb.sb([128, 256], F32, "ndel")
        ind = kb.sb([128, 128], F32, "ind")
        NB = [kb.sb([128, 8, 128], BF16, "NB%d" % j) for j in range(5)]
        NBt = kb.sb([128, 8, 128], F32, "h2")
        h2 = View(NBt, NBt.t[:].rearrange("p a b -> p (a b)"))
        kb.dma("sp", qs[:], qs_in[:, :], writes=[qs])
        kb.dma("sp", thrtab[:], thrtab_in[:, :], writes=[thrtab])
        with nc.allow_non_contiguous_dma(reason="tiny param loads"):
            kb.dma("sp", rbb[:], rel_bias.t.rearrange("b h -> (b h)").partition_broadcast(128), writes=[rbb])
        kb.op("dve", lambda e: e.tensor_tensor(out=ndel[:, 8:256], in0=rbb[:, 0:248], in1=rbb[:, 8:256],
                                               op=ALU.subtract), reads=[rbb], writes=[ndel])
        for r5 in range(5):
            kb.op("dve", lambda e: e.memset(NBt[:], 0.0), writes=[NBt])
            for b in range(1, 32):
                col = r5 * 31 + (b - 1)
                kb.op("dve", lambda e, col=col: e.tensor_scalar(out=ind[:], in0=qs[:], scalar1=thrtab[:, col:col + 1],
                                                                scalar2=None, op0=ALU.is_lt),
                      reads=[qs, thrtab], writes=[ind])
                for h in range(8):
                    kb.op("dve", lambda e, b=b, h=h: e.scalar_tensor_tensor(
                        out=NBt[:, h, :], in0=ind[:], scalar=ndel[:, b * 8 + h:b * 8 + h + 1], in1=NBt[:, h, :],
                        op0=ALU.mult, op1=ALU.add), reads=[ind, ndel, NBt], writes=[NBt])
            kb.op("dve", lambda e, r5=r5: e.tensor_copy(out=NB[r5][:], in_=NBt[:]), reads=[NBt], writes=[NB[r5]])
    if do_tail:
        aoT = kb.sb([128, 4, 128], BF16, "aoT")
        hnTp = kb.sb([128, 8, 16], BF16, "hnTp")
        xpv = kb.sb([16, D], F32, "xpv")
        uT = kb.sb([128, 4, 144], F32, "uT")
        s1 = kb.sb([128, 4, 144], F32, "s1")
        s2 = kb.sb([128, 4, 144], F32, "s2")
        s3 = kb.sb([128, 4, 144], F32, "s3")
        pmf = kb.sb([128, 4, 128], F32, "pmf")
        pmT = kb.sb([128, 4, 128], BF16, "pmT")
        poT = kb.sb([128, 4, 128], BF16, "poT")
        sga = kb.sb([128, 8, 128], BF16, "sga")
        sgb = kb.sb([128, 8, 128], BF16, "sgb")
        tmpA = kb.sb([128, 512], F32, "tmpA")
        tmpB = kb.sb([128, 512], F32, "tmpB")
        mT = kb.sb([128, 8, 128], BF16, "mT")
        xnT = sgb
        qpT = sga
        g2b = kb.sb([128, D], F32, "g2b")
        wpl = kb.sb([128, 4, 128], BF16, "wpl")
        wplf = kb.sb([128, 4, 128], F32, "wplf")
        pscol = kb.sb([128, 4], F32, "pscol")
        SKf = kb.sb([128, 256], F32, "SKf")
        sktmp = kb.sb([128, 128], F32, "sktmp")
        SK = kb.sb([128, 256], BF16, "SK")
        rcnt = kb.sb([128, 4, 128], F32, "rcnt")
        iota16 = kb.sb([128, 16], F32, "iota16")
        tv = kb.sb([128, 8, 2, 16], F32, "tv")
        ti = kb.sb([128, 8, 2, 16], U32, "ti")
        tif = kb.sb([128, 8, 2, 16], F32, "tif")
        tsv = kb.sb([128, 8, 16], F32, "tsv")
        tpos = kb.sb([128, 8, 16], U32, "tpos")
        pa = kb.sb([128, 8, 16], U32, "pa")
        pbb = kb.sb([128, 8, 16], U32, "pbb")
        paf = kb.sb([128, 8, 16], F32, "paf")
        pbf = kb.sb([128, 8, 16], F32, "pbf")
        i1s = kb.sb([128, 8, 16], F32, "i1s")
        i2s = kb.sb([128, 8, 16], F32, "i2s")
        eidf = kb.sb([128, 128], F32, "eidf")
        eidi = kb.sb([128, 128], I32, "eidi")
        gmx = kb.sb([128, 8], F32, "gmx")
        gk = kb.sb([128, 8, 16], F32, "gk")
        actv = kb.sb([128, 128], F32, "actv")
        wgt = kb.sb([128, 128], F32, "wgt")
        m8 = kb.sb([128, 8], F32, "m8")
        UbS = [View(o, o.t[:].rearrange("p a b -> p (a b)").bitcast(F32)[:, 0:1024]) for o in (ktc[0], ktc[1], vch[0], vch[1])]
        Ubo = [kb.sb([128, D], BF16, "Ub%d" % j) for j in range(6)]
        Ub = [View(o, o.t[:]) for o in Ubo]
        kb.dma("sp", rcnt[:], rcnt_in[:, :, :], writes=[rcnt])
        kb.dma("sp", iota16[:], iota16_in[:, :], writes=[iota16])
        kb.op("dve", lambda e: e.memset(SKf[:], 0.0), writes=[SKf])
        with nc.allow_non_contiguous_dma(reason="small param loads"):
            kb.dma("sp", g2b[:], norm2_g.t.partition_broadcast(128), writes=[g2b])
            kb.dma("sp", pscol[:], pool_scale.t.rearrange("(g d) -> d g", d=128), writes=[pscol])
            kb.dma("sp", wplf[:], w_pool.t.rearrange("g c d -> c g d"), writes=[wplf])
            kb.dma("sp", sktmp[:, 0:64], subkeys.t[0], writes=[sktmp])
            kb.dma("sp", sktmp[:, 64:128], subkeys.t[1], writes=[sktmp])
        kb.op("pe", lambda e: e.transpose(out=pb[2][:, 0:128], in_=sktmp[:], identity=ident_f[:]),
              reads=[sktmp, ident_f], writes=[pb[2]])
        kb.op("dve", lambda e: e.tensor_copy(out=SKf[0:64, 0:128], in_=pb[2][0:64, 0:128]), reads=[pb[2], SKf], writes=[SKf])
        kb.op("dve", lambda e: e.tensor_copy(out=SKf[64:128, 128:256], in_=pb[2][64:128, 0:128]), reads=[pb[2], SKf],
              writes=[SKf])
        kb.op("dve", lambda e: e.tensor_copy(out=SK[:], in_=SKf[:]), reads=[SKf], writes=[SK])
        kb.op("dve", lambda e: e.tensor_copy(out=wpl[:], in_=wplf[:]), reads=[wplf], writes=[wpl])
        IscV = Isc.t[:]
        ssc = IscV[:, 0:2048]
        sscw = IscV[:, 2048:4096]
        cand = IscV[:, 4096:6144]
        candw = IscV[:, 6144:8192]
        ohv = IscV[:, 0:2048]
        ohv2 = IscV[:, 2048:4096]

    def tail_block(i, xb, sample=False):
        tpv = tp_bank.t[:].bitcast(BF16)
        for pr in range(4):
            kb.op("pe", lambda e, pr=pr: e.transpose(out=tpv[:, pr * 128:(pr + 1) * 128],
                                                     in_=ao[:, pr * 128:(pr + 1) * 128], identity=ident_b[:]),
                  reads=[ao, ident_b], writes=[tp_bank])
        kb.op("act", lambda e: e.copy(out=aoT[:], in_=tpv[:, 0:512].rearrange("p (a t) -> p a t", a=4)),
              reads=[tp_bank], writes=[aoT])
        if not sample:
            kb.dma("sp", xpv[:], xprev[i, :, :], writes=[xpv])
            norm_transpose(xpv, hnTp, ntok=16)
            wsl = load_w(win_bf, *CHUNKS[CH_U])
            for g in range(4):
                bk = pb[g // 2]
                c0 = (g % 2) * 144
                for kc in range(8):
                    kb.op("pe", lambda e, bk=bk, c0=c0, g=g, kc=kc, wsl=wsl: e.matmul(
                        bk[:, c0:c0 + 16], lhsT=wsl[:, kc, g * 128:(g + 1) * 128], rhs=hnTp[:, kc, :],
                        start=(kc == 0), stop=(kc == 7)), reads=[wsl, hnTp], writes=[bk])
                for kc in range(8):
                    kb.op("pe", lambda e, bk=bk, c0=c0, g=g, kc=kc, wsl=wsl: e.matmul(
                        bk[:, c0 + 16:c0 + 144], lhsT=wsl[:, kc, g * 128:(g + 1) * 128], rhs=hnT[:, kc, :],
                        start=(kc == 0), stop=(kc == 7)), reads=[wsl, hnT], writes=[bk])
            for hf in range(2):
                kb.op("act", lambda e, hf=hf: e.copy(out=uT[:, 2 * hf:2 * hf + 2, :],
                                                     in_=pb[hf][:, 0:288].rearrange("p (g t) -> p g t", g=2)),
                      reads=[pb[hf]], writes=[uT])
            if i == nq - 1:
                kb.dma("sp", u_last[:, :, :], uT[:], reads=[uT], writes=[u_last])
            kb.op("dve", lambda e: e.tensor_tensor(out=s1[:, :, 1:144], in0=uT[:, :, 1:144], in1=uT[:, :, 0:143],
                                                   op=ALU.add), reads=[uT], writes=[s1])
            kb.op("dve", lambda e: e.tensor_tensor(out=s2[:, 1:4, 3:144], in0=s1[:, 1:4, 3:144], in1=s1[:, 1:4, 1:142],
                                                   op=ALU.add), reads=[s1], writes=[s2])
            kb.op("dve", lambda e: e.tensor_tensor(out=s3[:, 2:4, 7:144], in0=s2[:, 2:4, 7:144], in1=s2[:, 2:4, 3:140],
                                                   op=ALU.add), reads=[s2], writes=[s3])
            kb.op("dve", lambda e: e.tensor_tensor(out=s1[:, 3, 15:144], in0=s3[:, 3, 15:144], in1=s3[:, 3, 7:136],
                                                   op=ALU.add), reads=[s3, s1], writes=[s1])
            wsum = [s1[:, 0, 16:144], s2[:, 1, 16:144], s3[:, 2, 16:144], s1[:, 3, 16:144]]
            wsrc = [s1, s2, s3, s1]
            for g in range(4):
                if i == 0:
                    kb.op("dve", lambda e, g=g: e.tensor_tensor(out=pmf[:, g, :], in0=wsum[g], in1=rcnt[:, g, :],
                                                                op=ALU.mult), reads=[wsrc[g], rcnt], writes=[pmf])
                    kb.op("dve", lambda e, g=g: e.tensor_tensor(out=pmT[:, g, :], in0=pmf[:, g, :], in1=uT[:, g, 16:144],
                                                                op=ALU.subtract), reads=[pmf, uT], writes=[pmT])
                else:
                    kb.op("dve", lambda e, g=g: e.scalar_tensor_tensor(
                        out=pmT[:, g, :], in0=wsum[g], scalar=1.0 / (2 ** (g + 1)), in1=uT[:, g, 16:144],
                        op0=ALU.mult, op1=ALU.subtract), reads=[wsrc[g], uT], writes=[pmT])
        for g in range(4):
            kb.op("pe", lambda e, g=g: e.matmul(pb[0][:, g * 128:(g + 1) * 128], lhsT=wpl[:, g, :], rhs=pmT[:, g, :],
                                                start=True, stop=True), reads=[wpl, pmT], writes=[pb[0]])
        kb.op("dve", lambda e: e.tensor_tensor(out=poT[:], in0=pb[0][:, 0:512].rearrange("p (g t) -> p g t", g=4),
                                               in1=pscol[:].unsqueeze(2).to_broadcast([128, 4, 128]), op=ALU.mult),
              reads=[pb[0], pscol], writes=[poT])
        for (chs, dst) in (((CH_GA0, CH_GA1), sga), ((CH_GB0, CH_GB1), sgb)):
            for hf, chn in enumerate(chs):
                wsl = load_w(win_bf, *CHUNKS[chn])
                bk = pb[hf]
                for ft in range(4):
                    for kc in range(8):
                        kb.op("pe", lambda e, bk=bk, ft=ft, kc=kc, wsl=wsl: e.matmul(
                            bk[:, ft * 128:(ft + 1) * 128], lhsT=wsl[:, kc, ft * 128:(ft + 1) * 128], rhs=hnT[:, kc, :],
                            start=(kc == 0), stop=(kc == 7)), reads=[wsl, hnT], writes=[bk])
                kb.op("act", lambda e, bk=bk, dst=dst, hf=hf: e.activation(
                    out=dst[:, 4 * hf:4 * hf + 4, :], in_=bk[:, 0:512].rearrange("p (f t) -> p f t", f=4),
                    func=AF.Sigmoid), reads=[bk], writes=[dst])
        for hf in range(2):
            wa = load_w(wba_bf, hf * 512, 512, nk=4)
            wb = load_w(wbp_bf, hf * 512, 512, nk=4)
            for ft in range(4):
                for kc in range(4):
                    kb.op("pe", lambda e, ft=ft, kc=kc, wa=wa: e.matmul(
                        pb[0][:, ft * 128:(ft + 1) * 128], lhsT=wa[:, kc, ft * 128:(ft + 1) * 128], rhs=aoT[:, kc, :],
                        start=(kc == 0), stop=(kc == 3)), reads=[wa, aoT], writes=[pb[0]])
                for kc in range(4):
                    kb.op("pe", lambda e, ft=ft, kc=kc, wb=wb: e.matmul(
                        pb[1][:, ft * 128:(ft + 1) * 128], lhsT=wb[:, kc, ft * 128:(ft + 1) * 128], rhs=poT[:, kc, :],
                        start=(kc == 0), stop=(kc == 3)), reads=[wb, poT], writes=[pb[1]])
            kb.op("dve", lambda e, hf=hf: e.tensor_tensor(
                out=tmpA[:], in0=pb[0][:, 0:512], in1=sga[:, 4 * hf:4 * hf + 4, :].rearrange("p f t -> p (f t)"),
                op=ALU.mult), reads=[pb[0], sga], writes=[tmpA])
            kb.op("dve", lambda e, hf=hf: e.tensor_tensor(
                out=tmpB[:], in0=pb[1][:, 0:512], in1=sgb[:, 4 * hf:4 * hf + 4, :].rearrange("p f t -> p (f t)"),
                op=ALU.mult), reads=[pb[1], sgb], writes=[tmpB])
            kb.op("dve", lambda e, hf=hf: e.tensor_tensor(
                out=mT[:, 4 * hf:4 * hf + 4, :].rearrange("p f t -> p (f t)"), in0=tmpA[:], in1=tmpB[:], op=ALU.add),
                reads=[tmpA, tmpB], writes=[mT])
        for hf in range(2):
            wo = load_w(wout_bf, hf * 512, 512, nk=8)
            for kc in range(8):
                kb.op("pe", lambda e, kc=kc, wo=wo, hf=hf: e.matmul(
                    pb[hf][:, 0:512], lhsT=mT[:, kc, :], rhs=wo[:, kc, :], start=(kc == 0), stop=(kc == 7)),
                    reads=[mT, wo], writes=[pb[hf]])
            kb.op("dve", lambda e, hf=hf: e.tensor_tensor(out=h2[:, hf * 512:(hf + 1) * 512], in0=pb[hf][:, 0:512],
                                                          in1=xb[:, hf * 512:(hf + 1) * 512], op=ALU.add),
                  reads=[pb[hf], xb], writes=[NBt])
        norm_transpose(h2, xnT, gtile=g2b, keep=xn, xowner=NBt)
        for hf in range(2):
            wq_ = load_w(pwq_bf, hf * 512, 512, nk=8)
            for ft in range(4):
                for kc in range(8):
                    kb.op("pe", lambda e, ft=ft, kc=kc, wq_=wq_, hf=hf: e.matmul(
                        pb[hf][:, ft * 128:(ft + 1) * 128], lhsT=wq_[:, kc, ft * 128:(ft + 1) * 128], rhs=xnT[:, kc, :],
                        start=(kc == 0), stop=(kc == 7)), reads=[wq_, xnT], writes=[pb[hf]])
            kb.op("act", lambda e, hf=hf: e.copy(out=qpT[:, 4 * hf:4 * hf + 4, :],
                                                 in_=pb[hf][:, 0:512].rearrange("p (f t) -> p f t", f=4)),
                  reads=[pb[hf]], writes=[qpT])
        sbanks = [pb[0], pb[1], pb[3], pb[4]]
        for h in range(8):
            bk = sbanks[h // 2]
            kb.op("pe", lambda e, bk=bk, h=h: e.matmul(bk[:, (h % 2) * 256:(h % 2) * 256 + 256], lhsT=qpT[:, h, :],
                                                      rhs=SK[:], start=True, stop=True),
                  reads=[qpT, SK], writes=[bk])
        for j4 in range(4):
            kb.op("act", lambda e, j4=j4: e.copy(out=ssc[:, j4 * 512:(j4 + 1) * 512], in_=sbanks[j4][:, 0:512]),
                  reads=[sbanks[j4]], writes=[Isc])
        tvv = tv[:].rearrange("p h s k -> p (h s) k")
        tiv = ti[:].rearrange("p h s k -> p (h s) k")
        for gi in range(16):
            sv = ssc[:, gi * 128:(gi + 1) * 128]
            sw = sscw[:, gi * 128:(gi + 1) * 128]
            kb.op("dve", lambda e, sv=sv, gi=gi: e.max(out=tvv[:, gi, 0:8], in_=sv), reads=[Isc], writes=[tv])
            kb.op("dve", lambda e, sv=sv, gi=gi: e.max_index(out=tiv[:, gi, 0:8], in_max=tvv[:, gi, 0:8], in_values=sv),
                  reads=[Isc, tv], writes=[ti])
            kb.op("dve", lambda e, sv=sv, sw=sw, gi=gi: e.match_replace(out=sw, in_to_replace=tvv[:, gi, 0:8],
                                                                       in_values=sv, imm_value=-1e30),
                  reads=[Isc, tv], writes=[Isc])
            kb.op("dve", lambda e, sw=sw, gi=gi: e.max(out=tvv[:, gi, 8:16], in_=sw), reads=[Isc], writes=[tv])
            kb.op("dve", lambda e, sw=sw, gi=gi: e.max_index(out=tiv[:, gi, 8:16], in_max=tvv[:, gi, 8:16], in_values=sw),
                  reads=[Isc, tv], writes=[ti])
        kb.op("dve", lambda e: e.tensor_copy(out=tif[:], in_=ti[:]), reads=[ti], writes=[tif])
        candv = cand.rearrange("p (h a b) -> p h a b", h=8, a=16)
        kb.op("dve", lambda e: e.tensor_tensor(out=candv, in0=tv[:, :, 0, :].unsqueeze(3).to_broadcast([128, 8, 16, 16]),
                                               in1=tv[:, :, 1, :].unsqueeze(2).to_broadcast([128, 8, 16, 16]), op=ALU.add),
              reads=[tv], writes=[Isc])
        for h in range(8):
            cv = cand[:, h * 256:(h + 1) * 256]
            cw = candw[:, h * 256:(h + 1) * 256]
            kb.op("dve", lambda e, cv=cv, h=h: e.max(out=tsv[:, h, 0:8], in_=cv), reads=[Isc], writes=[tsv])
            kb.op("dve", lambda e, cv=cv, h=h: e.max_index(out=tpos[:, h, 0:8], in_max=tsv[:, h, 0:8], in_values=cv),
                  reads=[Isc, tsv], writes=[tpos])
            kb.op("dve", lambda e, cv=cv, cw=cw, h=h: e.match_replace(out=cw, in_to_replace=tsv[:, h, 0:8],
                                                                     in_values=cv, imm_value=-1e30),
                  reads=[Isc, tsv], writes=[Isc])
            kb.op("dve", lambda e, cw=cw, h=h: e.max(out=tsv[:, h, 8:16], in_=cw), reads=[Isc], writes=[tsv])
            kb.op("dve", lambda e, cw=cw, h=h: e.max_index(out=tpos[:, h, 8:16], in_max=tsv[:, h, 8:16], in_values=cw),
                  reads=[Isc, tsv], writes=[tpos])
        kb.op("dve", lambda e: e.tensor_single_scalar(out=pa[:], in_=tpos[:], scalar=4, op=ALU.logical_shift_right),
              reads=[tpos], writes=[pa])
        kb.op("dve", lambda e: e.tensor_single_scalar(out=pbb[:], in_=tpos[:], scalar=15, op=ALU.bitwise_and),
              reads=[tpos], writes=[pbb])
        kb.op("dve", lambda e: e.tensor_copy(out=paf[:], in_=pa[:]), reads=[pa], writes=[paf])
        kb.op("dve", lambda e: e.tensor_copy(out=pbf[:], in_=pbb[:]), reads=[pbb], writes=[pbf])
        oh4 = ohv.rearrange("p (h k a) -> p h k a", h=8, k=16)
        oh42 = ohv2.rearrange("p (h k a) -> p h k a", h=8, k=16)
        io4 = iota16[:].unsqueeze(1).unsqueeze(1).to_broadcast([128, 8, 16, 16])
        for (pf, half, dst) in ((paf, 0, i1s), (pbf, 1, i2s)):
            kb.op("dve", lambda e, pf=pf: e.tensor_tensor(out=oh4, in0=pf[:].unsqueeze(3).to_broadcast([128, 8, 16, 16]),
                                                          in1=io4, op=ALU.is_equal),
                  reads=[pf, iota16], writes=[Isc])
            kb.op("dve", lambda e, half=half: e.tensor_tensor(
                out=oh42, in0=oh4, in1=tif[:, :, half, :].unsqueeze(2).to_broadcast([128, 8, 16, 16]), op=ALU.mult),
                reads=[Isc, tif], writes=[Isc])
            kb.op("dve", lambda e, dst=dst: e.tensor_reduce(out=dst[:], in_=oh42, axis=AX.X, op=ALU.add),
                  reads=[Isc], writes=[dst])
        kb.op("dve", lambda e: e.scalar_tensor_tensor(out=eidf[:].rearrange("p (h k) -> p h k", h=8), in0=i1s[:],
                                                      scalar=128.0, in1=i2s[:], op0=ALU.mult, op1=ALU.add),
              reads=[i1s, i2s], writes=[eidf])
        kb.op("dve", lambda e: e.tensor_copy(out=eidi[:], in_=eidf[:]), reads=[eidf], writes=[eidi])
        kb.op("dve", lambda e: e.tensor_reduce(out=gmx[:], in_=tsv[:], axis=AX.X, op=ALU.max), reads=[tsv], writes=[gmx])
        kb.op("dve", lambda e: e.tensor_tensor(out=gk[:], in0=tsv[:], in1=gmx[:].unsqueeze(2).to_broadcast([128, 8, 16]),
                                               op=ALU.subtract), reads=[tsv, gmx], writes=[gk])
        kb.op("act", lambda e: e.activation(out=gk[:], in_=gk[:], func=AF.Exp), reads=[gk], writes=[gk])
        kb.op("dve", lambda e: e.tensor_reduce(out=gmx[:], in_=gk[:], axis=AX.X, op=ALU.add), reads=[gk], writes=[gmx])
        kb.op("dve", lambda e: e.reciprocal(out=gmx[:], in_=gmx[:]), reads=[gmx], writes=[gmx])
        kb.op("dve", lambda e: e.tensor_tensor(out=gk[:], in0=gk[:], in1=gmx[:].unsqueeze(2).to_broadcast([128, 8, 16]),
                                               op=ALU.mult), reads=[gk, gmx], writes=[gk])
        def peer_steps():
            for hk in range(128):
                ub = Ub[hk % 6]
                kb.dma("pool", ub[:], pu_bf[:, :], reads=[eidi, pu_bf], writes=[ub.owner],
                       indirect=bass.IndirectOffsetOnAxis(ap=eidi[:, hk:hk + 1], axis=0), own_sem=hk % 6)
                kb.op("dve", lambda e, ub=ub, hk=hk: e.scalar_tensor_tensor(
                    out=ub[:], in0=ub[:], scalar=1.0, in1=xn[:], op0=ALU.mult, op1=ALU.mult,
                    accum_out=actv[:, hk:hk + 1]), reads=[ub.owner, xn], writes=[ub.owner, actv])
                yield
            kb.op("act", lambda e: e.activation(out=wgt[:], in_=actv[:], func=AF.Gelu), reads=[actv], writes=[wgt])
            kb.op("dve", lambda e: e.tensor_tensor(out=wgt[:], in0=wgt[:], in1=gk[:].rearrange("p h k -> p (h k)"),
                                                   op=ALU.mult), reads=[wgt, gk], writes=[wgt])
            for hk in range(128):
                ub = Ub[(2 + hk) % 6]
                kb.dma("pool", ub[:], pv_bf[:, :], reads=[eidi, pv_bf], writes=[ub.owner],
                       indirect=bass.IndirectOffsetOnAxis(ap=eidi[:, hk:hk + 1], axis=0), own_sem=(2 + hk) % 6)
                kb.op("dve", lambda e, ub=ub, hk=hk: e.scalar_tensor_tensor(
                    out=h2[:], in0=ub[:], scalar=wgt[:, hk:hk + 1], in1=h2[:], op0=ALU.mult, op1=ALU.add),
                    reads=[ub.owner, wgt, NBt], writes=[NBt])
                yield
            if sample:
                kb.dma("sp", y_s_out[:, :], h2[:], reads=[NBt], writes=[y_s_out])
            else:
                kb.dma("sp", y_own[i, :, :], h2[:], reads=[NBt], writes=[y_own])
            yield
        return peer_steps()

    pending = [None]
    nslots = [1]

    def advance(n=1):
        g = pending[0]
        if g is None:
            return
        for _ in range(n):
            try:
                next(g)
            except StopIteration:
                pending[0] = None
                return

    def drain():
        while pending[0] is not None:
            advance(64)

    for i in range(nq):
        xb = xt[i % 2]
        kb.dma("sp", xb[:], xown[i, :, :], writes=[xb])
        norm_transpose(xb, hnT)
        wsl = load_w(win_bf, *CHUNKS[CH_K])
        proj_tok(pb[0], wsl, 512)
        head_norm(pb[0], kg, ko)
        kb.dma("sp", k_own[i, :, :], ko[:], reads=[ko], writes=[k_own])
        wsl = load_w(win_bf, *CHUNKS[CH_V])
        proj_tok(pb[1], wsl, 512)
        kb.op("act", lambda e: e.copy(out=vo[:], in_=pb[1][:, 0:512]), reads=[pb[1]], writes=[vo])
        kb.dma("sp", v_own[i, :, :], vo[:], reads=[vo], writes=[v_own])
        wsl = load_w(win_bf, *CHUNKS[CH_KW])
        proj_tok(pb[0], wsl, 72)
        kb.op("dve", lambda e: e.tensor_copy(out=kio[:], in_=pb[0][:, 0:64]), reads=[pb[0]], writes=[kio])
        kb.dma("sp", ki_own[i, :, :], kio[:], reads=[kio], writes=[ki_own])
        kb.op("dve", lambda e: e.tensor_scalar(out=wis[:], in0=pb[0][:, 64:72], scalar1=0.125 * (8 ** -0.5),
                                               scalar2=None, op0=ALU.mult), reads=[pb[0]], writes=[wis])
        if not do_attn:
            continue
        tpv = tp_bank.t[:].bitcast(BF16)
        wsl = load_w(win_bf, *CHUNKS[CH_Q])
        proj_tok(pb[0], wsl, 512)
        head_norm(pb[0], qg, qf)
        kb.op("dve", lambda e: e.tensor_copy(out=qbf[:], in_=qf[:]), reads=[qf], writes=[qbf])
        for pr in range(4):
            kb.op("pe", lambda e, pr=pr: e.transpose(out=tpv[:, pr * 128:(pr + 1) * 128],
                                                     in_=qbf[:, pr * 128:(pr + 1) * 128], identity=ident_b[:]),
                  reads=[qbf, ident_b], writes=[tp_bank])
        kb.op("act", lambda e: e.copy(out=qT2[:], in_=tpv[:, 0:512].rearrange("p (a t) -> p a t", a=4)),
              reads=[tp_bank], writes=[qT2])
        wsl = load_w(win_bf, *CHUNKS[CH_QI])
        proj_tok(pb[1], wsl, 512)
        kb.op("act", lambda e: e.copy(out=qbf[:], in_=pb[1][:, 0:512]), reads=[pb[1]], writes=[qbf])
        for pr in range(4):
            kb.op("pe", lambda e, pr=pr: e.transpose(out=tpv[:, pr * 128:(pr + 1) * 128],
                                                     in_=qbf[:, pr * 128:(pr + 1) * 128], identity=ident_b[:]),
                  reads=[qbf, ident_b], writes=[tp_bank])
        kb.op("act", lambda e: e.copy(out=qiT2[:], in_=tpv[:, 0:512].rearrange("p (a t) -> p a t", a=4)),
              reads=[tp_bank], writes=[qiT2])
        nkb = min(4 * i + 4, nblk_a)
        nch = nkb // 4
        nk = nkb * 128
        ibanks = [pb[0], pb[1], pb[3], pb[4]]
        cnt_i = 0
        per_slot = -(-(260 - NIT * (4 if nkb * 128 >= 2048 else 1)) // (nch * 8 + nkb))
        for ch in range(nch):
            kc_ = kitc[ch % 2]
            kb.dma("sp", kc_[:], kit_s[:, ch * 512:(ch + 1) * 512], reads=[kit_s], writes=[kc_])
            Ic = Isc[:, ch * 512:(ch + 1) * 512]
            for h in range(8):
                a, half = h // 2, h % 2
                ps_ = slice(64 * half, 64 * half + 64)
                bk = ibanks[cnt_i % 4]
                rl_ = rl[cnt_i % 2]
                cnt_i += 1
                advance(per_slot)
                kb.op("pe", lambda e, bk=bk, a=a, ps_=ps_, kc_=kc_: e.matmul(
                    bk[:, 0:512], lhsT=qiT2[ps_, a, :], rhs=kc_[ps_, :], start=True, stop=True),
                    reads=[qiT2, kc_], writes=[bk])
                kb.op("act", lambda e, bk=bk, rl_=rl_: e.activation(out=rl_[:], in_=bk[:, 0:512], func=AF.Relu),
                      reads=[bk], writes=[rl_])
                if h == 0:
                    kb.op("dve", lambda e, rl_=rl_, Ic=Ic: e.tensor_scalar(
                        out=Ic, in0=rl_[:], scalar1=wis[:, 0:1], scalar2=None, op0=ALU.mult),
                        reads=[rl_, wis], writes=[Isc])
                else:
                    kb.op("dve", lambda e, rl_=rl_, Ic=Ic, h=h: e.scalar_tensor_tensor(
                        out=Ic, in0=rl_[:], scalar=wis[:, h:h + 1], in1=Ic, op0=ALU.mult, op1=ALU.add),
                        reads=[rl_, wis, Isc], writes=[Isc])
        kb.op("dve", lambda e: e.tensor_reduce(out=hi0[:], in_=Isc[:, 0:nk], axis=AX.X, op=ALU.max),
              reads=[Isc], writes=[hi0])
        kb.op("dve", lambda e: e.tensor_reduce(out=lo[:], in_=Isc[:, 0:nk], axis=AX.X, op=ALU.min),
              reads=[Isc], writes=[lo])
        kb.op("dve", lambda e: e.tensor_tensor(out=Isc[:, nk - 512:nk], in0=Isc[:, nk - 512:nk], in1=cmask[:],
                                               op=ALU.add), reads=[Isc, cmask], writes=[Isc])
        kb.op("dve", lambda e: e.tensor_tensor(out=hi0[:], in0=hi0[:], in1=lo[:], op=ALU.subtract),
              reads=[hi0, lo], writes=[hi0])
        kb.op("dve", lambda e: e.tensor_scalar(out=wh[:], in0=pw[:], scalar1=hi0[:, 0:1], scalar2=None,
                                               op0=ALU.mult), reads=[pw, hi0], writes=[wh])
        for n in range(NIT):
            kb.op("dve", lambda e, n=n: e.tensor_tensor(out=mid[:], in0=lo[:], in1=wh[:, n:n + 1], op=ALU.add),
                  reads=[lo, wh], writes=[mid])
            kb.op("dve", lambda e: e.tensor_scalar(out=nmid[:], in0=mid[:], scalar1=-1.0, scalar2=None, op0=ALU.mult),
                  reads=[mid], writes=[nmid])
            kb.op("dve", lambda e: e.memset(cnt4[:], 0.0), writes=[cnt4])
            for q4 in range((nk + 2175) // 2176):
                c0 = q4 * 2176
                c1 = min(nk, c0 + 2176)
                kb.op("act", lambda e, c0=c0, c1=c1, q4=q4: e.activation(
                    out=junkI[:, 0:c1 - c0], in_=Isc[:, c0:c1], func=AF.Sign, bias=nmid[:, 0:1], scale=1.0,
                    accum_out=cnt4[:, q4:q4 + 1]), reads=[Isc, nmid], writes=[junkI, cnt4])
            advance(4 if nk >= 2048 else 1)
            kb.op("dve", lambda e: e.tensor_reduce(out=cntt[:], in_=cnt4[:], axis=AX.X, op=ALU.add),
                  reads=[cnt4], writes=[cntt])
            kb.op("dve", lambda e, n=n: e.tensor_scalar(out=dl[:], in0=cntt[:], scalar1=511.5 - nk,
                                                        scalar2=wh[:, n:n + 1], op0=ALU.is_ge, op1=ALU.mult),
                  reads=[cntt, wh], writes=[dl])
            kb.op("dve", lambda e: e.tensor_tensor(out=lo[:], in0=lo[:], in1=dl[:], op=ALU.add),
                  reads=[lo, dl], writes=[lo])
        kb.op("dve", lambda e: e.memset(pb[5][:, 0:260], 0.0), writes=[pb[5]])
        kb.op("dve", lambda e: e.memset(pb[6][:, 0:260], 0.0), writes=[pb[6]])
        def emit_S(kbi):
            ch, kloc = kbi // 4, kbi % 4
            ktc_ = ktc[ch % 2]
            vch_ = vch[ch % 2]
            if kloc == 0:
                kb.dma("sp", ktc_[:], kt_s[:, :, ch * 512:(ch + 1) * 512], reads=[kt_s], writes=[ktc_])
                kb.dma("sp", vch_[:], v_s.t[ch * 512:(ch + 1) * 512, :, :].rearrange("(b p) h e -> p b (h e)", p=128),
                       reads=[v_s], writes=[vch_])
            nm = negm[kbi % 2]
            kb.op("dve", lambda e, nm=nm, kbi=kbi: e.tensor_scalar(
                out=nm[:], in0=Isc[:, kbi * 128:(kbi + 1) * 128], scalar1=lo[:, 0:1], scalar2=-30000.0,
                op0=ALU.is_lt, op1=ALU.mult), reads=[Isc, lo], writes=[nm])
            r = kbi - 4 * i
            near = (-1 <= r <= 3)
            if near:
                kb.op("dve", lambda e, nm=nm, r=r: e.tensor_tensor(
                    out=addh[:], in0=NB[r + 1][:], in1=nm[:].unsqueeze(1).to_broadcast([128, 8, 128]), op=ALU.add),
                    reads=[NB[r + 1], nm], writes=[addh])
            pt_ = PT[kbi % 2]
            for g in range(2):
                sb_ = pb[3 + g]
                for hh in range(4):
                    h = 4 * g + hh
                    a, half = h // 2, h % 2
                    ps_ = slice(64 * half, 64 * half + 64)
                    kb.op("pe", lambda e, sb_=sb_, hh=hh, a=a, ps_=ps_, ktc_=ktc_, kloc=kloc: e.matmul(
                        sb_[:, hh * 128:(hh + 1) * 128], lhsT=ktc_[ps_, a, kloc * 128:(kloc + 1) * 128],
                        rhs=qT2[ps_, a, :], start=True, stop=False), reads=[ktc_, qT2], writes=[sb_])
                    if near:
                        kb.op("pe", lambda e, sb_=sb_, hh=hh, h=h: e.matmul(
                            sb_[:, hh * 128:(hh + 1) * 128], lhsT=addh[:, h, :], rhs=ident_b[:],
                            start=False, stop=True), reads=[addh, ident_b], writes=[sb_])
                    else:
                        kb.op("pe", lambda e, sb_=sb_, hh=hh, nm=nm: e.matmul(
                            sb_[:, hh * 128:(hh + 1) * 128], lhsT=nm[:], rhs=ident_b[:],
                            start=False, stop=True), reads=[nm, ident_b], writes=[sb_])
                kb.op("act", lambda e, sb_=sb_, pt_=pt_, g=g: e.activation(
                    out=pt_[:, 4 * g:4 * g + 4, :], in_=sb_[:, 0:512].rearrange("p (h q) -> p h q", h=4),
                    func=AF.Exp), reads=[sb_], writes=[pt_])

        def emit_PV(kbi):
            ch, kloc = kbi // 4, kbi % 4
            vch_ = vch[ch % 2]
            pt_ = PT[kbi % 2]
            for h in range(8):
                ob = pb[5 + h // 4]
                kb.op("pe", lambda e, ob=ob, h=h, pt_=pt_, vch_=vch_, kloc=kloc: e.matmul(
                    ob[:, (h % 4) * 65:(h % 4) * 65 + 65], lhsT=pt_[:, h, :], rhs=vch_[:, kloc, h * 65:(h + 1) * 65],
                    start=False, stop=False, skip_group_check=True), reads=[pt_, vch_], writes=[ob])

        for kbi in range(nkb):
            emit_S(kbi)
            advance(per_slot)
            if kbi > 0:
                emit_PV(kbi - 1)
        emit_PV(nkb - 1)
        for g in range(2):
            ob = pb[5 + g]
            obv = ob[:, 0:260].rearrange("p (h e) -> p h e", h=4)
            kb.op("dve", lambda e, obv=obv, g=g: e.reciprocal(out=rden[:, 4 * g:4 * g + 4], in_=obv[:, :, 64]),
                  reads=[ob], writes=[rden])
            kb.op("dve", lambda e, obv=obv, g=g: e.tensor_tensor(
                out=ao[:, 256 * g:256 * g + 256].rearrange("p (h d) -> p h d", h=4), in0=obv[:, :, 0:64],
                in1=rden[:, 4 * g:4 * g + 4].unsqueeze(2).to_broadcast([128, 4, 64]), op=ALU.mult),
                reads=[ob, rden], writes=[ao])
        if dbg_ao is not None:
            kb.op("dve", lambda e: e.tensor_copy(out=aof[:], in_=ao[:]), reads=[ao], writes=[aof])
            kb.dma("sp", dbg_ao[i, :, :], aof[:], reads=[aof], writes=[dbg_ao])

        if do_tail:
            drain()
            pending[0] = tail_block(i, xb)


    drain()
    if do_sample:
        LOB = _bucket_lo()
        wrep = kb.sb([128, 8], F32, "wrep")
        idx2 = kb.sb([128, 2], I32, "idx2")
        pti = kb.sb([128, 16], I32, "pti")
        ptf = kb.sb([128, 16], F32, "ptf")
        dh = kb.sb([128, 128], F32, "dh")
        dh2 = kb.sb([128, 128], F32, "dh2")
        scs = kb.sb([128, 2, 128], F32, "scs")
        scn = kb.sb([128, 1], F32, "scn")
        dn8 = kb.sb([128, 8], F32, "dn8")
        sm8 = kb.sb([128, 8], F32, "sm8")
        tA = View(Ubo[0], Ubo[0].t[:].bitcast(F32)[:, 0:256])
        tE = View(Ubo[0], Ubo[0].t[:].bitcast(F32)[:, 256:512])
        tF = View(Ubo[1], Ubo[1].t[:].bitcast(F32)[:, 0:256])
        tG = View(Ubo[1], Ubo[1].t[:].bitcast(F32)[:, 256:512])
        idxs = View(Ubo[2], Ubo[2].t[:].bitcast(U32)[:, 0:256])
        tC = View(Ubo[2], Ubo[2].t[:].bitcast(U32)[:, 256:512])
        tD = View(Ubo[3], Ubo[3].t[:].bitcast(U32)[:, 0:256])
        rowT = kb.sb([128, 32], I32, "rowT")
        distT = kb.sb([128, 32], F32, "distT")
        negT = kb.sb([128, 32], F32, "negT")
        indT = kb.sb([128, 32], F32, "indT")
        biasT = kb.sb([128, 32, 8], F32, "biasT")
        tmp3 = kb.sb([128, 32, 8], F32, "tmp3")
        lg = kb.sb([128, 8], F32, "lg")
        pex = kb.sb([128, 8], F32, "pex")
        zsel = kb.sb([128, 31], F32, "zsel")
        numS = View(Ubo[4], Ubo[4].t[:].bitcast(F32)[:, 0:512])
        denS = kb.sb([128, 8], F32, "denS")
        seln = kb.sb([128, 1], F32, "seln")
        nb0 = kb.sb([128, 8], F32, "nb0")
        sq, sk_, sv_, ski = qf, ko, vo, kio
        sqi = hsq
        su = tmpA
        qrep = tmpB
        IscV = Isc.t[:]
        kb.dma("sp", zsel[:], zsel_in[:, :], writes=[zsel])
        kb.op("dve", lambda e: e.memset(pti[:], 0), writes=[pti])
        kb.dma("sp", pti[0:16, :], pt_in[:, :], writes=[pti])
        kb.op("dve", lambda e: e.tensor_copy(out=ptf[:], in_=pti[:]), reads=[pti], writes=[ptf])
        with nc.allow_non_contiguous_dma(reason="page table slices"):
            for jg in range(8):
                kb.dma("sp", idx2[jg * 16:(jg + 1) * 16, :], pt_in[:, 2 * jg:2 * jg + 2], writes=[idx2])
        xb = xt[0]
        kb.dma("sp", xb[:], xs_in[:, :], writes=[xb])
        norm_transpose(xb, hnT)
        wsl = load_w(win_bf, *CHUNKS[CH_Q])
        proj_tok(pb[0], wsl, 512)
        head_norm(pb[0], qg, sq)
        wsl = load_w(win_bf, *CHUNKS[CH_K])
        proj_tok(pb[1], wsl, 512)
        head_norm(pb[1], kg, sk_)
        kb.dma("sp", k_s_out[:, :], sk_[:], reads=[sk_], writes=[k_s_out])
        wsl = load_w(win_bf, *CHUNKS[CH_V])
        proj_tok(pb[0], wsl, 512)
        kb.op("act", lambda e: e.copy(out=sv_[:], in_=pb[0][:, 0:512]), reads=[pb[0]], writes=[sv_])
        kb.dma("sp", v_s_out[:, :], sv_[:], reads=[sv_], writes=[v_s_out])
        wsl = load_w(win_bf, *CHUNKS[CH_QI])
        proj_tok(pb[1], wsl, 512)
        kb.op("act", lambda e: e.copy(out=sqi[:], in_=pb[1][:, 0:512]), reads=[pb[1]], writes=[sqi])
        wsl = load_w(win_bf, *CHUNKS[CH_KW])
        proj_tok(pb[0], wsl, 72)
        kb.op("dve", lambda e: e.tensor_copy(out=ski[:], in_=pb[0][:, 0:64]), reads=[pb[0]], writes=[ski])
        kb.dma("sp", ki_s_out[:, :], ski[:], reads=[ski], writes=[ki_s_out])
        kb.op("dve", lambda e: e.tensor_scalar(out=wis[:], in0=pb[0][:, 64:72], scalar1=0.125 * (8 ** -0.5),
                                               scalar2=None, op0=ALU.mult), reads=[pb[0]], writes=[wis])
        wsl = load_w(win_bf, *CHUNKS[CH_U])
        proj_tok(pb[1], wsl, 512)
        kb.op("act", lambda e: e.copy(out=su[:], in_=pb[1][:, 0:512]), reads=[pb[1]], writes=[su])
        kb.dma("sp", pool_s_out[:, 0:14, :], state_in[:, 1:15, :], reads=[state_in], writes=[pool_s_out])
        kb.dma("sp", pool_s_out[:, 14, :], su[0:16, :], reads=[su], writes=[pool_s_out])
        kb.dma("sp", qi_d[:, :], sqi[0:16, :], reads=[sqi], writes=[qi_d])
        kb.dma("sp", wi_d[:, :], wis[0:16, :], reads=[wis], writes=[wi_d])
        kb.dma("sp", q_d[:, :], sq[0:16, :], reads=[sq], writes=[q_d])
        for jg in range(8):
            kb.dma("sp", qrep[jg * 16:(jg + 1) * 16, :], qi_d[:, :], reads=[qi_d], writes=[qrep])
            kb.dma("sp", wrep[jg * 16:(jg + 1) * 16, :], wi_d[:, :], reads=[wi_d], writes=[wrep])
        KI = IscV[:, 0:8192]
        prodc = View(ktc[0], ktc[0].t[:].rearrange("p a b -> p (a b)").bitcast(F32)[:, 0:1024])
        for s in range(2):
            kb.dma("pool", KI, cache_ik[:, :], reads=[idx2], writes=[Isc],
                   indirect=bass.IndirectOffsetOnAxis(ap=idx2[:, s:s + 1], axis=0))
            for h in range(8):
                for c8 in range(8):
                    kb.op("dve", lambda e, h=h, c8=c8: e.tensor_tensor(
                        out=prodc[:].rearrange("p (k d) -> p k d", k=16),
                        in0=KI[:, c8 * 1024:(c8 + 1) * 1024].rearrange("p (k d) -> p k d", k=16),
                        in1=qrep[:, h * 64:(h + 1) * 64].unsqueeze(1).to_broadcast([128, 16, 64]), op=ALU.mult),
                        reads=[Isc, qrep], writes=[prodc.owner])
                    kb.op("dve", lambda e, c8=c8: e.tensor_reduce(
                        out=dh[:, c8 * 16:(c8 + 1) * 16], in_=prodc[:].rearrange("p (k d) -> p k d", k=16),
                        axis=AX.X, op=ALU.add), reads=[prodc.owner], writes=[dh])
                if h == 0:
                    kb.op("dve", lambda e, s=s: e.tensor_scalar(out=scs[:, s, :], in0=dh[:], scalar1=0.0,
                                                                scalar2=wrep[:, 0:1], op0=ALU.max, op1=ALU.mult),
                          reads=[dh, wrep], writes=[scs])
                else:
                    kb.op("dve", lambda e, h=h: e.tensor_scalar(out=dh2[:], in0=dh[:], scalar1=0.0,
                                                                scalar2=wrep[:, h:h + 1], op0=ALU.max, op1=ALU.mult),
                          reads=[dh, wrep], writes=[dh2])
                    kb.op("dve", lambda e, s=s: e.tensor_tensor(out=scs[:, s, :], in0=scs[:, s, :], in1=dh2[:],
                                                                op=ALU.add), reads=[scs, dh2], writes=[scs])
        for jg in range(8):
            kb.dma("sp", sc_d[:, jg * 256:(jg + 1) * 256], scs[jg * 16:(jg + 1) * 16, :, :].rearrange("p s k -> p (s k)"),
                   reads=[scs], writes=[sc_d])
        xtmp = xn[:, 512:1024]
        kb.op("dve", lambda e: e.tensor_tensor(out=xtmp.rearrange("p (h d) -> p h d", h=8),
                                               in0=sqi[:].rearrange("p (h d) -> p h d", h=8),
                                               in1=ski[:].unsqueeze(1).to_broadcast([128, 8, 64]), op=ALU.mult),
              reads=[sqi, ski], writes=[xn])
        kb.op("dve", lambda e: e.tensor_reduce(out=dn8[:], in_=xtmp.rearrange("p (h d) -> p h d", h=8), axis=AX.X,
                                               op=ALU.add), reads=[xn], writes=[dn8])
        kb.op("dve", lambda e: e.tensor_scalar(out=dn8[:], in0=dn8[:], scalar1=0.0, scalar2=None, op0=ALU.max),
              reads=[dn8], writes=[dn8])
        kb.op("dve", lambda e: e.tensor_tensor(out=dn8[:], in0=dn8[:], in1=wis[:], op=ALU.mult),
              reads=[dn8, wis], writes=[dn8])
        kb.op("dve", lambda e: e.tensor_reduce(out=scn[:], in_=dn8[:], axis=AX.X, op=ALU.add),
              reads=[dn8], writes=[scn])
        Is = IscV[:, 0:2049]
        kb.op("dve", lambda e: e.memset(IscV[:, 0:2304], 0.0), writes=[Isc])
        kb.dma("sp", IscV[0:16, 0:2048], sc_d[:, :], reads=[sc_d], writes=[Isc])
        kb.op("dve", lambda e: e.tensor_copy(out=IscV[:, 2048:2049], in_=scn[:]), reads=[scn], writes=[Isc])
        for rd in range(32):
            kb.op("dve", lambda e: e.max(out=sm8[:], in_=Is), reads=[Isc], writes=[sm8])
            kb.op("dve", lambda e, rd=rd: e.max_index(out=idxs[:, rd * 8:(rd + 1) * 8], in_max=sm8[:], in_values=Is),
                  reads=[Isc, sm8], writes=[idxs])
            kb.op("dve", lambda e: e.match_replace(out=Is, in_to_replace=sm8[:], in_values=Is, imm_value=-1e30),
                  reads=[Isc, sm8], writes=[Isc])
        kb.op("dve", lambda e: e.tensor_copy(out=tA[:], in_=idxs[:]), reads=[idxs], writes=[tA])
        kb.op("dve", lambda e: e.tensor_scalar(out=tC[:], in0=tA[:], scalar1=2047.0, scalar2=None, op0=ALU.min),
              reads=[tA], writes=[tC])
        kb.op("dve", lambda e: e.tensor_single_scalar(out=tD[:], in_=tC[:], scalar=7, op=ALU.logical_shift_right),
              reads=[tC], writes=[tD])
        kb.op("dve", lambda e: e.tensor_single_scalar(out=tC[:], in_=tC[:], scalar=127, op=ALU.bitwise_and),
              reads=[tC], writes=[tC])
        kb.op("dve", lambda e: e.tensor_copy(out=tE[:], in_=tD[:]), reads=[tD], writes=[tE])
        kb.op("dve", lambda e: e.tensor_copy(out=tF[:], in_=tC[:]), reads=[tC], writes=[tF])
        ohs = IscV[:, 4608:8704].rearrange("p (k j) -> p k j", k=256)
        kb.op("dve", lambda e: e.tensor_tensor(out=ohs, in0=tE[:].unsqueeze(2).to_broadcast([128, 256, 16]),
                                               in1=iota16[:].unsqueeze(1).to_broadcast([128, 256, 16]), op=ALU.is_equal),
              reads=[tE, iota16], writes=[Isc])
        kb.op("dve", lambda e: e.tensor_tensor(out=ohs, in0=ohs, in1=ptf[:].unsqueeze(1).to_broadcast([128, 256, 16]),
                                               op=ALU.mult), reads=[Isc, ptf], writes=[Isc])
        kb.op("dve", lambda e: e.tensor_reduce(out=tG[:], in_=ohs, axis=AX.X, op=ALU.add), reads=[Isc], writes=[tG])
        kb.op("dve", lambda e: e.scalar_tensor_tensor(out=tG[:], in0=tG[:], scalar=128.0, in1=tF[:], op0=ALU.mult,
                                                      op1=ALU.add), reads=[tG, tF], writes=[tG])
        kb.op("dve", lambda e: e.tensor_scalar(out=tE[:], in0=tA[:], scalar1=-1.0, scalar2=2048.0, op0=ALU.mult,
                                               op1=ALU.add), reads=[tA], writes=[tE])
        kb.op("dve", lambda e: e.tensor_scalar(out=tF[:], in0=tA[:], scalar1=2047.5, scalar2=-30000.0, op0=ALU.is_ge,
                                               op1=ALU.mult), reads=[tA], writes=[tF])
        kb.op("dve", lambda e: e.tensor_reduce(out=seln[:], in_=tF[:], axis=AX.X, op=ALU.min), reads=[tF], writes=[seln])
        kb.op("dve", lambda e: e.tensor_scalar(out=seln[:], in0=seln[:], scalar1=-1.0 / 30000.0, scalar2=None,
                                               op0=ALU.mult), reads=[seln], writes=[seln])
        for (srcb, dstb) in ((tG, rowT), (tE, distT), (tF, negT)):
            for gq in range(2):
                kb.op("pe", lambda e, srcb=srcb, gq=gq: e.transpose(out=pb[2][:, gq * 128:(gq + 1) * 128],
                                                                   in_=srcb[:, gq * 128:(gq + 1) * 128],
                                                                   identity=ident_f[:]),
                      reads=[srcb, ident_f], writes=[pb[2]])
            kb.op("dve", lambda e, dstb=dstb: e.tensor_copy(
                out=dstb[:].rearrange("p (g b) -> p g b", g=2),
                in_=pb[2][:, 0:256].rearrange("p (g b) -> p g b", g=2)[:, :, 0:16]), reads=[pb[2]], writes=[dstb])
        kb.op("dve", lambda e: e.memset(biasT[:], 0.0), writes=[biasT])
        for bkt in range(1, 32):
            kb.op("dve", lambda e, bkt=bkt: e.tensor_scalar(out=indT[:], in0=distT[:], scalar1=float(LOB[bkt - 1]),
                                                            scalar2=None, op0=ALU.is_lt), reads=[distT], writes=[indT])
            kb.op("dve", lambda e, bkt=bkt: e.tensor_tensor(
                out=tmp3[:], in0=indT[:].unsqueeze(2).to_broadcast([128, 32, 8]),
                in1=ndel[:, bkt * 8:(bkt + 1) * 8].unsqueeze(1).to_broadcast([128, 32, 8]), op=ALU.mult),
                reads=[indT, ndel], writes=[tmp3])
            kb.op("dve", lambda e: e.tensor_tensor(out=biasT[:], in0=biasT[:], in1=tmp3[:], op=ALU.add),
                  reads=[biasT, tmp3], writes=[biasT])
        kb.op("dve", lambda e: e.memset(pb[5][:, 0:512], 0.0), writes=[pb[5]])
        kb.op("dve", lambda e: e.memset(pb[6][:, 0:8], 0.0), writes=[pb[6]])
        qbv = xn[:, 0:512]
        pvv = xn[:, 512:1024]
        for b in range(16):
            kb.dma("sp", qbv, q_d.t[b:b + 1, :].partition_broadcast(128).rearrange("p o d -> p (o d)"),
                   reads=[q_d], writes=[xn])
            for gq in range(2):
                col = gq * 16 + b
                kg_ = UbS[gq]
                vg_ = UbS[2 + gq]
                kb.dma("pool", kg_[:, 0:512], cache_k[:, :], reads=[rowT], writes=[kg_.owner],
                       indirect=bass.IndirectOffsetOnAxis(ap=rowT[:, col:col + 1], axis=0))
                kb.dma("pool", vg_[:, 0:512], cache_v[:, :], reads=[rowT], writes=[vg_.owner],
                       indirect=bass.IndirectOffsetOnAxis(ap=rowT[:, col:col + 1], axis=0))
                kb.op("dve", lambda e, kg_=kg_: e.tensor_tensor(out=pvv, in0=kg_[:, 0:512], in1=qbv, op=ALU.mult),
                      reads=[kg_.owner, xn], writes=[xn])
                kb.op("dve", lambda e: e.tensor_reduce(out=lg[:], in_=pvv.rearrange("p (h d) -> p h d", h=8),
                                                       axis=AX.X, op=ALU.add), reads=[xn], writes=[lg])
                kb.op("dve", lambda e, col=col: e.scalar_tensor_tensor(
                    out=lg[:], in0=lg[:], scalar=negT[:, col:col + 1], in1=biasT[:, col, :], op0=ALU.add, op1=ALU.add),
                    reads=[lg, negT, biasT], writes=[lg])
                kb.op("act", lambda e: e.activation(out=pex[:], in_=lg[:], func=AF.Exp), reads=[lg], writes=[pex])
                kb.op("dve", lambda e, vg_=vg_: e.tensor_tensor(
                    out=pvv.rearrange("p (h d) -> p h d", h=8), in0=vg_[:, 0:512].rearrange("p (h d) -> p h d", h=8),
                    in1=pex[:].unsqueeze(2).to_broadcast([128, 8, 64]), op=ALU.mult),
                    reads=[vg_.owner, pex], writes=[xn])
                kb.op("pe", lambda e, b=b: e.matmul(pb[5][0:16, 0:512], lhsT=zsel[:, 15 - b:31 - b], rhs=pvv,
                                                    start=False, stop=False, skip_group_check=True),
                      reads=[zsel, xn], writes=[pb[5]])
                kb.op("pe", lambda e, b=b: e.matmul(pb[6][0:16, 0:8], lhsT=zsel[:, 15 - b:31 - b], rhs=pex[:],
                                                    start=False, stop=False, skip_group_check=True),
                      reads=[zsel, pex], writes=[pb[6]])
        kb.op("dve", lambda e: e.memset(numS[:], 0.0), writes=[numS])
        kb.op("dve", lambda e: e.memset(denS[:], 1.0), writes=[denS])
        kb.op("dve", lambda e: e.tensor_copy(out=numS[0:16, :], in_=pb[5][0:16, 0:512]), reads=[pb[5]], writes=[numS])
        kb.op("dve", lambda e: e.tensor_copy(out=denS[0:16, :], in_=pb[6][0:16, 0:8]), reads=[pb[6]], writes=[denS])
        kb.op("dve", lambda e: e.tensor_reduce(out=nb0[:], in_=ndel[:, 8:256].rearrange("p (b h) -> p h b", h=8),
                                               axis=AX.X, op=ALU.add), reads=[ndel], writes=[nb0])
        kb.op("dve", lambda e: e.tensor_tensor(out=pvv, in0=sq[:], in1=sk_[:], op=ALU.mult), reads=[sq, sk_], writes=[xn])
        kb.op("dve", lambda e: e.tensor_reduce(out=lg[:], in_=pvv.rearrange("p (h d) -> p h d", h=8), axis=AX.X,
                                               op=ALU.add), reads=[xn], writes=[lg])
        kb.op("dve", lambda e: e.tensor_tensor(out=lg[:], in0=lg[:], in1=nb0[:], op=ALU.add), reads=[lg, nb0], writes=[lg])
        kb.op("act", lambda e: e.activation(out=pex[:], in_=lg[:], func=AF.Exp), reads=[lg], writes=[pex])
        kb.op("dve", lambda e: e.tensor_scalar(out=pex[:], in0=pex[:], scalar1=seln[:, 0:1], scalar2=None, op0=ALU.mult),
              reads=[pex, seln], writes=[pex])
        kb.op("dve", lambda e: e.tensor_tensor(out=pvv.rearrange("p (h d) -> p h d", h=8),
                                               in0=sv_[:].rearrange("p (h d) -> p h d", h=8),
                                               in1=pex[:].unsqueeze(2).to_broadcast([128, 8, 64]), op=ALU.mult),
              reads=[sv_, pex], writes=[xn])
        kb.op("dve", lambda e: e.tensor_tensor(out=numS[:], in0=numS[:], in1=pvv, op=ALU.add), reads=[numS, xn], writes=[numS])
        kb.op("dve", lambda e: e.tensor_tensor(out=denS[:], in0=denS[:], in1=pex[:], op=ALU.add), reads=[denS, pex],
              writes=[denS])
        kb.op("dve", lambda e: e.reciprocal(out=denS[:], in_=denS[:]), reads=[denS], writes=[denS])
        kb.op("dve", lambda e: e.tensor_tensor(out=ao[:].rearrange("p (h d) -> p h d", h=8),
                                               in0=numS[:].rearrange("p (h d) -> p h d", h=8),
                                               in1=denS[:].unsqueeze(2).to_broadcast([128, 8, 64]), op=ALU.mult),
              reads=[numS, denS], writes=[ao])
        stv = IscV[:, 0:7680].rearrange("p (r c) -> p r c", r=15)
        kb.op("dve", lambda e: e.memset(IscV[:, 0:7680], 0.0), writes=[Isc])
        kb.dma("sp", IscV[0:16, 0:7680], state_in.t.rearrange("b r c -> b (r c)"), reads=[state_in], writes=[Isc])
        for g in range(4):
            w = 2 ** (g + 1)
            kb.op("dve", lambda e, g=g, w=w: e.tensor_reduce(
                out=pmf[:, g, :], in_=stv[:, 16 - w:15, g * 128:(g + 1) * 128].rearrange("p r c -> p c r"),
                axis=AX.X, op=ALU.add), reads=[Isc], writes=[pmf])
            kb.op("dve", lambda e, g=g: e.tensor_tensor(out=pmf[:, g, :], in0=pmf[:, g, :], in1=su[:, g * 128:(g + 1) * 128],
                                                        op=ALU.add), reads=[pmf, su], writes=[pmf])
            kb.op("dve", lambda e, g=g, w=w: e.scalar_tensor_tensor(
                out=pmf[:, g, :], in0=pmf[:, g, :], scalar=1.0 / w, in1=su[:, g * 128:(g + 1) * 128], op0=ALU.mult,
                op1=ALU.subtract), reads=[pmf, su], writes=[pmf])
        for g in range(4):
            kb.op("pe", lambda e, g=g: e.transpose(out=pb[2][:, g * 128:(g + 1) * 128], in_=pmf[:, g, :],
                                                   identity=ident_f[:]), reads=[pmf, ident_f], writes=[pb[2]])
        kb.op("dve", lambda e: e.tensor_copy(out=pmT[:], in_=pb[2][:, 0:512].rearrange("p (g t) -> p g t", g=4)),
              reads=[pb[2]], writes=[pmT])
        pending[0] = tail_block(0, xb, sample=True)
        drain()

    kb.finish()
    return nc, kb


def _prep_inputs(inp, cfg, cores):
    nblk_a = cfg.get("nblk_a", NBLK_A)
    nq = cfg.get("nq", NQ)
    nkeys = nblk_a * 128
    xp = np.asarray(inp["x_prompt"], np.float32)
    meta = np.asarray(inp["meta_tokens"], np.float32)
    maps = []
    for c in cores:
        b, cc = c // 4, c % 4
        full = np.zeros((max(nkeys, (4 * nq + 4) * 128), D), np.float32)
        T = 16 + xp.shape[1]
        cat = np.concatenate([meta, xp[b]], axis=0)
        n = min(T, full.shape[0])
        full[:n] = cat[:n]
        xown = np.zeros((nq, 128, D), np.float32)
        xprev = np.zeros((nq, 16, D), np.float32)
        for i in range(nq):
            j = 4 * i + cc
            xown[i] = full[j * 128:(j + 1) * 128]
            if j > 0:
                xprev[i] = full[j * 128 - 16:j * 128]
        m = {
            "xcat": np.ascontiguousarray(full[:nkeys]),
            "xown": xown,
            "xprev": xprev,
            "w_in": np.ascontiguousarray(np.asarray(inp["w_in"], np.float32)[0]),
            "norm1_g": np.ascontiguousarray(np.asarray(inp["norm1_g"], np.float32)[0]),
            "q_norm_g": np.ascontiguousarray(np.asarray(inp["q_norm_g"], np.float32)[0]),
            "k_norm_g": np.ascontiguousarray(np.asarray(inp["k_norm_g"], np.float32)[0]),
            "ident": np.eye(128, dtype=np.float32),
            "rel_bias": np.ascontiguousarray(np.asarray(inp["rel_bias"], np.float32)),
            "qs": (np.arange(128, dtype=np.float32)[:, None] - np.arange(128, dtype=np.float32)[None, :]),
            "thrtab": _thrtab(cc),
            "cmask": _cmask(cc),
            "w_ba": np.ascontiguousarray(np.asarray(inp["w_branch_attn"], np.float32)[0]),
            "w_bp": np.ascontiguousarray(np.asarray(inp["w_branch_pool"], np.float32)[0]),
            "w_out": np.ascontiguousarray(np.asarray(inp["w_out"], np.float32)[0]),
            "peer_wq": np.ascontiguousarray(np.asarray(inp["peer_wq"], np.float32)[0]),
            "w_pool": np.ascontiguousarray(np.asarray(inp["w_pool"], np.float32)[0]),
            "pool_scale": np.ascontiguousarray(np.asarray(inp["pool_scale"], np.float32)[0]),
            "norm2_g": np.ascontiguousarray(np.asarray(inp["norm2_g"], np.float32)[0]),
            "subkeys": np.ascontiguousarray(np.asarray(inp["peer_subkeys"], np.float32)[0]),
            "peer_u": np.ascontiguousarray(np.asarray(inp["peer_u"], np.float32)[0]),
            "peer_v": np.ascontiguousarray(np.asarray(inp["peer_v"], np.float32)[0]),
            "rcnt": _rcnt(cc),
            "iota16": np.broadcast_to(np.arange(16, dtype=np.float32)[None, :], (128, 16)).copy(),
            "pw": np.broadcast_to((0.5 ** np.arange(1, NIT + 1)).astype(np.float32)[None, :], (128, NIT)).copy(),
        }
        if cfg.get("sample", True):
            xs = np.zeros((128, D), np.float32)
            xs[:16] = np.asarray(inp["x_sample"], np.float32)[16 * c:16 * c + 16, 0]
            z = np.zeros((128, 31), np.float32)
            z[:, 15] = 1.0
            m.update({
                "xs_own": xs,
                "cache_k": np.asarray(inp["cache_k"], np.float32).reshape(2560 * 128, 512),
                "cache_v": np.asarray(inp["cache_v"], np.float32).reshape(2560 * 128, 512),
                "cache_ik": np.asarray(inp["cache_idx_k"], np.float32).reshape(2560, 8192),
                "state_own": np.ascontiguousarray(np.asarray(inp["state_pool"], np.float32)[0, 16 * c:16 * c + 16]),
                "pt_own": np.ascontiguousarray(np.asarray(inp["page_table"], np.int32)[16 * c:16 * c + 16]),
                "zsel": z,
            })
        maps.append(m)
    return maps


def _bucket_lo():
    n = np.arange(0, 256)
    nf = np.maximum(n, 16).astype(np.float32)
    large = 16 + (np.log(nf / np.float32(16)) / np.float32(np.log(128 / 16)) * np.float32(16)).astype(np.int32)
    large = np.minimum(large, 31)
    bkt = np.where(n < 16, n, large)
    return [int(np.min(n[bkt >= b])) for b in range(1, 32)]


def _thrtab(cc):
    lo_b = _bucket_lo()
    t = np.zeros((155,), np.float32)
    for r5 in range(5):
        r = r5 - 1
        for b in range(31):
            t[r5 * 31 + b] = lo_b[b] - 128 * (cc - r)
    return np.broadcast_to(t[None, :], (128, 155)).copy()


def _rcnt(cc):
    r = np.zeros((128, 4, 128), np.float32)
    t = np.arange(128)
    for g, w in enumerate((2, 4, 8, 16)):
        cnt = np.minimum(w, t + 1) if cc == 0 else np.full(128, w)
        r[:, g, :] = (1.0 / cnt.astype(np.float32))[None, :]
    return r


def _cmask(cc):
    m = np.zeros((128, 512), np.float32)
    q = np.arange(128)[:, None]
    s = np.arange(128)[None, :]
    for r in range(4):
        if r > cc:
            m[:, r * 128:(r + 1) * 128] = -1e30
        elif r == cc:
            m[:, r * 128:(r + 1) * 128] = np.where(s > q, -1e30, 0.0)
    return m


def kernel(**inputs):
    cfg = {}
    nc = build(cfg)
    cores = list(range(8))
    maps = _prep_inputs(inputs, cfg, cores)
    res = run_bass_kernel_spmd(nc, maps, core_ids=cores)
    rs = res.results
    B, S = 2, 8192
    T = S + 16
    y_prompt = np.zeros((B, S, D), np.float32)
    k_p = np.zeros((1, B, T, 8, 64), np.float32)
    v_p = np.zeros((1, B, T, 8, 64), np.float32)
    i_p = np.zeros((1, B, T, 64), np.float32)
    pool_p = np.zeros((1, B, 15, 512), np.float32)
    for c in cores:
        b, cc = c // 4, c % 4
        r = rs[c]
        for i in range(NQ):
            j = 4 * i + cc
            p0 = j * 128
            if p0 >= T:
                continue
            p1 = min(p0 + 128, T)
            n = p1 - p0
            k_p[0, b, p0:p1] = r["k_own"][i][:n].reshape(n, 8, 64)
            v_p[0, b, p0:p1] = r["v_own"][i][:n].reshape(n, 8, 64)
            i_p[0, b, p0:p1] = r["ki_own"][i][:n]
            lo = max(p0, 16)
            y_prompt[b, lo - 16:p1 - 16] = r["y_own"][i][lo - p0:n]
        if cc == 0:
            ul = r["u_last"]
            rows = ul[:, :, 17:32]
            pool_p[0, b] = np.transpose(rows, (2, 1, 0)).reshape(15, 512)
    y_s = np.zeros((128, 1, D), np.float32)
    k_s = np.zeros((1, 128, 1, 8, 64), np.float32)
    v_s = np.zeros((1, 128, 1, 8, 64), np.float32)
    i_s = np.zeros((1, 128, 1, 64), np.float32)
    pool_s = np.zeros((1, 128, 15, 512), np.float32)
    for c in cores:
        r = rs[c]
        sl = slice(16 * c, 16 * c + 16)
        y_s[sl, 0] = r["y_s"][:16]
        k_s[0, sl, 0] = r["ks_o"][:16].reshape(16, 8, 64)
        v_s[0, sl, 0] = r["vs_o"][:16].reshape(16, 8, 64)
        i_s[0, sl, 0] = r["kis_o"][:16]
        pool_s[0, sl] = r["pool_s"]
    return (y_prompt, y_s, k_p, v_p, i_p, pool_p, k_s, v_s, i_s, pool_s)
```

```python
import numpy as np
from contextlib import ExitStack
import concourse.bass as bass
import concourse.mybir as mybir
from concourse.bass_utils import run_bass_kernel_spmd

F32 = mybir.dt.float32
BF16 = mybir.dt.bfloat16
I32 = mybir.dt.int32
U32 = mybir.dt.uint32
ALU = mybir.AluOpType
AF = mybir.ActivationFunctionType
AX = mybir.AxisListType

D = 1024
NBLK_A = 68
NQ = 17
EPS = 1e-6
IN_W = 4680
CH_Q, CH_K, CH_V, CH_QI, CH_KW, CH_U, CH_GA0, CH_GA1, CH_GB0, CH_GB1 = range(10)
CHUNKS = [(0, 512), (512, 512), (1024, 512), (1536, 512), (2048, 72), (2120, 512),
          (2632, 512), (3144, 512), (3656, 512), (4168, 512)]
N_DMA_SEMS = 12
NIT = 16


class Buf:
    def __init__(self, t, name):
        self.t = t
        self.name = name
        self.w = None
        self.r = {}

    def __getitem__(self, idx):
        return self.t[idx]


class KB:
    def __init__(self, nc, es, plan=None):
        self.nc = nc
        self.es = es
        self.plan = plan
        self.targets = {e: set() for e in ("pe", "dve", "act", "pool", "sp")}
        self.rank = None
        if plan is not None:
            self.rank = {e: {idx: r + 1 for r, idx in enumerate(sorted(plan[e]))} for e in plan}
        self.eng = {"pe": nc.tensor, "dve": nc.vector, "act": nc.scalar, "pool": nc.gpsimd, "sp": nc.sync}
        self.sem = {e: es.enter_context(nc.semaphore("s_" + e)) for e in self.eng}
        self.cnt = {e: 0 for e in self.eng}
        self.seen = {e: {} for e in self.eng}
        self.dsem = [es.enter_context(nc.semaphore("d_%d" % i)) for i in range(N_DMA_SEMS + 6)]
        self.dcnt = [0] * (N_DMA_SEMS + 6)
        self.drr = 0
        self.nbuf = 0

    def sb(self, shape, dt, name=None):
        self.nbuf += 1
        name = "sb_" + (name or ("%d" % self.nbuf))
        return Buf(self.es.enter_context(self.nc.sbuf_tensor(name, list(shape), dt)), name)

    def ps(self, shape, dt, name=None):
        self.nbuf += 1
        name = name or ("ps%d" % self.nbuf)
        return Buf(self.es.enter_context(self.nc.psum_tensor(name, list(shape), dt)), name)

    def dram(self, name, shape, dt, kind="Internal"):
        return Buf(self.nc.dram_tensor(name, list(shape), dt, kind=kind).ap(), name)

    def _deps(self, e, reads, writes):
        need = {}

        def add(tok):
            if tok is None:
                return
            key, sem, val = tok
            if key == "pe" and e == "pe":
                return
            if need.get(key, (None, 0))[1] < val:
                need[key] = (sem, val)

        for b in reads:
            add(b.w)
        for b in writes:
            add(b.w)
            for tok in b.r.values():
                add(tok)
        eo = self.eng[e]
        for key, (sem, val) in need.items():
            if self.seen[e].get(key, 0) < val:
                self.seen[e][key] = val
                if key in self.targets:
                    if self.plan is None:
                        self.targets[key].add(val)
                    else:
                        eo.wait_ge(sem, self.rank[key][val])
                elif self.plan is not None:
                    eo.wait_ge(sem, val)

    def _record(self, tok, reads, writes):
        for b in reads:
            if b.r.get(tok[0], (None, None, 0))[2] < tok[2]:
                b.r[tok[0]] = tok
        for b in writes:
            b.w = tok
            b.r = {}

    def op(self, e, fn, reads=(), writes=()):
        self._deps(e, reads, writes)
        self.cnt[e] += 1
        if self.plan is not None:
            ins = fn(self.eng[e])
            if self.cnt[e] in self.plan[e]:
                ins.then_inc(self.sem[e], 1)
        self._record((e, self.sem[e], self.cnt[e]), reads, writes)

    def dma(self, q, out, in_, reads=(), writes=(), indirect=None, own_sem=None):
        if own_sem is None:
            i = self.drr
            self.drr = (i + 1) % N_DMA_SEMS
        else:
            i = N_DMA_SEMS + own_sem
        eo = self.eng[q]
        key = "d%d" % i
        if own_sem is None and self.dcnt[i] > 0 and self.seen[q].get(key, 0) < 16 * self.dcnt[i]:
            if self.plan is not None:
                eo.wait_ge(self.dsem[i], 16 * self.dcnt[i])
            self.seen[q][key] = 16 * self.dcnt[i]
        self._deps(q, reads, writes)
        self.dcnt[i] += 1
        if self.plan is not None:
            if indirect is None:
                ins = eo.dma_start(out=out, in_=in_)
            else:
                ins = eo.indirect_dma_start(out=out, out_offset=None, in_=in_, in_offset=indirect)
            ins.then_inc(self.dsem[i], 16)
        self._record((key, self.dsem[i], 16 * self.dcnt[i]), reads, writes)

    def finish(self):
        eo = self.eng["sp"]
        if self.plan is None:
            for e in ("pe", "dve", "act", "pool"):
                if self.cnt[e] > 0:
                    self.targets[e].add(self.cnt[e])
            return
        for i in range(N_DMA_SEMS + 6):
            if self.dcnt[i] > 0:
                eo.wait_ge(self.dsem[i], 16 * self.dcnt[i])
        for e in ("pe", "dve", "act", "pool"):
            if self.cnt[e] > 0:
                eo.wait_ge(self.sem[e], self.rank[e][self.cnt[e]])


def build(cfg):
    _, kb_dry = _build(cfg, None)
    nc, _ = _build(cfg, kb_dry.targets)
    return nc


def _build(cfg, plan):
    nblk_a = cfg.get("nblk_a", NBLK_A)
    nq = cfg.get("nq", NQ)
    do_attn = cfg.get("attn", True)
    do_tail = cfg.get("tail", True)
    nkeys = nblk_a * 128
    nc = bass.Bass("TRN2", target_bir_lowering=False)
    es = ExitStack()
    kb = KB(nc, es, plan)
    nc._es_keep = es

    def din(name, shape, dt=F32):
        return Buf(nc.dram_tensor(name, list(shape), dt, kind="ExternalInput").ap(), name)

    def dout(name, shape, dt=F32):
        return Buf(nc.dram_tensor(name, list(shape), dt, kind="ExternalOutput").ap(), name)

    xcat = din("xcat", [nkeys, D])
    xown = din("xown", [nq, 128, D])
    xprev = din("xprev", [nq, 16, D])
    w_in = din("w_in", [D, IN_W])
    norm1_g = din("norm1_g", [D])
    q_norm_g = din("q_norm_g", [64])
    k_norm_g = din("k_norm_g", [64])
    ident_in = din("ident", [128, 128])
    rel_bias = din("rel_bias", [32, 8])
    qs_in = din("qs", [128, 128])
    thrtab_in = din("thrtab", [128, 155])
    cmask_in = din("cmask", [128, 512])
    pw_in = din("pw", [128, NIT])
    w_ba = din("w_ba", [512, D])
    w_bp = din("w_bp", [512, D])
    w_out = din("w_out", [D, D])
    peer_wq = din("peer_wq", [D, D])
    w_pool = din("w_pool", [4, 128, 128])
    pool_scale = din("pool_scale", [512])
    norm2_g = din("norm2_g", [D])
    subkeys = din("subkeys", [2, 128, 64])
    peer_u = din("peer_u", [16384, D])
    peer_v = din("peer_v", [16384, D])
    rcnt_in = din("rcnt", [128, 4, 128])
    iota16_in = din("iota16", [128, 16])
    k_own = dout("k_own", [nq, 128, 512])
    v_own = dout("v_own", [nq, 128, 512])
    ki_own = dout("ki_own", [nq, 128, 64])
    y_own = dout("y_own", [nq, 128, D])
    u_last = dout("u_last", [128, 4, 144])
    y_s_out = dout("y_s", [128, D])
    do_sample = cfg.get("sample", True)
    if do_sample:
        xs_in = din("xs_own", [128, D])
        cache_k = din("cache_k", [2560 * 128, 512])
        cache_v = din("cache_v", [2560 * 128, 512])
        cache_ik = din("cache_ik", [2560, 8192])
        state_in = din("state_own", [16, 15, 512])
        pt_in = din("pt_own", [16, 16], I32)
        zsel_in = din("zsel", [128, 31])
        k_s_out = dout("ks_o", [128, 512])
        v_s_out = dout("vs_o", [128, 512])
        ki_s_out = dout("kis_o", [128, 64])
        pool_s_out = dout("pool_s", [16, 15, 512])
        qi_d = kb.dram("qi_d", [16, 512], F32)
        wi_d = kb.dram("wi_d", [16, 8], F32)
        q_d = kb.dram("q_d", [16, 512], F32)
        sc_d = kb.dram("sc_d", [16, 2048], F32)
    wba_bf = kb.dram("wba_bf", [128, 4, D], BF16)
    wbp_bf = kb.dram("wbp_bf", [128, 4, D], BF16)
    wout_bf = kb.dram("wout_bf", [128, 8, D], BF16)
    pwq_bf = kb.dram("pwq_bf", [128, 8, D], BF16)
    pu_bf = kb.dram("pu_bf", [16384, D], BF16)
    pv_bf = kb.dram("pv_bf", [16384, D], BF16)
    win_bf = kb.dram("win_bf", [128, 8, IN_W], BF16)
    kt_s = kb.dram("kt_s", [128, 4, nkeys], BF16)
    v_s = kb.dram("v_s", [nkeys, 8, 65], BF16)
    kit_s = kb.dram("kit_s", [128, nkeys], BF16)

    ident_f = kb.sb([128, 128], F32, "ident_f")
    ident_b = kb.sb([128, 128], BF16, "ident_b")
    g1col = kb.sb([128, 8], F32, "g1col")
    qg = kb.sb([128, 64], F32, "qg")
    kg = kb.sb([128, 64], F32, "kg")
    kb.dma("sp", ident_f[:], ident_in[:, :], writes=[ident_f])
    kb.op("dve", lambda e: e.tensor_copy(out=ident_b[:], in_=ident_f[:]), reads=[ident_f], writes=[ident_b])
    ident4 = kb.sb([128, 512], BF16, "ident4")
    for j4 in range(4):
        kb.op("dve", lambda e, j4=j4: e.tensor_copy(out=ident4[:, j4 * 128:(j4 + 1) * 128], in_=ident_f[:]),
              reads=[ident_f, ident4], writes=[ident4])
    with nc.allow_non_contiguous_dma(reason="tiny param loads"):
        kb.dma("sp", g1col[:], norm1_g.t.rearrange("(k p) -> p k", p=128), writes=[g1col])
        kb.dma("sp", qg[:], q_norm_g.t.partition_broadcast(128), writes=[qg])
        kb.dma("sp", kg[:], k_norm_g.t.partition_broadcast(128), writes=[kg])
    kb.op("dve", lambda e: e.tensor_scalar(out=qg[:], in0=qg[:], scalar1=0.125, scalar2=None, op0=ALU.mult),
          reads=[qg], writes=[qg])

    pb = [kb.ps([128, 512], F32, "bank%d" % i) for i in range(8)]

    ktc = [kb.sb([128, 4, 512], BF16, "ktc%d" % j) for j in range(2)]
    vch = [kb.sb([128, 4, 520], BF16, "vch%d" % j) for j in range(2)]
    Isc = kb.sb([128, max(nkeys, 8704)], F32, "Isc")

    class View:
        def __init__(self, owner, ap):
            self.owner = owner
            self.ap = ap

        def __getitem__(self, idx):
            return self.ap[idx]

        @property
        def w(self):
            return self.owner.w

        @w.setter
        def w(self, v):
            self.owner.w = v

        @property
        def r(self):
            return self.owner.r

        @r.setter
        def r(self, v):
            self.owner.r = v

    wst_v = [View(ktc[j], ktc[j].t[:].rearrange("p a b -> p (a b)").bitcast(F32)[:, 0:1024]) for j in range(2)]
    wsb_v = [View(vch[j], vch[j].t[:].rearrange("p a b -> p (a b)")[:, 0:1024]) for j in range(2)]
    pcount = [0]

    def conv_w(src_ap, dst_ap, ncol, scal):
        s = pcount[0] % 2
        pcount[0] += 1
        kb.dma("sp", wst_v[s][:, 0:ncol], src_ap, writes=[wst_v[s].owner])
        kb.op("dve", lambda e: e.tensor_scalar(out=wsb_v[s][:, 0:ncol], in0=wst_v[s][:, 0:ncol], scalar1=scal,
                                               scalar2=None, op0=ALU.mult),
              reads=[wst_v[s].owner, g1col], writes=[wsb_v[s].owner])
        kb.dma("sp", dst_ap, wsb_v[s][:, 0:ncol], reads=[wsb_v[s].owner], writes=[win_bf])

    for kc in range(8):
        for pc in range(5):
            conv_w(w_in[kc * 128:(kc + 1) * 128, pc * 936:(pc + 1) * 936], win_bf[:, kc, pc * 936:(pc + 1) * 936],
                   936, g1col[:, kc:kc + 1])
    if do_tail:
        for (wsrc, wdst, nkc) in ((w_ba, wba_bf, 4), (w_bp, wbp_bf, 4), (w_out, wout_bf, 8), (peer_wq, pwq_bf, 8)):
            for kc in range(nkc):
                conv_w(wsrc[kc * 128:(kc + 1) * 128, :], wdst[:, kc, :], 1024, 1.0)

    wslot = [kb.sb([128, 8, 512], BF16, "wslot%d" % i) for i in range(2)]
    wstate = {"i": 0}

    def load_w(src, c0, cw, nk=8):
        s = wslot[wstate["i"] % 2]
        wstate["i"] += 1
        kb.dma("sp", s[:, 0:nk, 0:cw], src[:, 0:nk, c0:c0 + cw], reads=[src], writes=[s])
        return s

    xt = [kb.sb([128, D], F32, "xt%d" % i) for i in range(2)]
    xn = kb.sb([128, D], F32, "xn")
    junk = kb.sb([128, D], BF16, "junk")
    ssq = kb.sb([128, 1], F32, "ssq")
    rstd = kb.sb([128, 1], F32, "rstd")
    xs = kb.sb([128, D], BF16, "xs")
    hnT = kb.sb([128, 8, 128], BF16, "hnT")
    tp_bank = pb[2]

    def norm_transpose(xb, dstT, ntok=128, gtile=None, keep=None, xowner=None):
        xo = xowner or xb
        kb.op("act", lambda e: e.activation(out=junk[0:ntok, :], in_=xb[0:ntok, :], func=AF.Square,
                                            accum_out=ssq[0:ntok, :]),
              reads=[xo], writes=[junk, ssq])
        kb.op("act", lambda e: e.activation(out=rstd[0:ntok, :], in_=ssq[0:ntok, :], func=AF.Sqrt,
                                            scale=1.0 / D, bias=EPS),
              reads=[ssq], writes=[rstd])
        kb.op("dve", lambda e: e.reciprocal(out=rstd[0:ntok, :], in_=rstd[0:ntok, :]), reads=[rstd], writes=[rstd])
        if gtile is None:
            kb.op("dve", lambda e: e.tensor_scalar(out=xs[0:ntok, :], in0=xb[0:ntok, :], scalar1=rstd[0:ntok, :],
                                                   scalar2=None, op0=ALU.mult),
                  reads=[xo, rstd], writes=[xs])
        else:
            kb.op("dve", lambda e: e.scalar_tensor_tensor(out=keep[0:ntok, :], in0=xb[0:ntok, :],
                                                          scalar=rstd[0:ntok, :], in1=gtile[0:ntok, :],
                                                          op0=ALU.mult, op1=ALU.mult),
                  reads=[xo, rstd, gtile], writes=[keep])
            kb.op("dve", lambda e: e.tensor_copy(out=xs[0:ntok, :], in_=keep[0:ntok, :]), reads=[keep], writes=[xs])
        tpv = tp_bank.t[:].bitcast(BF16)
        for kc in range(8):
            kb.op("pe", lambda e, kc=kc: e.transpose(out=tpv[:, kc * 128:kc * 128 + ntok],
                                                     in_=xs[0:ntok, kc * 128:(kc + 1) * 128],
                                                     identity=ident_b[0:ntok, 0:ntok]),
                  reads=[xs, ident_b], writes=[tp_bank])
        kb.op("dve", lambda e: e.tensor_copy(
            out=dstT[:, :, 0:ntok], in_=tpv.rearrange("p (k t) -> p k t", k=8)[:, :, 0:ntok]),
            reads=[tp_bank], writes=[dstT])

    def proj_tok(dst_bank, wsl, cw, srcT=None, ntok=128):
        srcT = srcT or hnT
        for kc in range(8):
            kb.op("pe", lambda e, kc=kc: e.matmul(dst_bank[0:ntok, 0:cw], lhsT=srcT[:, kc, 0:ntok],
                                                  rhs=wsl[:, kc, 0:cw], start=(kc == 0), stop=(kc == 7)),
                  reads=[srcT, wsl], writes=[dst_bank])

    hsq = kb.sb([128, 512], F32, "hsq")
    hss = kb.sb([128, 8], F32, "hss")
    hrs = kb.sb([128, 8], F32, "hrs")

    def head_norm(src_bank, gain, dst):
        kb.op("act", lambda e: e.activation(out=hsq[:], in_=src_bank[:, 0:512], func=AF.Square),
              reads=[src_bank], writes=[hsq])
        kb.op("dve", lambda e: e.tensor_reduce(out=hss[:], in_=hsq[:].rearrange("p (h d) -> p h d", h=8),
                                               axis=AX.X, op=ALU.add),
              reads=[hsq], writes=[hss])
        kb.op("act", lambda e: e.activation(out=hrs[:], in_=hss[:], func=AF.Sqrt, scale=1.0 / 64, bias=EPS),
              reads=[hss], writes=[hrs])
        kb.op("dve", lambda e: e.reciprocal(out=hrs[:], in_=hrs[:]), reads=[hrs], writes=[hrs])
        kb.op("dve", lambda e: e.tensor_tensor(out=hsq[:].rearrange("p (h d) -> p h d", h=8),
                                               in0=src_bank[:, 0:512].rearrange("p (h d) -> p h d", h=8),
                                               in1=hrs[:].unsqueeze(2).to_broadcast([128, 8, 64]), op=ALU.mult),
              reads=[src_bank, hrs], writes=[hsq])
        kb.op("dve", lambda e: e.tensor_tensor(out=dst[:].rearrange("p (h d) -> p h d", h=8),
                                               in0=hsq[:].rearrange("p (h d) -> p h d", h=8),
                                               in1=gain[:].unsqueeze(1).to_broadcast([128, 8, 64]), op=ALU.mult),
              reads=[hsq, gain], writes=[dst])

    wk = wslot[0]
    wv = wslot[1]
    wkw = kb.sb([128, 8, 72], BF16, "wkw")
    kb.dma("sp", wk[:], win_bf[:, :, 512:1024], reads=[win_bf], writes=[wk])
    kb.dma("sp", wv[:], win_bf[:, :, 1024:1536], reads=[win_bf], writes=[wv])
    kb.dma("sp", wkw[:], win_bf[:, :, 2048:2120], reads=[win_bf], writes=[wkw])
    kf = kb.sb([128, 512], F32, "kf")
    kbf = kb.sb([128, 512], BF16, "kbf")
    ktb = kb.sb([128, 4, 128], BF16, "ktb")
    vb = kb.sb([128, 8, 65], BF16, "vb")
    kib = kb.sb([128, 128], BF16, "kib")
    kitb = kb.sb([128, 128], BF16, "kitb")
    kb.op("dve", lambda e: e.memset(vb[:], 1.0), writes=[vb])
    def table_conv_steps():
        it = 0
        for (tsrc, tdst) in ((peer_u, pu_bf), (peer_v, pv_bf)):
            for c in range(32):
                half = it % 2
                it += 1
                fst = Isc.t[:, half * 4096:(half + 1) * 4096]
                bst = (ktc if half == 0 else vch)
                kb.dma("sp", fst, tsrc[c * 512:(c + 1) * 512, :].rearrange("(p r) d -> p (r d)", p=128),
                       writes=[Isc])
                for j2 in range(2):
                    bv = bst[j2].t[:].rearrange("p a b -> p (a b)")[:, 0:2048]
                    kb.op("act", lambda e, bv=bv, fst=fst, j2=j2: e.copy(out=bv, in_=fst[:, j2 * 2048:(j2 + 1) * 2048]),
                          reads=[Isc], writes=[bst[j2]])
                    kb.dma("sp", tdst[c * 512:(c + 1) * 512, :].rearrange("(p r) d -> p r d", p=128)[:, 2 * j2:2 * j2 + 2, :],
                           bv.rearrange("p (r d) -> p r d", r=2), reads=[bst[j2]], writes=[tdst])
                yield

    tconv = table_conv_steps() if (do_tail and do_attn) else iter(())
    for blk in range(nblk_a):
        next(tconv, None)
        xb = xt[blk % 2]
        kb.dma("sp", xb[:], xcat[blk * 128:(blk + 1) * 128, :], writes=[xb])
        norm_transpose(xb, hnT)
        proj_tok(pb[0], wk, 512)
        head_norm(pb[0], kg, kf)
        kb.op("dve", lambda e: e.tensor_copy(out=kbf[:], in_=kf[:]), reads=[kf], writes=[kbf])
        tpv = tp_bank.t[:].bitcast(BF16)
        for pr in range(4):
            kb.op("pe", lambda e, pr=pr: e.transpose(out=tpv[:, pr * 128:(pr + 1) * 128],
                                                     in_=kbf[:, pr * 128:(pr + 1) * 128], identity=ident_b[:]),
                  reads=[kbf, ident_b], writes=[tp_bank])
        kb.op("act", lambda e: e.copy(out=ktb[:], in_=tpv[:, 0:512].rearrange("p (a t) -> p a t", a=4)),
              reads=[tp_bank], writes=[ktb])
        kb.dma("sp", kt_s[:, :, blk * 128:(blk + 1) * 128], ktb[:], reads=[ktb], writes=[kt_s])
        proj_tok(pb[1], wv, 512)
        kb.op("act", lambda e: e.copy(out=vb[:, :, 0:64], in_=pb[1][:, 0:512].rearrange("p (h d) -> p h d", h=8)),
              reads=[pb[1]], writes=[vb])
        kb.dma("sp", v_s[blk * 128:(blk + 1) * 128, :, :], vb[:], reads=[vb], writes=[v_s])
        proj_tok(pb[0], wkw, 72)
        kb.op("dve", lambda e: e.tensor_copy(out=kib[:, 0:64], in_=pb[0][:, 0:64]), reads=[pb[0]], writes=[kib])
        kb.op("dve", lambda e: e.tensor_copy(out=kib[:, 64:128], in_=pb[0][:, 0:64]), reads=[pb[0]], writes=[kib])
        kb.op("pe", lambda e: e.transpose(out=tpv[:, 512:640], in_=kib[:], identity=ident_b[:]),
              reads=[kib, ident_b], writes=[tp_bank])
        kb.op("act", lambda e: e.copy(out=kitb[:], in_=tpv[:, 512:640]), reads=[tp_bank], writes=[kitb])
        kb.dma("sp", kit_s[:, blk * 128:(blk + 1) * 128], kitb[:], reads=[kitb], writes=[kit_s])

    for _ in tconv:
        pass
    ko = kb.sb([128, 512], F32, "ko")
    vo = kb.sb([128, 512], F32, "vo")
    kio = kb.sb([128, 64], F32, "kio")
    wis = kb.sb([128, 8], F32, "wis")
    dbg_ao = dout("dbg_ao", [nq, 128, 512]) if cfg.get("dbg") else None
    if do_attn:
        qf = kb.sb([128, 512], F32, "qf")
        qbf = kb.sb([128, 512], BF16, "qbf")
        qT2 = kb.sb([128, 4, 128], BF16, "qT2")
        qiT2 = kb.sb([128, 4, 128], BF16, "qiT2")
        kitc = [kb.sb([128, 512], BF16, "kitc%d" % j) for j in range(2)]
        rl = [kb.sb([128, 512], BF16, "rl%d" % j) for j in range(2)]
        junkI = kb.sb([128, 2176], BF16, "junkI")
        cnt4 = kb.sb([128, 4], F32, "cnt4")
        nmid = kb.sb([128, 1], F32, "nmid")
        hi0 = kb.sb([128, 1], F32, "hi0")
        lo = kb.sb([128, 1], F32, "lo")
        mid = kb.sb([128, 1], F32, "mid")
        cntt = kb.sb([128, 1], F32, "cntt")
        dl = kb.sb([128, 1], F32, "dl")
        wh = kb.sb([128, NIT], F32, "wh")
        pw = kb.sb([128, NIT], F32, "pw")
        cmask = kb.sb([128, 512], F32, "cmask")
        negm = [kb.sb([128, 128], BF16, "negm%d" % j) for j in range(2)]
        addh = kb.sb([128, 8, 128], BF16, "addh")
        PT = [kb.sb([128, 8, 128], BF16, "PT%d" % j) for j in range(2)]
        rden = kb.sb([128, 8], F32, "rden")
        ao = kb.sb([128, 512], BF16, "ao")
        aof = kb.sb([128, 512], F32, "aof") if cfg.get("dbg") else None
        kb.dma("sp", pw[:], pw_in[:, :], writes=[pw])
        kb.dma("sp", cmask[:], cmask_in[:, :], writes=[cmask])
        qs = kb.sb([128, 128], F32, "qs")
        thrtab = kb.sb([128, 155], F32, "thrtab")
        rbb = kb.sb([128, 256], F32, "rbb")
        ndel = kb.sb([128, 256], F32, "ndel")
        ind = kb.sb([128, 128], F32, "ind")
        NB = [kb.sb([128, 8, 128], BF16, "NB%d" % j) for j in range(5)]
        NBt = kb.sb([128, 8, 128], F32, "h2")
        h2 = View(NBt, NBt.t[:].rearrange("p a b -> p (a b)"))
        kb.dma("sp", qs[:], qs_in[:, :], writes=[qs])
        kb.dma("sp", thrtab[:], thrtab_in[:, :], writes=[thrtab])
        with nc.allow_non_contiguous_dma(reason="tiny param loads"):
            kb.dma("sp", rbb[:], rel_bias.t.rearrange("b h -> (b h)").partition_broadcast(128), writes=[rbb])
        kb.op("dve", lambda e: e.tensor_tensor(out=ndel[:, 8:256], in0=rbb[:, 0:248], in1=rbb[:, 8:256],
                                               op=ALU.subtract), reads=[rbb], writes=[ndel])
        for r5 in range(5):
            kb.op("dve", lambda e: e.memset(NBt[:], 0.0), writes=[NBt])
            for b in range(1, 32):
                col = r5 * 31 + (b - 1)
                kb.op("dve", lambda e, col=col: e.tensor_scalar(out=ind[:], in0=qs[:], scalar1=thrtab[:, col:col + 1],
                                                                scalar2=None, op0=ALU.is_lt),
                      reads=[qs, thrtab], writes=[ind])
                for h in range(8):
                    kb.op("dve", lambda e, b=b, h=h: e.scalar_tensor_tensor(
                        out=NBt[:, h, :], in0=ind[:], scalar=ndel[:, b * 8 + h:b * 8 + h + 1], in1=NBt[:, h, :],
                        op0=ALU.mult, op1=ALU.add), reads=[ind, ndel, NBt], writes=[NBt])
            kb.op("dve", lambda e, r5=r5: e.tensor_copy(out=NB[r5][:], in_=NBt[:]), reads=[NBt], writes=[NB[r5]])
    if do_tail:
        aoT = kb.sb([128, 4, 128], BF16, "aoT")
        hnTp = kb.sb([128, 8, 16], BF16, "hnTp")
        xpv = kb.sb([16, D], F32, "xpv")
        uT = kb.sb([128, 4, 144], F32, "uT")
        s1 = kb.sb([128, 4, 144], F32, "s1")
        s2 = kb.sb([128, 4, 144], F32, "s2")
        s3 = kb.sb([128, 4, 144], F32, "s3")
        pmf = kb.sb([128, 4, 128], F32, "pmf")
        pmT = kb.sb([128, 4, 128], BF16, "pmT")
        poT = kb.sb([128, 4, 128], BF16, "poT")
        sga = kb.sb([128, 8, 128], BF16, "sga")
        sgb = kb.sb([128, 8, 128], BF16, "sgb")
        tmpA = kb.sb([128, 512], F32, "tmpA")
        tmpB = kb.sb([128, 512], F32, "tmpB")
        mT = kb.sb([128, 8, 128], BF16, "mT")
        xnT = sgb
        qpT = sga
        g2b = kb.sb([128, D], F32, "g2b")
        wpl = kb.sb([128, 4, 128], BF16, "wpl")
        wplf = kb.sb([128, 4, 128], F32, "wplf")
        pscol = kb.sb([128, 4], F32, "pscol")
        SKf = kb.sb([128, 256], F32, "SKf")
        sktmp = kb.sb([128, 128], F32, "sktmp")
        SK = kb.sb([128, 256], BF16, "SK")
        rcnt = kb.sb([128, 4, 128], F32, "rcnt")
        iota16 = kb.sb([128, 16], F32, "iota16")
        tv = kb.sb([128, 8, 2, 16], F32, "tv")
        ti = kb.sb([128, 8, 2, 16], U32, "ti")
        tif = kb.sb([128, 8, 2, 16], F32, "tif")
        tsv = kb.sb([128, 8, 16], F32, "tsv")
        tpos = kb.sb([128, 8, 16], U32, "tpos")
        pa = kb.sb([128, 8, 16], U32, "pa")
        pbb = kb.sb([128, 8, 16], U32, "pbb")
        paf = kb.sb([128, 8, 16], F32, "paf")
        pbf = kb.sb([128, 8, 16], F32, "pbf")
        i1s = kb.sb([128, 8, 16], F32, "i1s")
        i2s = kb.sb([128, 8, 16], F32, "i2s")
        eidf = kb.sb([128, 128], F32, "eidf")
        eidi = kb.sb([128, 128], I32, "eidi")
        gmx = kb.sb([128, 8], F32, "gmx")
        gk = kb.sb([128, 8, 16], F32, "gk")
        actv = kb.sb([128, 128], F32, "actv")
        wgt = kb.sb([128, 128], F32, "wgt")
        m8 = kb.sb([128, 8], F32, "m8")
        UbS = [View(o, o.t[:].rearrange("p a b -> p (a b)").bitcast(F32)[:, 0:1024]) for o in (ktc[0], ktc[1], vch[0], vch[1])]
        Ubo = [kb.sb([128, D], BF16, "Ub%d" % j) for j in range(6)]
        Ub = [View(o, o.t[:]) for o in Ubo]
        kb.dma("sp", rcnt[:], rcnt_in[:, :, :], writes=[rcnt])
        kb.dma("sp", iota16[:], iota16_in[:, :], writes=[iota16])
        kb.op("dve", lambda e: e.memset(SKf[:], 0.0), writes=[SKf])
        with nc.allow_non_contiguous_dma(reason="small param loads"):
            kb.dma("sp", g2b[:], norm2_g.t.partition_broadcast(128), writes=[g2b])
            kb.dma("sp", pscol[:], pool_scale.t.rearrange("(g d) -> d g", d=128), writes=[pscol])
            kb.dma("sp", wplf[:], w_pool.t.rearrange("g c d -> c g d"), writes=[wplf])
            kb.dma("sp", sktmp[:, 0:64], subkeys.t[0], writes=[sktmp])
            kb.dma("sp", sktmp[:, 64:128], subkeys.t[1], writes=[sktmp])
        kb.op("pe", lambda e: e.transpose(out=pb[2][:, 0:128], in_=sktmp[:], identity=ident_f[:]),
              reads=[sktmp, ident_f], writes=[pb[2]])
        kb.op("dve", lambda e: e.tensor_copy(out=SKf[0:64, 0:128], in_=pb[2][0:64, 0:128]), reads=[pb[2], SKf], writes=[SKf])
        kb.op("dve", lambda e: e.tensor_copy(out=SKf[64:128, 128:256], in_=pb[2][64:128, 0:128]), reads=[pb[2], SKf],
              writes=[SKf])
        kb.op("dve", lambda e: e.tensor_copy(out=SK[:], in_=SKf[:]), reads=[SKf], writes=[SK])
        kb.op("dve", lambda e: e.tensor_copy(out=wpl[:], in_=wplf[:]), reads=[wplf], writes=[wpl])
        IscV = Isc.t[:]
        ssc = IscV[:, 0:2048]
        sscw = IscV[:, 2048:4096]
        cand = IscV[:, 4096:6144]
        candw = IscV[:, 6144:8192]
        ohv = IscV[:, 0:2048]
        ohv2 = IscV[:, 2048:4096]

    def tail_block(i, xb, sample=False):
        tpv = tp_bank.t[:].bitcast(BF16)
        for pr in range(4):
            kb.op("pe", lambda e, pr=pr: e.transpose(out=tpv[:, pr * 128:(pr + 1) * 128],
                                                     in_=ao[:, pr * 128:(pr + 1) * 128], identity=ident_b[:]),
                  reads=[ao, ident_b], writes=[tp_bank])
        kb.op("act", lambda e: e.copy(out=aoT[:], in_=tpv[:, 0:512].rearrange("p (a t) -> p a t", a=4)),
              reads=[tp_bank], writes=[aoT])
        if not sample:
            kb.dma("sp", xpv[:], xprev[i, :, :], writes=[xpv])
            norm_transpose(xpv, hnTp, ntok=16)
            wsl = load_w(win_bf, *CHUNKS[CH_U])
            for g in range(4):
                bk = pb[g // 2]
                c0 = (g % 2) * 144
                for kc in range(8):
                    kb.op("pe", lambda e, bk=bk, c0=c0, g=g, kc=kc, wsl=wsl: e.matmul(
                        bk[:, c0:c0 + 16], lhsT=wsl[:, kc, g * 128:(g + 1) * 128], rhs=hnTp[:, kc, :],
                        start=(kc == 0), stop=(kc == 7)), reads=[wsl, hnTp], writes=[bk])
                for kc in range(8):
                    kb.op("pe", lambda e, bk=bk, c0=c0, g=g, kc=kc, wsl=wsl: e.matmul(
                        bk[:, c0 + 16:c0 + 144], lhsT=wsl[:, kc, g * 128:(g + 1) * 128], rhs=hnT[:, kc, :],
                        start=(kc == 0), stop=(kc == 7)), reads=[wsl, hnT], writes=[bk])
            for hf in range(2):
                kb.op("act", lambda e, hf=hf: e.copy(out=uT[:, 2 * hf:2 * hf + 2, :],
                                                     in_=pb[hf][:, 0:288].rearrange("p (g t) -> p g t", g=2)),
                      reads=[pb[hf]], writes=[uT])
            if i == nq - 1:
                kb.dma("sp", u_last[:, :, :], uT[:], reads=[uT], writes=[u_last])
            kb.op("dve", lambda e: e.tensor_tensor(out=s1[:, :, 1:144], in0=uT[:, :, 1:144], in1=uT[:, :, 0:143],
                                                   op=ALU.add), reads=[uT], writes=[s1])
            kb.op("dve", lambda e: e.tensor_tensor(out=s2[:, 1:4, 3:144], in0=s1[:, 1:4, 3:144], in1=s1[:, 1:4, 1:142],
                                                   op=ALU.add), reads=[s1], writes=[s2])
            kb.op("dve", lambda e: e.tensor_tensor(out=s3[:, 2:4, 7:144], in0=s2[:, 2:4, 7:144], in1=s2[:, 2:4, 3:140],
                                                   op=ALU.add), reads=[s2], writes=[s3])
            kb.op("dve", lambda e: e.tensor_tensor(out=s1[:, 3, 15:144], in0=s3[:, 3, 15:144], in1=s3[:, 3, 7:136],
                                                   op=ALU.add), reads=[s3, s1], writes=[s1])
            wsum = [s1[:, 0, 16:144], s2[:, 1, 16:144], s3[:, 2, 16:144], s1[:, 3, 16:144]]
            wsrc = [s1, s2, s3, s1]
            for g in range(4):
                if i == 0:
                    kb.op("dve", lambda e, g=g: e.tensor_tensor(out=pmf[:, g, :], in0=wsum[g], in1=rcnt[:, g, :],
                                                                op=ALU.mult), reads=[wsrc[g], rcnt], writes=[pmf])
                    kb.op("dve", lambda e, g=g: e.tensor_tensor(out=pmT[:, g, :], in0=pmf[:, g, :], in1=uT[:, g, 16:144],
                                                                op=ALU.subtract), reads=[pmf, uT], writes=[pmT])
                else:
                    kb.op("dve", lambda e, g=g: e.scalar_tensor_tensor(
                        out=pmT[:, g, :], in0=wsum[g], scalar=1.0 / (2 ** (g + 1)), in1=uT[:, g, 16:144],
                        op0=ALU.mult, op1=ALU.subtract), reads=[wsrc[g], uT], writes=[pmT])
        for g in range(4):
            kb.op("pe", lambda e, g=g: e.matmul(pb[0][:, g * 128:(g + 1) * 128], lhsT=wpl[:, g, :], rhs=pmT[:, g, :],
                                                start=True, stop=True), reads=[wpl, pmT], writes=[pb[0]])
        kb.op("dve", lambda e: e.tensor_tensor(out=poT[:], in0=pb[0][:, 0:512].rearrange("p (g t) -> p g t", g=4),
                                               in1=pscol[:].unsqueeze(2).to_broadcast([128, 4, 128]), op=ALU.mult),
              reads=[pb[0], pscol], writes=[poT])
        for (chs, dst) in (((CH_GA0, CH_GA1), sga), ((CH_GB0, CH_GB1), sgb)):
            for hf, chn in enumerate(chs):
                wsl = load_w(win_bf, *CHUNKS[chn])
                bk = pb[hf]
                for ft in range(4):
                    for kc in range(8):
                        kb.op("pe", lambda e, bk=bk, ft=ft, kc=kc, wsl=wsl: e.matmul(
                            bk[:, ft * 128:(ft + 1) * 128], lhsT=wsl[:, kc, ft * 128:(ft + 1) * 128], rhs=hnT[:, kc, :],
                            start=(kc == 0), stop=(kc == 7)), reads=[wsl, hnT], writes=[bk])
                kb.op("act", lambda e, bk=bk, dst=dst, hf=hf: e.activation(
                    out=dst[:, 4 * hf:4 * hf + 4, :], in_=bk[:, 0:512].rearrange("p (f t) -> p f t", f=4),
                    func=AF.Sigmoid), reads=[bk], writes=[dst])
        for hf in range(2):
            wa = load_w(wba_bf, hf * 512, 512, nk=4)
            wb = load_w(wbp_bf, hf * 512, 512, nk=4)
            for ft in range(4):
                for kc in range(4):
                    kb.op("pe", lambda e, ft=ft, kc=kc, wa=wa: e.matmul(
                        pb[0][:, ft * 128:(ft + 1) * 128], lhsT=wa[:, kc, ft * 128:(ft + 1) * 128], rhs=aoT[:, kc, :],
                        start=(kc == 0), stop=(kc == 3)), reads=[wa, aoT], writes=[pb[0]])
                for kc in range(4):
                    kb.op("pe", lambda e, ft=ft, kc=kc, wb=wb: e.matmul(
                        pb[1][:, ft * 128:(ft + 1) * 128], lhsT=wb[:, kc, ft * 128:(ft + 1) * 128], rhs=poT[:, kc, :],
                        start=(kc == 0), stop=(kc == 3)), reads=[wb, poT], writes=[pb[1]])
            kb.op("dve", lambda e, hf=hf: e.tensor_tensor(
                out=tmpA[:], in0=pb[0][:, 0:512], in1=sga[:, 4 * hf:4 * hf + 4, :].rearrange("p f t -> p (f t)"),
                op=ALU.mult), reads=[pb[0], sga], writes=[tmpA])
            kb.op("dve", lambda e, hf=hf: e.tensor_tensor(
                out=tmpB[:], in0=pb[1][:, 0:512], in1=sgb[:, 4 * hf:4 * hf + 4, :].rearrange("p f t -> p (f t)"),
                op=ALU.mult), reads=[pb[1], sgb], writes=[tmpB])
            kb.op("dve", lambda e, hf=hf: e.tensor_tensor(
                out=mT[:, 4 * hf:4 * hf + 4, :].rearrange("p f t -> p (f t)"), in0=tmpA[:], in1=tmpB[:], op=ALU.add),
                reads=[tmpA, tmpB], writes=[mT])
        for hf in range(2):
            wo = load_w(wout_bf, hf * 512, 512, nk=8)
            for kc in range(8):
                kb.op("pe", lambda e, kc=kc, wo=wo, hf=hf: e.matmul(
                    pb[hf][:, 0:512], lhsT=mT[:, kc, :], rhs=wo[:, kc, :], start=(kc == 0), stop=(kc == 7)),
                    reads=[mT, wo], writes=[pb[hf]])
            kb.op("dve", lambda e, hf=hf: e.tensor_tensor(out=h2[:, hf * 512:(hf + 1) * 512], in0=pb[hf][:, 0:512],
                                                          in1=xb[:, hf * 512:(hf + 1) * 512], op=ALU.add),
                  reads=[pb[hf], xb], writes=[NBt])
        norm_transpose(h2, xnT, gtile=g2b, keep=xn, xowner=NBt)
        for hf in range(2):
            wq_ = load_w(pwq_bf, hf * 512, 512, nk=8)
            for ft in range(4):
                for kc in range(8):
                    kb.op("pe", lambda e, ft=ft, kc=kc, wq_=wq_, hf=hf: e.matmul(
                        pb[hf][:, ft * 128:(ft + 1) * 128], lhsT=wq_[:, kc, ft * 128:(ft + 1) * 128], rhs=xnT[:, kc, :],
                        start=(kc == 0), stop=(kc == 7)), reads=[wq_, xnT], writes=[pb[hf]])
            kb.op("act", lambda e, hf=hf: e.copy(out=qpT[:, 4 * hf:4 * hf + 4, :],
                                                 in_=pb[hf][:, 0:512].rearrange("p (f t) -> p f t", f=4)),
                  reads=[pb[hf]], writes=[qpT])
        sbanks = [pb[0], pb[1], pb[3], pb[4]]
        for h in range(8):
            bk = sbanks[h // 2]
            kb.op("pe", lambda e, bk=bk, h=h: e.matmul(bk[:, (h % 2) * 256:(h % 2) * 256 + 256], lhsT=qpT[:, h, :],
                                                      rhs=SK[:], start=True, stop=True),
                  reads=[qpT, SK], writes=[bk])
        for j4 in range(4):
            kb.op("act", lambda e, j4=j4: e.copy(out=ssc[:, j4 * 512:(j4 + 1) * 512], in_=sbanks[j4][:, 0:512]),
                  reads=[sbanks[j4]], writes=[Isc])
        tvv = tv[:].rearrange("p h s k -> p (h s) k")
        tiv = ti[:].rearrange("p h s k -> p (h s) k")
        for gi in range(16):
            sv = ssc[:, gi * 128:(gi + 1) * 128]
            sw = sscw[:, gi * 128:(gi + 1) * 128]
            kb.op("dve", lambda e, sv=sv, gi=gi: e.max(out=tvv[:, gi, 0:8], in_=sv), reads=[Isc], writes=[tv])
            kb.op("dve", lambda e, sv=sv, gi=gi: e.max_index(out=tiv[:, gi, 0:8], in_max=tvv[:, gi, 0:8], in_values=sv),
                  reads=[Isc, tv], writes=[ti])
            kb.op("dve", lambda e, sv=sv, sw=sw, gi=gi: e.match_replace(out=sw, in_to_replace=tvv[:, gi, 0:8],
                                                                       in_values=sv, imm_value=-1e30),
                  reads=[Isc, tv], writes=[Isc])
            kb.op("dve", lambda e, sw=sw, gi=gi: e.max(out=tvv[:, gi, 8:16], in_=sw), reads=[Isc], writes=[tv])
            kb.op("dve", lambda e, sw=sw, gi=gi: e.max_index(out=tiv[:, gi, 8:16], in_max=tvv[:, gi, 8:16], in_values=sw),
                  reads=[Isc, tv], writes=[ti])
        kb.op("dve", lambda e: e.tensor_copy(out=tif[:], in_=ti[:]), reads=[ti], writes=[tif])
        candv = cand.rearrange("p (h a b) -> p h a b", h=8, a=16)
        kb.op("dve", lambda e: e.tensor_tensor(out=candv, in0=tv[:, :, 0, :].unsqueeze(3).to_broadcast([128, 8, 16, 16]),
                                               in1=tv[:, :, 1, :].unsqueeze(2).to_broadcast([128, 8, 16, 16]), op=ALU.add),
              reads=[tv], writes=[Isc])
        for h in range(8):
            cv = cand[:, h * 256:(h + 1) * 256]
            cw = candw[:, h * 256:(h + 1) * 256]
            kb.op("dve", lambda e, cv=cv, h=h: e.max(out=tsv[:, h, 0:8], in_=cv), reads=[Isc], writes=[tsv])
            kb.op("dve", lambda e, cv=cv, h=h: e.max_index(out=tpos[:, h, 0:8], in_max=tsv[:, h, 0:8], in_values=cv),
                  reads=[Isc, tsv], writes=[tpos])
            kb.op("dve", lambda e, cv=cv, cw=cw, h=h: e.match_replace(out=cw, in_to_replace=tsv[:, h, 0:8],
                                                                     in_values=cv, imm_value=-1e30),
                  reads=[Isc, tsv], writes=[Isc])
            kb.op("dve", lambda e, cw=cw, h=h: e.max(out=tsv[:, h, 8:16], in_=cw), reads=[Isc], writes=[tsv])
            kb.op("dve", lambda e, cw=cw, h=h: e.max_index(out=tpos[:, h, 8:16], in_max=tsv[:, h, 8:16], in_values=cw),
                  reads=[Isc, tsv], writes=[tpos])
        kb.op("dve", lambda e: e.tensor_single_scalar(out=pa[:], in_=tpos[:], scalar=4, op=ALU.logical_shift_right),
              reads=[tpos], writes=[pa])
        kb.op("dve", lambda e: e.tensor_single_scalar(out=pbb[:], in_=tpos[:], scalar=15, op=ALU.bitwise_and),
              reads=[tpos], writes=[pbb])
        kb.op("dve", lambda e: e.tensor_copy(out=paf[:], in_=pa[:]), reads=[pa], writes=[paf])
        kb.op("dve", lambda e: e.tensor_copy(out=pbf[:], in_=pbb[:]), reads=[pbb], writes=[pbf])
        oh4 = ohv.rearrange("p (h k a) -> p h k a", h=8, k=16)
        oh42 = ohv2.rearrange("p (h k a) -> p h k a", h=8, k=16)
        io4 = iota16[:].unsqueeze(1).unsqueeze(1).to_broadcast([128, 8, 16, 16])
        for (pf, half, dst) in ((paf, 0, i1s), (pbf, 1, i2s)):
            kb.op("dve", lambda e, pf=pf: e.tensor_tensor(out=oh4, in0=pf[:].unsqueeze(3).to_broadcast([128, 8, 16, 16]),
                                                          in1=io4, op=ALU.is_equal),
                  reads=[pf, iota16], writes=[Isc])
            kb.op("dve", lambda e, half=half: e.tensor_tensor(
                out=oh42, in0=oh4, in1=tif[:, :, half, :].unsqueeze(2).to_broadcast([128, 8, 16, 16]), op=ALU.mult),
                reads=[Isc, tif], writes=[Isc])
            kb.op("dve", lambda e, dst=dst: e.tensor_reduce(out=dst[:], in_=oh42, axis=AX.X, op=ALU.add),
                  reads=[Isc], writes=[dst])
        kb.op("dve", lambda e: e.scalar_tensor_tensor(out=eidf[:].rearrange("p (h k) -> p h k", h=8), in0=i1s[:],
                                                      scalar=128.0, in1=i2s[:], op0=ALU.mult, op1=ALU.add),
              reads=[i1s, i2s], writes=[eidf])
        kb.op("dve", lambda e: e.tensor_copy(out=eidi[:], in_=eidf[:]), reads=[eidf], writes=[eidi])
        kb.op("dve", lambda e: e.tensor_reduce(out=gmx[:], in_=tsv[:], axis=AX.X, op=ALU.max), reads=[tsv], writes=[gmx])
        kb.op("dve", lambda e: e.tensor_tensor(out=gk[:], in0=tsv[:], in1=gmx[:].unsqueeze(2).to_broadcast([128, 8, 16]),
                                               op=ALU.subtract), reads=[tsv, gmx], writes=[gk])
        kb.op("act", lambda e: e.activation(out=gk[:], in_=gk[:], func=AF.Exp), reads=[gk], writes=[gk])
        kb.op("dve", lambda e: e.tensor_reduce(out=gmx[:], in_=gk[:], axis=AX.X, op=ALU.add), reads=[gk], writes=[gmx])
        kb.op("dve", lambda e: e.reciprocal(out=gmx[:], in_=gmx[:]), reads=[gmx], writes=[gmx])
        kb.op("dve", lambda e: e.tensor_tensor(out=gk[:], in0=gk[:], in1=gmx[:].unsqueeze(2).to_broadcast([128, 8, 16]),
                                               op=ALU.mult), reads=[gk, gmx], writes=[gk])
        def peer_steps():
            for hk in range(128):
                ub = Ub[hk % 6]
                kb.dma("pool", ub[:], pu_bf[:, :], reads=[eidi, pu_bf], writes=[ub.owner],
                       indirect=bass.IndirectOffsetOnAxis(ap=eidi[:, hk:hk + 1], axis=0), own_sem=hk % 6)
                kb.op("dve", lambda e, ub=ub, hk=hk: e.scalar_tensor_tensor(
                    out=ub[:], in0=ub[:], scalar=1.0, in1=xn[:], op0=ALU.mult, op1=ALU.mult,
                    accum_out=actv[:, hk:hk + 1]), reads=[ub.owner, xn], writes=[ub.owner, actv])
                yield
            kb.op("act", lambda e: e.activation(out=wgt[:], in_=actv[:], func=AF.Gelu), reads=[actv], writes=[wgt])
            kb.op("dve", lambda e: e.tensor_tensor(out=wgt[:], in0=wgt[:], in1=gk[:].rearrange("p h k -> p (h k)"),
                                                   op=ALU.mult), reads=[wgt, gk], writes=[wgt])
            for hk in range(128):
                ub = Ub[(2 + hk) % 6]
                kb.dma("pool", ub[:], pv_bf[:, :], reads=[eidi, pv_bf], writes=[ub.owner],
                       indirect=bass.IndirectOffsetOnAxis(ap=eidi[:, hk:hk + 1], axis=0), own_sem=(2 + hk) % 6)
                kb.op("dve", lambda e, ub=ub, hk=hk: e.scalar_tensor_tensor(
                    out=h2[:], in0=ub[:], scalar=wgt[:, hk:hk + 1], in1=h2[:], op0=ALU.mult, op1=ALU.add),
                    reads=[ub.owner, wgt, NBt], writes=[NBt])
                yield
            if sample:
                kb.dma("sp", y_s_out[:, :], h2[:], reads=[NBt], writes=[y_s_out])
            else:
                kb.dma("sp", y_own[i, :, :], h2[:], reads=[NBt], writes=[y_own])
            yield
        return peer_steps()

    pending = [None]
    nslots = [1]

    def advance(n=1):
        g = pending[0]
        if g is None:
            return
        for _ in range(n):
            try:
                next(g)
            except StopIteration:
                pending[0] = None
                return

    def drain():
        while pending[0] is not None:
            advance(64)

    for i in range(nq):
        xb = xt[i % 2]
        kb.dma("sp", xb[:], xown[i, :, :], writes=[xb])
        norm_transpose(xb, hnT)
        wsl = load_w(win_bf, *CHUNKS[CH_K])
        proj_tok(pb[0], wsl, 512)
        head_norm(pb[0], kg, ko)
        kb.dma("sp", k_own[i, :, :], ko[:], reads=[ko], writes=[k_own])
        wsl = load_w(win_bf, *CHUNKS[CH_V])
        proj_tok(pb[1], wsl, 512)
        kb.op("act", lambda e: e.copy(out=vo[:], in_=pb[1][:, 0:512]), reads=[pb[1]], writes=[vo])
        kb.dma("sp", v_own[i, :, :], vo[:], reads=[vo], writes=[v_own])
        wsl = load_w(win_bf, *CHUNKS[CH_KW])
        proj_tok(pb[0], wsl, 72)
        kb.op("dve", lambda e: e.tensor_copy(out=kio[:], in_=pb[0][:, 0:64]), reads=[pb[0]], writes=[kio])
        kb.dma("sp", ki_own[i, :, :], kio[:], reads=[kio], writes=[ki_own])
        kb.op("dve", lambda e: e.tensor_scalar(out=wis[:], in0=pb[0][:, 64:72], scalar1=0.125 * (8 ** -0.5),
                                               scalar2=None, op0=ALU.mult), reads=[pb[0]], writes=[wis])
        if not do_attn:
            continue
        tpv = tp_bank.t[:].bitcast(BF16)
        wsl = load_w(win_bf, *CHUNKS[CH_Q])
        proj_tok(pb[0], wsl, 512)
        head_norm(pb[0], qg, qf)
        kb.op("dve", lambda e: e.tensor_copy(out=qbf[:], in_=qf[:]), reads=[qf], writes=[qbf])
        for pr in range(4):
            kb.op("pe", lambda e, pr=pr: e.transpose(out=tpv[:, pr * 128:(pr + 1) * 128],
                                                     in_=qbf[:, pr * 128:(pr + 1) * 128], identity=ident_b[:]),
                  reads=[qbf, ident_b], writes=[tp_bank])
        kb.op("act", lambda e: e.copy(out=qT2[:], in_=tpv[:, 0:512].rearrange("p (a t) -> p a t", a=4)),
              reads=[tp_bank], writes=[qT2])
        wsl = load_w(win_bf, *CHUNKS[CH_QI])
        proj_tok(pb[1], wsl, 512)
        kb.op("act", lambda e: e.copy(out=qbf[:], in_=pb[1][:, 0:512]), reads=[pb[1]], writes=[qbf])
        for pr in range(4):
            kb.op("pe", lambda e, pr=pr: e.transpose(out=tpv[:, pr * 128:(pr + 1) * 128],
                                                     in_=qbf[:, pr * 128:(pr + 1) * 128], identity=ident_b[:]),
                  reads=[qbf, ident_b], writes=[tp_bank])
        kb.op("act", lambda e: e.copy(out=qiT2[:], in_=tpv[:, 0:512].rearrange("p (a t) -> p a t", a=4)),
              reads=[tp_bank], writes=[qiT2])
        nkb = min(4 * i + 4, nblk_a)
        nch = nkb // 4
        nk = nkb * 128
        ibanks = [pb[0], pb[1], pb[3], pb[4]]
        cnt_i = 0
        per_slot = -(-(260 - NIT * (6 if nkb * 128 >= 4096 else (4 if nkb * 128 >= 2048 else 1))) // (nch * 8 + nkb))
        for ch in range(nch):
            kc_ = kitc[ch % 2]
            kb.dma("sp", kc_[:], kit_s[:, ch * 512:(ch + 1) * 512], reads=[kit_s], writes=[kc_])
            Ic = Isc[:, ch * 512:(ch + 1) * 512]
            for h in range(8):
                a, half = h // 2, h % 2
                ps_ = slice(64 * half, 64 * half + 64)
                bk = ibanks[cnt_i % 4]
                rl_ = rl[cnt_i % 2]
                cnt_i += 1
                advance(per_slot)
                kb.op("pe", lambda e, bk=bk, a=a, ps_=ps_, kc_=kc_: e.matmul(
                    bk[:, 0:512], lhsT=qiT2[ps_, a, :], rhs=kc_[ps_, :], start=True, stop=True),
                    reads=[qiT2, kc_], writes=[bk])
                kb.op("act", lambda e, bk=bk, rl_=rl_: e.activation(out=rl_[:], in_=bk[:, 0:512], func=AF.Relu),
                      reads=[bk], writes=[rl_])
                if h == 0:
                    kb.op("dve", lambda e, rl_=rl_, Ic=Ic: e.tensor_scalar(
                        out=Ic, in0=rl_[:], scalar1=wis[:, 0:1], scalar2=None, op0=ALU.mult),
                        reads=[rl_, wis], writes=[Isc])
                else:
                    kb.op("dve", lambda e, rl_=rl_, Ic=Ic, h=h: e.scalar_tensor_tensor(
                        out=Ic, in0=rl_[:], scalar=wis[:, h:h + 1], in1=Ic, op0=ALU.mult, op1=ALU.add),
                        reads=[rl_, wis, Isc], writes=[Isc])
        kb.op("dve", lambda e: e.tensor_reduce(out=hi0[:], in_=Isc[:, 0:nk], axis=AX.X, op=ALU.max),
              reads=[Isc], writes=[hi0])
        kb.op("dve", lambda e: e.tensor_reduce(out=lo[:], in_=Isc[:, 0:nk], axis=AX.X, op=ALU.min),
              reads=[Isc], writes=[lo])
        kb.op("dve", lambda e: e.tensor_tensor(out=Isc[:, nk - 512:nk], in0=Isc[:, nk - 512:nk], in1=cmask[:],
                                               op=ALU.add), reads=[Isc, cmask], writes=[Isc])
        kb.op("dve", lambda e: e.tensor_tensor(out=hi0[:], in0=hi0[:], in1=lo[:], op=ALU.subtract),
              reads=[hi0, lo], writes=[hi0])
        kb.op("dve", lambda e: e.tensor_scalar(out=wh[:], in0=pw[:], scalar1=hi0[:, 0:1], scalar2=None,
                                               op0=ALU.mult), reads=[pw, hi0], writes=[wh])
        for n in range(NIT):
            kb.op("dve", lambda e, n=n: e.tensor_tensor(out=mid[:], in0=lo[:], in1=wh[:, n:n + 1], op=ALU.add),
                  reads=[lo, wh], writes=[mid])
            kb.op("dve", lambda e: e.tensor_scalar(out=nmid[:], in0=mid[:], scalar1=-1.0, scalar2=None, op0=ALU.mult),
                  reads=[mid], writes=[nmid])
            kb.op("dve", lambda e: e.memset(cnt4[:], 0.0), writes=[cnt4])
            for q4 in range((nk + 2175) // 2176):
                c0 = q4 * 2176
                c1 = min(nk, c0 + 2176)
                kb.op("act", lambda e, c0=c0, c1=c1, q4=q4: e.activation(
                    out=junkI[:, 0:c1 - c0], in_=Isc[:, c0:c1], func=AF.Sign, bias=nmid[:, 0:1], scale=1.0,
                    accum_out=cnt4[:, q4:q4 + 1]), reads=[Isc, nmid], writes=[junkI, cnt4])
            advance(6 if nk >= 4096 else (4 if nk >= 2048 else 1))
            kb.op("dve", lambda e: e.tensor_reduce(out=cntt[:], in_=cnt4[:], axis=AX.X, op=ALU.add),
                  reads=[cnt4], writes=[cntt])
            kb.op("dve", lambda e, n=n: e.tensor_scalar(out=dl[:], in0=cntt[:], scalar1=511.5 - nk,
                                                        scalar2=wh[:, n:n + 1], op0=ALU.is_ge, op1=ALU.mult),
                  reads=[cntt, wh], writes=[dl])
            kb.op("dve", lambda e: e.tensor_tensor(out=lo[:], in0=lo[:], in1=dl[:], op=ALU.add),
                  reads=[lo, dl], writes=[lo])
        kb.op("dve", lambda e: e.memset(pb[5][:, 0:260], 0.0), writes=[pb[5]])
        kb.op("dve", lambda e: e.memset(pb[6][:, 0:260], 0.0), writes=[pb[6]])
        def emit_S(kbi):
            ch, kloc = kbi // 4, kbi % 4
            ktc_ = ktc[ch % 2]
            vch_ = vch[ch % 2]
            if kloc == 0:
                kb.dma("sp", ktc_[:], kt_s[:, :, ch * 512:(ch + 1) * 512], reads=[kt_s], writes=[ktc_])
                kb.dma("sp", vch_[:], v_s.t[ch * 512:(ch + 1) * 512, :, :].rearrange("(b p) h e -> p b (h e)", p=128),
                       reads=[v_s], writes=[vch_])
            nm = negm[kbi % 2]
            kb.op("dve", lambda e, nm=nm, kbi=kbi: e.tensor_scalar(
                out=nm[:], in0=Isc[:, kbi * 128:(kbi + 1) * 128], scalar1=lo[:, 0:1], scalar2=-30000.0,
                op0=ALU.is_lt, op1=ALU.mult), reads=[Isc, lo], writes=[nm])
            r = kbi - 4 * i
            near = (-1 <= r <= 3)
            if near:
                kb.op("dve", lambda e, nm=nm, r=r: e.tensor_tensor(
                    out=addh[:], in0=NB[r + 1][:], in1=nm[:].unsqueeze(1).to_broadcast([128, 8, 128]), op=ALU.add),
                    reads=[NB[r + 1], nm], writes=[addh])
            pt_ = PT[kbi % 2]
            for g in range(2):
                sb_ = pb[3 + g]
                for hh in range(4):
                    h = 4 * g + hh
                    a, half = h // 2, h % 2
                    ps_ = slice(64 * half, 64 * half + 64)
                    kb.op("pe", lambda e, sb_=sb_, hh=hh, a=a, ps_=ps_, ktc_=ktc_, kloc=kloc: e.matmul(
                        sb_[:, hh * 128:(hh + 1) * 128], lhsT=ktc_[ps_, a, kloc * 128:(kloc + 1) * 128],
                        rhs=qT2[ps_, a, :], start=True, stop=False), reads=[ktc_, qT2], writes=[sb_])
                    if near:
                        kb.op("pe", lambda e, sb_=sb_, hh=hh, h=h: e.matmul(
                            sb_[:, hh * 128:(hh + 1) * 128], lhsT=addh[:, h, :], rhs=ident_b[:],
                            start=False, stop=True), reads=[addh, ident_b], writes=[sb_])
                    else:
                        kb.op("pe", lambda e, sb_=sb_, hh=hh, nm=nm: e.matmul(
                            sb_[:, hh * 128:(hh + 1) * 128], lhsT=nm[:], rhs=ident_b[:],
                            start=False, stop=True), reads=[nm, ident_b], writes=[sb_])
                kb.op("act", lambda e, sb_=sb_, pt_=pt_, g=g: e.activation(
                    out=pt_[:, 4 * g:4 * g + 4, :], in_=sb_[:, 0:512].rearrange("p (h q) -> p h q", h=4),
                    func=AF.Exp), reads=[sb_], writes=[pt_])

        def emit_PV(kbi):
            ch, kloc = kbi // 4, kbi % 4
            vch_ = vch[ch % 2]
            pt_ = PT[kbi % 2]
            for h in range(8):
                ob = pb[5 + h // 4]
                kb.op("pe", lambda e, ob=ob, h=h, pt_=pt_, vch_=vch_, kloc=kloc: e.matmul(
                    ob[:, (h % 4) * 65:(h % 4) * 65 + 65], lhsT=pt_[:, h, :], rhs=vch_[:, kloc, h * 65:(h + 1) * 65],
                    start=False, stop=False, skip_group_check=True), reads=[pt_, vch_], writes=[ob])

        for kbi in range(nkb):
            emit_S(kbi)
            advance(per_slot)
            if kbi > 0:
                emit_PV(kbi - 1)
        emit_PV(nkb - 1)
        for g in range(2):
            ob = pb[5 + g]
            obv = ob[:, 0:260].rearrange("p (h e) -> p h e", h=4)
            kb.op("dve", lambda e, obv=obv, g=g: e.reciprocal(out=rden[:, 4 * g:4 * g + 4], in_=obv[:, :, 64]),
                  reads=[ob], writes=[rden])
            kb.op("dve", lambda e, obv=obv, g=g: e.tensor_tensor(
                out=ao[:, 256 * g:256 * g + 256].rearrange("p (h d) -> p h d", h=4), in0=obv[:, :, 0:64],
                in1=rden[:, 4 * g:4 * g + 4].unsqueeze(2).to_broadcast([128, 4, 64]), op=ALU.mult),
                reads=[ob, rden], writes=[ao])
        if dbg_ao is not None:
            kb.op("dve", lambda e: e.tensor_copy(out=aof[:], in_=ao[:]), reads=[ao], writes=[aof])
            kb.dma("sp", dbg_ao[i, :, :], aof[:], reads=[aof], writes=[dbg_ao])

        if do_tail:
            drain()
            pending[0] = tail_block(i, xb)


    drain()
    if do_sample:
        LOB = _bucket_lo()
        wrep = kb.sb([128, 8], F32, "wrep")
        idx2 = kb.sb([128, 2], I32, "idx2")
        pti = kb.sb([128, 16], I32, "pti")
        ptf = kb.sb([128, 16], F32, "ptf")
        dh = kb.sb([128, 128], F32, "dh")
        dh2 = kb.sb([128, 128], F32, "dh2")
        scs = kb.sb([128, 2, 128], F32, "scs")
        scn = kb.sb([128, 1], F32, "scn")
        dn8 = kb.sb([128, 8], F32, "dn8")
        sm8 = kb.sb([128, 8], F32, "sm8")
        tA = View(Ubo[0], Ubo[0].t[:].bitcast(F32)[:, 0:256])
        tE = View(Ubo[0], Ubo[0].t[:].bitcast(F32)[:, 256:512])
        tF = View(Ubo[1], Ubo[1].t[:].bitcast(F32)[:, 0:256])
        tG = View(Ubo[1], Ubo[1].t[:].bitcast(F32)[:, 256:512])
        idxs = View(Ubo[2], Ubo[2].t[:].bitcast(U32)[:, 0:256])
        tC = View(Ubo[2], Ubo[2].t[:].bitcast(U32)[:, 256:512])
        tD = View(Ubo[3], Ubo[3].t[:].bitcast(U32)[:, 0:256])
        rowT = kb.sb([128, 32], I32, "rowT")
        distT = kb.sb([128, 32], F32, "distT")
        negT = kb.sb([128, 32], F32, "negT")
        indT = kb.sb([128, 32], F32, "indT")
        biasT = kb.sb([128, 32, 8], F32, "biasT")
        tmp3 = kb.sb([128, 32, 8], F32, "tmp3")
        lg = kb.sb([128, 8], F32, "lg")
        pex = kb.sb([128, 8], F32, "pex")
        zsel = kb.sb([128, 31], F32, "zsel")
        numS = View(Ubo[4], Ubo[4].t[:].bitcast(F32)[:, 0:512])
        denS = kb.sb([128, 8], F32, "denS")
        seln = kb.sb([128, 1], F32, "seln")
        nb0 = kb.sb([128, 8], F32, "nb0")
        sq, sk_, sv_, ski = qf, ko, vo, kio
        sqi = hsq
        su = tmpA
        qrep = tmpB
        IscV = Isc.t[:]
        kb.dma("sp", zsel[:], zsel_in[:, :], writes=[zsel])
        kb.op("dve", lambda e: e.memset(pti[:], 0), writes=[pti])
        kb.dma("sp", pti[0:16, :], pt_in[:, :], writes=[pti])
        kb.op("dve", lambda e: e.tensor_copy(out=ptf[:], in_=pti[:]), reads=[pti], writes=[ptf])
        with nc.allow_non_contiguous_dma(reason="page table slices"):
            for jg in range(8):
                kb.dma("sp", idx2[jg * 16:(jg + 1) * 16, :], pt_in[:, 2 * jg:2 * jg + 2], writes=[idx2])
        xb = xt[0]
        kb.dma("sp", xb[:], xs_in[:, :], writes=[xb])
        norm_transpose(xb, hnT)
        wsl = load_w(win_bf, *CHUNKS[CH_Q])
        proj_tok(pb[0], wsl, 512)
        head_norm(pb[0], qg, sq)
        wsl = load_w(win_bf, *CHUNKS[CH_K])
        proj_tok(pb[1], wsl, 512)
        head_norm(pb[1], kg, sk_)
        kb.dma("sp", k_s_out[:, :], sk_[:], reads=[sk_], writes=[k_s_out])
        wsl = load_w(win_bf, *CHUNKS[CH_V])
        proj_tok(pb[0], wsl, 512)
        kb.op("act", lambda e: e.copy(out=sv_[:], in_=pb[0][:, 0:512]), reads=[pb[0]], writes=[sv_])
        kb.dma("sp", v_s_out[:, :], sv_[:], reads=[sv_], writes=[v_s_out])
        wsl = load_w(win_bf, *CHUNKS[CH_QI])
        proj_tok(pb[1], wsl, 512)
        kb.op("act", lambda e: e.copy(out=sqi[:], in_=pb[1][:, 0:512]), reads=[pb[1]], writes=[sqi])
        wsl = load_w(win_bf, *CHUNKS[CH_KW])
        proj_tok(pb[0], wsl, 72)
        kb.op("dve", lambda e: e.tensor_copy(out=ski[:], in_=pb[0][:, 0:64]), reads=[pb[0]], writes=[ski])
        kb.dma("sp", ki_s_out[:, :], ski[:], reads=[ski], writes=[ki_s_out])
        kb.op("dve", lambda e: e.tensor_scalar(out=wis[:], in0=pb[0][:, 64:72], scalar1=0.125 * (8 ** -0.5),
                                               scalar2=None, op0=ALU.mult), reads=[pb[0]], writes=[wis])
        wsl = load_w(win_bf, *CHUNKS[CH_U])
        proj_tok(pb[1], wsl, 512)
        kb.op("act", lambda e: e.copy(out=su[:], in_=pb[1][:, 0:512]), reads=[pb[1]], writes=[su])
        kb.dma("sp", pool_s_out[:, 0:14, :], state_in[:, 1:15, :], reads=[state_in], writes=[pool_s_out])
        kb.dma("sp", pool_s_out[:, 14, :], su[0:16, :], reads=[su], writes=[pool_s_out])
        kb.dma("sp", qi_d[:, :], sqi[0:16, :], reads=[sqi], writes=[qi_d])
        kb.dma("sp", wi_d[:, :], wis[0:16, :], reads=[wis], writes=[wi_d])
        kb.dma("sp", q_d[:, :], sq[0:16, :], reads=[sq], writes=[q_d])
        for jg in range(8):
            kb.dma("sp", qrep[jg * 16:(jg + 1) * 16, :], qi_d[:, :], reads=[qi_d], writes=[qrep])
            kb.dma("sp", wrep[jg * 16:(jg + 1) * 16, :], wi_d[:, :], reads=[wi_d], writes=[wrep])
        KI = IscV[:, 0:8192]
        prodc = View(ktc[0], ktc[0].t[:].rearrange("p a b -> p (a b)").bitcast(F32)[:, 0:1024])
        for s in range(2):
            kb.dma("pool", KI, cache_ik[:, :], reads=[idx2], writes=[Isc],
                   indirect=bass.IndirectOffsetOnAxis(ap=idx2[:, s:s + 1], axis=0))
            for h in range(8):
                for c8 in range(8):
                    kb.op("dve", lambda e, h=h, c8=c8: e.tensor_tensor(
                        out=prodc[:].rearrange("p (k d) -> p k d", k=16),
                        in0=KI[:, c8 * 1024:(c8 + 1) * 1024].rearrange("p (k d) -> p k d", k=16),
                        in1=qrep[:, h * 64:(h + 1) * 64].unsqueeze(1).to_broadcast([128, 16, 64]), op=ALU.mult),
                        reads=[Isc, qrep], writes=[prodc.owner])
                    kb.op("dve", lambda e, c8=c8: e.tensor_reduce(
                        out=dh[:, c8 * 16:(c8 + 1) * 16], in_=prodc[:].rearrange("p (k d) -> p k d", k=16),
                        axis=AX.X, op=ALU.add), reads=[prodc.owner], writes=[dh])
                if h == 0:
                    kb.op("dve", lambda e, s=s: e.tensor_scalar(out=scs[:, s, :], in0=dh[:], scalar1=0.0,
                                                                scalar2=wrep[:, 0:1], op0=ALU.max, op1=ALU.mult),
                          reads=[dh, wrep], writes=[scs])
                else:
                    kb.op("dve", lambda e, h=h: e.tensor_scalar(out=dh2[:], in0=dh[:], scalar1=0.0,
                                                                scalar2=wrep[:, h:h + 1], op0=ALU.max, op1=ALU.mult),
                          reads=[dh, wrep], writes=[dh2])
                    kb.op("dve", lambda e, s=s: e.tensor_tensor(out=scs[:, s, :], in0=scs[:, s, :], in1=dh2[:],
                                                                op=ALU.add), reads=[scs, dh2], writes=[scs])
        for jg in range(8):
            kb.dma("sp", sc_d[:, jg * 256:(jg + 1) * 256], scs[jg * 16:(jg + 1) * 16, :, :].rearrange("p s k -> p (s k)"),
                   reads=[scs], writes=[sc_d])
        xtmp = xn[:, 512:1024]
        kb.op("dve", lambda e: e.tensor_tensor(out=xtmp.rearrange("p (h d) -> p h d", h=8),
                                               in0=sqi[:].rearrange("p (h d) -> p h d", h=8),
                                               in1=ski[:].unsqueeze(1).to_broadcast([128, 8, 64]), op=ALU.mult),
              reads=[sqi, ski], writes=[xn])
        kb.op("dve", lambda e: e.tensor_reduce(out=dn8[:], in_=xtmp.rearrange("p (h d) -> p h d", h=8), axis=AX.X,
                                               op=ALU.add), reads=[xn], writes=[dn8])
        kb.op("dve", lambda e: e.tensor_scalar(out=dn8[:], in0=dn8[:], scalar1=0.0, scalar2=None, op0=ALU.max),
              reads=[dn8], writes=[dn8])
        kb.op("dve", lambda e: e.tensor_tensor(out=dn8[:], in0=dn8[:], in1=wis[:], op=ALU.mult),
              reads=[dn8, wis], writes=[dn8])
        kb.op("dve", lambda e: e.tensor_reduce(out=scn[:], in_=dn8[:], axis=AX.X, op=ALU.add),
              reads=[dn8], writes=[scn])
        Is = IscV[:, 0:2049]
        kb.op("dve", lambda e: e.memset(IscV[:, 0:2304], 0.0), writes=[Isc])
        kb.dma("sp", IscV[0:16, 0:2048], sc_d[:, :], reads=[sc_d], writes=[Isc])
        kb.op("dve", lambda e: e.tensor_copy(out=IscV[:, 2048:2049], in_=scn[:]), reads=[scn], writes=[Isc])
        for rd in range(32):
            kb.op("dve", lambda e: e.max(out=sm8[:], in_=Is), reads=[Isc], writes=[sm8])
            kb.op("dve", lambda e, rd=rd: e.max_index(out=idxs[:, rd * 8:(rd + 1) * 8], in_max=sm8[:], in_values=Is),
                  reads=[Isc, sm8], writes=[idxs])
            kb.op("dve", lambda e: e.match_replace(out=Is, in_to_replace=sm8[:], in_values=Is, imm_value=-1e30),
                  reads=[Isc, sm8], writes=[Isc])
        kb.op("dve", lambda e: e.tensor_copy(out=tA[:], in_=idxs[:]), reads=[idxs], writes=[tA])
        kb.op("dve", lambda e: e.tensor_scalar(out=tC[:], in0=tA[:], scalar1=2047.0, scalar2=None, op0=ALU.min),
              reads=[tA], writes=[tC])
        kb.op("dve", lambda e: e.tensor_single_scalar(out=tD[:], in_=tC[:], scalar=7, op=ALU.logical_shift_right),
              reads=[tC], writes=[tD])
        kb.op("dve", lambda e: e.tensor_single_scalar(out=tC[:], in_=tC[:], scalar=127, op=ALU.bitwise_and),
              reads=[tC], writes=[tC])
        kb.op("dve", lambda e: e.tensor_copy(out=tE[:], in_=tD[:]), reads=[tD], writes=[tE])
        kb.op("dve", lambda e: e.tensor_copy(out=tF[:], in_=tC[:]), reads=[tC], writes=[tF])
        ohs = IscV[:, 4608:8704].rearrange("p (k j) -> p k j", k=256)
        kb.op("dve", lambda e: e.tensor_tensor(out=ohs, in0=tE[:].unsqueeze(2).to_broadcast([128, 256, 16]),
                                               in1=iota16[:].unsqueeze(1).to_broadcast([128, 256, 16]), op=ALU.is_equal),
              reads=[tE, iota16], writes=[Isc])
        kb.op("dve", lambda e: e.tensor_tensor(out=ohs, in0=ohs, in1=ptf[:].unsqueeze(1).to_broadcast([128, 256, 16]),
                                               op=ALU.mult), reads=[Isc, ptf], writes=[Isc])
        kb.op("dve", lambda e: e.tensor_reduce(out=tG[:], in_=ohs, axis=AX.X, op=ALU.add), reads=[Isc], writes=[tG])
        kb.op("dve", lambda e: e.scalar_tensor_tensor(out=tG[:], in0=tG[:], scalar=128.0, in1=tF[:], op0=ALU.mult,
                                                      op1=ALU.add), reads=[tG, tF], writes=[tG])
        kb.op("dve", lambda e: e.tensor_scalar(out=tE[:], in0=tA[:], scalar1=-1.0, scalar2=2048.0, op0=ALU.mult,
                                               op1=ALU.add), reads=[tA], writes=[tE])
        kb.op("dve", lambda e: e.tensor_scalar(out=tF[:], in0=tA[:], scalar1=2047.5, scalar2=-30000.0, op0=ALU.is_ge,
                                               op1=ALU.mult), reads=[tA], writes=[tF])
        kb.op("dve", lambda e: e.tensor_reduce(out=seln[:], in_=tF[:], axis=AX.X, op=ALU.min), reads=[tF], writes=[seln])
        kb.op("dve", lambda e: e.tensor_scalar(out=seln[:], in0=seln[:], scalar1=-1.0 / 30000.0, scalar2=None,
                                               op0=ALU.mult), reads=[seln], writes=[seln])
        for (srcb, dstb) in ((tG, rowT), (tE, distT), (tF, negT)):
            for gq in range(2):
                kb.op("pe", lambda e, srcb=srcb, gq=gq: e.transpose(out=pb[2][:, gq * 128:(gq + 1) * 128],
                                                                   in_=srcb[:, gq * 128:(gq + 1) * 128],
                                                                   identity=ident_f[:]),
                      reads=[srcb, ident_f], writes=[pb[2]])
            kb.op("dve", lambda e, dstb=dstb: e.tensor_copy(
                out=dstb[:].rearrange("p (g b) -> p g b", g=2),
                in_=pb[2][:, 0:256].rearrange("p (g b) -> p g b", g=2)[:, :, 0:16]), reads=[pb[2]], writes=[dstb])
        kb.op("dve", lambda e: e.memset(biasT[:], 0.0), writes=[biasT])
        for bkt in range(1, 32):
            kb.op("dve", lambda e, bkt=bkt: e.tensor_scalar(out=indT[:], in0=distT[:], scalar1=float(LOB[bkt - 1]),
                                                            scalar2=None, op0=ALU.is_lt), reads=[distT], writes=[indT])
            kb.op("dve", lambda e, bkt=bkt: e.tensor_tensor(
                out=tmp3[:], in0=indT[:].unsqueeze(2).to_broadcast([128, 32, 8]),
                in1=ndel[:, bkt * 8:(bkt + 1) * 8].unsqueeze(1).to_broadcast([128, 32, 8]), op=ALU.mult),
                reads=[indT, ndel], writes=[tmp3])
            kb.op("dve", lambda e: e.tensor_tensor(out=biasT[:], in0=biasT[:], in1=tmp3[:], op=ALU.add),
                  reads=[biasT, tmp3], writes=[biasT])
        kb.op("dve", lambda e: e.memset(pb[5][:, 0:512], 0.0), writes=[pb[5]])
        kb.op("dve", lambda e: e.memset(pb[6][:, 0:8], 0.0), writes=[pb[6]])
        qbv = xn[:, 0:512]
        pvv = xn[:, 512:1024]
        for b in range(16):
            kb.dma("sp", qbv, q_d.t[b:b + 1, :].partition_broadcast(128).rearrange("p o d -> p (o d)"),
                   reads=[q_d], writes=[xn])
            for gq in range(2):
                col = gq * 16 + b
                kg_ = UbS[gq]
                vg_ = UbS[2 + gq]
                kb.dma("pool", kg_[:, 0:512], cache_k[:, :], reads=[rowT], writes=[kg_.owner],
                       indirect=bass.IndirectOffsetOnAxis(ap=rowT[:, col:col + 1], axis=0))
                kb.dma("pool", vg_[:, 0:512], cache_v[:, :], reads=[rowT], writes=[vg_.owner],
                       indirect=bass.IndirectOffsetOnAxis(ap=rowT[:, col:col + 1], axis=0))
                kb.op("dve", lambda e, kg_=kg_: e.tensor_tensor(out=pvv, in0=kg_[:, 0:512], in1=qbv, op=ALU.mult),
                      reads=[kg_.owner, xn], writes=[xn])
                kb.op("dve", lambda e: e.tensor_reduce(out=lg[:], in_=pvv.rearrange("p (h d) -> p h d", h=8),
                                                       axis=AX.X, op=ALU.add), reads=[xn], writes=[lg])
                kb.op("dve", lambda e, col=col: e.scalar_tensor_tensor(
                    out=lg[:], in0=lg[:], scalar=negT[:, col:col + 1], in1=biasT[:, col, :], op0=ALU.add, op1=ALU.add),
                    reads=[lg, negT, biasT], writes=[lg])
                kb.op("act", lambda e: e.activation(out=pex[:], in_=lg[:], func=AF.Exp), reads=[lg], writes=[pex])
                kb.op("dve", lambda e, vg_=vg_: e.tensor_tensor(
                    out=pvv.rearrange("p (h d) -> p h d", h=8), in0=vg_[:, 0:512].rearrange("p (h d) -> p h d", h=8),
                    in1=pex[:].unsqueeze(2).to_broadcast([128, 8, 64]), op=ALU.mult),
                    reads=[vg_.owner, pex], writes=[xn])
                kb.op("pe", lambda e, b=b: e.matmul(pb[5][0:16, 0:512], lhsT=zsel[:, 15 - b:31 - b], rhs=pvv,
                                                    start=False, stop=False, skip_group_check=True),
                      reads=[zsel, xn], writes=[pb[5]])
                kb.op("pe", lambda e, b=b: e.matmul(pb[6][0:16, 0:8], lhsT=zsel[:, 15 - b:31 - b], rhs=pex[:],
                                                    start=False, stop=False, skip_group_check=True),
                      reads=[zsel, pex], writes=[pb[6]])
        kb.op("dve", lambda e: e.memset(numS[:], 0.0), writes=[numS])
        kb.op("dve", lambda e: e.memset(denS[:], 1.0), writes=[denS])
        kb.op("dve", lambda e: e.tensor_copy(out=numS[0:16, :], in_=pb[5][0:16, 0:512]), reads=[pb[5]], writes=[numS])
        kb.op("dve", lambda e: e.tensor_copy(out=denS[0:16, :], in_=pb[6][0:16, 0:8]), reads=[pb[6]], writes=[denS])
        kb.op("dve", lambda e: e.tensor_reduce(out=nb0[:], in_=ndel[:, 8:256].rearrange("p (b h) -> p h b", h=8),
                                               axis=AX.X, op=ALU.add), reads=[ndel], writes=[nb0])
        kb.op("dve", lambda e: e.tensor_tensor(out=pvv, in0=sq[:], in1=sk_[:], op=ALU.mult), reads=[sq, sk_], writes=[xn])
        kb.op("dve", lambda e: e.tensor_reduce(out=lg[:], in_=pvv.rearrange("p (h d) -> p h d", h=8), axis=AX.X,
                                               op=ALU.add), reads=[xn], writes=[lg])
        kb.op("dve", lambda e: e.tensor_tensor(out=lg[:], in0=lg[:], in1=nb0[:], op=ALU.add), reads=[lg, nb0], writes=[lg])
        kb.op("act", lambda e: e.activation(out=pex[:], in_=lg[:], func=AF.Exp), reads=[lg], writes=[pex])
        kb.op("dve", lambda e: e.tensor_scalar(out=pex[:], in0=pex[:], scalar1=seln[:, 0:1], scalar2=None, op0=ALU.mult),
              reads=[pex, seln], writes=[pex])
        kb.op("dve", lambda e: e.tensor_tensor(out=pvv.rearrange("p (h d) -> p h d", h=8),
                                               in0=sv_[:].rearrange("p (h d) -> p h d", h=8),
                                               in1=pex[:].unsqueeze(2).to_broadcast([128, 8, 64]), op=ALU.mult),
              reads=[sv_, pex], writes=[xn])
        kb.op("dve", lambda e: e.tensor_tensor(out=numS[:], in0=numS[:], in1=pvv, op=ALU.add), reads=[numS, xn], writes=[numS])
        kb.op("dve", lambda e: e.tensor_tensor(out=denS[:], in0=denS[:], in1=pex[:], op=ALU.add), reads=[denS, pex],
              writes=[denS])
        kb.op("dve", lambda e: e.reciprocal(out=denS[:], in_=denS[:]), reads=[denS], writes=[denS])
        kb.op("dve", lambda e: e.tensor_tensor(out=ao[:].rearrange("p (h d) -> p h d", h=8),
                                               in0=numS[:].rearrange("p (h d) -> p h d", h=8),
                                               in1=denS[:].unsqueeze(2).to_broadcast([128, 8, 64]), op=ALU.mult),
              reads=[numS, denS], writes=[ao])
        stv = IscV[:, 0:7680].rearrange("p (r c) -> p r c", r=15)
        kb.op("dve", lambda e: e.memset(IscV[:, 0:7680], 0.0), writes=[Isc])
        kb.dma("sp", IscV[0:16, 0:7680], state_in.t.rearrange("b r c -> b (r c)"), reads=[state_in], writes=[Isc])
        for g in range(4):
            w = 2 ** (g + 1)
            kb.op("dve", lambda e, g=g, w=w: e.tensor_reduce(
                out=pmf[:, g, :], in_=stv[:, 16 - w:15, g * 128:(g + 1) * 128].rearrange("p r c -> p c r"),
                axis=AX.X, op=ALU.add), reads=[Isc], writes=[pmf])
            kb.op("dve", lambda e, g=g: e.tensor_tensor(out=pmf[:, g, :], in0=pmf[:, g, :], in1=su[:, g * 128:(g + 1) * 128],
                                                        op=ALU.add), reads=[pmf, su], writes=[pmf])
            kb.op("dve", lambda e, g=g, w=w: e.scalar_tensor_tensor(
                out=pmf[:, g, :], in0=pmf[:, g, :], scalar=1.0 / w, in1=su[:, g * 128:(g + 1) * 128], op0=ALU.mult,
                op1=ALU.subtract), reads=[pmf, su], writes=[pmf])
        for g in range(4):
            kb.op("pe", lambda e, g=g: e.transpose(out=pb[2][:, g * 128:(g + 1) * 128], in_=pmf[:, g, :],
                                                   identity=ident_f[:]), reads=[pmf, ident_f], writes=[pb[2]])
        kb.op("dve", lambda e: e.tensor_copy(out=pmT[:], in_=pb[2][:, 0:512].rearrange("p (g t) -> p g t", g=4)),
              reads=[pb[2]], writes=[pmT])
        pending[0] = tail_block(0, xb, sample=True)
        drain()

    kb.finish()
    return nc, kb


def _prep_inputs(inp, cfg, cores):
    nblk_a = cfg.get("nblk_a", NBLK_A)
    nq = cfg.get("nq", NQ)
    nkeys = nblk_a * 128
    xp = np.asarray(inp["x_prompt"], np.float32)
    meta = np.asarray(inp["meta_tokens"], np.float32)
    maps = []
    for c in cores:
        b, cc = c // 4, c % 4
        full = np.zeros((max(nkeys, (4 * nq + 4) * 128), D), np.float32)
        T = 16 + xp.shape[1]
        cat = np.concatenate([meta, xp[b]], axis=0)
        n = min(T, full.shape[0])
        full[:n] = cat[:n]
        xown = np.zeros((nq, 128, D), np.float32)
        xprev = np.zeros((nq, 16, D), np.float32)
        for i in range(nq):
            j = 4 * i + cc
            xown[i] = full[j * 128:(j + 1) * 128]
            if j > 0:
                xprev[i] = full[j * 128 - 16:j * 128]
        m = {
            "xcat": np.ascontiguousarray(full[:nkeys]),
            "xown": xown,
            "xprev": xprev,
            "w_in": np.ascontiguousarray(np.asarray(inp["w_in"], np.float32)[0]),
            "norm1_g": np.ascontiguousarray(np.asarray(inp["norm1_g"], np.float32)[0]),
            "q_norm_g": np.ascontiguousarray(np.asarray(inp["q_norm_g"], np.float32)[0]),
            "k_norm_g": np.ascontiguousarray(np.asarray(inp["k_norm_g"], np.float32)[0]),
            "ident": np.eye(128, dtype=np.float32),
            "rel_bias": np.ascontiguousarray(np.asarray(inp["rel_bias"], np.float32)),
            "qs": (np.arange(128, dtype=np.float32)[:, None] - np.arange(128, dtype=np.float32)[None, :]),
            "thrtab": _thrtab(cc),
            "cmask": _cmask(cc),
            "w_ba": np.ascontiguousarray(np.asarray(inp["w_branch_attn"], np.float32)[0]),
            "w_bp": np.ascontiguousarray(np.asarray(inp["w_branch_pool"], np.float32)[0]),
            "w_out": np.ascontiguousarray(np.asarray(inp["w_out"], np.float32)[0]),
            "peer_wq": np.ascontiguousarray(np.asarray(inp["peer_wq"], np.float32)[0]),
            "w_pool": np.ascontiguousarray(np.asarray(inp["w_pool"], np.float32)[0]),
            "pool_scale": np.ascontiguousarray(np.asarray(inp["pool_scale"], np.float32)[0]),
            "norm2_g": np.ascontiguousarray(np.asarray(inp["norm2_g"], np.float32)[0]),
            "subkeys": np.ascontiguousarray(np.asarray(inp["peer_subkeys"], np.float32)[0]),
            "peer_u": np.ascontiguousarray(np.asarray(inp["peer_u"], np.float32)[0]),
            "peer_v": np.ascontiguousarray(np.asarray(inp["peer_v"], np.float32)[0]),
            "rcnt": _rcnt(cc),
            "iota16": np.broadcast_to(np.arange(16, dtype=np.float32)[None, :], (128, 16)).copy(),
            "pw": np.broadcast_to((0.5 ** np.arange(1, NIT + 1)).astype(np.float32)[None, :], (128, NIT)).copy(),
        }
        if cfg.get("sample", True):
            xs = np.zeros((128, D), np.float32)
            xs[:16] = np.asarray(inp["x_sample"], np.float32)[16 * c:16 * c + 16, 0]
            z = np.zeros((128, 31), np.float32)
            z[:, 15] = 1.0
            m.update({
                "xs_own": xs,
                "cache_k": np.asarray(inp["cache_k"], np.float32).reshape(2560 * 128, 512),
                "cache_v": np.asarray(inp["cache_v"], np.float32).reshape(2560 * 128, 512),
                "cache_ik": np.asarray(inp["cache_idx_k"], np.float32).reshape(2560, 8192),
                "state_own": np.ascontiguousarray(np.asarray(inp["state_pool"], np.float32)[0, 16 * c:16 * c + 16]),
                "pt_own": np.ascontiguousarray(np.asarray(inp["page_table"], np.int32)[16 * c:16 * c + 16]),
                "zsel": z,
            })
        maps.append(m)
    return maps


def _bucket_lo():
    n = np.arange(0, 256)
    nf = np.maximum(n, 16).astype(np.float32)
    large = 16 + (np.log(nf / np.float32(16)) / np.float32(np.log(128 / 16)) * np.float32(16)).astype(np.int32)
    large = np.minimum(large, 31)
    bkt = np.where(n < 16, n, large)
    return [int(np.min(n[bkt >= b])) for b in range(1, 32)]


def _thrtab(cc):
    lo_b = _bucket_lo()
    t = np.zeros((155,), np.float32)
    for r5 in range(5):
        r = r5 - 1
        for b in range(31):
            t[r5 * 31 + b] = lo_b[b] - 128 * (cc - r)
    return np.broadcast_to(t[None, :], (128, 155)).copy()


def _rcnt(cc):
    r = np.zeros((128, 4, 128), np.float32)
    t = np.arange(128)
    for g, w in enumerate((2, 4, 8, 16)):
        cnt = np.minimum(w, t + 1) if cc == 0 else np.full(128, w)
        r[:, g, :] = (1.0 / cnt.astype(np.float32))[None, :]
    return r


def _cmask(cc):
    m = np.zeros((128, 512), np.float32)
    q = np.arange(128)[:, None]
    s = np.arange(128)[None, :]
    for r in range(4):
        if r > cc:
            m[:, r * 128:(r + 1) * 128] = -1e30
        elif r == cc:
            m[:, r * 128:(r + 1) * 128] = np.where(s > q, -1e30, 0.0)
    return m


def kernel(**inputs):
    cfg = {}
    nc = build(cfg)
    cores = list(range(8))
    maps = _prep_inputs(inputs, cfg, cores)
    res = run_bass_kernel_spmd(nc, maps, core_ids=cores)
    rs = res.results
    B, S = 2, 8192
    T = S + 16
    y_prompt = np.zeros((B, S, D), np.float32)
    k_p = np.zeros((1, B, T, 8, 64), np.float32)
    v_p = np.zeros((1, B, T, 8, 64), np.float32)
    i_p = np.zeros((1, B, T, 64), np.float32)
    pool_p = np.zeros((1, B, 15, 512), np.float32)
    for c in cores:
        b, cc = c // 4, c % 4
        r = rs[c]
        for i in range(NQ):
            j = 4 * i + cc
            p0 = j * 128
            if p0 >= T:
                continue
            p1 = min(p0 + 128, T)
            n = p1 - p0
            k_p[0, b, p0:p1] = r["k_own"][i][:n].reshape(n, 8, 64)
            v_p[0, b, p0:p1] = r["v_own"][i][:n].reshape(n, 8, 64)
            i_p[0, b, p0:p1] = r["ki_own"][i][:n]
            lo = max(p0, 16)
            y_prompt[b, lo - 16:p1 - 16] = r["y_own"][i][lo - p0:n]
        if cc == 0:
            ul = r["u_last"]
            rows = ul[:, :, 17:32]
            pool_p[0, b] = np.transpose(rows, (2, 1, 0)).reshape(15, 512)
    y_s = np.zeros((128, 1, D), np.float32)
    k_s = np.zeros((1, 128, 1, 8, 64), np.float32)
    v_s = np.zeros((1, 128, 1, 8, 64), np.float32)
    i_s = np.zeros((1, 128, 1, 64), np.float32)
    pool_s = np.zeros((1, 128, 15, 512), np.float32)
    for c in cores:
        r = rs[c]
        sl = slice(16 * c, 16 * c + 16)
        y_s[sl, 0] = r["y_s"][:16]
        k_s[0, sl, 0] = r["ks_o"][:16].reshape(16, 8, 64)
        v_s[0, sl, 0] = r["vs_o"][:16].reshape(16, 8, 64)
        i_s[0, sl, 0] = r["kis_o"][:16]
        pool_s[0, sl] = r["pool_s"]
    return (y_prompt, y_s, k_p, v_p, i_p, pool_p, k_s, v_s, i_s, pool_s)
```

```python
import numpy as np
from contextlib import ExitStack
import concourse.bass as bass
import concourse.mybir as mybir
from concourse.bass_utils import run_bass_kernel_spmd

F32 = mybir.dt.float32
BF16 = mybir.dt.bfloat16
I32 = mybir.dt.int32
U32 = mybir.dt.uint32
ALU = mybir.AluOpType
AF = mybir.ActivationFunctionType
AX = mybir.AxisListType

D = 1024
NBLK_A = 68
NQ = 17
EPS = 1e-6
IN_W = 4680
CH_Q, CH_K, CH_V, CH_QI, CH_KW, CH_U, CH_GA0, CH_GA1, CH_GB0, CH_GB1 = range(10)
CHUNKS = [(0, 512), (512, 512), (1024, 512), (1536, 512), (2048, 72), (2120, 512),
          (2632, 512), (3144, 512), (3656, 512), (4168, 512)]
N_DMA_SEMS = 12
NIT = 16


class Buf:
    def __init__(self, t, name):
        self.t = t
        self.name = name
        self.w = None
        self.r = {}

    def __getitem__(self, idx):
        return self.t[idx]


class KB:
    def __init__(self, nc, es, plan=None):
        self.nc = nc
        self.es = es
        self.plan = plan
        self.targets = {e: set() for e in ("pe", "dve", "act", "pool", "sp")}
        self.rank = None
        if plan is not None:
            self.rank = {e: {idx: r + 1 for r, idx in enumerate(sorted(plan[e]))} for e in plan}
        self.eng = {"pe": nc.tensor, "dve": nc.vector, "act": nc.scalar, "pool": nc.gpsimd, "sp": nc.sync}
        self.sem = {e: es.enter_context(nc.semaphore("s_" + e)) for e in self.eng}
        self.cnt = {e: 0 for e in self.eng}
        self.seen = {e: {} for e in self.eng}
        self.dsem = [es.enter_context(nc.semaphore("d_%d" % i)) for i in range(N_DMA_SEMS + 6)]
        self.dcnt = [0] * (N_DMA_SEMS + 6)
        self.drr = 0
        self.nbuf = 0

    def sb(self, shape, dt, name=None):
        self.nbuf += 1
        name = "sb_" + (name or ("%d" % self.nbuf))
        return Buf(self.es.enter_context(self.nc.sbuf_tensor(name, list(shape), dt)), name)

    def ps(self, shape, dt, name=None):
        self.nbuf += 1
        name = name or ("ps%d" % self.nbuf)
        return Buf(self.es.enter_context(self.nc.psum_tensor(name, list(shape), dt)), name)

    def dram(self, name, shape, dt, kind="Internal"):
        return Buf(self.nc.dram_tensor(name, list(shape), dt, kind=kind).ap(), name)

    def _deps(self, e, reads, writes):
        need = {}

        def add(tok):
            if tok is None:
                return
            key, sem, val = tok
            if key == "pe" and e == "pe":
                return
            if need.get(key, (None, 0))[1] < val:
                need[key] = (sem, val)

        for b in reads:
            add(b.w)
        for b in writes:
            add(b.w)
            for tok in b.r.values():
                add(tok)
        eo = self.eng[e]
        for key, (sem, val) in need.items():
            if self.seen[e].get(key, 0) < val:
                self.seen[e][key] = val
                if key in self.targets:
                    if self.plan is None:
                        self.targets[key].add(val)
                    else:
                        eo.wait_ge(sem, self.rank[key][val])
                elif self.plan is not None:
                    eo.wait_ge(sem, val)

    def _record(self, tok, reads, writes):
        for b in reads:
            if b.r.get(tok[0], (None, None, 0))[2] < tok[2]:
                b.r[tok[0]] = tok
        for b in writes:
            b.w = tok
            b.r = {}

    def op(self, e, fn, reads=(), writes=()):
        self._deps(e, reads, writes)
        self.cnt[e] += 1
        if self.plan is not None:
            ins = fn(self.eng[e])
            if self.cnt[e] in self.plan[e]:
                ins.then_inc(self.sem[e], 1)
        self._record((e, self.sem[e], self.cnt[e]), reads, writes)

    def dma(self, q, out, in_, reads=(), writes=(), indirect=None, own_sem=None):
        if own_sem is None:
            i = self.drr
            self.drr = (i + 1) % N_DMA_SEMS
        else:
            i = N_DMA_SEMS + own_sem
        eo = self.eng[q]
        key = "d%d" % i
        if own_sem is None and self.dcnt[i] > 0 and self.seen[q].get(key, 0) < 16 * self.dcnt[i]:
            if self.plan is not None:
                eo.wait_ge(self.dsem[i], 16 * self.dcnt[i])
            self.seen[q][key] = 16 * self.dcnt[i]
        self._deps(q, reads, writes)
        self.dcnt[i] += 1
        if self.plan is not None:
            if indirect is None:
                ins = eo.dma_start(out=out, in_=in_)
            else:
                ins = eo.indirect_dma_start(out=out, out_offset=None, in_=in_, in_offset=indirect)
            ins.then_inc(self.dsem[i], 16)
        self._record((key, self.dsem[i], 16 * self.dcnt[i]), reads, writes)

    def finish(self):
        eo = self.eng["sp"]
        if self.plan is None:
            for e in ("pe", "dve", "act", "pool"):
                if self.cnt[e] > 0:
                    self.targets[e].add(self.cnt[e])
            return
        for i in range(N_DMA_SEMS + 6):
            if self.dcnt[i] > 0:
                eo.wait_ge(self.dsem[i], 16 * self.dcnt[i])
        for e in ("pe", "dve", "act", "pool"):
            if self.cnt[e] > 0:
                eo.wait_ge(self.sem[e], self.rank[e][self.cnt[e]])


def build(cfg):
    _, kb_dry = _build(cfg, None)
    nc, _ = _build(cfg, kb_dry.targets)
    return nc


def _build(cfg, plan):
    nblk_a = cfg.get("nblk_a", NBLK_A)
    nq = cfg.get("nq", NQ)
    do_attn = cfg.get("attn", True)
    do_tail = cfg.get("tail", True)
    nkeys = nblk_a * 128
    nc = bass.Bass("TRN2", target_bir_lowering=False)
    es = ExitStack()
    kb = KB(nc, es, plan)
    nc._es_keep = es

    def din(name, shape, dt=F32):
        return Buf(nc.dram_tensor(name, list(shape), dt, kind="ExternalInput").ap(), name)

    def dout(name, shape, dt=F32):
        return Buf(nc.dram_tensor(name, list(shape), dt, kind="ExternalOutput").ap(), name)

    xcat = din("xcat", [nkeys, D])
    xown = din("xown", [nq, 128, D])
    xprev = din("xprev", [nq, 16, D])
    w_in = din("w_in", [D, IN_W])
    norm1_g = din("norm1_g", [D])
    q_norm_g = din("q_norm_g", [64])
    k_norm_g = din("k_norm_g", [64])
    ident_in = din("ident", [128, 128])
    rel_bias = din("rel_bias", [32, 8])
    qs_in = din("qs", [128, 128])
    thrtab_in = din("thrtab", [128, 155])
    cmask_in = din("cmask", [128, 512])
    pw_in = din("pw", [128, NIT])
    w_ba = din("w_ba", [512, D])
    w_bp = din("w_bp", [512, D])
    w_out = din("w_out", [D, D])
    peer_wq = din("peer_wq", [D, D])
    w_pool = din("w_pool", [4, 128, 128])
    pool_scale = din("pool_scale", [512])
    norm2_g = din("norm2_g", [D])
    subkeys = din("subkeys", [2, 128, 64])
    peer_u = din("peer_u", [16384, D])
    peer_v = din("peer_v", [16384, D])
    rcnt_in = din("rcnt", [128, 4, 128])
    iota16_in = din("iota16", [128, 16])
    k_own = dout("k_own", [nq, 128, 512])
    v_own = dout("v_own", [nq, 128, 512])
    ki_own = dout("ki_own", [nq, 128, 64])
    y_own = dout("y_own", [nq, 128, D])
    u_last = dout("u_last", [128, 4, 144])
    y_s_out = dout("y_s", [128, D])
    do_sample = cfg.get("sample", True)
    if do_sample:
        xs_in = din("xs_own", [128, D])
        cache_k = din("cache_k", [2560 * 128, 512])
        cache_v = din("cache_v", [2560 * 128, 512])
        cache_ik = din("cache_ik", [2560, 8192])
        state_in = din("state_own", [16, 15, 512])
        pt_in = din("pt_own", [16, 16], I32)
        zsel_in = din("zsel", [128, 31])
        k_s_out = dout("ks_o", [128, 512])
        v_s_out = dout("vs_o", [128, 512])
        ki_s_out = dout("kis_o", [128, 64])
        pool_s_out = dout("pool_s", [16, 15, 512])
        qi_d = kb.dram("qi_d", [16, 512], F32)
        wi_d = kb.dram("wi_d", [16, 8], F32)
        q_d = kb.dram("q_d", [16, 512], F32)
        sc_d = kb.dram("sc_d", [16, 2048], F32)
    wba_bf = kb.dram("wba_bf", [128, 4, D], BF16)
    wbp_bf = kb.dram("wbp_bf", [128, 4, D], BF16)
    wout_bf = kb.dram("wout_bf", [128, 8, D], BF16)
    pwq_bf = kb.dram("pwq_bf", [128, 8, D], BF16)
    pu_bf = kb.dram("pu_bf", [16384, D], BF16)
    pv_bf = kb.dram("pv_bf", [16384, D], BF16)
    win_bf = kb.dram("win_bf", [128, 8, IN_W], BF16)
    kt_s = kb.dram("kt_s", [128, 4, nkeys], BF16)
    v_s = kb.dram("v_s", [nkeys, 8, 65], BF16)
    kit_s = kb.dram("kit_s", [128, nkeys], BF16)

    ident_f = kb.sb([128, 128], F32, "ident_f")
    ident_b = kb.sb([128, 128], BF16, "ident_b")
    g1col = kb.sb([128, 8], F32, "g1col")
    qg = kb.sb([128, 64], F32, "qg")
    kg = kb.sb([128, 64], F32, "kg")
    kb.dma("sp", ident_f[:], ident_in[:, :], writes=[ident_f])
    kb.op("dve", lambda e: e.tensor_copy(out=ident_b[:], in_=ident_f[:]), reads=[ident_f], writes=[ident_b])
    ident4 = kb.sb([128, 512], BF16, "ident4")
    for j4 in range(4):
        kb.op("dve", lambda e, j4=j4: e.tensor_copy(out=ident4[:, j4 * 128:(j4 + 1) * 128], in_=ident_f[:]),
              reads=[ident_f, ident4], writes=[ident4])
    with nc.allow_non_contiguous_dma(reason="tiny param loads"):
        kb.dma("sp", g1col[:], norm1_g.t.rearrange("(k p) -> p k", p=128), writes=[g1col])
        kb.dma("sp", qg[:], q_norm_g.t.partition_broadcast(128), writes=[qg])
        kb.dma("sp", kg[:], k_norm_g.t.partition_broadcast(128), writes=[kg])
    kb.op("dve", lambda e: e.tensor_scalar(out=qg[:], in0=qg[:], scalar1=0.125, scalar2=None, op0=ALU.mult),
          reads=[qg], writes=[qg])

    pb = [kb.ps([128, 512], F32, "bank%d" % i) for i in range(8)]

    ktc = [kb.sb([128, 4, 512], BF16, "ktc%d" % j) for j in range(2)]
    vch = [kb.sb([128, 4, 520], BF16, "vch%d" % j) for j in range(2)]
    Isc = kb.sb([128, max(nkeys, 8704)], F32, "Isc")

    class View:
        def __init__(self, owner, ap):
            self.owner = owner
            self.ap = ap

        def __getitem__(self, idx):
            return self.ap[idx]

        @property
        def w(self):
            return self.owner.w

        @w.setter
        def w(self, v):
            self.owner.w = v

        @property
        def r(self):
            return self.owner.r

        @r.setter
        def r(self, v):
            self.owner.r = v

    wst_v = [View(ktc[j], ktc[j].t[:].rearrange("p a b -> p (a b)").bitcast(F32)[:, 0:1024]) for j in range(2)]
    wsb_v = [View(vch[j], vch[j].t[:].rearrange("p a b -> p (a b)")[:, 0:1024]) for j in range(2)]
    pcount = [0]

    def conv_w(src_ap, dst_ap, ncol, scal):
        s = pcount[0] % 2
        pcount[0] += 1
        kb.dma("sp", wst_v[s][:, 0:ncol], src_ap, writes=[wst_v[s].owner])
        kb.op("dve", lambda e: e.tensor_scalar(out=wsb_v[s][:, 0:ncol], in0=wst_v[s][:, 0:ncol], scalar1=scal,
                                               scalar2=None, op0=ALU.mult),
              reads=[wst_v[s].owner, g1col], writes=[wsb_v[s].owner])
        kb.dma("sp", dst_ap, wsb_v[s][:, 0:ncol], reads=[wsb_v[s].owner], writes=[win_bf])

    for kc in range(8):
        for pc in range(5):
            conv_w(w_in[kc * 128:(kc + 1) * 128, pc * 936:(pc + 1) * 936], win_bf[:, kc, pc * 936:(pc + 1) * 936],
                   936, g1col[:, kc:kc + 1])
    if do_tail:
        for (wsrc, wdst, nkc) in ((w_ba, wba_bf, 4), (w_bp, wbp_bf, 4), (w_out, wout_bf, 8), (peer_wq, pwq_bf, 8)):
            for kc in range(nkc):
                conv_w(wsrc[kc * 128:(kc + 1) * 128, :], wdst[:, kc, :], 1024, 1.0)

    wslot = [kb.sb([128, 8, 512], BF16, "wslot%d" % i) for i in range(2)]
    wstate = {"i": 0}

    def load_w(src, c0, cw, nk=8):
        s = wslot[wstate["i"] % 2]
        wstate["i"] += 1
        kb.dma("sp", s[:, 0:nk, 0:cw], src[:, 0:nk, c0:c0 + cw], reads=[src], writes=[s])
        return s

    xt = [kb.sb([128, D], F32, "xt%d" % i) for i in range(2)]
    xn = kb.sb([128, D], F32, "xn")
    junk = kb.sb([128, D], BF16, "junk")
    ssq = kb.sb([128, 1], F32, "ssq")
    rstd = kb.sb([128, 1], F32, "rstd")
    xs = kb.sb([128, D], BF16, "xs")
    hnT = kb.sb([128, 8, 128], BF16, "hnT")
    tp_bank = pb[2]

    def norm_transpose(xb, dstT, ntok=128, gtile=None, keep=None, xowner=None):
        xo = xowner or xb
        kb.op("act", lambda e: e.activation(out=junk[0:ntok, :], in_=xb[0:ntok, :], func=AF.Square,
                                            accum_out=ssq[0:ntok, :]),
              reads=[xo], writes=[junk, ssq])
        kb.op("act", lambda e: e.activation(out=rstd[0:ntok, :], in_=ssq[0:ntok, :], func=AF.Sqrt,
                                            scale=1.0 / D, bias=EPS),
              reads=[ssq], writes=[rstd])
        kb.op("dve", lambda e: e.reciprocal(out=rstd[0:ntok, :], in_=rstd[0:ntok, :]), reads=[rstd], writes=[rstd])
        if gtile is None:
            kb.op("dve", lambda e: e.tensor_scalar(out=xs[0:ntok, :], in0=xb[0:ntok, :], scalar1=rstd[0:ntok, :],
                                                   scalar2=None, op0=ALU.mult),
                  reads=[xo, rstd], writes=[xs])
        else:
            kb.op("dve", lambda e: e.scalar_tensor_tensor(out=keep[0:ntok, :], in0=xb[0:ntok, :],
                                                          scalar=rstd[0:ntok, :], in1=gtile[0:ntok, :],
                                                          op0=ALU.mult, op1=ALU.mult),
                  reads=[xo, rstd, gtile], writes=[keep])
            kb.op("dve", lambda e: e.tensor_copy(out=xs[0:ntok, :], in_=keep[0:ntok, :]), reads=[keep], writes=[xs])
        tpv = tp_bank.t[:].bitcast(BF16)
        for kc in range(8):
            kb.op("pe", lambda e, kc=kc: e.transpose(out=tpv[:, kc * 128:kc * 128 + ntok],
                                                     in_=xs[0:ntok, kc * 128:(kc + 1) * 128],
                                                     identity=ident_b[0:ntok, 0:ntok]),
                  reads=[xs, ident_b], writes=[tp_bank])
        kb.op("dve", lambda e: e.tensor_copy(
            out=dstT[:, :, 0:ntok], in_=tpv.rearrange("p (k t) -> p k t", k=8)[:, :, 0:ntok]),
            reads=[tp_bank], writes=[dstT])

    def proj_tok(dst_bank, wsl, cw, srcT=None, ntok=128):
        srcT = srcT or hnT
        for kc in range(8):
            kb.op("pe", lambda e, kc=kc: e.matmul(dst_bank[0:ntok, 0:cw], lhsT=srcT[:, kc, 0:ntok],
                                                  rhs=wsl[:, kc, 0:cw], start=(kc == 0), stop=(kc == 7)),
                  reads=[srcT, wsl], writes=[dst_bank])

    hsq = kb.sb([128, 512], F32, "hsq")
    hss = kb.sb([128, 8], F32, "hss")
    hrs = kb.sb([128, 8], F32, "hrs")

    def head_norm(src_bank, gain, dst):
        kb.op("act", lambda e: e.activation(out=hsq[:], in_=src_bank[:, 0:512], func=AF.Square),
              reads=[src_bank], writes=[hsq])
        kb.op("dve", lambda e: e.tensor_reduce(out=hss[:], in_=hsq[:].rearrange("p (h d) -> p h d", h=8),
                                               axis=AX.X, op=ALU.add),
              reads=[hsq], writes=[hss])
        kb.op("act", lambda e: e.activation(out=hrs[:], in_=hss[:], func=AF.Sqrt, scale=1.0 / 64, bias=EPS),
              reads=[hss], writes=[hrs])
        kb.op("dve", lambda e: e.reciprocal(out=hrs[:], in_=hrs[:]), reads=[hrs], writes=[hrs])
        kb.op("dve", lambda e: e.tensor_tensor(out=hsq[:].rearrange("p (h d) -> p h d", h=8),
                                               in0=src_bank[:, 0:512].rearrange("p (h d) -> p h d", h=8),
                                               in1=hrs[:].unsqueeze(2).to_broadcast([128, 8, 64]), op=ALU.mult),
              reads=[src_bank, hrs], writes=[hsq])
        kb.op("dve", lambda e: e.tensor_tensor(out=dst[:].rearrange("p (h d) -> p h d", h=8),
                                               in0=hsq[:].rearrange("p (h d) -> p h d", h=8),
                                               in1=gain[:].unsqueeze(1).to_broadcast([128, 8, 64]), op=ALU.mult),
              reads=[hsq, gain], writes=[dst])

    wk = wslot[0]
    wv = wslot[1]
    wkw = kb.sb([128, 8, 72], BF16, "wkw")
    kb.dma("sp", wk[:], win_bf[:, :, 512:1024], reads=[win_bf], writes=[wk])
    kb.dma("sp", wv[:], win_bf[:, :, 1024:1536], reads=[win_bf], writes=[wv])
    kb.dma("sp", wkw[:], win_bf[:, :, 2048:2120], reads=[win_bf], writes=[wkw])
    kf = kb.sb([128, 512], F32, "kf")
    kbf = kb.sb([128, 512], BF16, "kbf")
    ktb = kb.sb([128, 4, 128], BF16, "ktb")
    vb = kb.sb([128, 8, 65], BF16, "vb")
    kib = kb.sb([128, 128], BF16, "kib")
    kitb = kb.sb([128, 128], BF16, "kitb")
    kb.op("dve", lambda e: e.memset(vb[:], 1.0), writes=[vb])
    def table_conv_steps():
        it = 0
        for (tsrc, tdst) in ((peer_u, pu_bf), (peer_v, pv_bf)):
            for c in range(32):
                half = it % 2
                it += 1
                fst = Isc.t[:, half * 4096:(half + 1) * 4096]
                bst = (ktc if half == 0 else vch)
                kb.dma("sp", fst, tsrc[c * 512:(c + 1) * 512, :].rearrange("(p r) d -> p (r d)", p=128),
                       writes=[Isc])
                for j2 in range(2):
                    bv = bst[j2].t[:].rearrange("p a b -> p (a b)")[:, 0:2048]
                    kb.op("act", lambda e, bv=bv, fst=fst, j2=j2: e.copy(out=bv, in_=fst[:, j2 * 2048:(j2 + 1) * 2048]),
                          reads=[Isc], writes=[bst[j2]])
                    kb.dma("sp", tdst[c * 512:(c + 1) * 512, :].rearrange("(p r) d -> p r d", p=128)[:, 2 * j2:2 * j2 + 2, :],
                           bv.rearrange("p (r d) -> p r d", r=2), reads=[bst[j2]], writes=[tdst])
                yield

    tconv = table_conv_steps() if (do_tail and do_attn) else iter(())
    for blk in range(nblk_a):
        next(tconv, None)
        xb = xt[blk % 2]
        kb.dma("sp", xb[:], xcat[blk * 128:(blk + 1) * 128, :], writes=[xb])
        norm_transpose(xb, hnT)
        proj_tok(pb[0], wk, 512)
        head_norm(pb[0], kg, kf)
        kb.op("dve", lambda e: e.tensor_copy(out=kbf[:], in_=kf[:]), reads=[kf], writes=[kbf])
        tpv = tp_bank.t[:].bitcast(BF16)
        for pr in range(4):
            kb.op("pe", lambda e, pr=pr: e.transpose(out=tpv[:, pr * 128:(pr + 1) * 128],
                                                     in_=kbf[:, pr * 128:(pr + 1) * 128], identity=ident_b[:]),
                  reads=[kbf, ident_b], writes=[tp_bank])
        kb.op("act", lambda e: e.copy(out=ktb[:], in_=tpv[:, 0:512].rearrange("p (a t) -> p a t", a=4)),
              reads=[tp_bank], writes=[ktb])
        kb.dma("sp", kt_s[:, :, blk * 128:(blk + 1) * 128], ktb[:], reads=[ktb], writes=[kt_s])
        proj_tok(pb[1], wv, 512)
        kb.op("act", lambda e: e.copy(out=vb[:, :, 0:64], in_=pb[1][:, 0:512].rearrange("p (h d) -> p h d", h=8)),
              reads=[pb[1]], writes=[vb])
        kb.dma("sp", v_s[blk * 128:(blk + 1) * 128, :, :], vb[:], reads=[vb], writes=[v_s])
        proj_tok(pb[0], wkw, 72)
        kb.op("dve", lambda e: e.tensor_copy(out=kib[:, 0:64], in_=pb[0][:, 0:64]), reads=[pb[0]], writes=[kib])
        kb.op("dve", lambda e: e.tensor_copy(out=kib[:, 64:128], in_=pb[0][:, 0:64]), reads=[pb[0]], writes=[kib])
        kb.op("pe", lambda e: e.transpose(out=tpv[:, 512:640], in_=kib[:], identity=ident_b[:]),
              reads=[kib, ident_b], writes=[tp_bank])
        kb.op("act", lambda e: e.copy(out=kitb[:], in_=tpv[:, 512:640]), reads=[tp_bank], writes=[kitb])
        kb.dma("sp", kit_s[:, blk * 128:(blk + 1) * 128], kitb[:], reads=[kitb], writes=[kit_s])

    for _ in tconv:
        pass
    ko = kb.sb([128, 512], F32, "ko")
    vo = kb.sb([128, 512], F32, "vo")
    kio = kb.sb([128, 64], F32, "kio")
    wis = kb.sb([128, 8], F32, "wis")
    dbg_ao = dout("dbg_ao", [nq, 128, 512]) if cfg.get("dbg") else None
    if do_attn:
        qf = kb.sb([128, 512], F32, "qf")
        qbf = kb.sb([128, 512], BF16, "qbf")
        qT2 = kb.sb([128, 4, 128], BF16, "qT2")
        qiT2 = kb.sb([128, 4, 128], BF16, "qiT2")
        kitc = [kb.sb([128, 512], BF16, "kitc%d" % j) for j in range(2)]
        rl = [kb.sb([128, 512], BF16, "rl%d" % j) for j in range(2)]
        junkI = kb.sb([128, 2176], BF16, "junkI")
        cnt4 = kb.sb([128, 4], F32, "cnt4")
        nmid = kb.sb([128, 1], F32, "nmid")
        hi0 = kb.sb([128, 1], F32, "hi0")
        lo = kb.sb([128, 1], F32, "lo")
        mid = kb.sb([128, 1], F32, "mid")
        cntt = kb.sb([128, 1], F32, "cntt")
        dl = kb.sb([128, 1], F32, "dl")
        wh = kb.sb([128, NIT], F32, "wh")
        pw = kb.sb([128, NIT], F32, "pw")
        cmask = kb.sb([128, 512], F32, "cmask")
        negm = [kb.sb([128, 128], BF16, "negm%d" % j) for j in range(2)]
        addh = kb.sb([128, 8, 128], BF16, "addh")
        PT = [kb.sb([128, 8, 128], BF16, "PT%d" % j) for j in range(2)]
        rden = kb.sb([128, 8], F32, "rden")
        ao = kb.sb([128, 512], BF16, "ao")
        aof = kb.sb([128, 512], F32, "aof") if cfg.get("dbg") else None
        kb.dma("sp", pw[:], pw_in[:, :], writes=[pw])
        kb.dma("sp", cmask[:], cmask_in[:, :], writes=[cmask])
        qs = kb.sb([128, 128], F32, "qs")
        thrtab = kb.sb([128, 155], F32, "thrtab")
        rbb = kb.sb([128, 256], F32, "rbb")
        ndel = kb.sb([128, 256], F32, "ndel")
        ind = kb.sb([128, 128], F32, "ind")
        NB = [kb.sb([128, 8, 128], BF16, "NB%d" % j) for j in range(5)]
        NBt = kb.sb([128, 8, 128], F32, "h2")
        h2 = View(NBt, NBt.t[:].rearrange("p a b -> p (a b)"))
        kb.dma("sp", qs[:], qs_in[:, :], writes=[qs])
        kb.dma("sp", thrtab[:], thrtab_in[:, :], writes=[thrtab])
        with nc.allow_non_contiguous_dma(reason="tiny param loads"):
            kb.dma("sp", rbb[:], rel_bias.t.rearrange("b h -> (b h)").partition_broadcast(128), writes=[rbb])
        kb.op("dve", lambda e: e.tensor_tensor(out=ndel[:, 8:256], in0=rbb[:, 0:248], in1=rbb[:, 8:256],
                                               op=ALU.subtract), reads=[rbb], writes=[ndel])
        for r5 in range(5):
            kb.op("dve", lambda e: e.memset(NBt[:], 0.0), writes=[NBt])
            for b in range(1, 32):
                col = r5 * 31 + (b - 1)
                kb.op("dve", lambda e, col=col: e.tensor_scalar(out=ind[:], in0=qs[:], scalar1=thrtab[:, col:col + 1],
                                                                scalar2=None, op0=ALU.is_lt),
                      reads=[qs, thrtab], writes=[ind])
                for h in range(8):
                    kb.op("dve", lambda e, b=b, h=h: e.scalar_tensor_tensor(
                        out=NBt[:, h, :], in0=ind[:], scalar=ndel[:, b * 8 + h:b * 8 + h + 1], in1=NBt[:, h, :],
                        op0=ALU.mult, op1=ALU.add), reads=[ind, ndel, NBt], writes=[NBt])
            kb.op("dve", lambda e, r5=r5: e.tensor_copy(out=NB[r5][:], in_=NBt[:]), reads=[NBt], writes=[NB[r5]])
    if do_tail:
        aoT = kb.sb([128, 4, 128], BF16, "aoT")
        hnTp = kb.sb([128, 8, 16], BF16, "hnTp")
        xpv = kb.sb([16, D], F32, "xpv")
        uT = kb.sb([128, 4, 144], F32, "uT")
        s1 = kb.sb([128, 4, 144], F32, "s1")
        s2 = kb.sb([128, 4, 144], F32, "s2")
        s3 = kb.sb([128, 4, 144], F32, "s3")
        pmf = kb.sb([128, 4, 128], F32, "pmf")
        pmT = kb.sb([128, 4, 128], BF16, "pmT")
        poT = kb.sb([128, 4, 128], BF16, "poT")
        sga = kb.sb([128, 8, 128], BF16, "sga")
        sgb = kb.sb([128, 8, 128], BF16, "sgb")
        tmpA = kb.sb([128, 512], F32, "tmpA")
        tmpB = kb.sb([128, 512], F32, "tmpB")
        mT = kb.sb([128, 8, 128], BF16, "mT")
        xnT = sgb
        qpT = sga
        g2b = kb.sb([128, D], F32, "g2b")
        wpl = kb.sb([128, 4, 128], BF16, "wpl")
        wplf = kb.sb([128, 4, 128], F32, "wplf")
        pscol = kb.sb([128, 4], F32, "pscol")
        SKf = kb.sb([128, 256], F32, "SKf")
        sktmp = kb.sb([128, 128], F32, "sktmp")
        SK = kb.sb([128, 256], BF16, "SK")
        rcnt = kb.sb([128, 4, 128], F32, "rcnt")
        iota16 = kb.sb([128, 16], F32, "iota16")
        tv = kb.sb([128, 8, 2, 16], F32, "tv")
        ti = kb.sb([128, 8, 2, 16], U32, "ti")
        tif = kb.sb([128, 8, 2, 16], F32, "tif")
        tsv = kb.sb([128, 8, 16], F32, "tsv")
        tpos = kb.sb([128, 8, 16], U32, "tpos")
        pa = kb.sb([128, 8, 16], U32, "pa")
        pbb = kb.sb([128, 8, 16], U32, "pbb")
        paf = kb.sb([128, 8, 16], F32, "paf")
        pbf = kb.sb([128, 8, 16], F32, "pbf")
        i1s = kb.sb([128, 8, 16], F32, "i1s")
        i2s = kb.sb([128, 8, 16], F32, "i2s")
        eidf = kb.sb([128, 128], F32, "eidf")
        eidi = kb.sb([128, 128], I32, "eidi")
        gmx = kb.sb([128, 8], F32, "gmx")
        gk = kb.sb([128, 8, 16], F32, "gk")
        actv = kb.sb([128, 128], F32, "actv")
        wgt = kb.sb([128, 128], F32, "wgt")
        m8 = kb.sb([128, 8], F32, "m8")
        UbS = [View(o, o.t[:].rearrange("p a b -> p (a b)").bitcast(F32)[:, 0:1024]) for o in (ktc[0], ktc[1], vch[0], vch[1])]
        Ubo = [kb.sb([128, D], BF16, "Ub%d" % j) for j in range(6)]
        Ub = [View(o, o.t[:]) for o in Ubo]
        kb.dma("sp", rcnt[:], rcnt_in[:, :, :], writes=[rcnt])
        kb.dma("sp", iota16[:], iota16_in[:, :], writes=[iota16])
        kb.op("dve", lambda e: e.memset(SKf[:], 0.0), writes=[SKf])
        with nc.allow_non_contiguous_dma(reason="small param loads"):
            kb.dma("sp", g2b[:], norm2_g.t.partition_broadcast(128), writes=[g2b])
            kb.dma("sp", pscol[:], pool_scale.t.rearrange("(g d) -> d g", d=128), writes=[pscol])
            kb.dma("sp", wplf[:], w_pool.t.rearrange("g c d -> c g d"), writes=[wplf])
            kb.dma("sp", sktmp[:, 0:64], subkeys.t[0], writes=[sktmp])
            kb.dma("sp", sktmp[:, 64:128], subkeys.t[1], writes=[sktmp])
        kb.op("pe", lambda e: e.transpose(out=pb[2][:, 0:128], in_=sktmp[:], identity=ident_f[:]),
              reads=[sktmp, ident_f], writes=[pb[2]])
        kb.op("dve", lambda e: e.tensor_copy(out=SKf[0:64, 0:128], in_=pb[2][0:64, 0:128]), reads=[pb[2], SKf], writes=[SKf])
        kb.op("dve", lambda e: e.tensor_copy(out=SKf[64:128, 128:256], in_=pb[2][64:128, 0:128]), reads=[pb[2], SKf],
              writes=[SKf])
        kb.op("dve", lambda e: e.tensor_copy(out=SK[:], in_=SKf[:]), reads=[SKf], writes=[SK])
        kb.op("dve", lambda e: e.tensor_copy(out=wpl[:], in_=wplf[:]), reads=[wplf], writes=[wpl])
        IscV = Isc.t[:]
        ssc = IscV[:, 0:2048]
        sscw = IscV[:, 2048:4096]
        cand = IscV[:, 4096:6144]
        candw = IscV[:, 6144:8192]
        ohv = IscV[:, 0:2048]
        ohv2 = IscV[:, 2048:4096]

    def tail_block(i, xb, sample=False):
        tpv = tp_bank.t[:].bitcast(BF16)
        for pr in range(4):
            kb.op("pe", lambda e, pr=pr: e.transpose(out=tpv[:, pr * 128:(pr + 1) * 128],
                                                     in_=ao[:, pr * 128:(pr + 1) * 128], identity=ident_b[:]),
                  reads=[ao, ident_b], writes=[tp_bank])
        kb.op("act", lambda e: e.copy(out=aoT[:], in_=tpv[:, 0:512].rearrange("p (a t) -> p a t", a=4)),
              reads=[tp_bank], writes=[aoT])
        if not sample:
            kb.dma("sp", xpv[:], xprev[i, :, :], writes=[xpv])
            norm_transpose(xpv, hnTp, ntok=16)
            wsl = load_w(win_bf, *CHUNKS[CH_U])
            for g in range(4):
                bk = pb[g // 2]
                c0 = (g % 2) * 144
                for kc in range(8):
                    kb.op("pe", lambda e, bk=bk, c0=c0, g=g, kc=kc, wsl=wsl: e.matmul(
                        bk[:, c0:c0 + 16], lhsT=wsl[:, kc, g * 128:(g + 1) * 128], rhs=hnTp[:, kc, :],
                        start=(kc == 0), stop=(kc == 7)), reads=[wsl, hnTp], writes=[bk])
                for kc in range(8):
                    kb.op("pe", lambda e, bk=bk, c0=c0, g=g, kc=kc, wsl=wsl: e.matmul(
                        bk[:, c0 + 16:c0 + 144], lhsT=wsl[:, kc, g * 128:(g + 1) * 128], rhs=hnT[:, kc, :],
                        start=(kc == 0), stop=(kc == 7)), reads=[wsl, hnT], writes=[bk])
            for hf in range(2):
                kb.op("act", lambda e, hf=hf: e.copy(out=uT[:, 2 * hf:2 * hf + 2, :],
                                                     in_=pb[hf][:, 0:288].rearrange("p (g t) -> p g t", g=2)),
                      reads=[pb[hf]], writes=[uT])
            if i == nq - 1:
                kb.dma("sp", u_last[:, :, :], uT[:], reads=[uT], writes=[u_last])
            kb.op("dve", lambda e: e.tensor_tensor(out=s1[:, :, 1:144], in0=uT[:, :, 1:144], in1=uT[:, :, 0:143],
                                                   op=ALU.add), reads=[uT], writes=[s1])
            kb.op("dve", lambda e: e.tensor_tensor(out=s2[:, 1:4, 3:144], in0=s1[:, 1:4, 3:144], in1=s1[:, 1:4, 1:142],
                                                   op=ALU.add), reads=[s1], writes=[s2])
            kb.op("dve", lambda e: e.tensor_tensor(out=s3[:, 2:4, 7:144], in0=s2[:, 2:4, 7:144], in1=s2[:, 2:4, 3:140],
                                                   op=ALU.add), reads=[s2], writes=[s3])
            kb.op("dve", lambda e: e.tensor_tensor(out=s1[:, 3, 15:144], in0=s3[:, 3, 15:144], in1=s3[:, 3, 7:136],
                                                   op=ALU.add), reads=[s3, s1], writes=[s1])
            wsum = [s1[:, 0, 16:144], s2[:, 1, 16:144], s3[:, 2, 16:144], s1[:, 3, 16:144]]
            wsrc = [s1, s2, s3, s1]
            for g in range(4):
                if i == 0:
                    kb.op("dve", lambda e, g=g: e.tensor_tensor(out=pmf[:, g, :], in0=wsum[g], in1=rcnt[:, g, :],
                                                                op=ALU.mult), reads=[wsrc[g], rcnt], writes=[pmf])
                    kb.op("dve", lambda e, g=g: e.tensor_tensor(out=pmT[:, g, :], in0=pmf[:, g, :], in1=uT[:, g, 16:144],
                                                                op=ALU.subtract), reads=[pmf, uT], writes=[pmT])
                else:
                    kb.op("dve", lambda e, g=g: e.scalar_tensor_tensor(
                        out=pmT[:, g, :], in0=wsum[g], scalar=1.0 / (2 ** (g + 1)), in1=uT[:, g, 16:144],
                        op0=ALU.mult, op1=ALU.subtract), reads=[wsrc[g], uT], writes=[pmT])
        for g in range(4):
            kb.op("pe", lambda e, g=g: e.matmul(pb[0][:, g * 128:(g + 1) * 128], lhsT=wpl[:, g, :], rhs=pmT[:, g, :],
                                                start=True, stop=True), reads=[wpl, pmT], writes=[pb[0]])
        kb.op("dve", lambda e: e.tensor_tensor(out=poT[:], in0=pb[0][:, 0:512].rearrange("p (g t) -> p g t", g=4),
                                               in1=pscol[:].unsqueeze(2).to_broadcast([128, 4, 128]), op=ALU.mult),
              reads=[pb[0], pscol], writes=[poT])
        for (chs, dst) in (((CH_GA0, CH_GA1), sga), ((CH_GB0, CH_GB1), sgb)):
            for hf, chn in enumerate(chs):
                wsl = load_w(win_bf, *CHUNKS[chn])
                bk = pb[hf]
                for ft in range(4):
                    for kc in range(8):
                        kb.op("pe", lambda e, bk=bk, ft=ft, kc=kc, wsl=wsl: e.matmul(
                            bk[:, ft * 128:(ft + 1) * 128], lhsT=wsl[:, kc, ft * 128:(ft + 1) * 128], rhs=hnT[:, kc, :],
                            start=(kc == 0), stop=(kc == 7)), reads=[wsl, hnT], writes=[bk])
                kb.op("act", lambda e, bk=bk, dst=dst, hf=hf: e.activation(
                    out=dst[:, 4 * hf:4 * hf + 4, :], in_=bk[:, 0:512].rearrange("p (f t) -> p f t", f=4),
                    func=AF.Sigmoid), reads=[bk], writes=[dst])
        for hf in range(2):
            wa = load_w(wba_bf, hf * 512, 512, nk=4)
            wb = load_w(wbp_bf, hf * 512, 512, nk=4)
            for ft in range(4):
                for kc in range(4):
                    kb.op("pe", lambda e, ft=ft, kc=kc, wa=wa: e.matmul(
                        pb[0][:, ft * 128:(ft + 1) * 128], lhsT=wa[:, kc, ft * 128:(ft + 1) * 128], rhs=aoT[:, kc, :],
                        start=(kc == 0), stop=(kc == 3)), reads=[wa, aoT], writes=[pb[0]])
                for kc in range(4):
                    kb.op("pe", lambda e, ft=ft, kc=kc, wb=wb: e.matmul(
                        pb[1][:, ft * 128:(ft + 1) * 128], lhsT=wb[:, kc, ft * 128:(ft + 1) * 128], rhs=poT[:, kc, :],
                        start=(kc == 0), stop=(kc == 3)), reads=[wb, poT], writes=[pb[1]])
            kb.op("dve", lambda e, hf=hf: e.tensor_tensor(
                out=tmpA[:], in0=pb[0][:, 0:512], in1=sga[:, 4 * hf:4 * hf + 4, :].rearrange("p f t -> p (f t)"),
                op=ALU.mult), reads=[pb[0], sga], writes=[tmpA])
            kb.op("dve", lambda e, hf=hf: e.tensor_tensor(
                out=tmpB[:], in0=pb[1][:, 0:512], in1=sgb[:, 4 * hf:4 * hf + 4, :].rearrange("p f t -> p (f t)"),
                op=ALU.mult), reads=[pb[1], sgb], writes=[tmpB])
            kb.op("dve", lambda e, hf=hf: e.tensor_tensor(
                out=mT[:, 4 * hf:4 * hf + 4, :].rearrange("p f t -> p (f t)"), in0=tmpA[:], in1=tmpB[:], op=ALU.add),
                reads=[tmpA, tmpB], writes=[mT])
        for hf in range(2):
            wo = load_w(wout_bf, hf * 512, 512, nk=8)
            for kc in range(8):
                kb.op("pe", lambda e, kc=kc, wo=wo, hf=hf: e.matmul(
                    pb[hf][:, 0:512], lhsT=mT[:, kc, :], rhs=wo[:, kc, :], start=(kc == 0), stop=(kc == 7)),
                    reads=[mT, wo], writes=[pb[hf]])
            kb.op("dve", lambda e, hf=hf: e.tensor_tensor(out=h2[:, hf * 512:(hf + 1) * 512], in0=pb[hf][:, 0:512],
                                                          in1=xb[:, hf * 512:(hf + 1) * 512], op=ALU.add),
                  reads=[pb[hf], xb], writes=[NBt])
        norm_transpose(h2, xnT, gtile=g2b, keep=xn, xowner=NBt)
        for hf in range(2):
            wq_ = load_w(pwq_bf, hf * 512, 512, nk=8)
            for ft in range(4):
                for kc in range(8):
                    kb.op("pe", lambda e, ft=ft, kc=kc, wq_=wq_, hf=hf: e.matmul(
                        pb[hf][:, ft * 128:(ft + 1) * 128], lhsT=wq_[:, kc, ft * 128:(ft + 1) * 128], rhs=xnT[:, kc, :],
                        start=(kc == 0), stop=(kc == 7)), reads=[wq_, xnT], writes=[pb[hf]])
            kb.op("act", lambda e, hf=hf: e.copy(out=qpT[:, 4 * hf:4 * hf + 4, :],
                                                 in_=pb[hf][:, 0:512].rearrange("p (f t) -> p f t", f=4)),
                  reads=[pb[hf]], writes=[qpT])
        sbanks = [pb[0], pb[1], pb[3], pb[4]]
        for h in range(8):
            bk = sbanks[h // 2]
            kb.op("pe", lambda e, bk=bk, h=h: e.matmul(bk[:, (h % 2) * 256:(h % 2) * 256 + 256], lhsT=qpT[:, h, :],
                                                      rhs=SK[:], start=True, stop=True),
                  reads=[qpT, SK], writes=[bk])
        for j4 in range(4):
            kb.op("act", lambda e, j4=j4: e.copy(out=ssc[:, j4 * 512:(j4 + 1) * 512], in_=sbanks[j4][:, 0:512]),
                  reads=[sbanks[j4]], writes=[Isc])
        tvv = tv[:].rearrange("p h s k -> p (h s) k")
        tiv = ti[:].rearrange("p h s k -> p (h s) k")
        for gi in range(16):
            sv = ssc[:, gi * 128:(gi + 1) * 128]
            sw = sscw[:, gi * 128:(gi + 1) * 128]
            kb.op("dve", lambda e, sv=sv, gi=gi: e.max(out=tvv[:, gi, 0:8], in_=sv), reads=[Isc], writes=[tv])
            kb.op("dve", lambda e, sv=sv, gi=gi: e.max_index(out=tiv[:, gi, 0:8], in_max=tvv[:, gi, 0:8], in_values=sv),
                  reads=[Isc, tv], writes=[ti])
            kb.op("dve", lambda e, sv=sv, sw=sw, gi=gi: e.match_replace(out=sw, in_to_replace=tvv[:, gi, 0:8],
                                                                       in_values=sv, imm_value=-1e30),
                  reads=[Isc, tv], writes=[Isc])
            kb.op("dve", lambda e, sw=sw, gi=gi: e.max(out=tvv[:, gi, 8:16], in_=sw), reads=[Isc], writes=[tv])
            kb.op("dve", lambda e, sw=sw, gi=gi: e.max_index(out=tiv[:, gi, 8:16], in_max=tvv[:, gi, 8:16], in_values=sw),
                  reads=[Isc, tv], writes=[ti])
        kb.op("dve", lambda e: e.tensor_copy(out=tif[:], in_=ti[:]), reads=[ti], writes=[tif])
        candv = cand.rearrange("p (h a b) -> p h a b", h=8, a=16)
        kb.op("dve", lambda e: e.tensor_tensor(out=candv, in0=tv[:, :, 0, :].unsqueeze(3).to_broadcast([128, 8, 16, 16]),
                                               in1=tv[:, :, 1, :].unsqueeze(2).to_broadcast([128, 8, 16, 16]), op=ALU.add),
              reads=[tv], writes=[Isc])
        for h in range(8):
            cv = cand[:, h * 256:(h + 1) * 256]
            cw = candw[:, h * 256:(h + 1) * 256]
            kb.op("dve", lambda e, cv=cv, h=h: e.max(out=tsv[:, h, 0:8], in_=cv), reads=[Isc], writes=[tsv])
            kb.op("dve", lambda e, cv=cv, h=h: e.max_index(out=tpos[:, h, 0:8], in_max=tsv[:, h, 0:8], in_values=cv),
                  reads=[Isc, tsv], writes=[tpos])
            kb.op("dve", lambda e, cv=cv, cw=cw, h=h: e.match_replace(out=cw, in_to_replace=tsv[:, h, 0:8],
                                                                     in_values=cv, imm_value=-1e30),
                  reads=[Isc, tsv], writes=[Isc])
            kb.op("dve", lambda e, cw=cw, h=h: e.max(out=tsv[:, h, 8:16], in_=cw), reads=[Isc], writes=[tsv])
            kb.op("dve", lambda e, cw=cw, h=h: e.max_index(out=tpos[:, h, 8:16], in_max=tsv[:, h, 8:16], in_values=cw),
                  reads=[Isc, tsv], writes=[tpos])
        kb.op("dve", lambda e: e.tensor_single_scalar(out=pa[:], in_=tpos[:], scalar=4, op=ALU.logical_shift_right),
              reads=[tpos], writes=[pa])
        kb.op("dve", lambda e: e.tensor_single_scalar(out=pbb[:], in_=tpos[:], scalar=15, op=ALU.bitwise_and),
              reads=[tpos], writes=[pbb])
        kb.op("dve", lambda e: e.tensor_copy(out=paf[:], in_=pa[:]), reads=[pa], writes=[paf])
        kb.op("dve", lambda e: e.tensor_copy(out=pbf[:], in_=pbb[:]), reads=[pbb], writes=[pbf])
        oh4 = ohv.rearrange("p (h k a) -> p h k a", h=8, k=16)
        oh42 = ohv2.rearrange("p (h k a) -> p h k a", h=8, k=16)
        io4 = iota16[:].unsqueeze(1).unsqueeze(1).to_broadcast([128, 8, 16, 16])
        for (pf, half, dst) in ((paf, 0, i1s), (pbf, 1, i2s)):
            kb.op("dve", lambda e, pf=pf: e.tensor_tensor(out=oh4, in0=pf[:].unsqueeze(3).to_broadcast([128, 8, 16, 16]),
                                                          in1=io4, op=ALU.is_equal),
                  reads=[pf, iota16], writes=[Isc])
            kb.op("dve", lambda e, half=half: e.tensor_tensor(
                out=oh42, in0=oh4, in1=tif[:, :, half, :].unsqueeze(2).to_broadcast([128, 8, 16, 16]), op=ALU.mult),
                reads=[Isc, tif], writes=[Isc])
            kb.op("dve", lambda e, dst=dst: e.tensor_reduce(out=dst[:], in_=oh42, axis=AX.X, op=ALU.add),
                  reads=[Isc], writes=[dst])
        kb.op("dve", lambda e: e.scalar_tensor_tensor(out=eidf[:].rearrange("p (h k) -> p h k", h=8), in0=i1s[:],
                                                      scalar=128.0, in1=i2s[:], op0=ALU.mult, op1=ALU.add),
              reads=[i1s, i2s], writes=[eidf])
        kb.op("dve", lambda e: e.tensor_copy(out=eidi[:], in_=eidf[:]), reads=[eidf], writes=[eidi])
        kb.op("dve", lambda e: e.tensor_reduce(out=gmx[:], in_=tsv[:], axis=AX.X, op=ALU.max), reads=[tsv], writes=[gmx])
        kb.op("dve", lambda e: e.tensor_tensor(out=gk[:], in0=tsv[:], in1=gmx[:].unsqueeze(2).to_broadcast([128, 8, 16]),
                                               op=ALU.subtract), reads=[tsv, gmx], writes=[gk])
        kb.op("act", lambda e: e.activation(out=gk[:], in_=gk[:], func=AF.Exp), reads=[gk], writes=[gk])
        kb.op("dve", lambda e: e.tensor_reduce(out=gmx[:], in_=gk[:], axis=AX.X, op=ALU.add), reads=[gk], writes=[gmx])
        kb.op("dve", lambda e: e.reciprocal(out=gmx[:], in_=gmx[:]), reads=[gmx], writes=[gmx])
        kb.op("dve", lambda e: e.tensor_tensor(out=gk[:], in0=gk[:], in1=gmx[:].unsqueeze(2).to_broadcast([128, 8, 16]),
                                               op=ALU.mult), reads=[gk, gmx], writes=[gk])
        def peer_steps():
            for hk in range(128):
                ub = Ub[hk % 6]
                kb.dma("pool", ub[:], pu_bf[:, :], reads=[eidi, pu_bf], writes=[ub.owner],
                       indirect=bass.IndirectOffsetOnAxis(ap=eidi[:, hk:hk + 1], axis=0), own_sem=hk % 6)
                kb.op("dve", lambda e, ub=ub, hk=hk: e.scalar_tensor_tensor(
                    out=ub[:], in0=ub[:], scalar=1.0, in1=xn[:], op0=ALU.mult, op1=ALU.mult,
                    accum_out=actv[:, hk:hk + 1]), reads=[ub.owner, xn], writes=[ub.owner, actv])
                yield
            kb.op("act", lambda e: e.activation(out=wgt[:], in_=actv[:], func=AF.Gelu), reads=[actv], writes=[wgt])
            kb.op("dve", lambda e: e.tensor_tensor(out=wgt[:], in0=wgt[:], in1=gk[:].rearrange("p h k -> p (h k)"),
                                                   op=ALU.mult), reads=[wgt, gk], writes=[wgt])
            for hk in range(128):
                ub = Ub[(2 + hk) % 6]
                kb.dma("pool", ub[:], pv_bf[:, :], reads=[eidi, pv_bf], writes=[ub.owner],
                       indirect=bass.IndirectOffsetOnAxis(ap=eidi[:, hk:hk + 1], axis=0), own_sem=(2 + hk) % 6)
                kb.op("dve", lambda e, ub=ub, hk=hk: e.scalar_tensor_tensor(
                    out=h2[:], in0=ub[:], scalar=wgt[:, hk:hk + 1], in1=h2[:], op0=ALU.mult, op1=ALU.add),
                    reads=[ub.owner, wgt, NBt], writes=[NBt])
                yield
            if sample:
                kb.dma("sp", y_s_out[:, :], h2[:], reads=[NBt], writes=[y_s_out])
            else:
                kb.dma("sp", y_own[i, :, :], h2[:], reads=[NBt], writes=[y_own])
            yield
        return peer_steps()

    pending = [None]
    nslots = [1]

    def advance(n=1):
        g = pending[0]
        if g is None:
            return
        for _ in range(n):
            try:
                next(g)
            except StopIteration:
                pending[0] = None
                return

    def drain():
        while pending[0] is not None:
            advance(64)

    for i in range(nq):
        xb = xt[i % 2]
        kb.dma("sp", xb[:], xown[i, :, :], writes=[xb])
        norm_transpose(xb, hnT)
        wsl = load_w(win_bf, *CHUNKS[CH_K])
        proj_tok(pb[0], wsl, 512)
        head_norm(pb[0], kg, ko)
        kb.dma("sp", k_own[i, :, :], ko[:], reads=[ko], writes=[k_own])
        wsl = load_w(win_bf, *CHUNKS[CH_V])
        proj_tok(pb[1], wsl, 512)
        kb.op("act", lambda e: e.copy(out=vo[:], in_=pb[1][:, 0:512]), reads=[pb[1]], writes=[vo])
        kb.dma("sp", v_own[i, :, :], vo[:], reads=[vo], writes=[v_own])
        wsl = load_w(win_bf, *CHUNKS[CH_KW])
        proj_tok(pb[0], wsl, 72)
        kb.op("dve", lambda e: e.tensor_copy(out=kio[:], in_=pb[0][:, 0:64]), reads=[pb[0]], writes=[kio])
        kb.dma("sp", ki_own[i, :, :], kio[:], reads=[kio], writes=[ki_own])
        kb.op("dve", lambda e: e.tensor_scalar(out=wis[:], in0=pb[0][:, 64:72], scalar1=0.125 * (8 ** -0.5),
                                               scalar2=None, op0=ALU.mult), reads=[pb[0]], writes=[wis])
        if not do_attn:
            continue
        tpv = tp_bank.t[:].bitcast(BF16)
        wsl = load_w(win_bf, *CHUNKS[CH_Q])
        proj_tok(pb[0], wsl, 512)
        head_norm(pb[0], qg, qf)
        kb.op("dve", lambda e: e.tensor_copy(out=qbf[:], in_=qf[:]), reads=[qf], writes=[qbf])
        for pr in range(4):
            kb.op("pe", lambda e, pr=pr: e.transpose(out=tpv[:, pr * 128:(pr + 1) * 128],
                                                     in_=qbf[:, pr * 128:(pr + 1) * 128], identity=ident_b[:]),
                  reads=[qbf, ident_b], writes=[tp_bank])
        kb.op("act", lambda e: e.copy(out=qT2[:], in_=tpv[:, 0:512].rearrange("p (a t) -> p a t", a=4)),
              reads=[tp_bank], writes=[qT2])
        wsl = load_w(win_bf, *CHUNKS[CH_QI])
        proj_tok(pb[1], wsl, 512)
        kb.op("act", lambda e: e.copy(out=qbf[:], in_=pb[1][:, 0:512]), reads=[pb[1]], writes=[qbf])
        for pr in range(4):
            kb.op("pe", lambda e, pr=pr: e.transpose(out=tpv[:, pr * 128:(pr + 1) * 128],
                                                     in_=qbf[:, pr * 128:(pr + 1) * 128], identity=ident_b[:]),
                  reads=[qbf, ident_b], writes=[tp_bank])
        kb.op("act", lambda e: e.copy(out=qiT2[:], in_=tpv[:, 0:512].rearrange("p (a t) -> p a t", a=4)),
              reads=[tp_bank], writes=[qiT2])
        nkb = min(4 * i + 4, nblk_a)
        nch = nkb // 4
        nk = nkb * 128
        ibanks = [pb[0], pb[1], pb[3], pb[4]]
        cnt_i = 0
        bis_steps = NIT * (6 if nkb * 128 >= 4096 else (4 if nkb * 128 >= 2048 else 1))
        per_slot = 1
        att_slot = max(1, -(-(260 - bis_steps - nch * 8) // nkb))
        for ch in range(nch):
            kc_ = kitc[ch % 2]
            kb.dma("sp", kc_[:], kit_s[:, ch * 512:(ch + 1) * 512], reads=[kit_s], writes=[kc_])
            Ic = Isc[:, ch * 512:(ch + 1) * 512]
            for h in range(8):
                a, half = h // 2, h % 2
                ps_ = slice(64 * half, 64 * half + 64)
                bk = ibanks[cnt_i % 4]
                rl_ = rl[cnt_i % 2]
                cnt_i += 1
                advance(per_slot)
                kb.op("pe", lambda e, bk=bk, a=a, ps_=ps_, kc_=kc_: e.matmul(
                    bk[:, 0:512], lhsT=qiT2[ps_, a, :], rhs=kc_[ps_, :], start=True, stop=True),
                    reads=[qiT2, kc_], writes=[bk])
                kb.op("act", lambda e, bk=bk, rl_=rl_: e.activation(out=rl_[:], in_=bk[:, 0:512], func=AF.Relu),
                      reads=[bk], writes=[rl_])
                if h == 0:
                    kb.op("dve", lambda e, rl_=rl_, Ic=Ic: e.tensor_scalar(
                        out=Ic, in0=rl_[:], scalar1=wis[:, 0:1], scalar2=None, op0=ALU.mult),
                        reads=[rl_, wis], writes=[Isc])
                else:
                    kb.op("dve", lambda e, rl_=rl_, Ic=Ic, h=h: e.scalar_tensor_tensor(
                        out=Ic, in0=rl_[:], scalar=wis[:, h:h + 1], in1=Ic, op0=ALU.mult, op1=ALU.add),
                        reads=[rl_, wis, Isc], writes=[Isc])
        kb.op("dve", lambda e: e.tensor_reduce(out=hi0[:], in_=Isc[:, 0:nk], axis=AX.X, op=ALU.max),
              reads=[Isc], writes=[hi0])
        kb.op("dve", lambda e: e.tensor_reduce(out=lo[:], in_=Isc[:, 0:nk], axis=AX.X, op=ALU.min),
              reads=[Isc], writes=[lo])
        kb.op("dve", lambda e: e.tensor_tensor(out=Isc[:, nk - 512:nk], in0=Isc[:, nk - 512:nk], in1=cmask[:],
                                               op=ALU.add), reads=[Isc, cmask], writes=[Isc])
        kb.op("dve", lambda e: e.tensor_tensor(out=hi0[:], in0=hi0[:], in1=lo[:], op=ALU.subtract),
              reads=[hi0, lo], writes=[hi0])
        kb.op("dve", lambda e: e.tensor_scalar(out=wh[:], in0=pw[:], scalar1=hi0[:, 0:1], scalar2=None,
                                               op0=ALU.mult), reads=[pw, hi0], writes=[wh])
        for n in range(NIT):
            kb.op("dve", lambda e, n=n: e.tensor_tensor(out=mid[:], in0=lo[:], in1=wh[:, n:n + 1], op=ALU.add),
                  reads=[lo, wh], writes=[mid])
            kb.op("dve", lambda e: e.tensor_scalar(out=nmid[:], in0=mid[:], scalar1=-1.0, scalar2=None, op0=ALU.mult),
                  reads=[mid], writes=[nmid])
            kb.op("dve", lambda e: e.memset(cnt4[:], 0.0), writes=[cnt4])
            for q4 in range((nk + 2175) // 2176):
                c0 = q4 * 2176
                c1 = min(nk, c0 + 2176)
                kb.op("act", lambda e, c0=c0, c1=c1, q4=q4: e.activation(
                    out=junkI[:, 0:c1 - c0], in_=Isc[:, c0:c1], func=AF.Sign, bias=nmid[:, 0:1], scale=1.0,
                    accum_out=cnt4[:, q4:q4 + 1]), reads=[Isc, nmid], writes=[junkI, cnt4])
            advance(6 if nk >= 4096 else (4 if nk >= 2048 else 1))
            kb.op("dve", lambda e: e.tensor_reduce(out=cntt[:], in_=cnt4[:], axis=AX.X, op=ALU.add),
                  reads=[cnt4], writes=[cntt])
            kb.op("dve", lambda e, n=n: e.tensor_scalar(out=dl[:], in0=cntt[:], scalar1=511.5 - nk,
                                                        scalar2=wh[:, n:n + 1], op0=ALU.is_ge, op1=ALU.mult),
                  reads=[cntt, wh], writes=[dl])
            kb.op("dve", lambda e: e.tensor_tensor(out=lo[:], in0=lo[:], in1=dl[:], op=ALU.add),
                  reads=[lo, dl], writes=[lo])
        kb.op("dve", lambda e: e.memset(pb[5][:, 0:260], 0.0), writes=[pb[5]])
        kb.op("dve", lambda e: e.memset(pb[6][:, 0:260], 0.0), writes=[pb[6]])
        def emit_S(kbi):
            ch, kloc = kbi // 4, kbi % 4
            ktc_ = ktc[ch % 2]
            vch_ = vch[ch % 2]
            if kloc == 0:
                kb.dma("sp", ktc_[:], kt_s[:, :, ch * 512:(ch + 1) * 512], reads=[kt_s], writes=[ktc_])
                kb.dma("sp", vch_[:], v_s.t[ch * 512:(ch + 1) * 512, :, :].rearrange("(b p) h e -> p b (h e)", p=128),
                       reads=[v_s], writes=[vch_])
            nm = negm[kbi % 2]
            kb.op("dve", lambda e, nm=nm, kbi=kbi: e.tensor_scalar(
                out=nm[:], in0=Isc[:, kbi * 128:(kbi + 1) * 128], scalar1=lo[:, 0:1], scalar2=-30000.0,
                op0=ALU.is_lt, op1=ALU.mult), reads=[Isc, lo], writes=[nm])
            r = kbi - 4 * i
            near = (-1 <= r <= 3)
            if near:
                kb.op("dve", lambda e, nm=nm, r=r: e.tensor_tensor(
                    out=addh[:], in0=NB[r + 1][:], in1=nm[:].unsqueeze(1).to_broadcast([128, 8, 128]), op=ALU.add),
                    reads=[NB[r + 1], nm], writes=[addh])
            pt_ = PT[kbi % 2]
            for g in range(2):
                sb_ = pb[3 + g]
                for hh in range(4):
                    h = 4 * g + hh
                    a, half = h // 2, h % 2
                    ps_ = slice(64 * half, 64 * half + 64)
                    kb.op("pe", lambda e, sb_=sb_, hh=hh, a=a, ps_=ps_, ktc_=ktc_, kloc=kloc: e.matmul(
                        sb_[:, hh * 128:(hh + 1) * 128], lhsT=ktc_[ps_, a, kloc * 128:(kloc + 1) * 128],
                        rhs=qT2[ps_, a, :], start=True, stop=False), reads=[ktc_, qT2], writes=[sb_])
                    if near:
                        kb.op("pe", lambda e, sb_=sb_, hh=hh, h=h: e.matmul(
                            sb_[:, hh * 128:(hh + 1) * 128], lhsT=addh[:, h, :], rhs=ident_b[:],
                            start=False, stop=True), reads=[addh, ident_b], writes=[sb_])
                    else:
                        kb.op("pe", lambda e, sb_=sb_, hh=hh, nm=nm: e.matmul(
                            sb_[:, hh * 128:(hh + 1) * 128], lhsT=nm[:], rhs=ident_b[:],
                            start=False, stop=True), reads=[nm, ident_b], writes=[sb_])
                kb.op("act", lambda e, sb_=sb_, pt_=pt_, g=g: e.activation(
                    out=pt_[:, 4 * g:4 * g + 4, :], in_=sb_[:, 0:512].rearrange("p (h q) -> p h q", h=4),
                    func=AF.Exp), reads=[sb_], writes=[pt_])

        def emit_PV(kbi):
            ch, kloc = kbi // 4, kbi % 4
            vch_ = vch[ch % 2]
            pt_ = PT[kbi % 2]
            for h in range(8):
                ob = pb[5 + h // 4]
                kb.op("pe", lambda e, ob=ob, h=h, pt_=pt_, vch_=vch_, kloc=kloc: e.matmul(
                    ob[:, (h % 4) * 65:(h % 4) * 65 + 65], lhsT=pt_[:, h, :], rhs=vch_[:, kloc, h * 65:(h + 1) * 65],
                    start=False, stop=False, skip_group_check=True), reads=[pt_, vch_], writes=[ob])

        for kbi in range(nkb):
            emit_S(kbi)
            advance(att_slot)
            if kbi > 0:
                emit_PV(kbi - 1)
        emit_PV(nkb - 1)
        for g in range(2):
            ob = pb[5 + g]
            obv = ob[:, 0:260].rearrange("p (h e) -> p h e", h=4)
            kb.op("dve", lambda e, obv=obv, g=g: e.reciprocal(out=rden[:, 4 * g:4 * g + 4], in_=obv[:, :, 64]),
                  reads=[ob], writes=[rden])
            kb.op("dve", lambda e, obv=obv, g=g: e.tensor_tensor(
                out=ao[:, 256 * g:256 * g + 256].rearrange("p (h d) -> p h d", h=4), in0=obv[:, :, 0:64],
                in1=rden[:, 4 * g:4 * g + 4].unsqueeze(2).to_broadcast([128, 4, 64]), op=ALU.mult),
                reads=[ob, rden], writes=[ao])
        if dbg_ao is not None:
            kb.op("dve", lambda e: e.tensor_copy(out=aof[:], in_=ao[:]), reads=[ao], writes=[aof])
            kb.dma("sp", dbg_ao[i, :, :], aof[:], reads=[aof], writes=[dbg_ao])

        if do_tail:
            drain()
            pending[0] = tail_block(i, xb)


    drain()
    if do_sample:
        LOB = _bucket_lo()
        wrep = kb.sb([128, 8], F32, "wrep")
        idx2 = kb.sb([128, 2], I32, "idx2")
        pti = kb.sb([128, 16], I32, "pti")
        ptf = kb.sb([128, 16], F32, "ptf")
        dh = kb.sb([128, 128], F32, "dh")
        dh2 = kb.sb([128, 128], F32, "dh2")
        scs = kb.sb([128, 2, 128], F32, "scs")
        scn = kb.sb([128, 1], F32, "scn")
        dn8 = kb.sb([128, 8], F32, "dn8")
        sm8 = kb.sb([128, 8], F32, "sm8")
        tA = View(Ubo[0], Ubo[0].t[:].bitcast(F32)[:, 0:256])
        tE = View(Ubo[0], Ubo[0].t[:].bitcast(F32)[:, 256:512])
        tF = View(Ubo[1], Ubo[1].t[:].bitcast(F32)[:, 0:256])
        tG = View(Ubo[1], Ubo[1].t[:].bitcast(F32)[:, 256:512])
        idxs = View(Ubo[2], Ubo[2].t[:].bitcast(U32)[:, 0:256])
        tC = View(Ubo[2], Ubo[2].t[:].bitcast(U32)[:, 256:512])
        tD = View(Ubo[3], Ubo[3].t[:].bitcast(U32)[:, 0:256])
        rowT = kb.sb([128, 32], I32, "rowT")
        distT = kb.sb([128, 32], F32, "distT")
        negT = kb.sb([128, 32], F32, "negT")
        indT = kb.sb([128, 32], F32, "indT")
        biasT = kb.sb([128, 32, 8], F32, "biasT")
        tmp3 = kb.sb([128, 32, 8], F32, "tmp3")
        lg = kb.sb([128, 8], F32, "lg")
        pex = kb.sb([128, 8], F32, "pex")
        zsel = kb.sb([128, 31], F32, "zsel")
        numS = View(Ubo[4], Ubo[4].t[:].bitcast(F32)[:, 0:512])
        denS = kb.sb([128, 8], F32, "denS")
        seln = kb.sb([128, 1], F32, "seln")
        nb0 = kb.sb([128, 8], F32, "nb0")
        sq, sk_, sv_, ski = qf, ko, vo, kio
        sqi = hsq
        su = tmpA
        qrep = tmpB
        IscV = Isc.t[:]
        kb.dma("sp", zsel[:], zsel_in[:, :], writes=[zsel])
        kb.op("dve", lambda e: e.memset(pti[:], 0), writes=[pti])
        kb.dma("sp", pti[0:16, :], pt_in[:, :], writes=[pti])
        kb.op("dve", lambda e: e.tensor_copy(out=ptf[:], in_=pti[:]), reads=[pti], writes=[ptf])
        with nc.allow_non_contiguous_dma(reason="page table slices"):
            for jg in range(8):
                kb.dma("sp", idx2[jg * 16:(jg + 1) * 16, :], pt_in[:, 2 * jg:2 * jg + 2], writes=[idx2])
        xb = xt[0]
        kb.dma("sp", xb[:], xs_in[:, :], writes=[xb])
        norm_transpose(xb, hnT)
        wsl = load_w(win_bf, *CHUNKS[CH_Q])
        proj_tok(pb[0], wsl, 512)
        head_norm(pb[0], qg, sq)
        wsl = load_w(win_bf, *CHUNKS[CH_K])
        proj_tok(pb[1], wsl, 512)
        head_norm(pb[1], kg, sk_)
        kb.dma("sp", k_s_out[:, :], sk_[:], reads=[sk_], writes=[k_s_out])
        wsl = load_w(win_bf, *CHUNKS[CH_V])
        proj_tok(pb[0], wsl, 512)
        kb.op("act", lambda e: e.copy(out=sv_[:], in_=pb[0][:, 0:512]), reads=[pb[0]], writes=[sv_])
        kb.dma("sp", v_s_out[:, :], sv_[:], reads=[sv_], writes=[v_s_out])
        wsl = load_w(win_bf, *CHUNKS[CH_QI])
        proj_tok(pb[1], wsl, 512)
        kb.op("act", lambda e: e.copy(out=sqi[:], in_=pb[1][:, 0:512]), reads=[pb[1]], writes=[sqi])
        wsl = load_w(win_bf, *CHUNKS[CH_KW])
        proj_tok(pb[0], wsl, 72)
        kb.op("dve", lambda e: e.tensor_copy(out=ski[:], in_=pb[0][:, 0:64]), reads=[pb[0]], writes=[ski])
        kb.dma("sp", ki_s_out[:, :], ski[:], reads=[ski], writes=[ki_s_out])
        kb.op("dve", lambda e: e.tensor_scalar(out=wis[:], in0=pb[0][:, 64:72], scalar1=0.125 * (8 ** -0.5),
                                               scalar2=None, op0=ALU.mult), reads=[pb[0]], writes=[wis])
        wsl = load_w(win_bf, *CHUNKS[CH_U])
        proj_tok(pb[1], wsl, 512)
        kb.op("act", lambda e: e.copy(out=su[:], in_=pb[1][:, 0:512]), reads=[pb[1]], writes=[su])
        kb.dma("sp", pool_s_out[:, 0:14, :], state_in[:, 1:15, :], reads=[state_in], writes=[pool_s_out])
        kb.dma("sp", pool_s_out[:, 14, :], su[0:16, :], reads=[su], writes=[pool_s_out])
        kb.dma("sp", qi_d[:, :], sqi[0:16, :], reads=[sqi], writes=[qi_d])
        kb.dma("sp", wi_d[:, :], wis[0:16, :], reads=[wis], writes=[wi_d])
        kb.dma("sp", q_d[:, :], sq[0:16, :], reads=[sq], writes=[q_d])
        for jg in range(8):
            kb.dma("sp", qrep[jg * 16:(jg + 1) * 16, :], qi_d[:, :], reads=[qi_d], writes=[qrep])
            kb.dma("sp", wrep[jg * 16:(jg + 1) * 16, :], wi_d[:, :], reads=[wi_d], writes=[wrep])
        KI = IscV[:, 0:8192]
        prodc = View(ktc[0], ktc[0].t[:].rearrange("p a b -> p (a b)").bitcast(F32)[:, 0:1024])
        for s in range(2):
            kb.dma("pool", KI, cache_ik[:, :], reads=[idx2], writes=[Isc],
                   indirect=bass.IndirectOffsetOnAxis(ap=idx2[:, s:s + 1], axis=0))
            for h in range(8):
                for c8 in range(8):
                    kb.op("dve", lambda e, h=h, c8=c8: e.tensor_tensor(
                        out=prodc[:].rearrange("p (k d) -> p k d", k=16),
                        in0=KI[:, c8 * 1024:(c8 + 1) * 1024].rearrange("p (k d) -> p k d", k=16),
                        in1=qrep[:, h * 64:(h + 1) * 64].unsqueeze(1).to_broadcast([128, 16, 64]), op=ALU.mult),
                        reads=[Isc, qrep], writes=[prodc.owner])
                    kb.op("dve", lambda e, c8=c8: e.tensor_reduce(
                        out=dh[:, c8 * 16:(c8 + 1) * 16], in_=prodc[:].rearrange("p (k d) -> p k d", k=16),
                        axis=AX.X, op=ALU.add), reads=[prodc.owner], writes=[dh])
                if h == 0:
                    kb.op("dve", lambda e, s=s: e.tensor_scalar(out=scs[:, s, :], in0=dh[:], scalar1=0.0,
                                                                scalar2=wrep[:, 0:1], op0=ALU.max, op1=ALU.mult),
                          reads=[dh, wrep], writes=[scs])
                else:
                    kb.op("dve", lambda e, h=h: e.tensor_scalar(out=dh2[:], in0=dh[:], scalar1=0.0,
                                                                scalar2=wrep[:, h:h + 1], op0=ALU.max, op1=ALU.mult),
                          reads=[dh, wrep], writes=[dh2])
                    kb.op("dve", lambda e, s=s: e.tensor_tensor(out=scs[:, s, :], in0=scs[:, s, :], in1=dh2[:],
                                                                op=ALU.add), reads=[scs, dh2], writes=[scs])
        for jg in range(8):
            kb.dma("sp", sc_d[:, jg * 256:(jg + 1) * 256], scs[jg * 16:(jg + 1) * 16, :, :].rearrange("p s k -> p (s k)"),
                   reads=[scs], writes=[sc_d])
        xtmp = xn[:, 512:1024]
        kb.op("dve", lambda e: e.tensor_tensor(out=xtmp.rearrange("p (h d) -> p h d", h=8),
                                               in0=sqi[:].rearrange("p (h d) -> p h d", h=8),
                                               in1=ski[:].unsqueeze(1).to_broadcast([128, 8, 64]), op=ALU.mult),
              reads=[sqi, ski], writes=[xn])
        kb.op("dve", lambda e: e.tensor_reduce(out=dn8[:], in_=xtmp.rearrange("p (h d) -> p h d", h=8), axis=AX.X,
                                               op=ALU.add), reads=[xn], writes=[dn8])
        kb.op("dve", lambda e: e.tensor_scalar(out=dn8[:], in0=dn8[:], scalar1=0.0, scalar2=None, op0=ALU.max),
              reads=[dn8], writes=[dn8])
        kb.op("dve", lambda e: e.tensor_tensor(out=dn8[:], in0=dn8[:], in1=wis[:], op=ALU.mult),
              reads=[dn8, wis], writes=[dn8])
        kb.op("dve", lambda e: e.tensor_reduce(out=scn[:], in_=dn8[:], axis=AX.X, op=ALU.add),
              reads=[dn8], writes=[scn])
        Is = IscV[:, 0:2049]
        kb.op("dve", lambda e: e.memset(IscV[:, 0:2304], 0.0), writes=[Isc])
        kb.dma("sp", IscV[0:16, 0:2048], sc_d[:, :], reads=[sc_d], writes=[Isc])
        kb.op("dve", lambda e: e.tensor_copy(out=IscV[:, 2048:2049], in_=scn[:]), reads=[scn], writes=[Isc])
        for rd in range(32):
            kb.op("dve", lambda e: e.max(out=sm8[:], in_=Is), reads=[Isc], writes=[sm8])
            kb.op("dve", lambda e, rd=rd: e.max_index(out=idxs[:, rd * 8:(rd + 1) * 8], in_max=sm8[:], in_values=Is),
                  reads=[Isc, sm8], writes=[idxs])
            kb.op("dve", lambda e: e.match_replace(out=Is, in_to_replace=sm8[:], in_values=Is, imm_value=-1e30),
                  reads=[Isc, sm8], writes=[Isc])
        kb.op("dve", lambda e: e.tensor_copy(out=tA[:], in_=idxs[:]), reads=[idxs], writes=[tA])
        kb.op("dve", lambda e: e.tensor_scalar(out=tC[:], in0=tA[:], scalar1=2047.0, scalar2=None, op0=ALU.min),
              reads=[tA], writes=[tC])
        kb.op("dve", lambda e: e.tensor_single_scalar(out=tD[:], in_=tC[:], scalar=7, op=ALU.logical_shift_right),
              reads=[tC], writes=[tD])
        kb.op("dve", lambda e: e.tensor_single_scalar(out=tC[:], in_=tC[:], scalar=127, op=ALU.bitwise_and),
              reads=[tC], writes=[tC])
        kb.op("dve", lambda e: e.tensor_copy(out=tE[:], in_=tD[:]), reads=[tD], writes=[tE])
        kb.op("dve", lambda e: e.tensor_copy(out=tF[:], in_=tC[:]), reads=[tC], writes=[tF])
        ohs = IscV[:, 4608:8704].rearrange("p (k j) -> p k j", k=256)
        kb.op("dve", lambda e: e.tensor_tensor(out=ohs, in0=tE[:].unsqueeze(2).to_broadcast([128, 256, 16]),
                                               in1=iota16[:].unsqueeze(1).to_broadcast([128, 256, 16]), op=ALU.is_equal),
              reads=[tE, iota16], writes=[Isc])
        kb.op("dve", lambda e: e.tensor_tensor(out=ohs, in0=ohs, in1=ptf[:].unsqueeze(1).to_broadcast([128, 256, 16]),
                                               op=ALU.mult), reads=[Isc, ptf], writes=[Isc])
        kb.op("dve", lambda e: e.tensor_reduce(out=tG[:], in_=ohs, axis=AX.X, op=ALU.add), reads=[Isc], writes=[tG])
        kb.op("dve", lambda e: e.scalar_tensor_tensor(out=tG[:], in0=tG[:], scalar=128.0, in1=tF[:], op0=ALU.mult,
                                                      op1=ALU.add), reads=[tG, tF], writes=[tG])
        kb.op("dve", lambda e: e.tensor_scalar(out=tE[:], in0=tA[:], scalar1=-1.0, scalar2=2048.0, op0=ALU.mult,
                                               op1=ALU.add), reads=[tA], writes=[tE])
        kb.op("dve", lambda e: e.tensor_scalar(out=tF[:], in0=tA[:], scalar1=2047.5, scalar2=-30000.0, op0=ALU.is_ge,
                                               op1=ALU.mult), reads=[tA], writes=[tF])
        kb.op("dve", lambda e: e.tensor_reduce(out=seln[:], in_=tF[:], axis=AX.X, op=ALU.min), reads=[tF], writes=[seln])
        kb.op("dve", lambda e: e.tensor_scalar(out=seln[:], in0=seln[:], scalar1=-1.0 / 30000.0, scalar2=None,
                                               op0=ALU.mult), reads=[seln], writes=[seln])
        for (srcb, dstb) in ((tG, rowT), (tE, distT), (tF, negT)):
            for gq in range(2):
                kb.op("pe", lambda e, srcb=srcb, gq=gq: e.transpose(out=pb[2][:, gq * 128:(gq + 1) * 128],
                                                                   in_=srcb[:, gq * 128:(gq + 1) * 128],
                                                                   identity=ident_f[:]),
                      reads=[srcb, ident_f], writes=[pb[2]])
            kb.op("dve", lambda e, dstb=dstb: e.tensor_copy(
                out=dstb[:].rearrange("p (g b) -> p g b", g=2),
                in_=pb[2][:, 0:256].rearrange("p (g b) -> p g b", g=2)[:, :, 0:16]), reads=[pb[2]], writes=[dstb])
        kb.op("dve", lambda e: e.memset(biasT[:], 0.0), writes=[biasT])
        for bkt in range(1, 32):
            kb.op("dve", lambda e, bkt=bkt: e.tensor_scalar(out=indT[:], in0=distT[:], scalar1=float(LOB[bkt - 1]),
                                                            scalar2=None, op0=ALU.is_lt), reads=[distT], writes=[indT])
            kb.op("dve", lambda e, bkt=bkt: e.tensor_tensor(
                out=tmp3[:], in0=indT[:].unsqueeze(2).to_broadcast([128, 32, 8]),
                in1=ndel[:, bkt * 8:(bkt + 1) * 8].unsqueeze(1).to_broadcast([128, 32, 8]), op=ALU.mult),
                reads=[indT, ndel], writes=[tmp3])
            kb.op("dve", lambda e: e.tensor_tensor(out=biasT[:], in0=biasT[:], in1=tmp3[:], op=ALU.add),
                  reads=[biasT, tmp3], writes=[biasT])
        kb.op("dve", lambda e: e.memset(pb[5][:, 0:512], 0.0), writes=[pb[5]])
        kb.op("dve", lambda e: e.memset(pb[6][:, 0:8], 0.0), writes=[pb[6]])
        qbv = xn[:, 0:512]
        pvv = xn[:, 512:1024]
        for b in range(16):
            kb.dma("sp", qbv, q_d.t[b:b + 1, :].partition_broadcast(128).rearrange("p o d -> p (o d)"),
                   reads=[q_d], writes=[xn])
            for gq in range(2):
                col = gq * 16 + b
                kg_ = UbS[gq]
                vg_ = UbS[2 + gq]
                kb.dma("pool", kg_[:, 0:512], cache_k[:, :], reads=[rowT], writes=[kg_.owner],
                       indirect=bass.IndirectOffsetOnAxis(ap=rowT[:, col:col + 1], axis=0))
                kb.dma("pool", vg_[:, 0:512], cache_v[:, :], reads=[rowT], writes=[vg_.owner],
                       indirect=bass.IndirectOffsetOnAxis(ap=rowT[:, col:col + 1], axis=0))
                kb.op("dve", lambda e, kg_=kg_: e.tensor_tensor(out=pvv, in0=kg_[:, 0:512], in1=qbv, op=ALU.mult),
                      reads=[kg_.owner, xn], writes=[xn])
                kb.op("dve", lambda e: e.tensor_reduce(out=lg[:], in_=pvv.rearrange("p (h d) -> p h d", h=8),
                                                       axis=AX.X, op=ALU.add), reads=[xn], writes=[lg])
                kb.op("dve", lambda e, col=col: e.scalar_tensor_tensor(
                    out=lg[:], in0=lg[:], scalar=negT[:, col:col + 1], in1=biasT[:, col, :], op0=ALU.add, op1=ALU.add),
                    reads=[lg, negT, biasT], writes=[lg])
                kb.op("act", lambda e: e.activation(out=pex[:], in_=lg[:], func=AF.Exp), reads=[lg], writes=[pex])
                kb.op("dve", lambda e, vg_=vg_: e.tensor_tensor(
                    out=pvv.rearrange("p (h d) -> p h d", h=8), in0=vg_[:, 0:512].rearrange("p (h d) -> p h d", h=8),
                    in1=pex[:].unsqueeze(2).to_broadcast([128, 8, 64]), op=ALU.mult),
                    reads=[vg_.owner, pex], writes=[xn])
                kb.op("pe", lambda e, b=b: e.matmul(pb[5][0:16, 0:512], lhsT=zsel[:, 15 - b:31 - b], rhs=pvv,
                                                    start=False, stop=False, skip_group_check=True),
                      reads=[zsel, xn], writes=[pb[5]])
                kb.op("pe", lambda e, b=b: e.matmul(pb[6][0:16, 0:8], lhsT=zsel[:, 15 - b:31 - b], rhs=pex[:],
                                                    start=False, stop=False, skip_group_check=True),
                      reads=[zsel, pex], writes=[pb[6]])
        kb.op("dve", lambda e: e.memset(numS[:], 0.0), writes=[numS])
        kb.op("dve", lambda e: e.memset(denS[:], 1.0), writes=[denS])
        kb.op("dve", lambda e: e.tensor_copy(out=numS[0:16, :], in_=pb[5][0:16, 0:512]), reads=[pb[5]], writes=[numS])
        kb.op("dve", lambda e: e.tensor_copy(out=denS[0:16, :], in_=pb[6][0:16, 0:8]), reads=[pb[6]], writes=[denS])
        kb.op("dve", lambda e: e.tensor_reduce(out=nb0[:], in_=ndel[:, 8:256].rearrange("p (b h) -> p h b", h=8),
                                               axis=AX.X, op=ALU.add), reads=[ndel], writes=[nb0])
        kb.op("dve", lambda e: e.tensor_tensor(out=pvv, in0=sq[:], in1=sk_[:], op=ALU.mult), reads=[sq, sk_], writes=[xn])
        kb.op("dve", lambda e: e.tensor_reduce(out=lg[:], in_=pvv.rearrange("p (h d) -> p h d", h=8), axis=AX.X,
                                               op=ALU.add), reads=[xn], writes=[lg])
        kb.op("dve", lambda e: e.tensor_tensor(out=lg[:], in0=lg[:], in1=nb0[:], op=ALU.add), reads=[lg, nb0], writes=[lg])
        kb.op("act", lambda e: e.activation(out=pex[:], in_=lg[:], func=AF.Exp), reads=[lg], writes=[pex])
        kb.op("dve", lambda e: e.tensor_scalar(out=pex[:], in0=pex[:], scalar1=seln[:, 0:1], scalar2=None, op0=ALU.mult),
              reads=[pex, seln], writes=[pex])
        kb.op("dve", lambda e: e.tensor_tensor(out=pvv.rearrange("p (h d) -> p h d", h=8),
                                               in0=sv_[:].rearrange("p (h d) -> p h d", h=8),
                                               in1=pex[:].unsqueeze(2).to_broadcast([128, 8, 64]), op=ALU.mult),
              reads=[sv_, pex], writes=[xn])
        kb.op("dve", lambda e: e.tensor_tensor(out=numS[:], in0=numS[:], in1=pvv, op=ALU.add), reads=[numS, xn], writes=[numS])
        kb.op("dve", lambda e: e.tensor_tensor(out=denS[:], in0=denS[:], in1=pex[:], op=ALU.add), reads=[denS, pex],
              writes=[denS])
        kb.op("dve", lambda e: e.reciprocal(out=denS[:], in_=denS[:]), reads=[denS], writes=[denS])
        kb.op("dve", lambda e: e.tensor_tensor(out=ao[:].rearrange("p (h d) -> p h d", h=8),
                                               in0=numS[:].rearrange("p (h d) -> p h d", h=8),
                                               in1=denS[:].unsqueeze(2).to_broadcast([128, 8, 64]), op=ALU.mult),
              reads=[numS, denS], writes=[ao])
        stv = IscV[:, 0:7680].rearrange("p (r c) -> p r c", r=15)
        kb.op("dve", lambda e: e.memset(IscV[:, 0:7680], 0.0), writes=[Isc])
        kb.dma("sp", IscV[0:16, 0:7680], state_in.t.rearrange("b r c -> b (r c)"), reads=[state_in], writes=[Isc])
        for g in range(4):
            w = 2 ** (g + 1)
            kb.op("dve", lambda e, g=g, w=w: e.tensor_reduce(
                out=pmf[:, g, :], in_=stv[:, 16 - w:15, g * 128:(g + 1) * 128].rearrange("p r c -> p c r"),
                axis=AX.X, op=ALU.add), reads=[Isc], writes=[pmf])
            kb.op("dve", lambda e, g=g: e.tensor_tensor(out=pmf[:, g, :], in0=pmf[:, g, :], in1=su[:, g * 128:(g + 1) * 128],
                                                        op=ALU.add), reads=[pmf, su], writes=[pmf])
            kb.op("dve", lambda e, g=g, w=w: e.scalar_tensor_tensor(
                out=pmf[:, g, :], in0=pmf[:, g, :], scalar=1.0 / w, in1=su[:, g * 128:(g + 1) * 128], op0=ALU.mult,
                op1=ALU.subtract), reads=[pmf, su], writes=[pmf])
        for g in range(4):
            kb.op("pe", lambda e, g=g: e.transpose(out=pb[2][:, g * 128:(g + 1) * 128], in_=pmf[:, g, :],
                                                   identity=ident_f[:]), reads=[pmf, ident_f], writes=[pb[2]])
        kb.op("dve", lambda e: e.tensor_copy(out=pmT[:], in_=pb[2][:, 0:512].rearrange("p (g t) -> p g t", g=4)),
              reads=[pb[2]], writes=[pmT])
        pending[0] = tail_block(0, xb, sample=True)
        drain()

    kb.finish()
    return nc, kb


def _prep_inputs(inp, cfg, cores):
    nblk_a = cfg.get("nblk_a", NBLK_A)
    nq = cfg.get("nq", NQ)
    nkeys = nblk_a * 128
    xp = np.asarray(inp["x_prompt"], np.float32)
    meta = np.asarray(inp["meta_tokens"], np.float32)
    maps = []
    for c in cores:
        b, cc = c // 4, c % 4
        full = np.zeros((max(nkeys, (4 * nq + 4) * 128), D), np.float32)
        T = 16 + xp.shape[1]
        cat = np.concatenate([meta, xp[b]], axis=0)
        n = min(T, full.shape[0])
        full[:n] = cat[:n]
        xown = np.zeros((nq, 128, D), np.float32)
        xprev = np.zeros((nq, 16, D), np.float32)
        for i in range(nq):
            j = 4 * i + cc
            xown[i] = full[j * 128:(j + 1) * 128]
            if j > 0:
                xprev[i] = full[j * 128 - 16:j * 128]
        m = {
            "xcat": np.ascontiguousarray(full[:nkeys]),
            "xown": xown,
            "xprev": xprev,
            "w_in": np.ascontiguousarray(np.asarray(inp["w_in"], np.float32)[0]),
            "norm1_g": np.ascontiguousarray(np.asarray(inp["norm1_g"], np.float32)[0]),
            "q_norm_g": np.ascontiguousarray(np.asarray(inp["q_norm_g"], np.float32)[0]),
            "k_norm_g": np.ascontiguousarray(np.asarray(inp["k_norm_g"], np.float32)[0]),
            "ident": np.eye(128, dtype=np.float32),
            "rel_bias": np.ascontiguousarray(np.asarray(inp["rel_bias"], np.float32)),
            "qs": (np.arange(128, dtype=np.float32)[:, None] - np.arange(128, dtype=np.float32)[None, :]),
            "thrtab": _thrtab(cc),
            "cmask": _cmask(cc),
            "w_ba": np.ascontiguousarray(np.asarray(inp["w_branch_attn"], np.float32)[0]),
            "w_bp": np.ascontiguousarray(np.asarray(inp["w_branch_pool"], np.float32)[0]),
            "w_out": np.ascontiguousarray(np.asarray(inp["w_out"], np.float32)[0]),
            "peer_wq": np.ascontiguousarray(np.asarray(inp["peer_wq"], np.float32)[0]),
            "w_pool": np.ascontiguousarray(np.asarray(inp["w_pool"], np.float32)[0]),
            "pool_scale": np.ascontiguousarray(np.asarray(inp["pool_scale"], np.float32)[0]),
            "norm2_g": np.ascontiguousarray(np.asarray(inp["norm2_g"], np.float32)[0]),
            "subkeys": np.ascontiguousarray(np.asarray(inp["peer_subkeys"], np.float32)[0]),
            "peer_u": np.ascontiguousarray(np.asarray(inp["peer_u"], np.float32)[0]),
            "peer_v": np.ascontiguousarray(np.asarray(inp["peer_v"], np.float32)[0]),
            "rcnt": _rcnt(cc),
            "iota16": np.broadcast_to(np.arange(16, dtype=np.float32)[None, :], (128, 16)).copy(),
            "pw": np.broadcast_to((0.5 ** np.arange(1, NIT + 1)).astype(np.float32)[None, :], (128, NIT)).copy(),
        }
        if cfg.get("sample", True):
            xs = np.zeros((128, D), np.float32)
            xs[:16] = np.asarray(inp["x_sample"], np.float32)[16 * c:16 * c + 16, 0]
            z = np.zeros((128, 31), np.float32)
            z[:, 15] = 1.0
            m.update({
                "xs_own": xs,
                "cache_k": np.asarray(inp["cache_k"], np.float32).reshape(2560 * 128, 512),
                "cache_v": np.asarray(inp["cache_v"], np.float32).reshape(2560 * 128, 512),
                "cache_ik": np.asarray(inp["cache_idx_k"], np.float32).reshape(2560, 8192),
                "state_own": np.ascontiguousarray(np.asarray(inp["state_pool"], np.float32)[0, 16 * c:16 * c + 16]),
                "pt_own": np.ascontiguousarray(np.asarray(inp["page_table"], np.int32)[16 * c:16 * c + 16]),
                "zsel": z,
            })
        maps.append(m)
    return maps


def _bucket_lo():
    n = np.arange(0, 256)
    nf = np.maximum(n, 16).astype(np.float32)
    large = 16 + (np.log(nf / np.float32(16)) / np.float32(np.log(128 / 16)) * np.float32(16)).astype(np.int32)
    large = np.minimum(large, 31)
    bkt = np.where(n < 16, n, large)
    return [int(np.min(n[bkt >= b])) for b in range(1, 32)]


def _thrtab(cc):
    lo_b = _bucket_lo()
    t = np.zeros((155,), np.float32)
    for r5 in range(5):
        r = r5 - 1
        for b in range(31):
            t[r5 * 31 + b] = lo_b[b] - 128 * (cc - r)
    return np.broadcast_to(t[None, :], (128, 155)).copy()


def _rcnt(cc):
    r = np.zeros((128, 4, 128), np.float32)
    t = np.arange(128)
    for g, w in enumerate((2, 4, 8, 16)):
        cnt = np.minimum(w, t + 1) if cc == 0 else np.full(128, w)
        r[:, g, :] = (1.0 / cnt.astype(np.float32))[None, :]
    return r


def _cmask(cc):
    m = np.zeros((128, 512), np.float32)
    q = np.arange(128)[:, None]
    s = np.arange(128)[None, :]
    for r in range(4):
        if r > cc:
            m[:, r * 128:(r + 1) * 128] = -1e30
        elif r == cc:
            m[:, r * 128:(r + 1) * 128] = np.where(s > q, -1e30, 0.0)
    return m


def kernel(**inputs):
    cfg = {}
    nc = build(cfg)
    cores = list(range(8))
    maps = _prep_inputs(inputs, cfg, cores)
    res = run_bass_kernel_spmd(nc, maps, core_ids=cores)
    rs = res.results
    B, S = 2, 8192
    T = S + 16
    y_prompt = np.zeros((B, S, D), np.float32)
    k_p = np.zeros((1, B, T, 8, 64), np.float32)
    v_p = np.zeros((1, B, T, 8, 64), np.float32)
    i_p = np.zeros((1, B, T, 64), np.float32)
    pool_p = np.zeros((1, B, 15, 512), np.float32)
    for c in cores:
        b, cc = c // 4, c % 4
        r = rs[c]
        for i in range(NQ):
            j = 4 * i + cc
            p0 = j * 128
            if p0 >= T:
                continue
            p1 = min(p0 + 128, T)
            n = p1 - p0
            k_p[0, b, p0:p1] = r["k_own"][i][:n].reshape(n, 8, 64)
            v_p[0, b, p0:p1] = r["v_own"][i][:n].reshape(n, 8, 64)
            i_p[0, b, p0:p1] = r["ki_own"][i][:n]
            lo = max(p0, 16)
            y_prompt[b, lo - 16:p1 - 16] = r["y_own"][i][lo - p0:n]
        if cc == 0:
            ul = r["u_last"]
            rows = ul[:, :, 17:32]
            pool_p[0, b] = np.transpose(rows, (2, 1, 0)).reshape(15, 512)
    y_s = np.zeros((128, 1, D), np.float32)
    k_s = np.zeros((1, 128, 1, 8, 64), np.float32)
    v_s = np.zeros((1, 128, 1, 8, 64), np.float32)
    i_s = np.zeros((1, 128, 1, 64), np.float32)
    pool_s = np.zeros((1, 128, 15, 512), np.float32)
    for c in cores:
        r = rs[c]
        sl = slice(16 * c, 16 * c + 16)
        y_s[sl, 0] = r["y_s"][:16]
        k_s[0, sl, 0] = r["ks_o"][:16].reshape(16, 8, 64)
        v_s[0, sl, 0] = r["vs_o"][:16].reshape(16, 8, 64)
        i_s[0, sl, 0] = r["kis_o"][:16]
        pool_s[0, sl] = r["pool_s"]
    return (y_prompt, y_s, k_p, v_p, i_p, pool_p, k_s, v_s, i_s, pool_s)
```
